# Optimizing a Trainium2 kernel written in Bass

```python
import math
import jax, jax.numpy as jnp
from jax import lax
import numpy as np

D_MODEL = 2048
BATCH = 2
SEQ = 8192
DEPTH = 4

HEAD_DIM = 64
BLK = 128
NORM_EPS = 1e-5
N_BRANCH = 3
A_WINDOW = 128
A_HQ = 8
A_HKV = 2
A_G = A_HQ // A_HKV
B_HEADS = 12
B_WIDTH = B_HEADS * HEAD_DIM
LORA_DECAY = 96
LORA_ICLR = 96
LORA_GATE = 256
B_GN_EPS = 64e-5
C_GROUPS = ((128, 1), (512, 4), (2048, 16))
C_HG = 4
C_HEADS = C_HG * len(C_GROUPS)
C_WIDTH = C_HEADS * HEAD_DIM
C_OUT = C_HG * HEAD_DIM
N_BUCKETS = 32
MAX_EXACT = 16
REL_MAX_DIST = 2048
N_BIAS_HEADS = A_HQ + C_HEADS
D_FF = 5504
CONV_W = 3
B_COL_SIZES = (B_WIDTH, B_WIDTH, B_WIDTH, LORA_DECAY, LORA_ICLR, LORA_GATE)
B_COLS = sum(B_COL_SIZES)
IN_COL_SIZES = (A_HQ * HEAD_DIM, A_HKV * HEAD_DIM, A_HKV * HEAD_DIM, B_COLS,
                C_WIDTH, C_WIDTH, C_WIDTH, N_BRANCH * D_MODEL)
D_IN = sum(IN_COL_SIZES)

kernel_name = 'hybrid_swa_rwkv7_dilated_gated_trunk'


def _split(t, sizes):
    return jnp.split(t, [int(c) for c in np.cumsum(sizes)[:-1]], axis=-1)


def _rmsnorm(x, g):
    xf = x.astype(jnp.float32)
    y = xf * lax.rsqrt(jnp.mean(xf * xf, axis=-1, keepdims=True) + NORM_EPS)
    return (y * g.astype(jnp.float32)).astype(x.dtype)


def _shift(t):
    return jnp.pad(t, ((0, 0), (1, 0), (0, 0)))[:, :-1]


def _band_offsets():
    i = jnp.arange(BLK)[:, None]
    j = jnp.arange(2 * BLK)[None, :]
    return i + BLK - j


def _t5_bucket(dist):
    small = dist < MAX_EXACT
    nf = jnp.maximum(dist, 1).astype(jnp.float32)
    large = MAX_EXACT + (jnp.log(nf / MAX_EXACT) / math.log(REL_MAX_DIST / MAX_EXACT)
                         * (N_BUCKETS - MAX_EXACT)).astype(jnp.int32)
    return jnp.where(small, dist, jnp.minimum(large, N_BUCKETS - 1))


def _rel_bias(table, dilation):
    dist = jnp.maximum(_band_offsets(), 0) * dilation
    return jnp.transpose(table[_t5_bucket(dist)], (2, 0, 1))


def _banded_attention(q, k, v, max_steps, bias, sink):
    n, L, hkv, g, hd = q.shape
    nb = -(-L // BLK)
    pad = nb * BLK - L
    q = jnp.pad(q, ((0, 0), (0, pad), (0, 0), (0, 0), (0, 0)))
    k = jnp.pad(k, ((0, 0), (0, pad), (0, 0), (0, 0)))
    v = jnp.pad(v, ((0, 0), (0, pad), (0, 0), (0, 0)))
    qb = q.reshape(n, nb, BLK, hkv, g, hd).astype(jnp.float32)

    def window(t):
        tb = t.reshape(n, nb, BLK, hkv, hd)
        prev = jnp.pad(tb, ((0, 0), (1, 0), (0, 0), (0, 0), (0, 0)))[:, :-1]
        return jnp.concatenate([prev, tb], axis=2).astype(jnp.float32)

    kw, vw = window(k), window(v)
    s = jnp.einsum('nbqhgd,nbkhd->nbhgqk', qb, kw) * (hd ** -0.5) + bias.astype(jnp.float32)
    dist = _band_offsets()
    kpos = jnp.arange(nb)[:, None, None] * BLK - BLK + jnp.arange(2 * BLK)[None, None, :]
    valid = (dist >= 0) & (dist <= max_steps) & (kpos >= 0)
    s = jnp.where(valid[None, :, None, None], s, -jnp.inf)
    m = jnp.max(s, axis=-1)
    if sink is not None:
        sk = sink.astype(jnp.float32)[:, :, None]
        m = jnp.maximum(m, sk)
    p = jnp.exp(s - m[..., None])
    l = jnp.sum(p, axis=-1)
    denom = l + jnp.exp(sk - m) if sink is not None else l
    o = jnp.einsum('nbhgqk,nbkhd->nbqhgd', p, vw) / jnp.moveaxis(denom, -1, 2)[..., None]
    lse = jnp.moveaxis(m + jnp.log(l), -1, 2)
    o = o.reshape(n, nb * BLK, hkv, g, hd)[:, :L].astype(q.dtype)
    lse = lse.reshape(n, nb * BLK, hkv, g)[:, :L]
    return o, lse


def _swa_sink_attention(q, k, v, bias, sink):
    B, S, _ = q.shape
    q = q.reshape(B, S, A_HKV, A_G, HEAD_DIM)
    k = k.reshape(B, S, A_HKV, HEAD_DIM)
    v = v.reshape(B, S, A_HKV, HEAD_DIM)
    o, _ = _banded_attention(q, k, v, A_WINDOW - 1, bias, sink.reshape(A_HKV, A_G))
    return o.reshape(B, S, A_HQ * HEAD_DIM)


def _fold(t, dil):
    B, S = t.shape[:2]
    rest = t.shape[2:]
    return jnp.swapaxes(t.reshape(B, S // dil, dil, *rest), 1, 2).reshape(B * dil, S // dil, *rest)


def _unfold(t, B, dil):
    L = t.shape[1]
    rest = t.shape[2:]
    return jnp.swapaxes(t.reshape(B, dil, L, *rest), 1, 2).reshape(B, L * dil, *rest)


def _dilated_mixture(q, k, v, biases):
    B, S, _ = q.shape
    shp = (B, S, len(C_GROUPS), C_HG, HEAD_DIM)
    q, k, v = q.reshape(shp), k.reshape(shp), v.reshape(shp)
    outs, lses = [], []
    for gi, ((win, dil), bias) in enumerate(zip(C_GROUPS, biases)):
        o, lse = _banded_attention(_fold(q[:, :, gi], dil)[:, :, :, None], _fold(k[:, :, gi], dil),
                                   _fold(v[:, :, gi], dil), win // dil, bias, None)
        outs.append(_unfold(o[:, :, :, 0], B, dil).astype(jnp.float32))
        lses.append(_unfold(lse[..., 0], B, dil))
    wts = jax.nn.softmax(jnp.stack(lses), axis=0)
    y = jnp.sum(wts[..., None] * jnp.stack(outs), axis=0)
    return y.reshape(B, S, C_OUT).astype(q.dtype)


def _wkv7_scan(r, w, k, v, a, b):
    B, S, H, N = r.shape

    def step(state, inp):
        r_t, w_t, k_t, v_t, a_t, b_t = inp
        sa = jnp.einsum('bhvk,bhk->bhv', state, a_t)
        state = state * w_t[:, :, None, :] + sa[..., None] * b_t[:, :, None, :] + v_t[..., None] * k_t[:, :, None, :]
        return state, jnp.einsum('bhvk,bhk->bhv', state, r_t)

    xs = tuple(jnp.swapaxes(t, 0, 1) for t in (r, w, k, v, a, b))
    _, ys = lax.scan(step, jnp.zeros((B, H, N, N), jnp.float32), xs)
    return jnp.swapaxes(ys, 0, 1)


def _rwkv7_time_mix(pb, mu, w0, w_up, a0, a_up, g_up, k_k, k_a, r_k, lnx_g, lnx_b):
    B, S, _ = pb.shape
    pf = pb.astype(jnp.float32)
    pf = pf + (_shift(pf) - pf) * mu
    r, k, v, wd, ad, gd = _split(pf, B_COL_SIZES)
    w = -jax.nn.softplus(-(w0 + jnp.tanh(wd) @ w_up)) - 0.5
    a = jax.nn.sigmoid(a0 + ad @ a_up)
    g = jax.nn.sigmoid(gd) @ g_up
    heads = lambda t: t.reshape(B, S, B_HEADS, HEAD_DIM)
    kk = heads(k * k_k)
    kk = kk / jnp.maximum(jnp.sqrt(jnp.sum(kk * kk, axis=-1, keepdims=True)), 1e-12)
    k = k * (1.0 + (a - 1.0) * k_a)
    r, k, v, a = heads(r), heads(k), heads(v), heads(a)
    y = _wkv7_scan(r, jnp.exp(-jnp.exp(heads(w))), k, v, -kk, kk * a)
    mean = jnp.mean(y, axis=-1, keepdims=True)
    var = jnp.mean(jnp.square(y - mean), axis=-1, keepdims=True)
    y = ((y - mean) * lax.rsqrt(var + B_GN_EPS)).reshape(B, S, B_WIDTH) * lnx_g + lnx_b
    y = y + (jnp.sum(r * k * r_k, axis=-1, keepdims=True) * v).reshape(B, S, B_WIDTH)
    return (y * g).astype(pb.dtype)


def _conv_ffn(h, w_up, conv_w, w_down):
    u = h @ w_up
    gate, val = jnp.split(u, 2, axis=-1)
    gp = jnp.pad(gate, ((0, 0), (CONV_W - 1, 0), (0, 0)))
    S = gate.shape[1]
    gate = conv_w[0] * gp[:, 0:S] + conv_w[1] * gp[:, 1:S + 1] + conv_w[2] * gp[:, 2:S + 2]
    return (jax.nn.silu(gate) * val) @ w_down


def setup_inputs(seed: int = 0) -> dict:
    key = jax.random.key(seed)
    ks = jax.random.split(key, 26)
    nrm = lambda kk, shape, scale: jax.random.normal(kk, shape, jnp.float32) * scale
    L = DEPTH
    return {
        'x': nrm(ks[0], (BATCH, SEQ, D_MODEL), 1.0),
        'rel_bias': nrm(ks[1], (N_BUCKETS, N_BIAS_HEADS), 0.3),
        'norm1_g': 1.0 + nrm(ks[2], (L, D_MODEL), 0.02),
        'w_in': nrm(ks[3], (L, D_MODEL, D_IN), D_MODEL ** -0.5),
        'attn_sinks': nrm(ks[4], (L, A_HQ), 0.5),
        'rwkv_mu': jax.random.uniform(ks[5], (L, B_COLS), jnp.float32),
        'rwkv_w0': -0.5 + nrm(ks[6], (L, B_WIDTH), 0.5),
        'rwkv_w_up': nrm(ks[7], (L, LORA_DECAY, B_WIDTH), 0.5 * LORA_DECAY ** -0.5),
        'rwkv_a0': nrm(ks[8], (L, B_WIDTH), 0.1),
        'rwkv_a_up': nrm(ks[9], (L, LORA_ICLR, B_WIDTH), 0.5 * LORA_ICLR ** -0.5),
        'rwkv_g_up': nrm(ks[10], (L, LORA_GATE, B_WIDTH), 2.0 * LORA_GATE ** -0.5),
        'rwkv_k_k': 0.85 + nrm(ks[11], (L, B_WIDTH), 0.05),
        'rwkv_k_a': 1.0 + nrm(ks[12], (L, B_WIDTH), 0.05),
        'rwkv_r_k': nrm(ks[13], (L, B_HEADS, HEAD_DIM), 0.1),
        'rwkv_lnx_g': 1.0 + nrm(ks[14], (L, B_WIDTH), 0.02),
        'rwkv_lnx_b': nrm(ks[15], (L, B_WIDTH), 0.02),
        'proj_a': nrm(ks[16], (L, A_HQ * HEAD_DIM, D_MODEL), (A_HQ * HEAD_DIM) ** -0.5),
        'proj_b': nrm(ks[17], (L, B_WIDTH, D_MODEL), B_WIDTH ** -0.5),
        'proj_c': nrm(ks[18], (L, C_OUT, D_MODEL), C_OUT ** -0.5),
        'w_out': nrm(ks[19], (L, D_MODEL, D_MODEL), D_MODEL ** -0.5),
        'norm2_g': 1.0 + nrm(ks[20], (L, D_MODEL), 0.02),
        'ffn_up': nrm(ks[21], (L, D_MODEL, 2 * D_FF), D_MODEL ** -0.5),
        'ffn_conv': nrm(ks[22], (L, CONV_W, D_FF), 0.6),
        'ffn_down': nrm(ks[23], (L, D_FF, D_MODEL), D_FF ** -0.5),
        'final_g': 1.0 + nrm(ks[24], (D_MODEL,), 0.02),
    }


def reference(x, rel_bias, norm1_g, w_in, attn_sinks, rwkv_mu, rwkv_w0, rwkv_w_up, rwkv_a0, rwkv_a_up,
              rwkv_g_up, rwkv_k_k, rwkv_k_a, rwkv_r_k, rwkv_lnx_g, rwkv_lnx_b, proj_a, proj_b, proj_c,
              w_out, norm2_g, ffn_up, ffn_conv, ffn_down, final_g):
    bias_a = _rel_bias(rel_bias[:, :A_HQ], 1).reshape(A_HKV, A_G, BLK, 2 * BLK)
    bias_c = [_rel_bias(rel_bias[:, A_HQ + gi * C_HG:A_HQ + (gi + 1) * C_HG], dil)[:, None]
              for gi, (_, dil) in enumerate(C_GROUPS)]
    for l in range(DEPTH):
        h = _rmsnorm(x, norm1_g[l])
        aq, ak, av, pb, cq, ck, cv, gates = _split(h @ w_in[l], IN_COL_SIZES)
        y_a = _swa_sink_attention(aq, ak, av, bias_a, attn_sinks[l])
        y_b = _rwkv7_time_mix(pb, rwkv_mu[l], rwkv_w0[l], rwkv_w_up[l], rwkv_a0[l], rwkv_a_up[l],
                              rwkv_g_up[l], rwkv_k_k[l], rwkv_k_a[l], rwkv_r_k[l], rwkv_lnx_g[l], rwkv_lnx_b[l])
        y_c = _dilated_mixture(cq, ck, cv, bias_c)
        g_a, g_b, g_c = jnp.split(jax.nn.sigmoid(gates.astype(jnp.float32)).astype(h.dtype), N_BRANCH, axis=-1)
        merged = g_a * (y_a @ proj_a[l]) + g_b * (y_b @ proj_b[l]) + g_c * (y_c @ proj_c[l])
        x = x + merged @ w_out[l]
        x = x + _conv_ffn(_rmsnorm(x, norm2_g[l]), ffn_up[l], ffn_conv[l], ffn_down[l])
    return _rmsnorm(x, final_g)
```

```python
import concourse.bass as bass
import concourse.mybir as mybir

ENGS = ("pe", "dve", "act", "pool", "sp")


_PH = [0]


def uname(n):
    return "%s_%d" % (n, _PH[0])


class Sched:
    _uid = 0

    def __init__(self, nc, n_dma_sems=24):
        self.nc = nc
        self.streams = {e: [] for e in ENGS}
        self.seq = {e: 0 for e in ENGS}
        self.waited = {}
        self.lastw = {}
        self.readers = {}
        self.n_dma = n_dma_sems
        self.dma_cnt = [0] * n_dma_sems
        self.dma_rr = 0
        self.sems = {}
        self.n_wait = 0
        self.alias = {}

    def canon(self, keys):
        return [self.alias.get(k, k) for k in keys]

    def eng(self, e):
        nc = self.nc
        return {"pe": nc.tensor, "dve": nc.vector, "act": nc.scalar, "pool": nc.gpsimd, "sp": nc.sync}[e]

    def _need(self, cons, deps):
        best = {}
        for p, v in deps:
            if p is None:
                continue
            if v > best.get(p, 0):
                best[p] = v
        out = []
        for p, v in best.items():
            if self.waited.get((cons, p), 0) >= v:
                continue
            self.waited[(cons, p)] = v
            out.append((p, v))
        return out

    def _deps(self, e, reads, writes, same_engine_war=False):
        deps = []
        me = ("eng", e)
        for k in reads:
            w = self.lastw.get(k)
            if w is not None:
                deps.append(w)
        for k in writes:
            w = self.lastw.get(k)
            if w is not None:
                deps.append(w)
            for p, v in self.readers.get(k, {}).items():
                deps.append((p, v))
        return deps

    def _record(self, prod, val, reads, writes):
        for k in reads:
            d = self.readers.setdefault(k, {})
            if d.get(prod, 0) < val:
                d[prod] = val
        for k in writes:
            self.lastw[k] = (prod, val)
            self.readers[k] = {}

    def op(self, e, fn, reads=(), writes=()):
        reads = self.canon(reads); writes = self.canon(writes)
        deps = self._deps(e, reads, writes)
        if e == "pe":
            deps = [d for d in deps if d[0] != ("eng", "pe")]
        waits = self._need(e, deps)
        self.seq[e] += 1
        val = self.seq[e]
        self.streams[e].append(("op", fn, waits, None))
        self._record(("eng", e), val, reads, writes)
        return val

    def dma(self, q, fn, reads=(), writes=()):
        reads = self.canon(reads); writes = self.canon(writes)
        deps = self._deps(q, reads, writes, same_engine_war=True)
        i = self.dma_rr
        self.dma_rr = (self.dma_rr + 1) % self.n_dma
        prod = ("dma", i)
        if self.dma_cnt[i] > 0:
            deps.append((prod, self.dma_cnt[i]))
        waits = self._need(q, deps)
        self.dma_cnt[i] += 16
        val = self.dma_cnt[i]
        self.streams[q].append(("dma", fn, waits, (i, 16)))
        self._record(prod, val, reads, writes)
        return prod, val

    def finish_waits(self, e="sp"):
        deps = [(("dma", i), c) for i, c in enumerate(self.dma_cnt) if c > 0]
        deps += [(("eng", x), self.seq[x]) for x in ENGS if self.seq[x] > 0 and x != e]
        waits = self._need(e, deps)
        self.streams[e].append(("wait", None, waits, None))

    def barrier_all(self):
        for e in ENGS:
            deps = [(("dma", i), c) for i, c in enumerate(self.dma_cnt) if c > 0]
            deps += [(("eng", x), self.seq[x]) for x in ENGS if self.seq[x] > 0 and x != e]
            waits = self._need(e, deps)
            self.streams[e].append(("wait", None, waits, None))

    def emit(self):
        nc = self.nc
        Sched._uid += 1
        u = Sched._uid
        esem = {e: nc.alloc_semaphore("s%d_%s" % (u, e)) for e in ENGS}
        dsem = [nc.alloc_semaphore("d%d_%d" % (u, i)) for i in range(self.n_dma)]

        def semof(p):
            return esem[p[1]] if p[0] == "eng" else dsem[p[1]]

        def run(e):
            def body(engine):
                for kind, fn, waits, dinfo in self.streams[e]:
                    for p, v in waits:
                        engine.wait_ge(semof(p), v)
                        self.n_wait += 1
                    if kind == "op":
                        fn(engine).then_inc(esem[e], 1)
                    elif kind == "dma":
                        fn(engine).then_inc(dsem[dinfo[0]], dinfo[1])
            return body

        with nc.Block() as block:
            block.tensor(run("pe"))
            block.vector(run("dve"))
            block.scalar(run("act"))
            block.gpsimd(run("pool"))
            block.sync(run("sp"))
        nc.all_engine_barrier()
        nc.clear_and_free_semaphores(list(esem.values()) + dsem)
        nc.all_engine_barrier()


import numpy as np
from concourse.bass_utils import run_bass_kernel_spmd

F32 = mybir.dt.float32
BF16 = mybir.dt.bfloat16
ALU = mybir.AluOpType
AF = mybir.ActivationFunctionType
AX = mybir.AxisListType

D_MODEL = 2048
NORM_EPS = 1e-5
KC = D_MODEL // 128


def emit_rmsnorm_T(S, nc, xT, hT, g_sb, ones_bf, sq, ps, rstd, ntok, keys):
    kx, kh, ksq, kps, krs = keys["x"], keys["h"], keys["sq"], keys["ps"], keys["rstd"]
    for c in range(KC):
        S.op("act", lambda e, c=c: e.activation(out=sq[:, c, :], in_=xT[:, c, :], func=AF.Square),
             reads=[kx], writes=[(ksq, c)])
    for c in range(KC):
        S.op("pe", lambda e, c=c: e.matmul(ps, ones_bf, sq[:, c, :], start=(c == 0), stop=(c == KC - 1)),
             reads=[(ksq, c)], writes=[kps])
    S.op("dve", lambda e: e.tensor_scalar(out=rstd, in0=ps, scalar1=1.0 / D_MODEL, scalar2=NORM_EPS,
                                          op0=ALU.mult, op1=ALU.add), reads=[kps], writes=[krs])
    S.op("act", lambda e: e.activation(out=rstd, in_=rstd, func=AF.Sqrt), reads=[krs], writes=[krs])
    S.op("dve", lambda e: e.reciprocal(out=rstd, in_=rstd), reads=[krs], writes=[krs])
    for c in range(KC):
        S.op("dve", lambda e, c=c: e.scalar_tensor_tensor(out=hT[:, c, :], in0=xT[:, c, :], scalar=g_sb[:, c:c + 1],
                                                         in1=rstd, op0=ALU.mult, op1=ALU.mult),
             reads=[kx, krs], writes=[kh])


def build_proj(T, ncols, TT=512):
    nc = bass.Bass("TRN2", target_bir_lowering=False)
    xT = nc.dram_tensor("xT", [D_MODEL, T], F32, kind="ExternalInput").ap()
    g = nc.dram_tensor("g", [128, KC], F32, kind="ExternalInput").ap()
    W = nc.dram_tensor("W", [D_MODEL, ncols], F32, kind="ExternalInput").ap()
    pT = nc.dram_tensor("pT", [ncols, T], F32, kind="ExternalOutput").ap()
    ntt = T // TT
    CB = 512
    ncb = (ncols + CB - 1) // CB
    import contextlib
    with contextlib.ExitStack() as st:
        sb = lambda name, shape, dt: st.enter_context(nc.sbuf_tensor(uname(name), shape, dt))
        ones_bf = sb("ones", [128, 128], BF16)
        g_sb = sb("g_sb", [128, KC], F32)
        hT = sb("hT", [128, KC, T], BF16)
        xin = [sb("xin%d" % i, [128, KC, TT], F32) for i in range(2)]
        sq = sb("sq", [128, KC, TT], BF16)
        rstd = sb("rstd", [128, TT], F32)
        wt = [sb("wt%d" % i, [128, KC, CB], BF16) for i in range(2)]
        ot = [sb("ot%d" % i, [128, TT], F32) for i in range(4)]
        pss = [st.enter_context(nc.psum_tensor(uname("ps%d" % i), [128, 512], F32)) for i in range(8)]
        _PH[0] += 1
        S = Sched(nc)
        S.op("pool", lambda e: e.memset(ones_bf[:], 1.0), writes=["ones"])
        S.dma("sp", lambda e: e.dma_start(out=g_sb[:], in_=g), writes=["g"])
        xTv = xT.rearrange("(c p) t -> p c t", p=128)
        Wv = W.rearrange("(c p) n -> p c n", p=128)
        for tt in range(ntt):
            xb = xin[tt % 2]
            S.dma("sp", lambda e, xb=xb, tt=tt: e.dma_start(out=xb[:], in_=xTv[:, :, tt * TT:(tt + 1) * TT]),
                  writes=[("xin", tt % 2)])
            keys = dict(x=("xin", tt % 2), h=("h", tt), sq="sq", ps=("ps", 0), rstd="rstd")
            emit_rmsnorm_T(S, nc, xb[:], hT[:, :, tt * TT:(tt + 1) * TT], g_sb[:], ones_bf[:], sq[:], pss[0][:, :TT],
                           rstd[:], TT, keys)
        n_o = 0
        n_ps = 0
        for cb in range(ncb):
            c0 = cb * CB
            cw = min(CB, ncols - c0)
            wb = wt[cb % 2]
            S.dma("pool", lambda e, wb=wb, c0=c0, cw=cw: e.dma_start(out=wb[:, :, :cw], in_=Wv[:, :, c0:c0 + cw]),
                  writes=[("wt", cb % 2)])
            for m0 in range(0, cw, 128):
                mw = min(128, cw - m0)
                for tt in range(ntt):
                    pi = 1 + (n_ps % 7); n_ps += 1
                    ps = pss[pi]
                    for c in range(KC):
                        S.op("pe", lambda e, ps=ps, wb=wb, c=c, m0=m0, mw=mw, tt=tt:
                             e.matmul(ps[:mw, :TT], wb[:, c, m0:m0 + mw], hT[:, c, tt * TT:(tt + 1) * TT],
                                      start=(c == 0), stop=(c == KC - 1)),
                             reads=[("wt", cb % 2), ("h", tt), "ones", "g"], writes=[("ps", pi)])
                    oi = n_o % 4; n_o += 1
                    ob = ot[oi]
                    eng = "act" if (n_o % 2) else "dve"
                    if eng == "act":
                        S.op("act", lambda e, ob=ob, ps=ps, mw=mw: e.copy(out=ob[:mw, :], in_=ps[:mw, :TT]),
                             reads=[("ps", pi)], writes=[("ot", oi)])
                    else:
                        S.op("dve", lambda e, ob=ob, ps=ps, mw=mw: e.tensor_copy(out=ob[:mw, :], in_=ps[:mw, :TT]),
                             reads=[("ps", pi)], writes=[("ot", oi)])
                    S.dma("sp", lambda e, ob=ob, mw=mw, r0=c0 + m0, tt=tt:
                          e.dma_start(out=pT[r0:r0 + mw, tt * TT:(tt + 1) * TT], in_=ob[:mw, :]),
                          reads=[("ot", oi)])
        S.finish_waits("sp")
        S.emit()
    return nc


import math

ROW_AQ, ROW_AK, ROW_AV = 0, 512, 640
ROW_BR, ROW_BK, ROW_BV, ROW_WD, ROW_AD, ROW_GD = 768, 1536, 2304, 3072, 3168, 3264
ROW_CQ, ROW_CK, ROW_CV = 3520, 4288, 5056
NP_ROWS = 5824
YROW_A, YROW_B, YROW_C = 0, 512, 1280
C_DILS = (1, 4, 16)


def ssl(c0, n, d):
    return slice(c0, c0 + d * (n - 1) + 1, d)


def t5_bucket_np(dist):
    dist = np.asarray(dist, np.int64)
    nf = np.maximum(dist, 1).astype(np.float32)
    large = 16 + (np.log(nf / np.float32(16)) / np.float32(math.log(2048 / 16)) * np.float32(16)).astype(np.int32)
    return np.where(dist < 16, dist, np.minimum(large, 31))


def attn_tables(rel_bias):
    i = np.arange(128)[None, :]
    j = np.arange(128)[:, None]
    dists = (i - j, i + 128 - j)
    specs = [(h, 1, 127) for h in range(8)] + [(8 + g * 4 + hh, dil, 128) for g, dil in enumerate(C_DILS)
                                              for hh in range(4)]
    gathered = np.zeros((20, 128, 256), np.float32)
    mul = np.zeros((20, 128, 256), np.float32)
    add = np.zeros((20, 128, 256), np.float32)
    for n, (col, dil, ms) in enumerate(specs):
        for half, d in enumerate(dists):
            valid = (d >= 0) & (d <= ms)
            idx = t5_bucket_np(np.maximum(d, 0) * dil)
            gathered[n, :, half * 128:(half + 1) * 128] = rel_bias[idx, col]
            mul[n, :, half * 128:(half + 1) * 128] = np.where(valid, 8.0, 0.0)
            add[n, :, half * 128:(half + 1) * 128] = np.where(valid, 0.0, -240000.0)
    return gathered, mul, add


def emit_attention(S, nc, st, T, pT, yT, tabs_g, tabs_m, tabs_a, sinks_rep, ident_d, pss, heads_a, heads_c):
    sb = lambda name, shape, dt: st.enter_context(nc.sbuf_tensor(uname(name), shape, dt))
    NB = T // 128
    q_bf = sb("at_q", [64, T], BF16)
    k_bf = sb("at_k", [64, T], BF16)
    v_f = sb("at_v", [64, T], F32)
    stg = sb("at_stg", [64, T], F32)
    vaug = sb("at_vaug", [128, NB, 65], BF16)
    acc = sb("at_acc", [65, T], F32)
    tb_g = sb("at_tbg", [128, 256], F32)
    tb_m = sb("at_tbm", [128, 256], F32)
    tb_a = sb("at_tba", [128, 256], F32)
    tb = sb("at_tb", [128, 256], BF16)
    pt_sb = [sb("at_pt%d" % i, [128, 512], BF16) for i in range(2)]
    ident_f = sb("at_identf", [128, 128], F32)
    ident_b = sb("at_identb", [128, 128], BF16)
    sel = sb("at_sel", [65, 64], F32)
    esink = sb("at_esink", [64, 8], F32)
    den = sb("at_den", [64, 512], F32)
    yo = [sb("at_yo%d" % i, [64, 512], F32) for i in range(2)]

    S.dma("sp", lambda e: e.dma_start(out=ident_f[:], in_=ident_d), writes=["at_identf"])
    S.op("dve", lambda e: e.tensor_copy(out=ident_b[:], in_=ident_f[:]), reads=["at_identf"], writes=["at_identb"])
    S.op("pool", lambda e: e.memset(sel[:], 0.0), writes=["at_sel"])
    S.op("pool", lambda e: e.memset(sel[64:65, :], 1.0), writes=["at_sel"])
    S.op("pool", lambda e: e.memset(vaug[:, :, 64:65], 1.0), writes=["at_vaug1"])
    S.dma("sp", lambda e: e.dma_start(out=esink[:], in_=sinks_rep), writes=["at_esink"])
    S.op("act", lambda e: e.activation(out=esink[:], in_=esink[:], func=AF.Exp), reads=["at_esink"],
         writes=["at_esink"])

    ps_s = [pss[0], pss[1]]
    ps_o = [pss[2], pss[3]]
    ps_t = pss[4]
    ps_d = pss[5]
    cnt = dict(s=0, o=0, y=0)

    def load_table(n):
        S.dma("sp", lambda e: e.dma_start(out=tb_g[:], in_=tabs_g[n]), writes=["at_tbg"])
        S.dma("sp", lambda e: e.dma_start(out=tb_m[:], in_=tabs_m[n]), writes=["at_tbm"])
        S.dma("sp", lambda e: e.dma_start(out=tb_a[:], in_=tabs_a[n]), writes=["at_tba"])
        S.op("pool", lambda e: e.tensor_tensor(out=tb_g[:], in0=tb_g[:], in1=tb_m[:], op=ALU.mult),
             reads=["at_tbg", "at_tbm"], writes=["at_tbg"])
        S.op("pool", lambda e: e.tensor_tensor(out=tb[:], in0=tb_g[:], in1=tb_a[:], op=ALU.add),
             reads=["at_tbg", "at_tba"], writes=["at_tb"])

    def load_kv(krow, vrow, dil):
        S.dma("act", lambda e: e.dma_start(out=stg[:], in_=pT[krow:krow + 64, :]), reads=["pT"], writes=["at_stg"])
        S.op("pool", lambda e: e.tensor_copy(out=k_bf[:], in_=stg[:]), reads=["at_stg"], writes=["at_k"])
        S.dma("sp", lambda e: e.dma_start(out=v_f[:], in_=pT[vrow:vrow + 64, :]), reads=["pT"], writes=["at_v"])
        Lf = T // dil
        bps = Lf // 128
        for vb0 in range(0, NB, 8):
            for u in range(8):
                vb = vb0 + u
                s_, jb = vb // bps, vb % bps
                c0 = s_ + dil * 128 * jb
                src = v_f[0:64, ssl(c0, 128, dil)]
                S.op("pe", lambda e, u=u, src=src: e.transpose(ps_t[:, u * 64:(u + 1) * 64], src, ident_f[0:64, 0:64]),
                     reads=["at_v", "at_identf"], writes=["ps_t"])
            S.op("dve", lambda e, vb0=vb0: e.tensor_copy(out=vaug[:, vb0:vb0 + 8, 0:64],
                                                        in_=ps_t[:, :].rearrange("p (u d) -> p u d", d=64)),
                 reads=["ps_t"], writes=["at_vaug"])

    def run_seq(dil, first_group):
        Lf = T // dil
        bps = Lf // 128
        nbt = min(4, bps)
        for s_ in range(dil):
            for jb0 in range(0, bps, nbt):
                oi = cnt["o"] % 2; cnt["o"] += 1
                po = ps_o[oi]
                for half in range(0, nbt, 2):
                    si = cnt["s"] % 2; cnt["s"] += 1
                    pst = ps_s[si]
                    ptb = pt_sb[si]
                    nb2 = min(2, nbt - half)
                    lo = 512
                    for r in range(nb2):
                        jb = jb0 + half + r
                        qs = s_ + dil * 128 * jb
                        qv = q_bf[:, ssl(qs, 128, dil)]
                        kv = k_bf[:, ssl(qs, 128, dil)]
                        sl_prev = slice((2 * r) * 128, (2 * r + 1) * 128)
                        sl_cur = slice((2 * r + 1) * 128, (2 * r + 2) * 128)
                        if jb > 0:
                            ks = s_ + dil * 128 * (jb - 1)
                            kpv = k_bf[:, ssl(ks, 128, dil)]
                            S.op("pe", lambda e, pst=pst, sl=sl_prev, kpv=kpv, qv=qv:
                                 e.matmul(pst[:, sl], kpv, qv, start=True, stop=False),
                                 reads=["at_k", "at_q"], writes=[("ps_s", si)])
                            S.op("pe", lambda e, pst=pst, sl=sl_prev:
                                 e.matmul(pst[:, sl], ident_b[:], tb[:, 128:256], start=False, stop=True),
                                 reads=["at_tb", "at_identb"], writes=[("ps_s", si)])
                            lo = min(lo, sl_prev.start)
                        S.op("pe", lambda e, pst=pst, sl=sl_cur, kv=kv, qv=qv:
                             e.matmul(pst[:, sl], kv, qv, start=True, stop=False),
                             reads=["at_k", "at_q"], writes=[("ps_s", si)])
                        S.op("pe", lambda e, pst=pst, sl=sl_cur:
                             e.matmul(pst[:, sl], ident_b[:], tb[:, 0:128], start=False, stop=True),
                             reads=["at_tb", "at_identb"], writes=[("ps_s", si)])
                        lo = min(lo, sl_cur.start)
                    hi = nb2 * 256
                    S.op("act", lambda e, ptb=ptb, pst=pst, lo=lo, hi=hi:
                         e.activation(out=ptb[:, lo:hi], in_=pst[:, lo:hi], func=AF.Exp, scale=0.125),
                         reads=[("ps_s", si)], writes=[("at_pt", si)])
                    for r in range(nb2):
                        jb = jb0 + half + r
                        vb = s_ * bps + jb
                        osl = slice((half + r) * 128, (half + r + 1) * 128)
                        S.op("pe", lambda e, po=po, osl=osl, vb=vb, ptb=ptb, r=r, last=(jb == 0):
                             e.matmul(po[0:65, osl], vaug[:, vb, :], ptb[:, (2 * r + 1) * 128:(2 * r + 2) * 128],
                                      start=True, stop=last),
                             reads=["at_vaug", "at_vaug1", ("at_pt", si)], writes=[("ps_o", oi)])
                        if jb > 0:
                            S.op("pe", lambda e, po=po, osl=osl, vb=vb, ptb=ptb, r=r:
                                 e.matmul(po[0:65, osl], vaug[:, vb - 1, :], ptb[:, (2 * r) * 128:(2 * r + 1) * 128],
                                          start=False, stop=True),
                                 reads=["at_vaug", "at_vaug1", ("at_pt", si)], writes=[("ps_o", oi)])
                t0 = s_ + dil * 128 * jb0
                n_el = nbt * 128
                av = acc[:, ssl(t0, n_el, dil)]
                if first_group:
                    S.op("dve", lambda e, av=av, po=po, n_el=n_el: e.tensor_copy(out=av, in_=po[0:65, 0:n_el]),
                         reads=[("ps_o", oi)], writes=["at_acc"])
                else:
                    S.op("dve", lambda e, av=av, po=po, n_el=n_el:
                         e.tensor_tensor(out=av, in0=po[0:65, 0:n_el], in1=av, op=ALU.add),
                         reads=[("ps_o", oi), "at_acc"], writes=["at_acc"])

    def normalize(yrow, sink_col):
        for t0 in range(0, T, 512):
            S.op("pe", lambda e, t0=t0: e.matmul(ps_d[0:64, :], sel[:], acc[:, t0:t0 + 512], start=True, stop=True),
                 reads=["at_acc", "at_sel"], writes=["ps_d"])
            if sink_col is not None:
                S.op("dve", lambda e: e.tensor_scalar(out=den[:], in0=ps_d[0:64, :],
                                                      scalar1=esink[:, sink_col:sink_col + 1], scalar2=None,
                                                      op0=ALU.add),
                     reads=["ps_d", "at_esink"], writes=["at_den"])
                S.op("dve", lambda e: e.reciprocal(out=den[:], in_=den[:]), reads=["at_den"], writes=["at_den"])
            else:
                S.op("dve", lambda e: e.reciprocal(out=den[:], in_=ps_d[0:64, :]), reads=["ps_d"], writes=["at_den"])
            yi = cnt["y"] % 2; cnt["y"] += 1
            yb = yo[yi]
            S.op("dve", lambda e, yb=yb, t0=t0: e.tensor_tensor(out=yb[:], in0=acc[0:64, t0:t0 + 512], in1=den[:],
                                                                op=ALU.mult),
                 reads=["at_acc", "at_den"], writes=[("at_yo", yi)])
            S.dma("sp", lambda e, yb=yb, t0=t0: e.dma_start(out=yT[yrow:yrow + 64, t0:t0 + 512], in_=yb[:]),
                  reads=[("at_yo", yi)], writes=["yT"])

    last_kv = None
    for h in heads_a:
        kvh = h // 4
        if last_kv != kvh:
            load_kv(ROW_AK + 64 * kvh, ROW_AV + 64 * kvh, 1)
            last_kv = kvh
        S.dma("act", lambda e, h=h: e.dma_start(out=stg[:], in_=pT[ROW_AQ + 64 * h:ROW_AQ + 64 * h + 64, :]),
              reads=["pT"], writes=["at_stg"])
        S.op("pool", lambda e: e.tensor_copy(out=q_bf[:], in_=stg[:]), reads=["at_stg"], writes=["at_q"])
        load_table(h)
        run_seq(1, True)
        normalize(YROW_A + 64 * h, h)
    for hh in heads_c:
        for g, dil in enumerate(C_DILS):
            off = g * 256 + hh * 64
            load_kv(ROW_CK + off, ROW_CV + off, dil)
            S.dma("act", lambda e, off=off: e.dma_start(out=stg[:], in_=pT[ROW_CQ + off:ROW_CQ + off + 64, :]),
                  reads=["pT"], writes=["at_stg"])
            S.op("pool", lambda e: e.tensor_copy(out=q_bf[:], in_=stg[:]), reads=["at_stg"], writes=["at_q"])
            load_table(8 + g * 4 + hh)
            run_seq(dil, g == 0)
        normalize(YROW_C + 64 * hh, None)


def build_attn_test(T, heads_a, heads_c):
    nc = bass.Bass("TRN2", target_bir_lowering=False)
    pT = nc.dram_tensor("pT", [NP_ROWS, T], F32, kind="ExternalInput").ap()
    tg = nc.dram_tensor("tabs_g", [20, 128, 256], F32, kind="ExternalInput").ap()
    tm = nc.dram_tensor("tabs_m", [20, 128, 256], F32, kind="ExternalInput").ap()
    ta = nc.dram_tensor("tabs_a", [20, 128, 256], F32, kind="ExternalInput").ap()
    sk = nc.dram_tensor("sinks", [64, 8], F32, kind="ExternalInput").ap()
    idd = nc.dram_tensor("ident", [128, 128], F32, kind="ExternalInput").ap()
    yT = nc.dram_tensor("yT", [1536, T], F32, kind="ExternalOutput").ap()
    import contextlib
    with contextlib.ExitStack() as st:
        pss = [st.enter_context(nc.psum_tensor(uname("ps%d" % i), [128, 512], F32)) for i in range(8)]
        _PH[0] += 1
        S = Sched(nc)
        emit_attention(S, nc, st, T, pT, yT, tg, tm, ta, sk, idd, pss, heads_a, heads_c)
        S.finish_waits("sp")
        S.emit()
    return nc


CH = 64
B_GN_EPS = 64e-5


def rwkv_consts():
    s_ = np.arange(64)[:, None]; t_ = np.arange(64)[None, :]
    lt = (s_ < t_).astype(np.float32); le = (s_ <= t_).astype(np.float32); gt = (s_ > t_).astype(np.float32)
    m_lt2 = np.ascontiguousarray(np.broadcast_to(np.concatenate([lt, lt], 1)[:, None, :], (64, 3, 128)))
    m_le2 = np.ascontiguousarray(np.broadcast_to(np.concatenate([le, le], 1)[:, None, :], (64, 3, 128)))
    m_gt = np.ascontiguousarray(np.broadcast_to(gt[:, None, :], (64, 3, 64)))
    rst = np.ones((64, 512), np.float32); rst[:, ::64] = 0.0
    return dict(m_lt2=m_lt2, m_le2=m_le2, m_gt=m_gt, rst=rst)


def phase_rwkv(nc, T, pT, yT, prm_d, lmu_d, wup_d, aup_d, gup_d, ident_d, mlt_d, mle_d, mgt_d, rst_d, heads, dbg=None, stop=None):
    import contextlib
    HG = 3
    TT = 512
    NCK = TT // CH
    NH = len(heads)
    with contextlib.ExitStack() as st:
        sb = lambda name, shape, dt=F32: st.enter_context(nc.sbuf_tensor(uname(name), shape, dt))
        pss = [st.enter_context(nc.psum_tensor(uname("rps%d" % i), [128, 512], F32)) for i in range(8)]
        _PH[0] += 1
        S = Sched(nc)
        ident = sb("rw_ident", [128, 128]); m_lt2 = sb("rw_mlt", [64, 3, 128]); m_le2 = sb("rw_mle", [64, 3, 128])
        m_gt = sb("rw_mgt", [64, 3, 64]); rst = sb("rw_rst", [64, 512])
        prm = sb("rw_prm", [64, NH, 10]); lmu = sb("rw_lmu", [128, 4])
        wup = sb("rw_wup", [96, NH * 64]); aup = sb("rw_aup", [96, NH * 64]); gup = sb("rw_gup", [128, 2, NH * 64])
        ones64 = sb("rw_ones", [64, 64]); avg64 = sb("rw_avg", [64, 64]); rkb = sb("rw_rkb", [64, NH, 64])
        for t_, d_ in ((ident, ident_d), (m_lt2, mlt_d), (m_le2, mle_d), (m_gt, mgt_d), (rst, rst_d), (prm, prm_d),
                       (lmu, lmu_d), (wup, wup_d), (aup, aup_d)):
            S.dma("sp", lambda e, t_=t_, d_=d_: e.dma_start(out=t_[:], in_=d_), writes=["const"])
        S.dma("sp", lambda e: e.dma_start(out=gup[:], in_=gup_d.rearrange("(c p) n -> p c n", p=128)), writes=["const"])
        S.op("pool", lambda e: e.memset(ones64[:], 1.0), writes=["const"])
        S.op("pool", lambda e: e.memset(avg64[:], 1.0 / 64), writes=["const"])
        for hi in range(NH):
            S.op("dve", lambda e, hi=hi: e.tensor_scalar(out=rkb[:, hi, :], in0=ones64[:], scalar1=prm[:, hi, 7:8],
                                                        scalar2=None, op0=ALU.mult), reads=["const"], writes=["const"])
        lin = [sb("rw_lin%d" % i, [128, TT + 1]) for i in range(4)]
        ltmp = sb("rw_ltmp", [128, TT])
        th = sb("rw_th", [96, TT]); adm = sb("rw_adm", [96, TT]); sg = sb("rw_sg", [128, 2, TT])
        xin = [sb("rw_xin%d" % i, [64, HG, TT + 1]) for i in range(3)]
        names = ["rm", "km", "vm", "logw", "iclr", "g", "kkn", "k2", "sbon", "Lc", "G", "t0", "t1", "t2",
                 "at"]
        B = {n: sb("rw_" + n, [64, HG, TT]) for n in names}
        for n in ("rt", "bt", "kt", "atb"):
            B[n] = sb("rw_" + n, [64, HG, TT], BF16)
        B["bh"] = B["iclr"]; B["kh"] = B["kkn"]; B["y"] = B["logw"]
        S.alias.update({"bh": "iclr", "kh": "kkn", "y": "logw", ("yo", 0): "Lc", ("yo", 1): "Lc"})
        RhT = sb("rw_RhT", [64, NCK, HG, 64]); Y0T = sb("rw_Y0T", [64, NCK, HG, 64])
        MTa = sb("rw_MT", [64, NCK, HG, 64]); Na = sb("rw_N", [64, NCK, HG, 64])
        Wp = [sb("rw_W%d" % p, [64, HG, 128], BF16) for p in range(2)]; Vtokp = [sb("rw_Vtok%d" % p, [64, HG, 64], BF16) for p in range(2)]
        BKp = [sb("rw_BK%d" % p, [64, HG, 128], BF16) for p in range(2)]; AQp = [sb("rw_AQ%d" % p, [64, HG, 128], BF16) for p in range(2)]
        QPp = [[sb("rw_QP%d_%d" % (p, i), [64, HG, 128], BF16) for i in range(2)] for p in range(2)]
        ARKp = [sb("rw_ARK%d" % p, [64, HG, 128], BF16) for p in range(2)]
        Hs = [sb("rw_H%d" % i, [64, HG, 64]) for i in range(2)]
        yout = [B["Lc"], B["Lc"]]
        cnt = dict(l=0, m=0, y=0)

        def ps_l():
            i = cnt["l"] % 2; cnt["l"] += 1
            return pss[i], ("ps", i)

        def ps_m():
            i = 4 + cnt["m"] % 2; cnt["m"] += 1
            return pss[i], ("ps", i)

        def dve(fn, r, w): S.op("dve", fn, reads=r, writes=w)
        def act(fn, r, w): S.op("act", fn, reads=r, writes=w)
        def pool(fn, r, w): S.op("pool", fn, reads=r, writes=w)
        def pe(fn, r, w): S.op("pe", fn, reads=r, writes=w)

        for g0 in range(0, NH, HG):
            hs = heads[g0:g0 + HG]
            pool(lambda e: e.memset(Hs[0][:], 0.0), [], ["H0"])
            st_h = dict(hcur=0)

            def do_tile(ti, g0=g0, hs=hs, st_h=st_h):
                t0 = ti * TT
                srcs = [(ROW_WD, 96), (ROW_AD, 96), (ROW_GD, 128), (ROW_GD + 128, 128)]
                for i, (row, n) in enumerate(srcs):
                    if t0 == 0:
                        pool(lambda e, i=i, n=n: e.memset(lin[i][0:n, 0:1], 0.0), [], ["lin%d" % i])
                        S.dma("sp", lambda e, i=i, row=row, n=n: e.dma_start(out=lin[i][0:n, 1:TT + 1],
                                                                          in_=pT[row:row + n, 0:TT]),
                              reads=["pT"], writes=["lin%d" % i])
                    else:
                        S.dma("sp", lambda e, i=i, row=row, n=n: e.dma_start(out=lin[i][0:n, :],
                                                                          in_=pT[row:row + n, t0 - 1:t0 + TT]),
                              reads=["pT"], writes=["lin%d" % i])
                for i, row0 in enumerate((ROW_BR, ROW_BK, ROW_BV)):
                    for j, h in enumerate(hs):
                        row = row0 + 64 * h
                        if t0 == 0:
                            pool(lambda e, i=i, j=j: e.memset(xin[i][:, j, 0:1], 0.0), [], ["xin%d" % i])
                            S.dma("act", lambda e, i=i, j=j, row=row: e.dma_start(out=xin[i][:, j, 1:TT + 1],
                                                                              in_=pT[row:row + 64, 0:TT]),
                                  reads=["pT"], writes=["xin%d" % i])
                        else:
                            S.dma("act", lambda e, i=i, j=j, row=row: e.dma_start(out=xin[i][:, j, :],
                                                                              in_=pT[row:row + 64, t0 - 1:t0 + TT]),
                                  reads=["pT"], writes=["xin%d" % i])
                outs = [th, adm, sg[:, 0, :], sg[:, 1, :]]
                for i, (row, n) in enumerate(srcs):
                    pool(lambda e, i=i, n=n: e.tensor_tensor(out=ltmp[0:n, :], in0=lin[i][0:n, 0:TT], in1=lin[i][0:n, 1:TT + 1],
                                                           op=ALU.subtract), ["lin%d" % i], ["ltmp"])
                    o = outs[i]
                    dve(lambda e, i=i, n=n, o=o: e.scalar_tensor_tensor(out=o[0:n, :] if i < 2 else o, in0=ltmp[0:n, :],
                                                                       scalar=lmu[0:n, i:i + 1], in1=lin[i][0:n, 1:TT + 1],
                                                                       op0=ALU.mult, op1=ALU.add),
                        ["ltmp", "lin%d" % i, "const"], ["lo%d" % i])
                act(lambda e: e.activation(out=th[:], in_=th[:], func=AF.Tanh), ["lo0"], ["lo0"])
                act(lambda e: e.activation(out=sg[:, 0, :], in_=sg[:, 0, :], func=AF.Sigmoid), ["lo2"], ["lo2"])
                act(lambda e: e.activation(out=sg[:, 1, :], in_=sg[:, 1, :], func=AF.Sigmoid), ["lo3"], ["lo3"])
                for i, nm in enumerate(("rm", "km", "vm")):
                    pool(lambda e, i=i: e.tensor_tensor(out=B["t0"][:], in0=xin[i][:, :, 0:TT], in1=xin[i][:, :, 1:TT + 1],
                                                      op=ALU.subtract), ["xin%d" % i], ["t0"])
                    for j in range(HG):
                        dve(lambda e, i=i, j=j, nm=nm: e.scalar_tensor_tensor(
                            out=B[nm][:, j, :], in0=B["t0"][:, j, :], scalar=prm[:, g0 + j, i:i + 1],
                            in1=xin[i][:, j, 1:TT + 1], op0=ALU.mult, op1=ALU.add),
                            ["t0", "xin%d" % i, "const"], [nm])
                for j in range(HG):
                    hj = g0 + j
                    cs = slice(hj * 64, hj * 64 + 64)
                    p1, k1 = ps_l()
                    pe(lambda e, p1=p1, cs=cs: e.matmul(p1[0:64, :], wup[:, cs], th[:], start=True, stop=True),
                       ["lo0", "const"], [k1])
                    act(lambda e, p1=p1, j=j, hj=hj: e.activation(out=B["logw"][:, j, :], in_=p1[0:64, :], func=AF.Sigmoid,
                                                               bias=prm[:, hj, 3:4]), [k1, "const"], ["logw"])
                    p2, k2_ = ps_l()
                    pe(lambda e, p2=p2, cs=cs: e.matmul(p2[0:64, :], aup[:, cs], adm[:], start=True, stop=True),
                       ["lo1", "const"], [k2_])
                    act(lambda e, p2=p2, j=j, hj=hj: e.activation(out=B["iclr"][:, j, :], in_=p2[0:64, :], func=AF.Sigmoid,
                                                               bias=prm[:, hj, 4:5]), [k2_, "const"], ["iclr"])
                    p3, k3 = ps_l()
                    for c in range(2):
                        pe(lambda e, p3=p3, cs=cs, c=c: e.matmul(p3[0:64, :], gup[:, c, cs], sg[:, c, :], start=(c == 0),
                                                               stop=(c == 1)), ["lo2", "lo3", "const"], [k3])
                    act(lambda e, p3=p3, j=j: e.copy(out=B["g"][:, j, :], in_=p3[0:64, :]), [k3], ["g"])
                    dve(lambda e, j=j, hj=hj: e.tensor_scalar(out=B["kkn"][:, j, :], in0=B["km"][:, j, :],
                                                             scalar1=prm[:, hj, 5:6], scalar2=None, op0=ALU.mult),
                        ["km", "const"], ["kkn"])
                    pool(lambda e, j=j: e.tensor_tensor(out=B["t1"][:, j, :], in0=B["kkn"][:, j, :], in1=B["kkn"][:, j, :],
                                                      op=ALU.mult), ["kkn"], ["t1"])
                    p4, k4 = ps_l()
                    pe(lambda e, p4=p4, j=j: e.matmul(p4[0:64, :], ones64[:], B["t1"][:, j, :], start=True, stop=True),
                       ["t1", "const"], [k4])
                    act(lambda e, p4=p4, j=j: e.activation(out=B["t2"][:, j, :], in_=p4[0:64, :], func=AF.Sqrt), [k4], ["t2"])
                    dve(lambda e, j=j: e.tensor_scalar(out=B["t2"][:, j, :], in0=B["t2"][:, j, :], scalar1=1e-12, scalar2=None,
                                                      op0=ALU.max), ["t2"], ["t2"])
                    dve(lambda e, j=j: e.reciprocal(out=B["t2"][:, j, :], in_=B["t2"][:, j, :]), ["t2"], ["t2"])
                    dve(lambda e, j=j, hj=hj: e.tensor_scalar(out=B["k2"][:, j, :], in0=B["iclr"][:, j, :], scalar1=-1.0,
                                                             scalar2=prm[:, hj, 6:7], op0=ALU.add, op1=ALU.mult),
                        ["iclr", "const"], ["k2"])
                dve(lambda e: e.tensor_tensor(out=B["kkn"][:], in0=B["kkn"][:], in1=B["t2"][:], op=ALU.mult),
                    ["kkn", "t2"], ["kkn"])
                dve(lambda e: e.scalar_tensor_tensor(out=B["k2"][:], in0=B["k2"][:], scalar=1.0, in1=B["km"][:],
                                                     op0=ALU.add, op1=ALU.mult), ["k2", "km"], ["k2"])
                pool(lambda e: e.tensor_tensor(out=B["t1"][:], in0=B["rm"][:], in1=B["k2"][:], op=ALU.mult),
                     ["rm", "k2"], ["t1"])
                for j in range(HG):
                    p5, k5 = ps_l()
                    pe(lambda e, p5=p5, j=j: e.matmul(p5[0:64, :], rkb[:, g0 + j, :], B["t1"][:, j, :], start=True, stop=True),
                       ["t1", "const"], [k5])
                    act(lambda e, p5=p5, j=j: e.copy(out=B["sbon"][:, j, :], in_=p5[0:64, :]), [k5], ["sbon"])
                dve(lambda e: e.tensor_scalar(out=B["logw"][:], in0=B["logw"][:], scalar1=-math.exp(-0.5), scalar2=None,
                                              op0=ALU.mult), ["logw"], ["logw"])
                for j in range(HG):
                    dve(lambda e, j=j: e.tensor_tensor_scan(out=B["Lc"][:, j, :], data0=rst[:], data1=B["logw"][:, j, :],
                                                           initial=0.0, op0=ALU.mult, op1=ALU.add),
                        ["logw", "const"], ["Lc"])
                act(lambda e: e.activation(out=B["G"][:], in_=B["Lc"][:], func=AF.Exp), ["Lc"], ["G"])
                pool(lambda e: e.tensor_tensor(out=B["rt"][:], in0=B["rm"][:], in1=B["G"][:], op=ALU.mult), ["rm", "G"], ["rt"])
                dve(lambda e: e.tensor_tensor(out=B["t0"][:], in0=B["Lc"][:], in1=B["logw"][:], op=ALU.subtract),
                    ["Lc", "logw"], ["t0"])
                act(lambda e: e.activation(out=B["t0"][:], in_=B["t0"][:], func=AF.Exp), ["t0"], ["t0"])
                dve(lambda e: e.scalar_tensor_tensor(out=B["at"][:], in0=B["kkn"][:], scalar=-1.0, in1=B["t0"][:],
                                                     op0=ALU.mult, op1=ALU.mult), ["kkn", "t0"], ["at"])
                pool(lambda e: e.tensor_copy(out=B["atb"][:], in_=B["at"][:]), ["at"], ["atb"])
                pool(lambda e: e.tensor_tensor(out=B["t2"][:], in0=B["kkn"][:], in1=B["iclr"][:], op=ALU.mult),
                     ["kkn", "iclr"], ["t2"])
                act(lambda e: e.activation(out=B["t1"][:], in_=B["Lc"][:], func=AF.Exp, scale=-1.0), ["Lc", "t1"], ["t1"])
                dve(lambda e: e.tensor_tensor(out=B["bt"][:], in0=B["t2"][:], in1=B["t1"][:], op=ALU.mult), ["t2", "t1"], ["bt"])
                pool(lambda e: e.tensor_tensor(out=B["kt"][:], in0=B["k2"][:], in1=B["t1"][:], op=ALU.mult), ["k2", "t1"], ["kt"])
                for j in range(HG):
                    lc3 = B["Lc"][:, j, :].rearrange("p (c t) -> p c t", t=CH)
                    o3 = B["t0"][:, j, :].rearrange("p (c t) -> p c t", t=CH)
                    dve(lambda e, lc3=lc3, o3=o3: e.tensor_tensor(out=o3, in0=lc3[:, :, CH - 1:CH].to_broadcast([64, NCK, CH]),
                                                                 in1=lc3, op=ALU.subtract), ["Lc", "at"], ["t0"])
                act(lambda e: e.activation(out=B["t0"][:], in_=B["t0"][:], func=AF.Exp), ["t0"], ["t0"])
                dve(lambda e: e.tensor_tensor(out=B["bh"][:], in0=B["t2"][:], in1=B["t0"][:], op=ALU.mult), ["t2", "t0"], ["bh"])
                pool(lambda e: e.tensor_tensor(out=B["kh"][:], in0=B["k2"][:], in1=B["t0"][:], op=ALU.mult), ["k2", "t0"], ["kh"])

                if dbg is not None and ti == 0 and g0 == 0:
                    for di_, nm_ in enumerate(["rm", "km", "vm", "logw", "iclr", "g", "kkn", "k2", "sbon", "Lc", "G", "rt", "at", "bt", "kt", "bh", "kh"]):
                        S.dma("sp", lambda e, di_=di_, nm_=nm_: e.dma_start(out=dbg[di_], in_=B[nm_][:]), reads=[nm_], writes=["dbg"])
                if stop == "prep":
                    return
                def do_chunk(c, pb):
                    W, Vtok, BK, AQ, QP, ARK = Wp[pb], Vtokp[pb], BKp[pb], AQp[pb], QPp[pb], ARKp[pb]
                    kW, kV, kBK, kAQ, kARK = 'W%d' % pb, 'Vtok%d' % pb, 'BK%d' % pb, 'AQ%d' % pb, 'ARK%d' % pb
                    pw_i, pq_i = (6, 0) if pb == 0 else (7, 1)
                    csl = slice(c * CH, (c + 1) * CH)
                    tp1, ktp1 = pss[2], ("ps", 2)
                    tp2, ktp2 = pss[3], ("ps", 3)
                    for j in range(HG):
                        pe(lambda e, j=j: e.transpose(tp1[0:64, j * 128:j * 128 + 64], B["at"][:, j, csl], ident[0:64, 0:64]),
                           ["at", "const"], [ktp1])
                        pe(lambda e, j=j: e.transpose(tp1[0:64, j * 128 + 64:j * 128 + 128], B["vm"][:, j, csl], ident[0:64, 0:64]),
                           ["vm", "const"], [ktp1])
                        pe(lambda e, j=j: e.transpose(tp2[0:64, j * 128:j * 128 + 64], B["bh"][:, j, csl], ident[0:64, 0:64]),
                           ["bh", "const"], [ktp2])
                        pe(lambda e, j=j: e.transpose(tp2[0:64, j * 128 + 64:j * 128 + 128], B["kh"][:, j, csl], ident[0:64, 0:64]),
                           ["kh", "const"], [ktp2])
                    tp1v = tp1[0:64, 0:HG * 128].rearrange("p (h x) -> p h x", x=128)
                    tp2v = tp2[0:64, 0:HG * 128].rearrange("p (h x) -> p h x", x=128)
                    act(lambda e, tp1v=tp1v: e.copy(out=W[:, :, 0:64], in_=tp1v[:, :, 0:64]), [ktp1], [kW])
                    act(lambda e, tp1v=tp1v: e.copy(out=Vtok[:], in_=tp1v[:, :, 64:128]), [ktp1], [kV])
                    act(lambda e, tp2v=tp2v: e.copy(out=BK[:], in_=tp2v), [ktp2], [kBK])
                    yield
                    if stop == "c1":
                        return
                    m1, km1 = ps_m()
                    for j in range(HG):
                        pe(lambda e, m1=m1, j=j: e.matmul(m1[0:64, j * 128:j * 128 + 64], B["kt"][:, j, csl], B["atb"][:, j, csl],
                                                        start=True, stop=True), ["kt", "atb"], [km1])
                        pe(lambda e, m1=m1, j=j: e.matmul(m1[0:64, j * 128 + 64:j * 128 + 128], B["bt"][:, j, csl], B["atb"][:, j, csl],
                                                        start=True, stop=True), ["bt", "atb"], [km1])
                    m1v = m1[0:64, 0:HG * 128].rearrange("p (h x) -> p h x", x=128)
                    dve(lambda e, m1v=m1v: e.tensor_tensor(out=AQ[:], in0=m1v, in1=m_lt2[:], op=ALU.mult), [km1, "const"], [kAQ])
                    m2, km2 = ps_m()
                    for j in range(HG):
                        pe(lambda e, m2=m2, j=j: e.matmul(m2[0:64, j * 64:j * 64 + 64], B["atb"][:, j, csl], B["bt"][:, j, csl],
                                                        start=True, stop=True), ["bt", "atb"], [km2])
                    m2v = m2[0:64, 0:HG * 64].rearrange("p (h x) -> p h x", x=64)
                    qp = 0
                    dve(lambda e: e.tensor_copy(out=QP[0][:, :, 0:64], in_=AQ[:, :, 64:128]), [kAQ], ["QP%d_0" % pb])
                    dve(lambda e, m2v=m2v: e.tensor_tensor(out=QP[0][:, :, 64:128], in0=m2v, in1=m_gt[:], op=ALU.mult),
                        [km2, "const"], ["QP%d_0" % pb])
                    m3, km3 = ps_m()
                    for j in range(HG):
                        pe(lambda e, m3=m3, j=j: e.matmul(m3[0:64, j * 128:j * 128 + 64], B["bt"][:, j, csl], B["rt"][:, j, csl],
                                                        start=True, stop=True), ["bt", "rt"], [km3])
                        pe(lambda e, m3=m3, j=j: e.matmul(m3[0:64, j * 128 + 64:j * 128 + 128], B["kt"][:, j, csl], B["rt"][:, j, csl],
                                                        start=True, stop=True), ["kt", "rt"], [km3])
                    m3v = m3[0:64, 0:HG * 128].rearrange("p (h x) -> p h x", x=128)
                    dve(lambda e, m3v=m3v: e.tensor_tensor(out=ARK[:], in0=m3v, in1=m_le2[:], op=ALU.mult), [km3, "const"], [kARK])
                    yield
                    if stop == "c2":
                        return
                    m4, km4 = ps_m()
                    for j in range(HG):
                        pe(lambda e, m4=m4, j=j: e.matmul(m4[0:64, j * 64:j * 64 + 64], AQ[:, j, 0:64], Vtok[:, j, :],
                                                        start=True, stop=True), [kAQ, kV], [km4])
                    m4v = m4[0:64, 0:HG * 64].rearrange("p (h x) -> p h x", x=64)
                    act(lambda e, m4v=m4v: e.copy(out=W[:, :, 64:128], in_=m4v), [km4], [kW])
                    yield
                    for it in range(6):
                        Qb = QP[qp]
                        kq = "QP%d_%d" % (pb, qp)
                        pw, kpw = pss[pw_i], ("ps", pw_i)
                        for j in range(HG):
                            pe(lambda e, j=j, Qb=Qb: e.matmul(pw[0:64, j * 128:j * 128 + 128], Qb[:, j, 0:64], W[:, j, :],
                                                             start=True, stop=True), [kq, kW], [kpw])
                        pwv = pw[0:64, 0:HG * 128].rearrange("p (h x) -> p h x", x=128)
                        dve(lambda e, pwv=pwv: e.tensor_tensor(out=W[:], in0=pwv, in1=W[:], op=ALU.add), [kpw, kW], [kW])
                        yield
                        if it < 5:
                            pq, kpq = pss[pq_i], ("ps", pq_i)
                            for j in range(HG):
                                pe(lambda e, j=j, Qb=Qb: e.matmul(pq[0:64, j * 128:j * 128 + 64], Qb[:, j, 64:128], Qb[:, j, 0:64],
                                                                 start=True, stop=True), [kq], [kpq])
                                pe(lambda e, j=j, Qb=Qb: e.matmul(pq[0:64, j * 128 + 64:j * 128 + 128], Qb[:, j, 0:64], Qb[:, j, 64:128],
                                                                 start=True, stop=True), [kq], [kpq])
                            pqv = pq[0:64, 0:HG * 128].rearrange("p (h x) -> p h x", x=128)
                            qn = 1 - qp
                            act(lambda e, pqv=pqv, qn=qn: e.copy(out=QP[qn][:], in_=pqv), [kpq], ["QP%d_%d" % (pb, qn)])
                            qp = qn
                            yield
                    if stop == "c3":
                        return
                    m5, km5 = ps_m()
                    for j in range(HG):
                        pe(lambda e, m5=m5, j=j: e.matmul(m5[0:64, j * 64:j * 64 + 64], W[:, j, 0:64], ARK[:, j, 0:64],
                                                        start=True, stop=True), [kW, kARK], [km5])
                    m5b, km5b = ps_m()
                    for j in range(HG):
                        pe(lambda e, m5b=m5b, j=j: e.matmul(m5b[0:64, j * 64:j * 64 + 64], W[:, j, 64:128], ARK[:, j, 0:64],
                                                          start=True, stop=False), [kW, kARK], [km5b])
                        pe(lambda e, m5b=m5b, j=j: e.matmul(m5b[0:64, j * 64:j * 64 + 64], Vtok[:, j, :], ARK[:, j, 64:128],
                                                          start=False, stop=True), [kV, kARK], [km5b])
                    m5v = m5[0:64, 0:HG * 64].rearrange("p (h x) -> p h x", x=64)
                    m5bv = m5b[0:64, 0:HG * 64].rearrange("p (h x) -> p h x", x=64)
                    dve(lambda e, m5v=m5v, c=c: e.tensor_tensor(out=RhT[:, c, :, :], in0=m5v, in1=B["rt"][:, :, csl],
                                                              op=ALU.add), [km5, "rt"], ["RhT"])
                    act(lambda e, m5bv=m5bv, c=c: e.copy(out=Y0T[:, c, :, :], in_=m5bv), [km5b], ["Y0T"])
                    if stop == "c4":
                        return
                    m6, km6 = ps_m()
                    for j in range(HG):
                        pe(lambda e, m6=m6, j=j: e.matmul(m6[0:64, j * 64:j * 64 + 64], W[:, j, 0:64], BK[:, j, 0:64],
                                                        start=True, stop=True), [kW, kBK], [km6])
                    m6b, km6b = ps_m()
                    for j in range(HG):
                        pe(lambda e, m6b=m6b, j=j: e.matmul(m6b[0:64, j * 64:j * 64 + 64], BK[:, j, 0:64], W[:, j, 64:128],
                                                          start=True, stop=False), [kW, kBK], [km6b])
                        pe(lambda e, m6b=m6b, j=j: e.matmul(m6b[0:64, j * 64:j * 64 + 64], BK[:, j, 64:128], Vtok[:, j, :],
                                                          start=False, stop=True), [kV, kBK], [km6b])
                    m6v = m6[0:64, 0:HG * 64].rearrange("p (h x) -> p h x", x=64)
                    m6bv = m6b[0:64, 0:HG * 64].rearrange("p (h x) -> p h x", x=64)
                    for j in range(HG):
                        gc = B["G"][:, j, c * CH + CH - 1:c * CH + CH]
                        dve(lambda e, m6v=m6v, j=j, c=c, gc=gc: e.scalar_tensor_tensor(
                            out=MTa[:, c, j, :], in0=ident[0:64, 0:64], scalar=gc, in1=m6v[:, j, :],
                            op0=ALU.mult, op1=ALU.add), [km6, "G", "const"], ["MT"])
                    act(lambda e, m6bv=m6bv, c=c: e.copy(out=Na[:, c, :, :], in_=m6bv), [km6b], ["N"])

                for c in range(0, NCK, 2):
                    alive = [do_chunk(c, 0), do_chunk(c + 1, 1)]
                    while alive:
                        for g_ in list(alive):
                            try:
                                next(g_)
                            except StopIteration:
                                alive.remove(g_)
                if stop in ("chunk", "c1", "c2", "c3", "c4"):
                    return

                def do_seq(c):
                    hcur = st_h["hcur"]
                    Hc = Hs[hcur]; Hn = Hs[1 - hcur]
                    kh_, kn_ = "H%d" % hcur, "H%d" % (1 - hcur)
                    py, kpy = ps_m()
                    for j in range(HG):
                        pe(lambda e, py=py, j=j, Hc=Hc, c=c: e.matmul(py[0:64, j * 64:j * 64 + 64], Hc[:, j, :], RhT[:, c, j, :],
                                                                    start=True, stop=True), [kh_, "RhT"], [kpy])
                    pyv = py[0:64, 0:HG * 64].rearrange("p (h x) -> p h x", x=64)
                    dve(lambda e, pyv=pyv, c=c: e.tensor_tensor(out=B["y"][:, :, c * CH:(c + 1) * CH], in0=pyv, in1=Y0T[:, c, :, :],
                                                              op=ALU.add), [kpy, "Y0T"], ["y"])
                    ph, kph = ps_m()
                    for j in range(HG):
                        pe(lambda e, ph=ph, j=j, Hc=Hc, c=c: e.matmul(ph[0:64, j * 64:j * 64 + 64], MTa[:, c, j, :], Hc[:, j, :],
                                                                    start=True, stop=True), [kh_, "MT"], [kph])
                    phv = ph[0:64, 0:HG * 64].rearrange("p (h x) -> p h x", x=64)
                    dve(lambda e, phv=phv, c=c, Hn=Hn: e.tensor_tensor(out=Hn[:], in0=phv, in1=Na[:, c, :, :], op=ALU.add),
                        [kph, "N"], [kn_])
                    st_h["hcur"] = 1 - hcur

                for c in range(NCK):
                    do_seq(c)
                if dbg is not None and ti == 0 and g0 == 0:
                    S.dma("sp", lambda e: e.dma_start(out=dbg[17], in_=B["y"][:]), reads=["y"], writes=["dbg"])
                    for di_, nm_ in enumerate([RhT, Y0T, MTa, Na]):
                        S.dma("sp", lambda e, di_=di_, nm_=nm_: e.dma_start(out=dbg[18 + di_].rearrange("p h x -> p (h x)"), in_=nm_[:].rearrange("p c h x -> p (c h x)")),
                              reads=["RhT", "Y0T", "MT", "N"], writes=["dbg"])
                yi = cnt["y"] % 2; cnt["y"] += 1
                yb = yout[yi]
                for j in range(HG):
                    hj = g0 + j
                    p6, k6 = ps_l()
                    pe(lambda e, p6=p6, j=j: e.matmul(p6[0:64, :], avg64[:], B["y"][:, j, :], start=True, stop=True),
                       ["y", "const"], [k6])
                    dve(lambda e, p6=p6, j=j: e.tensor_tensor(out=B["t0"][:, j, :], in0=B["y"][:, j, :], in1=p6[0:64, :],
                                                            op=ALU.subtract), [k6, "y"], ["t0"])
                    pool(lambda e, j=j: e.tensor_tensor(out=B["t1"][:, j, :], in0=B["t0"][:, j, :], in1=B["t0"][:, j, :],
                                                      op=ALU.mult), ["t0"], ["t1"])
                    p7, k7 = ps_l()
                    pe(lambda e, p7=p7, j=j: e.matmul(p7[0:64, :], avg64[:], B["t1"][:, j, :], start=True, stop=True),
                       ["t1", "const"], [k7])
                    dve(lambda e, p7=p7, j=j: e.tensor_scalar(out=B["t2"][:, j, :], in0=p7[0:64, :], scalar1=B_GN_EPS, scalar2=None,
                                                            op0=ALU.add), [k7], ["t2"])
                    act(lambda e, j=j: e.activation(out=B["t2"][:, j, :], in_=B["t2"][:, j, :], func=AF.Sqrt), ["t2"], ["t2"])
                    dve(lambda e, j=j: e.reciprocal(out=B["t2"][:, j, :], in_=B["t2"][:, j, :]), ["t2"], ["t2"])
                    dve(lambda e, j=j: e.tensor_tensor(out=B["t0"][:, j, :], in0=B["t0"][:, j, :], in1=B["t2"][:, j, :],
                                                      op=ALU.mult), ["t0", "t2"], ["t0"])
                    dve(lambda e, j=j, hj=hj: e.tensor_scalar(out=B["t0"][:, j, :], in0=B["t0"][:, j, :], scalar1=prm[:, hj, 8:9],
                                                             scalar2=prm[:, hj, 9:10], op0=ALU.mult, op1=ALU.add),
                        ["t0", "const"], ["t0"])
                pool(lambda e: e.tensor_tensor(out=B["t1"][:], in0=B["sbon"][:], in1=B["vm"][:], op=ALU.mult),
                     ["sbon", "vm", "t1"], ["t1"])
                dve(lambda e: e.tensor_tensor(out=B["t0"][:], in0=B["t0"][:], in1=B["t1"][:], op=ALU.add), ["t0", "t1"], ["t0"])
                dve(lambda e, yb=yb: e.tensor_tensor(out=yb[:], in0=B["t0"][:], in1=B["g"][:], op=ALU.mult),
                    ["t0", "g"], [("yo", yi)])
                for j, h in enumerate(hs):
                    S.dma("sp", lambda e, yb=yb, j=j, h=h: e.dma_start(out=yT[YROW_B + 64 * h:YROW_B + 64 * h + 64, t0:t0 + TT],
                                                                     in_=yb[:, j, :]), reads=[("yo", yi)], writes=["yT"])

            for ti in range(T // TT):
                do_tile(ti)
        S.barrier_all()
        S.emit()


def build_rwkv_test(T, heads, debug=False, stop=None):
    nc = bass.Bass("TRN2", target_bir_lowering=False)
    NH = len(heads)
    di = lambda n, s: nc.dram_tensor(n, s, F32, kind="ExternalInput").ap()
    pT = di("pT", [NP_ROWS, T]); prm = di("prm", [64, NH, 10]); lmu = di("lmu", [128, 4])
    wup = di("wup", [96, NH * 64]); aup = di("aup", [96, NH * 64]); gup = di("gup", [256, NH * 64])
    ident = di("ident", [128, 128]); mlt = di("m_lt2", [64, 3, 128]); mle = di("m_le2", [64, 3, 128])
    mgt = di("m_gt", [64, 3, 64]); rst = di("rst", [64, 512])
    yT = nc.dram_tensor("yT", [1536, T], F32, kind="ExternalOutput").ap()
    dbg = nc.dram_tensor("dbg", [22, 64, 3, 512], F32, kind="ExternalOutput").ap() if debug else None
    phase_rwkv(nc, T, pT, yT, prm, lmu, wup, aup, gup, ident, mlt, mle, mgt, rst, heads, dbg=dbg, stop=stop)
    return nc


def rwkv_host_params(heads, mu, w0, w_up, a0, a_up, g_up, k_k, k_a, r_k, lnx_g, lnx_b):
    NH = len(heads)
    prm = np.zeros((64, NH, 10), np.float32)
    cols = np.concatenate([np.arange(64 * h, 64 * h + 64) for h in heads])
    for i, h in enumerate(heads):
        sl = slice(64 * h, 64 * h + 64)
        prm[:, i, 0] = mu[0:768][sl]; prm[:, i, 1] = mu[768:1536][sl]; prm[:, i, 2] = mu[1536:2304][sl]
        prm[:, i, 3] = w0[sl]; prm[:, i, 4] = a0[sl]; prm[:, i, 5] = k_k[sl]; prm[:, i, 6] = k_a[sl]
        prm[:, i, 7] = r_k.reshape(-1)[sl]; prm[:, i, 8] = lnx_g[sl]; prm[:, i, 9] = lnx_b[sl]
    lmu = np.zeros((128, 4), np.float32)
    lmu[0:96, 0] = mu[2304:2400]; lmu[0:96, 1] = mu[2400:2496]; lmu[:, 2] = mu[2496:2624]; lmu[:, 3] = mu[2624:2752]
    return dict(prm=prm, lmu=lmu, wup=np.ascontiguousarray(w_up[:, cols]), aup=np.ascontiguousarray(a_up[:, cols]),
                gup=np.ascontiguousarray(g_up[:, cols]))


class WeightStream:
    def __init__(self, S, nc, st, name, max_elems, n_stage=2, n_bf=2, direct=False):
        self.S, self.nc, self.name = S, nc, name
        if direct:
            n_stage = 0
        self.stage = [st.enter_context(nc.sbuf_tensor(uname("%s_st%d" % (name, i)), [128, max_elems], F32)) for i in range(n_stage)]
        self.bf = [st.enter_context(nc.sbuf_tensor(uname("%s_bf%d" % (name, i)), [128, max_elems], BF16)) for i in range(n_bf)]
        self.i = 0
        self.q = 0

    def load_bf(self, scr, shapes, dram_key):
        S = self.S
        bi = self.i % len(self.bf); self.i += 1
        bfb = self.bf[bi]
        n_tot = sum(int(np.prod(shp[1:])) for shp in shapes)
        q = ("sp", "act")[self.q % 2]; self.q += 1
        S.dma(q, lambda e, bfb=bfb, n_tot=n_tot: e.dma_start(out=bfb[:, 0:n_tot], in_=scr[:, 0:n_tot]), reads=[dram_key],
              writes=[(self.name, "bf", bi)])
        off = 0
        outs = []
        for shp in shapes:
            n = int(np.prod(shp[1:]))
            pat = "p (a b) -> p a b" if len(shp) == 3 else "p (a b c) -> p a b c"
            kw = dict(a=shp[1], b=shp[2]) if len(shp) == 3 else dict(a=shp[1], b=shp[2], c=shp[3])
            outs.append(bfb[:, off:off + n].rearrange(pat, **kw))
            off += n
        return outs, (self.name, "bf", bi)

    def load(self, src_views, shapes, dram_key):
        S = self.S
        si = self.i % len(self.stage); bi = self.i % len(self.bf); self.i += 1
        stg, bfb = self.stage[si], self.bf[bi]
        off = 0
        outs = []
        for v, shp in zip(src_views, shapes):
            n = int(np.prod(shp[1:]))
            pat = "p (a b) -> p a b" if len(shp) == 3 else "p (a b c) -> p a b c"
            kw = dict(a=shp[1], b=shp[2]) if len(shp) == 3 else dict(a=shp[1], b=shp[2], c=shp[3])
            dst = stg[:, off:off + n].rearrange(pat, **kw)
            q = ("sp", "act")[self.q % 2]; self.q += 1
            S.dma(q, lambda e, dst=dst, v=v: e.dma_start(out=dst, in_=v), reads=[dram_key],
                  writes=[(self.name, "st", si)])
            outs.append(bfb[:, off:off + n].rearrange(pat, **kw))
            off += n
        S.op("pool", lambda e, stg=stg, bfb=bfb, off=off: e.tensor_copy(out=bfb[:, 0:off], in_=stg[:, 0:off]),
             reads=[(self.name, "st", si)], writes=[(self.name, "bf", bi)])
        return outs, (self.name, "bf", bi)


def phase_precast(nc, panels, scr, max_elems):
    import contextlib
    with contextlib.ExitStack() as st:
        _PH[0] += 1
        S = Sched(nc)
        ws = WeightStream(S, nc, st, "pc_w", max_elems)
        for i, (views, shapes) in enumerate(panels):
            outs, kw = ws.load(views, shapes, "Wsrc")
            n_tot = sum(int(np.prod(shp[1:])) for shp in shapes)
            bfb = ws.bf[(ws.i - 1) % len(ws.bf)]
            S.dma("sp", lambda e, i=i, bfb=bfb, n_tot=n_tot: e.dma_start(out=scr[i][:, 0:n_tot], in_=bfb[:, 0:n_tot]),
                  reads=[kw], writes=["scr"])
        S.barrier_all()
        S.emit()


def emit_norm_T(S, nc, x_sb, kx, h_out, kh, g_col, ones_bf, sq, rstd, ps, kps, n):
    for c in range(KC):
        S.op("act", lambda e, c=c: e.activation(out=sq[:, c, :n], in_=x_sb[:, c, :], func=AF.Square), reads=[kx], writes=["nrm_sq"])
    for c in range(KC):
        S.op("pe", lambda e, c=c: e.matmul(ps[:, :n], ones_bf[:], sq[:, c, :n], start=(c == 0), stop=(c == KC - 1)),
             reads=["nrm_sq", "ones"], writes=[kps])
    S.op("dve", lambda e: e.tensor_scalar(out=rstd[:, :n], in0=ps[:, :n], scalar1=1.0 / D_MODEL, scalar2=NORM_EPS,
                                          op0=ALU.mult, op1=ALU.add), reads=[kps], writes=["nrm_rstd"])
    S.op("act", lambda e: e.activation(out=rstd[:, :n], in_=rstd[:, :n], func=AF.Sqrt), reads=["nrm_rstd"], writes=["nrm_rstd"])
    S.op("dve", lambda e: e.reciprocal(out=rstd[:, :n], in_=rstd[:, :n]), reads=["nrm_rstd"], writes=["nrm_rstd"])
    for c in range(KC):
        S.op("dve", lambda e, c=c: e.scalar_tensor_tensor(out=h_out[:, c, :], in0=x_sb[:, c, :], scalar=g_col[:, c:c + 1],
                                                         in1=rstd[:, :n], op0=ALU.mult, op1=ALU.mult),
             reads=[kx, "nrm_rstd", "gcol"], writes=[kh])


def phase_proj(nc, TL, xT, g_d, W, pT, ncols, scr=None):
    import contextlib
    TS = min(2048, TL); TT = 512; CB = 256
    Wv0 = W.rearrange("(c p) n -> p c n", p=128)
    if scr is not None:
        panels = []
        for cb0 in range(0, ncols, CB):
            cw = min(CB, ncols - cb0)
            panels.append(([Wv0[:, :, cb0:cb0 + cw]], [(128, KC, cw)]))
        phase_precast(nc, panels, scr, KC * CB)
    with contextlib.ExitStack() as st:
        sb = lambda name, shape, dt=F32: st.enter_context(nc.sbuf_tensor(uname(name), shape, dt))
        pss = [st.enter_context(nc.psum_tensor(uname("pps%d" % i), [128, 512], F32)) for i in range(8)]
        _PH[0] += 1
        S = Sched(nc)
        ones_bf = sb("pj_ones", [128, 128], BF16); g_sb = sb("pj_g", [128, KC])
        hT = sb("pj_h", [128, KC, TS], BF16)
        xin = [sb("pj_x%d" % i, [128, KC, TT]) for i in range(1)]
        sq = sb("pj_sq", [128, KC, TT], BF16); rstd = sb("pj_rstd", [128, TT])
        ot = [sb("pj_o%d" % i, [128, TT]) for i in range(4)]
        ws = WeightStream(S, nc, st, "pj_w", KC * CB, direct=scr is not None)
        S.op("pool", lambda e: e.memset(ones_bf[:], 1.0), writes=["ones"])
        S.dma("sp", lambda e: e.dma_start(out=g_sb[:], in_=g_d), writes=["gcol"])
        xTv = xT.rearrange("(c p) t -> p c t", p=128)
        Wv = W.rearrange("(c p) n -> p c n", p=128)
        cnt = dict(o=0, ps=0)
        for s0 in range(0, TL, TS):
            for tt in range(TS // TT):
                c0 = s0 + tt * TT
                S.dma("sp", lambda e, c0=c0: e.dma_start(out=xin[0][:], in_=xTv[:, :, c0:c0 + TT]), reads=["xT"], writes=["pj_x"])
                emit_norm_T(S, nc, xin[0], "pj_x", hT[:, :, tt * TT:(tt + 1) * TT], ("pj_h", tt), g_sb, ones_bf, sq, rstd,
                            pss[0], ("ps", 0), TT)
            for cb0 in range(0, ncols, CB):
                cw = min(CB, ncols - cb0)
                if scr is not None:
                    (wv,), kw = ws.load_bf(scr[cb0 // CB], [(128, KC, cw)], "Wscr")
                else:
                    (wv,), kw = ws.load([Wv[:, :, cb0:cb0 + cw]], [(128, KC, cw)], "W")
                for m0 in range(0, cw, 128):
                    mw = min(128, cw - m0)
                    for tt in range(TS // TT):
                        pi = 1 + cnt["ps"] % 7; cnt["ps"] += 1
                        ps = pss[pi]
                        for c in range(KC):
                            S.op("pe", lambda e, ps=ps, wv=wv, c=c, m0=m0, mw=mw, tt=tt:
                                 e.matmul(ps[:mw, :TT], wv[:, c, m0:m0 + mw], hT[:, c, tt * TT:(tt + 1) * TT],
                                          start=(c == 0), stop=(c == KC - 1)),
                                 reads=[kw, ("pj_h", tt)], writes=[("ps", pi)])
                        oi = cnt["o"] % 4; cnt["o"] += 1
                        ob = ot[oi]
                        if oi % 2:
                            S.op("act", lambda e, ob=ob, ps=ps, mw=mw: e.copy(out=ob[:mw, :], in_=ps[:mw, :TT]),
                                 reads=[("ps", pi)], writes=[("pj_o", oi)])
                        else:
                            S.op("dve", lambda e, ob=ob, ps=ps, mw=mw: e.tensor_copy(out=ob[:mw, :], in_=ps[:mw, :TT]),
                                 reads=[("ps", pi)], writes=[("pj_o", oi)])
                        r0 = cb0 + m0; t0 = s0 + tt * TT
                        S.dma("sp", lambda e, ob=ob, mw=mw, r0=r0, t0=t0: e.dma_start(out=pT[r0:r0 + mw, t0:t0 + TT], in_=ob[:mw, :]),
                              reads=[("pj_o", oi)], writes=["pT"])
        S.barrier_all()
        S.emit()


def phase_merge(nc, TL, xT, g_d, Wg, gate_col0, yT, projw, wout, x1T, scr=None, scr_o=None):
    import contextlib
    TT = 512
    YK = (4, 6, 2)
    YO = (0, 4, 10)
    Wgv0 = Wg.rearrange("(c p) n -> p c n", p=128)
    Pv0 = projw.rearrange("(c p) n -> p c n", p=128)
    Wov0 = wout.rearrange("(c p) n -> p c n", p=128)

    def mg_views(m):
        views = [Wgv0[:, :, gate_col0 + i * D_MODEL + m * 128:gate_col0 + i * D_MODEL + m * 128 + 128] for i in range(3)]
        views.append(Pv0[:, :, m * 128:(m + 1) * 128])
        return views, [(128, KC, 128)] * 3 + [(128, 12, 128)]

    if scr is not None:
        phase_precast(nc, [mg_views(m) for m in range(KC)], scr, KC * 3 * 128 + 12 * 128)
        phase_precast(nc, [([Wov0[:, :, m * 128:(m + 1) * 128]], [(128, KC, 128)]) for m in range(KC)], scr_o, KC * 128)
    with contextlib.ExitStack() as st:
        sb = lambda name, shape, dt=F32: st.enter_context(nc.sbuf_tensor(uname(name), shape, dt))
        pss = [st.enter_context(nc.psum_tensor(uname("mps%d" % i), [128, 512], F32)) for i in range(8)]
        _PH[0] += 1
        S = Sched(nc)
        ones_bf = sb("mg_ones", [128, 128], BF16); g_sb = sb("mg_g", [128, KC])
        xin = sb("mg_x", [128, KC, TT]); hT = sb("mg_h", [128, KC, TT], BF16)
        rstd = sb("mg_rstd", [128, TT])
        yst = sb("mg_yst", [128, 12, TT]); ybf = sb("mg_ybf", [128, 12, TT], BF16)
        mrgb = sb("mg_mrgb", [128, KC, TT], BF16)
        sq = mrgb
        S.alias["nrm_sq"] = "mg_mrgb"
        sig = [sb("mg_sig%d" % i, [128, TT]) for i in range(2)]
        tmp = sb("mg_tmp", [128, TT]); mrg = sb("mg_mrg", [128, TT])
        xo = [sb("mg_xo%d" % i, [128, TT]) for i in range(2)]
        ws = WeightStream(S, nc, st, "mg_w", KC * 3 * 128 + 12 * 128, direct=scr is not None)
        S.op("pool", lambda e: e.memset(ones_bf[:], 1.0), writes=["ones"])
        S.dma("sp", lambda e: e.dma_start(out=g_sb[:], in_=g_d), writes=["gcol"])
        xTv = xT.rearrange("(c p) t -> p c t", p=128)
        yTv = yT.rearrange("(c p) t -> p c t", p=128)
        Wgv = Wg.rearrange("(c p) n -> p c n", p=128)
        Pv = projw.rearrange("(c p) n -> p c n", p=128)
        Wov = wout.rearrange("(c p) n -> p c n", p=128)
        cnt = dict(ps=0, s=0, o=0)

        def nps():
            i = 1 + cnt["ps"] % 7; cnt["ps"] += 1
            return pss[i], ("ps", i)

        for t0 in range(0, TL, TT):
            S.dma("sp", lambda e, t0=t0: e.dma_start(out=xin[:], in_=xTv[:, :, t0:t0 + TT]), reads=["xT"], writes=["mg_x"])
            S.dma("act", lambda e, t0=t0: e.dma_start(out=yst[:], in_=yTv[:, :, t0:t0 + TT]), reads=["yT"], writes=["mg_yst"])
            S.op("pool", lambda e: e.tensor_copy(out=ybf[:], in_=yst[:]), reads=["mg_yst"], writes=["mg_ybf"])
            emit_norm_T(S, nc, xin, "mg_x", hT, "mg_h", g_sb, ones_bf, sq, rstd, pss[0], ("ps", 0), TT)
            for m in range(KC):
                gsrc = Wgv[:, :, gate_col0 + m * 128:gate_col0 + m * 128 + 128]
                views = [Wgv[:, :, gate_col0 + i * D_MODEL + m * 128:gate_col0 + i * D_MODEL + m * 128 + 128] for i in range(3)]
                views.append(Pv[:, :, m * 128:(m + 1) * 128])
                shapes = [(128, KC, 128)] * 3 + [(128, 12, 128)]
                if scr is not None:
                    (w0v, w1v, w2v, pv), kw = ws.load_bf(scr[m], shapes, "Wmgs")
                else:
                    (w0v, w1v, w2v, pv), kw = ws.load(views, shapes, "Wmg")
                wgs = (w0v, w1v, w2v)
                for i in range(3):
                    pg, kpg = nps()
                    for c in range(KC):
                        S.op("pe", lambda e, pg=pg, i=i, c=c, wgs=wgs: e.matmul(pg[:, :TT], wgs[i][:, c, :], hT[:, c, :], start=(c == 0),
                                                                            stop=(c == KC - 1)), reads=[kw, "mg_h"], writes=[kpg])
                    pz, kpz = nps()
                    for u in range(YK[i]):
                        S.op("pe", lambda e, pz=pz, i=i, u=u, pv=pv: e.matmul(pz[:, :TT], pv[:, YO[i] + u, :], ybf[:, YO[i] + u, :],
                                                                           start=(u == 0), stop=(u == YK[i] - 1)),
                             reads=[kw, "mg_ybf"], writes=[kpz])
                    si = cnt["s"] % 2; cnt["s"] += 1
                    sg = sig[si]
                    S.op("act", lambda e, sg=sg, pg=pg: e.activation(out=sg[:], in_=pg[:, :TT], func=AF.Sigmoid), reads=[kpg],
                         writes=[("mg_sig", si)])
                    if i == 0:
                        S.op("dve", lambda e, sg=sg, pz=pz: e.tensor_tensor(out=mrg[:], in0=pz[:, :TT], in1=sg[:], op=ALU.mult),
                             reads=[kpz, ("mg_sig", si)], writes=["mg_mrg"])
                    else:
                        S.op("dve", lambda e, sg=sg, pz=pz: e.tensor_tensor(out=tmp[:], in0=pz[:, :TT], in1=sg[:], op=ALU.mult),
                             reads=[kpz, ("mg_sig", si)], writes=["mg_tmp"])
                        if i == 1:
                            S.op("pool", lambda e: e.tensor_tensor(out=mrg[:], in0=mrg[:], in1=tmp[:], op=ALU.add),
                                 reads=["mg_mrg", "mg_tmp"], writes=["mg_mrg"])
                        else:
                            S.op("pool", lambda e, m=m: e.tensor_tensor(out=mrgb[:, m, :], in0=mrg[:], in1=tmp[:], op=ALU.add),
                                 reads=["mg_mrg", "mg_tmp"], writes=["mg_mrgb"])
            for m in range(KC):
                if scr is not None:
                    (wov,), kw = ws.load_bf(scr_o[m], [(128, KC, 128)], "Wouts")
                else:
                    (wov,), kw = ws.load([Wov[:, :, m * 128:(m + 1) * 128]], [(128, KC, 128)], "Wout")
                po, kpo = nps()
                for c in range(KC):
                    S.op("pe", lambda e, po=po, c=c, wov=wov: e.matmul(po[:, :TT], wov[:, c, :], mrgb[:, c, :], start=(c == 0),
                                                                     stop=(c == KC - 1)), reads=[kw, "mg_mrgb"], writes=[kpo])
                oi = cnt["o"] % 2; cnt["o"] += 1
                ob = xo[oi]
                S.op("dve", lambda e, ob=ob, po=po, m=m: e.tensor_tensor(out=ob[:], in0=po[:, :TT], in1=xin[:, m, :], op=ALU.add),
                     reads=[kpo, "mg_x"], writes=[("mg_xo", oi)])
                S.dma("sp", lambda e, ob=ob, m=m, t0=t0: e.dma_start(out=x1T[m * 128:(m + 1) * 128, t0:t0 + TT], in_=ob[:]),
                      reads=[("mg_xo", oi)], writes=["x1T"])
        S.barrier_all()
        S.emit()


def phase_ffn(nc, TL, x1T, g_d, wup, conv_d, wdown, xoT, d_ff, halo_d=None, scr_u=None, scr_d=None):
    import contextlib
    TT = 512
    NF = d_ff // 128
    Wuv0 = wup.rearrange("(c p) n -> p c n", p=128)
    Wdv0 = wdown.rearrange("(f p) n -> p f n", p=128)
    if scr_u is not None:
        phase_precast(nc, [([Wuv0[:, :, f * 128:(f + 1) * 128], Wuv0[:, :, d_ff + f * 128:d_ff + (f + 1) * 128]], [(128, KC, 128)] * 2)
                           for f in range(NF)], scr_u, KC * 256)
        phase_precast(nc, [([Wdv0[:, :, m * 128:(m + 1) * 128]], [(128, NF, 128)]) for m in range(KC)], scr_d, NF * 128)
    with contextlib.ExitStack() as st:
        sb = lambda name, shape, dt=F32: st.enter_context(nc.sbuf_tensor(uname(name), shape, dt))
        pss = [st.enter_context(nc.psum_tensor(uname("fps%d" % i), [128, 512], F32)) for i in range(8)]
        _PH[0] += 1
        S = Sched(nc)
        ones_bf = sb("ff_ones", [128, 128], BF16); g_sb = sb("ff_g", [128, KC]); cw = sb("ff_cw", [128, NF, 3])
        xin = sb("ff_x", [128, KC, TT]); hT = sb("ff_h", [128, KC, TT], BF16)
        sq = sb("ff_sq", [128, KC, TT], BF16); rstd = sb("ff_rstd", [128, TT])
        actb = sb("ff_act", [128, NF, TT], BF16)
        carry = sb("ff_carry", [128, NF, 2])
        gbuf = [sb("ff_gb%d" % i, [128, TT + 2]) for i in range(2)]
        cv = [sb("ff_cv%d" % i, [128, TT]) for i in range(2)]
        xo = [sb("ff_xo%d" % i, [128, TT]) for i in range(2)]
        ws = WeightStream(S, nc, st, "ff_w", max(KC * 256, NF * 128), n_stage=2, n_bf=3 if scr_u is not None else 2,
                          direct=scr_u is not None)
        S.op("pool", lambda e: e.memset(ones_bf[:], 1.0), writes=["ones"])
        S.dma("sp", lambda e: e.dma_start(out=g_sb[:], in_=g_d), writes=["gcol"])
        S.dma("sp", lambda e: e.dma_start(out=cw[:], in_=conv_d), writes=["ff_cw"])
        if halo_d is None:
            S.op("pool", lambda e: e.memset(carry[:], 0.0), writes=["ff_carry"])
        else:
            S.dma("sp", lambda e: e.dma_start(out=carry[:], in_=halo_d), writes=["ff_carry"])
        xv = x1T.rearrange("(c p) t -> p c t", p=128)
        Wuv = wup.rearrange("(c p) n -> p c n", p=128)
        Wdv = wdown.rearrange("(f p) n -> p f n", p=128)
        cnt = dict(ps=0, g=0, o=0)

        def nps():
            i = 1 + cnt["ps"] % 7; cnt["ps"] += 1
            return pss[i], ("ps", i)

        for t0 in range(0, TL, TT):
            S.dma("sp", lambda e, t0=t0: e.dma_start(out=xin[:], in_=xv[:, :, t0:t0 + TT]), reads=["x1T"], writes=["ff_x"])
            emit_norm_T(S, nc, xin, "ff_x", hT, "ff_h", g_sb, ones_bf, sq, rstd, pss[0], ("ps", 0), TT)
            for f in range(NF):
                views = [Wuv[:, :, f * 128:(f + 1) * 128], Wuv[:, :, d_ff + f * 128:d_ff + (f + 1) * 128]]
                if scr_u is not None:
                    (wg, wv), kw = ws.load_bf(scr_u[f], [(128, KC, 128)] * 2, "Wups")
                else:
                    (wg, wv), kw = ws.load(views, [(128, KC, 128)] * 2, "Wup")
                pg, kpg = nps()
                for c in range(KC):
                    S.op("pe", lambda e, pg=pg, c=c, wg=wg: e.matmul(pg[:, :TT], wg[:, c, :], hT[:, c, :], start=(c == 0), stop=(c == KC - 1)),
                         reads=[kw, "ff_h"], writes=[kpg])
                pv, kpv = nps()
                for c in range(KC):
                    S.op("pe", lambda e, pv=pv, c=c, wv=wv: e.matmul(pv[:, :TT], wv[:, c, :], hT[:, c, :], start=(c == 0), stop=(c == KC - 1)),
                         reads=[kw, "ff_h"], writes=[kpv])
                gi = cnt["g"] % 2; cnt["g"] += 1
                gb = gbuf[gi]; cb = cv[gi]
                kgb, kcb = ("ff_gb", gi), ("ff_cv", gi)
                S.op("pool", lambda e, gb=gb, f=f: e.tensor_copy(out=gb[:, 0:2], in_=carry[:, f, :]), reads=["ff_carry"], writes=[kgb])
                S.op("act", lambda e, gb=gb, pg=pg: e.copy(out=gb[:, 2:TT + 2], in_=pg[:, :TT]), reads=[kpg], writes=[kgb])
                S.op("pool", lambda e, gb=gb, f=f: e.tensor_copy(out=carry[:, f, :], in_=gb[:, TT:TT + 2]), reads=[kgb], writes=["ff_carry"])
                S.op("dve", lambda e, gb=gb, cb=cb, f=f: e.tensor_scalar(out=cb[:], in0=gb[:, 0:TT], scalar1=cw[:, f, 0:1], scalar2=None,
                                                                       op0=ALU.mult), reads=[kgb, "ff_cw"], writes=[kcb])
                S.op("dve", lambda e, gb=gb, cb=cb, f=f: e.scalar_tensor_tensor(out=cb[:], in0=gb[:, 1:TT + 1], scalar=cw[:, f, 1:2], in1=cb[:],
                                                                              op0=ALU.mult, op1=ALU.add), reads=[kgb, "ff_cw", kcb], writes=[kcb])
                S.op("dve", lambda e, gb=gb, cb=cb, f=f: e.scalar_tensor_tensor(out=cb[:], in0=gb[:, 2:TT + 2], scalar=cw[:, f, 2:3], in1=cb[:],
                                                                              op0=ALU.mult, op1=ALU.add), reads=[kgb, "ff_cw", kcb], writes=[kcb])
                S.op("act", lambda e, cb=cb: e.activation(out=cb[:], in_=cb[:], func=AF.Silu), reads=[kcb], writes=[kcb])
                S.op("dve", lambda e, cb=cb, pv=pv, f=f: e.tensor_tensor(out=actb[:, f, :], in0=pv[:, :TT], in1=cb[:], op=ALU.mult),
                     reads=[kpv, kcb], writes=["ff_act"])
            for m in range(KC):
                if scr_d is not None:
                    (wd,), kw = ws.load_bf(scr_d[m], [(128, NF, 128)], "Wdowns")
                else:
                    (wd,), kw = ws.load([Wdv[:, :, m * 128:(m + 1) * 128]], [(128, NF, 128)], "Wdown")
                po, kpo = nps()
                for f in range(NF):
                    S.op("pe", lambda e, po=po, f=f, wd=wd: e.matmul(po[:, :TT], wd[:, f, :], actb[:, f, :], start=(f == 0), stop=(f == NF - 1)),
                         reads=[kw, "ff_act"], writes=[kpo])
                oi = cnt["o"] % 2; cnt["o"] += 1
                ob = xo[oi]
                S.op("dve", lambda e, ob=ob, po=po, m=m: e.tensor_tensor(out=ob[:], in0=po[:, :TT], in1=xin[:, m, :], op=ALU.add),
                     reads=[kpo, "ff_x"], writes=[("ff_xo", oi)])
                S.dma("sp", lambda e, ob=ob, m=m, t0=t0: e.dma_start(out=xoT[m * 128:(m + 1) * 128, t0:t0 + TT], in_=ob[:]),
                      reads=[("ff_xo", oi)], writes=["xoT"])
        S.barrier_all()
        S.emit()


def phase_final_norm(nc, TL, xT, g_d, outT):
    import contextlib
    TT = 512
    with contextlib.ExitStack() as st:
        sb = lambda name, shape, dt=F32: st.enter_context(nc.sbuf_tensor(uname(name), shape, dt))
        pss = [st.enter_context(nc.psum_tensor(uname("nps%d" % i), [128, 512], F32)) for i in range(2)]
        _PH[0] += 1
        S = Sched(nc)
        ones_bf = sb("fn_ones", [128, 128], BF16); g_sb = sb("fn_g", [128, KC])
        xin = [sb("fn_x%d" % i, [128, KC, TT]) for i in range(2)]
        ho = [sb("fn_h%d" % i, [128, KC, TT]) for i in range(2)]
        sq = sb("fn_sq", [128, KC, TT], BF16); rstd = sb("fn_rstd", [128, TT])
        S.op("pool", lambda e: e.memset(ones_bf[:], 1.0), writes=["ones"])
        S.dma("sp", lambda e: e.dma_start(out=g_sb[:], in_=g_d), writes=["gcol"])
        xv = xT.rearrange("(c p) t -> p c t", p=128)
        ov = outT.rearrange("(c p) t -> p c t", p=128)
        for i, t0 in enumerate(range(0, TL, TT)):
            b = i % 2
            S.dma("sp", lambda e, t0=t0, b=b: e.dma_start(out=xin[b][:], in_=xv[:, :, t0:t0 + TT]), reads=["xT"], writes=[("fn_x", b)])
            emit_norm_T(S, nc, xin[b], ("fn_x", b), ho[b], ("fn_h", b), g_sb, ones_bf, sq, rstd, pss[0], ("ps", 0), TT)
            S.dma("act", lambda e, t0=t0, b=b: e.dma_start(out=ov[:, :, t0:t0 + TT], in_=ho[b][:]), reads=[("fn_h", b)], writes=["outT"])
        S.barrier_all()
        S.emit()


def build_dense_test(TL, d_ff, ncols_p):
    nc = bass.Bass("TRN2", target_bir_lowering=False)
    di = lambda n, s: nc.dram_tensor(n, s, F32, kind="ExternalInput").ap()
    xT = di("xT", [D_MODEL, TL]); g1 = di("g1", [128, KC]); g2 = di("g2", [128, KC]); gf = di("gf", [128, KC])
    w_in = di("w_in", [D_MODEL, ncols_p + 6144]); yT = di("yT", [1536, TL]); projw = di("projw", [1536, D_MODEL])
    wout = di("wout", [D_MODEL, D_MODEL]); wup = di("wup", [D_MODEL, 2 * d_ff]); conv = di("conv", [128, d_ff // 128, 3])
    wdown = di("wdown", [d_ff, D_MODEL])
    pT = nc.dram_tensor("pT", [ncols_p, TL], F32, kind="ExternalOutput").ap()
    x1T = nc.dram_tensor("x1T", [D_MODEL, TL], F32, kind="ExternalOutput").ap()
    x2T = nc.dram_tensor("x2T", [D_MODEL, TL], F32, kind="ExternalOutput").ap()
    outT = nc.dram_tensor("outT", [D_MODEL, TL], F32, kind="ExternalOutput").ap()
    phase_proj(nc, TL, xT, g1, w_in, pT, ncols_p)
    phase_merge(nc, TL, xT, g1, w_in, ncols_p, yT, projw, wout, x1T)
    phase_ffn(nc, TL, x1T, g2, wup, conv, wdown, x2T, d_ff)
    phase_final_norm(nc, TL, x2T, gf, outT)
    return nc


def build_full(TL, n_layers, d_ff, heads_a, heads_c, heads_b, final=True):
    nc = bass.Bass("TRN2", target_bir_lowering=False)
    di = lambda n, s: nc.dram_tensor(n, list(s), F32, kind="ExternalInput").ap()
    L = n_layers
    NHB = len(heads_b)
    xT = di("xT", [D_MODEL, TL])
    g1 = di("g1", [L, 128, KC]); g2 = di("g2", [L, 128, KC]); gf = di("gf", [128, KC])
    w_in = di("w_in", [L, D_MODEL, NP_ROWS + 3 * D_MODEL])
    projw = di("projw", [L, 1536, D_MODEL]); wout = di("w_out", [L, D_MODEL, D_MODEL])
    wup = di("ffn_up", [L, D_MODEL, 2 * d_ff]); conv = di("conv", [L, 128, d_ff // 128, 3]); wdown = di("ffn_down", [L, d_ff, D_MODEL])
    tabs_g = di("tabs_g", [20, 128, 256]); tabs_m = di("tabs_m", [20, 128, 256]); tabs_a = di("tabs_a", [20, 128, 256])
    sinks = di("sinks", [L, 64, 8]); ident = di("ident", [128, 128])
    prm = di("prm", [L, 64, NHB, 10]); lmu = di("lmu", [L, 128, 4])
    rwup = di("rw_up", [L, 96, NHB * 64]); raup = di("ra_up", [L, 96, NHB * 64]); rgup = di("rg_up", [L, 256, NHB * 64])
    mlt = di("m_lt2", [64, 3, 128]); mle = di("m_le2", [64, 3, 128]); mgt = di("m_gt", [64, 3, 64]); rst = di("rst", [64, 512])
    outT = nc.dram_tensor("outT", [D_MODEL, TL], F32, kind="ExternalOutput").ap()
    pT = nc.dram_tensor("pT_s", [NP_ROWS, TL], F32).ap()
    yT = nc.dram_tensor("yT_s", [1536, TL], F32).ap()
    x1T = nc.dram_tensor("x1T_s", [D_MODEL, TL], F32).ap()
    xs = [nc.dram_tensor("xs%d" % i, [D_MODEL, TL], F32).ap() for i in range(2)]
    NF = d_ff // 128
    sc_pj = nc.dram_tensor("sc_pj", [(NP_ROWS + 255) // 256, 128, KC * 256], BF16).ap()
    sc_mg = nc.dram_tensor("sc_mg", [KC, 128, KC * 3 * 128 + 12 * 128], BF16).ap()
    sc_wo = nc.dram_tensor("sc_wo", [KC, 128, KC * 128], BF16).ap()
    sc_up = nc.dram_tensor("sc_up", [NF, 128, KC * 256], BF16).ap()
    sc_dn = nc.dram_tensor("sc_dn", [KC, 128, NF * 128], BF16).ap()
    cur = xT
    for l in range(L):
        phase_proj(nc, TL, cur, g1[l], w_in[l], pT, NP_ROWS, scr=sc_pj)
        phase_attn(nc, TL, pT, yT, tabs_g, tabs_m, tabs_a, sinks[l], ident, heads_a, heads_c)
        phase_rwkv(nc, TL, pT, yT, prm[l], lmu[l], rwup[l], raup[l], rgup[l], ident, mlt, mle, mgt, rst, heads_b)
        phase_merge(nc, TL, cur, g1[l], w_in[l], NP_ROWS, yT, projw[l], wout[l], x1T, scr=sc_mg, scr_o=sc_wo)
        nxt = outT if (l == L - 1 and not final) else xs[l % 2]
        phase_ffn(nc, TL, x1T, g2[l], wup[l], conv[l], wdown[l], nxt, d_ff, scr_u=sc_up, scr_d=sc_dn)
        cur = nxt
    if final:
        phase_final_norm(nc, TL, cur, gf, outT)
    return nc


def phase_attn(nc, T, pT, yT, tg, tm, ta, sk, idd, heads_a, heads_c):
    import contextlib
    with contextlib.ExitStack() as st:
        pss = [st.enter_context(nc.psum_tensor(uname("aps%d" % i), [128, 512], F32)) for i in range(8)]
        _PH[0] += 1
        S = Sched(nc)
        emit_attention(S, nc, st, T, pT, yT, tg, tm, ta, sk, idd, pss, heads_a, heads_c)
        S.barrier_all()
        S.emit()


def host_inputs(inp, n_layers, d_ff, heads_b):
    L = n_layers
    f32 = lambda a: np.ascontiguousarray(np.asarray(a, dtype=np.float32))
    gl = lambda g: np.ascontiguousarray(np.asarray(g, np.float32).reshape(-1, KC, 128).transpose(0, 2, 1))
    tg, tm, ta = attn_tables(np.asarray(inp["rel_bias"], np.float32))
    d = dict(g1=gl(inp["norm1_g"][:L]), g2=gl(inp["norm2_g"][:L]), gf=gl(inp["final_g"])[0],
             w_in=f32(inp["w_in"][:L]),
             projw=f32(np.concatenate([np.asarray(inp["proj_a"][:L]), np.asarray(inp["proj_b"][:L]), np.asarray(inp["proj_c"][:L])], axis=1)),
             w_out=f32(inp["w_out"][:L]), ffn_up=f32(inp["ffn_up"][:L]), ffn_down=f32(inp["ffn_down"][:L]),
             conv=f32(np.asarray(inp["ffn_conv"][:L]).transpose(0, 2, 1).reshape(L, d_ff // 128, 128, 3).transpose(0, 2, 1, 3)),
             tabs_g=tg, tabs_m=tm, tabs_a=ta,
             sinks=f32(np.broadcast_to(np.asarray(inp["attn_sinks"][:L])[:, None, :], (L, 64, 8))),
             ident=np.eye(128, dtype=np.float32))
    prm, lmu, wu, au, gu = [], [], [], [], []
    for l in range(L):
        hp = rwkv_host_params(heads_b, *[np.asarray(inp[k][l], np.float32) for k in
                                         ("rwkv_mu", "rwkv_w0", "rwkv_w_up", "rwkv_a0", "rwkv_a_up", "rwkv_g_up", "rwkv_k_k",
                                          "rwkv_k_a", "rwkv_r_k", "rwkv_lnx_g", "rwkv_lnx_b")])
        prm.append(hp["prm"]); lmu.append(hp["lmu"]); wu.append(hp["wup"]); au.append(hp["aup"]); gu.append(hp["gup"])
    d.update(prm=np.stack(prm), lmu=np.stack(lmu), rw_up=np.stack(wu), ra_up=np.stack(au), rg_up=np.stack(gu))
    d.update(rwkv_consts())
    return d


_NC_CACHE = {}
N_LAUNCH = 1


def kernel(**inp):
    x = np.asarray(inp["x"], np.float32)
    Bn, Sq, Dm = x.shape
    L = np.asarray(inp["w_in"]).shape[0]
    d_ff = np.asarray(inp["ffn_down"]).shape[1]
    heads_a, heads_c, heads_b = list(range(8)), list(range(4)), list(range(12))
    nl = N_LAUNCH if L % N_LAUNCH == 0 else 1
    Lp = L // nl
    xTs = [np.ascontiguousarray(x[b].T) for b in range(Bn)]
    per_layer = ("norm1_g", "w_in", "attn_sinks", "rwkv_mu", "rwkv_w0", "rwkv_w_up", "rwkv_a0", "rwkv_a_up", "rwkv_g_up", "rwkv_k_k",
                 "rwkv_k_a", "rwkv_r_k", "rwkv_lnx_g", "rwkv_lnx_b", "proj_a", "proj_b", "proj_c", "w_out", "norm2_g", "ffn_up",
                 "ffn_conv", "ffn_down")
    for li in range(nl):
        final = (li == nl - 1)
        key = (Sq, Lp, d_ff, final)
        if key not in _NC_CACHE:
            _NC_CACHE[key] = build_full(Sq, Lp, d_ff, heads_a, heads_c, heads_b, final=final)
        nc = _NC_CACHE[key]
        sub = dict(inp)
        for k in per_layer:
            sub[k] = np.asarray(inp[k])[li * Lp:(li + 1) * Lp]
        shared = host_inputs(sub, Lp, d_ff, heads_b)
        in_maps = []
        for b in range(Bn):
            m = dict(shared)
            m["xT"] = xTs[b]
            in_maps.append(m)
        res = run_bass_kernel_spmd(nc, in_maps, core_ids=list(range(Bn)))
        xTs = [np.ascontiguousarray(r["outT"]) for r in res.results]
    out = np.stack([np.ascontiguousarray(t.T) for t in xTs], axis=0)
    return out.astype(np.float32)
```

```python
import concourse.bass as bass
import concourse.mybir as mybir

ENGS = ("pe", "dve", "act", "pool", "sp")


_PH = [0]


def uname(n):
    return "%s_%d" % (n, _PH[0])


class Sched:
    _uid = 0

    def __init__(self, nc, n_dma_sems=24):
        self.nc = nc
        self.streams = {e: [] for e in ENGS}
        self.seq = {e: 0 for e in ENGS}
        self.waited = {}
        self.lastw = {}
        self.readers = {}
        self.n_dma = n_dma_sems
        self.dma_cnt = [0] * n_dma_sems
        self.dma_rr = 0
        self.sems = {}
        self.n_wait = 0
        self.alias = {}

    def canon(self, keys):
        return [self.alias.get(k, k) for k in keys]

    def eng(self, e):
        nc = self.nc
        return {"pe": nc.tensor, "dve": nc.vector, "act": nc.scalar, "pool": nc.gpsimd, "sp": nc.sync}[e]

    def _need(self, cons, deps):
        best = {}
        for p, v in deps:
            if p is None:
                continue
            if v > best.get(p, 0):
                best[p] = v
        out = []
        for p, v in best.items():
            if self.waited.get((cons, p), 0) >= v:
                continue
            self.waited[(cons, p)] = v
            out.append((p, v))
        return out

    def _deps(self, e, reads, writes, same_engine_war=False):
        deps = []
        me = ("eng", e)
        for k in reads:
            w = self.lastw.get(k)
            if w is not None:
                deps.append(w)
        for k in writes:
            w = self.lastw.get(k)
            if w is not None:
                deps.append(w)
            for p, v in self.readers.get(k, {}).items():
                deps.append((p, v))
        return deps

    def _record(self, prod, val, reads, writes):
        for k in reads:
            d = self.readers.setdefault(k, {})
            if d.get(prod, 0) < val:
                d[prod] = val
        for k in writes:
            self.lastw[k] = (prod, val)
            self.readers[k] = {}

    def op(self, e, fn, reads=(), writes=()):
        reads = self.canon(reads); writes = self.canon(writes)
        deps = self._deps(e, reads, writes)
        if e == "pe":
            deps = [d for d in deps if d[0] != ("eng", "pe")]
        waits = self._need(e, deps)
        self.seq[e] += 1
        val = self.seq[e]
        self.streams[e].append(("op", fn, waits, None))
        self._record(("eng", e), val, reads, writes)
        return val

    def dma(self, q, fn, reads=(), writes=()):
        reads = self.canon(reads); writes = self.canon(writes)
        deps = self._deps(q, reads, writes, same_engine_war=True)
        i = self.dma_rr
        self.dma_rr = (self.dma_rr + 1) % self.n_dma
        prod = ("dma", i)
        if self.dma_cnt[i] > 0:
            deps.append((prod, self.dma_cnt[i]))
        waits = self._need(q, deps)
        self.dma_cnt[i] += 16
        val = self.dma_cnt[i]
        self.streams[q].append(("dma", fn, waits, (i, 16)))
        self._record(prod, val, reads, writes)
        return prod, val

    def finish_waits(self, e="sp"):
        deps = [(("dma", i), c) for i, c in enumerate(self.dma_cnt) if c > 0]
        deps += [(("eng", x), self.seq[x]) for x in ENGS if self.seq[x] > 0 and x != e]
        waits = self._need(e, deps)
        self.streams[e].append(("wait", None, waits, None))

    def barrier_all(self):
        for e in ENGS:
            deps = [(("dma", i), c) for i, c in enumerate(self.dma_cnt) if c > 0]
            deps += [(("eng", x), self.seq[x]) for x in ENGS if self.seq[x] > 0 and x != e]
            waits = self._need(e, deps)
            self.streams[e].append(("wait", None, waits, None))

    def emit(self):
        nc = self.nc
        Sched._uid += 1
        u = Sched._uid
        esem = {e: nc.alloc_semaphore("s%d_%s" % (u, e)) for e in ENGS}
        dsem = [nc.alloc_semaphore("d%d_%d" % (u, i)) for i in range(self.n_dma)]

        def semof(p):
            return esem[p[1]] if p[0] == "eng" else dsem[p[1]]

        def run(e):
            def body(engine):
                for kind, fn, waits, dinfo in self.streams[e]:
                    for p, v in waits:
                        engine.wait_ge(semof(p), v)
                        self.n_wait += 1
                    if kind == "op":
                        fn(engine).then_inc(esem[e], 1)
                    elif kind == "dma":
                        fn(engine).then_inc(dsem[dinfo[0]], dinfo[1])
            return body

        with nc.Block() as block:
            block.tensor(run("pe"))
            block.vector(run("dve"))
            block.scalar(run("act"))
            block.gpsimd(run("pool"))
            block.sync(run("sp"))
        nc.all_engine_barrier()
        nc.clear_and_free_semaphores(list(esem.values()) + dsem)
        nc.all_engine_barrier()


import numpy as np
from concourse.bass_utils import run_bass_kernel_spmd

F32 = mybir.dt.float32
BF16 = mybir.dt.bfloat16
ALU = mybir.AluOpType
AF = mybir.ActivationFunctionType
AX = mybir.AxisListType

D_MODEL = 2048
NORM_EPS = 1e-5
KC = D_MODEL // 128


def emit_rmsnorm_T(S, nc, xT, hT, g_sb, ones_bf, sq, ps, rstd, ntok, keys):
    kx, kh, ksq, kps, krs = keys["x"], keys["h"], keys["sq"], keys["ps"], keys["rstd"]
    for c in range(KC):
        S.op("act", lambda e, c=c: e.activation(out=sq[:, c, :], in_=xT[:, c, :], func=AF.Square),
             reads=[kx], writes=[(ksq, c)])
    for c in range(KC):
        S.op("pe", lambda e, c=c: e.matmul(ps, ones_bf, sq[:, c, :], start=(c == 0), stop=(c == KC - 1)),
             reads=[(ksq, c)], writes=[kps])
    S.op("dve", lambda e: e.tensor_scalar(out=rstd, in0=ps, scalar1=1.0 / D_MODEL, scalar2=NORM_EPS,
                                          op0=ALU.mult, op1=ALU.add), reads=[kps], writes=[krs])
    S.op("act", lambda e: e.activation(out=rstd, in_=rstd, func=AF.Sqrt), reads=[krs], writes=[krs])
    S.op("dve", lambda e: e.reciprocal(out=rstd, in_=rstd), reads=[krs], writes=[krs])
    for c in range(KC):
        S.op("dve", lambda e, c=c: e.scalar_tensor_tensor(out=hT[:, c, :], in0=xT[:, c, :], scalar=g_sb[:, c:c + 1],
                                                         in1=rstd, op0=ALU.mult, op1=ALU.mult),
             reads=[kx, krs], writes=[kh])


def build_proj(T, ncols, TT=512):
    nc = bass.Bass("TRN2", target_bir_lowering=False)
    xT = nc.dram_tensor("xT", [D_MODEL, T], F32, kind="ExternalInput").ap()
    g = nc.dram_tensor("g", [128, KC], F32, kind="ExternalInput").ap()
    W = nc.dram_tensor("W", [D_MODEL, ncols], F32, kind="ExternalInput").ap()
    pT = nc.dram_tensor("pT", [ncols, T], F32, kind="ExternalOutput").ap()
    ntt = T // TT
    CB = 512
    ncb = (ncols + CB - 1) // CB
    import contextlib
    with contextlib.ExitStack() as st:
        sb = lambda name, shape, dt: st.enter_context(nc.sbuf_tensor(uname(name), shape, dt))
        ones_bf = sb("ones", [128, 128], BF16)
        g_sb = sb("g_sb", [128, KC], F32)
        hT = sb("hT", [128, KC, T], BF16)
        xin = [sb("xin%d" % i, [128, KC, TT], F32) for i in range(2)]
        sq = sb("sq", [128, KC, TT], BF16)
        rstd = sb("rstd", [128, TT], F32)
        wt = [sb("wt%d" % i, [128, KC, CB], BF16) for i in range(2)]
        ot = [sb("ot%d" % i, [128, TT], F32) for i in range(4)]
        pss = [st.enter_context(nc.psum_tensor(uname("ps%d" % i), [128, 512], F32)) for i in range(8)]
        _PH[0] += 1
        S = Sched(nc)
        S.op("pool", lambda e: e.memset(ones_bf[:], 1.0), writes=["ones"])
        S.dma("sp", lambda e: e.dma_start(out=g_sb[:], in_=g), writes=["g"])
        xTv = xT.rearrange("(c p) t -> p c t", p=128)
        Wv = W.rearrange("(c p) n -> p c n", p=128)
        for tt in range(ntt):
            xb = xin[tt % 2]
            S.dma("sp", lambda e, xb=xb, tt=tt: e.dma_start(out=xb[:], in_=xTv[:, :, tt * TT:(tt + 1) * TT]),
                  writes=[("xin", tt % 2)])
            keys = dict(x=("xin", tt % 2), h=("h", tt), sq="sq", ps=("ps", 0), rstd="rstd")
            emit_rmsnorm_T(S, nc, xb[:], hT[:, :, tt * TT:(tt + 1) * TT], g_sb[:], ones_bf[:], sq[:], pss[0][:, :TT],
                           rstd[:], TT, keys)
        n_o = 0
        n_ps = 0
        for cb in range(ncb):
            c0 = cb * CB
            cw = min(CB, ncols - c0)
            wb = wt[cb % 2]
            S.dma("pool", lambda e, wb=wb, c0=c0, cw=cw: e.dma_start(out=wb[:, :, :cw], in_=Wv[:, :, c0:c0 + cw]),
                  writes=[("wt", cb % 2)])
            for m0 in range(0, cw, 128):
                mw = min(128, cw - m0)
                for tt in range(ntt):
                    pi = 1 + (n_ps % 7); n_ps += 1
                    ps = pss[pi]
                    for c in range(KC):
                        S.op("pe", lambda e, ps=ps, wb=wb, c=c, m0=m0, mw=mw, tt=tt:
                             e.matmul(ps[:mw, :TT], wb[:, c, m0:m0 + mw], hT[:, c, tt * TT:(tt + 1) * TT],
                                      start=(c == 0), stop=(c == KC - 1)),
                             reads=[("wt", cb % 2), ("h", tt), "ones", "g"], writes=[("ps", pi)])
                    oi = n_o % 4; n_o += 1
                    ob = ot[oi]
                    eng = "act" if (n_o % 2) else "dve"
                    if eng == "act":
                        S.op("act", lambda e, ob=ob, ps=ps, mw=mw: e.copy(out=ob[:mw, :], in_=ps[:mw, :TT]),
                             reads=[("ps", pi)], writes=[("ot", oi)])
                    else:
                        S.op("dve", lambda e, ob=ob, ps=ps, mw=mw: e.tensor_copy(out=ob[:mw, :], in_=ps[:mw, :TT]),
                             reads=[("ps", pi)], writes=[("ot", oi)])
                    S.dma("sp", lambda e, ob=ob, mw=mw, r0=c0 + m0, tt=tt:
                          e.dma_start(out=pT[r0:r0 + mw, tt * TT:(tt + 1) * TT], in_=ob[:mw, :]),
                          reads=[("ot", oi)])
        S.finish_waits("sp")
        S.emit()
    return nc


import math

ROW_AQ, ROW_AK, ROW_AV = 0, 512, 640
ROW_BR, ROW_BK, ROW_BV, ROW_WD, ROW_AD, ROW_GD = 768, 1536, 2304, 3072, 3168, 3264
ROW_CQ, ROW_CK, ROW_CV = 3520, 4288, 5056
NP_ROWS = 5824
YROW_A, YROW_B, YROW_C = 0, 512, 1280
C_DILS = (1, 4, 16)


def ssl(c0, n, d):
    return slice(c0, c0 + d * (n - 1) + 1, d)


def t5_bucket_np(dist):
    dist = np.asarray(dist, np.int64)
    nf = np.maximum(dist, 1).astype(np.float32)
    large = 16 + (np.log(nf / np.float32(16)) / np.float32(math.log(2048 / 16)) * np.float32(16)).astype(np.int32)
    return np.where(dist < 16, dist, np.minimum(large, 31))


def attn_tables(rel_bias):
    i = np.arange(128)[None, :]
    j = np.arange(128)[:, None]
    dists = (i - j, i + 128 - j)
    specs = [(h, 1, 127) for h in range(8)] + [(8 + g * 4 + hh, dil, 128) for g, dil in enumerate(C_DILS)
                                              for hh in range(4)]
    gathered = np.zeros((20, 128, 256), np.float32)
    mul = np.zeros((20, 128, 256), np.float32)
    add = np.zeros((20, 128, 256), np.float32)
    for n, (col, dil, ms) in enumerate(specs):
        for half, d in enumerate(dists):
            valid = (d >= 0) & (d <= ms)
            idx = t5_bucket_np(np.maximum(d, 0) * dil)
            gathered[n, :, half * 128:(half + 1) * 128] = rel_bias[idx, col]
            mul[n, :, half * 128:(half + 1) * 128] = np.where(valid, 8.0, 0.0)
            add[n, :, half * 128:(half + 1) * 128] = np.where(valid, 0.0, -240000.0)
    return gathered, mul, add


def emit_attention(S, nc, st, T, pT, yT, tabs_g, tabs_m, tabs_a, sinks_rep, ident_d, pss, heads_a, heads_c):
    sb = lambda name, shape, dt: st.enter_context(nc.sbuf_tensor(uname(name), shape, dt))
    NB = T // 128
    q_bf = sb("at_q", [64, T], BF16)
    k_bf = sb("at_k", [64, T], BF16)
    v_f = sb("at_v", [64, T], F32)
    stg = sb("at_stg", [64, T], F32)
    vaug = sb("at_vaug", [128, NB, 65], BF16)
    acc = sb("at_acc", [65, T], F32)
    tb_g = sb("at_tbg", [128, 256], F32)
    tb_m = sb("at_tbm", [128, 256], F32)
    tb_a = sb("at_tba", [128, 256], F32)
    tb = sb("at_tb", [128, 256], BF16)
    pt_sb = [sb("at_pt%d" % i, [128, 512], BF16) for i in range(2)]
    ident_f = sb("at_identf", [128, 128], F32)
    ident_b = sb("at_identb", [128, 128], BF16)
    sel = sb("at_sel", [65, 64], F32)
    esink = sb("at_esink", [64, 8], F32)
    den = sb("at_den", [64, 512], F32)
    yo = [sb("at_yo%d" % i, [64, 512], F32) for i in range(2)]

    S.dma("sp", lambda e: e.dma_start(out=ident_f[:], in_=ident_d), writes=["at_identf"])
    S.op("dve", lambda e: e.tensor_copy(out=ident_b[:], in_=ident_f[:]), reads=["at_identf"], writes=["at_identb"])
    S.op("pool", lambda e: e.memset(sel[:], 0.0), writes=["at_sel"])
    S.op("pool", lambda e: e.memset(sel[64:65, :], 1.0), writes=["at_sel"])
    S.op("pool", lambda e: e.memset(vaug[:, :, 64:65], 1.0), writes=["at_vaug1"])
    S.dma("sp", lambda e: e.dma_start(out=esink[:], in_=sinks_rep), writes=["at_esink"])
    S.op("act", lambda e: e.activation(out=esink[:], in_=esink[:], func=AF.Exp), reads=["at_esink"],
         writes=["at_esink"])

    ps_s = [pss[0], pss[1]]
    ps_o = [pss[2], pss[3]]
    ps_t = pss[4]
    ps_d = pss[5]
    cnt = dict(s=0, o=0, y=0)

    def load_table(n):
        S.dma("sp", lambda e: e.dma_start(out=tb_g[:], in_=tabs_g[n]), writes=["at_tbg"])
        S.dma("sp", lambda e: e.dma_start(out=tb_m[:], in_=tabs_m[n]), writes=["at_tbm"])
        S.dma("sp", lambda e: e.dma_start(out=tb_a[:], in_=tabs_a[n]), writes=["at_tba"])
        S.op("pool", lambda e: e.tensor_tensor(out=tb_g[:], in0=tb_g[:], in1=tb_m[:], op=ALU.mult),
             reads=["at_tbg", "at_tbm"], writes=["at_tbg"])
        S.op("pool", lambda e: e.tensor_tensor(out=tb[:], in0=tb_g[:], in1=tb_a[:], op=ALU.add),
             reads=["at_tbg", "at_tba"], writes=["at_tb"])

    def load_kv(krow, vrow, dil):
        S.dma("act", lambda e: e.dma_start(out=stg[:], in_=pT[krow:krow + 64, :]), reads=["pT"], writes=["at_stg"])
        S.op("pool", lambda e: e.tensor_copy(out=k_bf[:], in_=stg[:]), reads=["at_stg"], writes=["at_k"])
        S.dma("sp", lambda e: e.dma_start(out=v_f[:], in_=pT[vrow:vrow + 64, :]), reads=["pT"], writes=["at_v"])
        Lf = T // dil
        bps = Lf // 128
        for vb0 in range(0, NB, 8):
            for u in range(8):
                vb = vb0 + u
                s_, jb = vb // bps, vb % bps
                c0 = s_ + dil * 128 * jb
                src = v_f[0:64, ssl(c0, 128, dil)]
                S.op("pe", lambda e, u=u, src=src: e.transpose(ps_t[:, u * 64:(u + 1) * 64], src, ident_f[0:64, 0:64]),
                     reads=["at_v", "at_identf"], writes=["ps_t"])
            S.op("dve", lambda e, vb0=vb0: e.tensor_copy(out=vaug[:, vb0:vb0 + 8, 0:64],
                                                        in_=ps_t[:, :].rearrange("p (u d) -> p u d", d=64)),
                 reads=["ps_t"], writes=["at_vaug"])

    def run_seq(dil, first_group):
        Lf = T // dil
        bps = Lf // 128
        nbt = min(4, bps)
        for s_ in range(dil):
            for jb0 in range(0, bps, nbt):
                oi = cnt["o"] % 2; cnt["o"] += 1
                po = ps_o[oi]
                for half in range(0, nbt, 2):
                    si = cnt["s"] % 2; cnt["s"] += 1
                    pst = ps_s[si]
                    ptb = pt_sb[si]
                    nb2 = min(2, nbt - half)
                    lo = 512
                    for r in range(nb2):
                        jb = jb0 + half + r
                        qs = s_ + dil * 128 * jb
                        qv = q_bf[:, ssl(qs, 128, dil)]
                        kv = k_bf[:, ssl(qs, 128, dil)]
                        sl_prev = slice((2 * r) * 128, (2 * r + 1) * 128)
                        sl_cur = slice((2 * r + 1) * 128, (2 * r + 2) * 128)
                        if jb > 0:
                            ks = s_ + dil * 128 * (jb - 1)
                            kpv = k_bf[:, ssl(ks, 128, dil)]
                            S.op("pe", lambda e, pst=pst, sl=sl_prev, kpv=kpv, qv=qv:
                                 e.matmul(pst[:, sl], kpv, qv, start=True, stop=False),
                                 reads=["at_k", "at_q"], writes=[("ps_s", si)])
                            S.op("pe", lambda e, pst=pst, sl=sl_prev:
                                 e.matmul(pst[:, sl], ident_b[:], tb[:, 128:256], start=False, stop=True),
                                 reads=["at_tb", "at_identb"], writes=[("ps_s", si)])
                            lo = min(lo, sl_prev.start)
                        S.op("pe", lambda e, pst=pst, sl=sl_cur, kv=kv, qv=qv:
                             e.matmul(pst[:, sl], kv, qv, start=True, stop=False),
                             reads=["at_k", "at_q"], writes=[("ps_s", si)])
                        S.op("pe", lambda e, pst=pst, sl=sl_cur:
                             e.matmul(pst[:, sl], ident_b[:], tb[:, 0:128], start=False, stop=True),
                             reads=["at_tb", "at_identb"], writes=[("ps_s", si)])
                        lo = min(lo, sl_cur.start)
                    hi = nb2 * 256
                    S.op("act", lambda e, ptb=ptb, pst=pst, lo=lo, hi=hi:
                         e.activation(out=ptb[:, lo:hi], in_=pst[:, lo:hi], func=AF.Exp, scale=0.125),
                         reads=[("ps_s", si)], writes=[("at_pt", si)])
                    for r in range(nb2):
                        jb = jb0 + half + r
                        vb = s_ * bps + jb
                        osl = slice((half + r) * 128, (half + r + 1) * 128)
                        S.op("pe", lambda e, po=po, osl=osl, vb=vb, ptb=ptb, r=r, last=(jb == 0):
                             e.matmul(po[0:65, osl], vaug[:, vb, :], ptb[:, (2 * r + 1) * 128:(2 * r + 2) * 128],
                                      start=True, stop=last),
                             reads=["at_vaug", "at_vaug1", ("at_pt", si)], writes=[("ps_o", oi)])
                        if jb > 0:
                            S.op("pe", lambda e, po=po, osl=osl, vb=vb, ptb=ptb, r=r:
                                 e.matmul(po[0:65, osl], vaug[:, vb - 1, :], ptb[:, (2 * r) * 128:(2 * r + 1) * 128],
                                          start=False, stop=True),
                                 reads=["at_vaug", "at_vaug1", ("at_pt", si)], writes=[("ps_o", oi)])
                t0 = s_ + dil * 128 * jb0
                n_el = nbt * 128
                av = acc[:, ssl(t0, n_el, dil)]
                if first_group:
                    S.op("dve", lambda e, av=av, po=po, n_el=n_el: e.tensor_copy(out=av, in_=po[0:65, 0:n_el]),
                         reads=[("ps_o", oi)], writes=["at_acc"])
                else:
                    S.op("dve", lambda e, av=av, po=po, n_el=n_el:
                         e.tensor_tensor(out=av, in0=po[0:65, 0:n_el], in1=av, op=ALU.add),
                         reads=[("ps_o", oi), "at_acc"], writes=["at_acc"])

    def normalize(yrow, sink_col):
        for t0 in range(0, T, 512):
            S.op("pe", lambda e, t0=t0: e.matmul(ps_d[0:64, :], sel[:], acc[:, t0:t0 + 512], start=True, stop=True),
                 reads=["at_acc", "at_sel"], writes=["ps_d"])
            if sink_col is not None:
                S.op("dve", lambda e: e.tensor_scalar(out=den[:], in0=ps_d[0:64, :],
                                                      scalar1=esink[:, sink_col:sink_col + 1], scalar2=None,
                                                      op0=ALU.add),
                     reads=["ps_d", "at_esink"], writes=["at_den"])
                S.op("dve", lambda e: e.reciprocal(out=den[:], in_=den[:]), reads=["at_den"], writes=["at_den"])
            else:
                S.op("dve", lambda e: e.reciprocal(out=den[:], in_=ps_d[0:64, :]), reads=["ps_d"], writes=["at_den"])
            yi = cnt["y"] % 2; cnt["y"] += 1
            yb = yo[yi]
            S.op("dve", lambda e, yb=yb, t0=t0: e.tensor_tensor(out=yb[:], in0=acc[0:64, t0:t0 + 512], in1=den[:],
                                                                op=ALU.mult),
                 reads=["at_acc", "at_den"], writes=[("at_yo", yi)])
            S.dma("sp", lambda e, yb=yb, t0=t0: e.dma_start(out=yT[yrow:yrow + 64, t0:t0 + 512], in_=yb[:]),
                  reads=[("at_yo", yi)], writes=["yT"])

    last_kv = None
    for h in heads_a:
        kvh = h // 4
        if last_kv != kvh:
            load_kv(ROW_AK + 64 * kvh, ROW_AV + 64 * kvh, 1)
            last_kv = kvh
        S.dma("act", lambda e, h=h: e.dma_start(out=stg[:], in_=pT[ROW_AQ + 64 * h:ROW_AQ + 64 * h + 64, :]),
              reads=["pT"], writes=["at_stg"])
        S.op("pool", lambda e: e.tensor_copy(out=q_bf[:], in_=stg[:]), reads=["at_stg"], writes=["at_q"])
        load_table(h)
        run_seq(1, True)
        normalize(YROW_A + 64 * h, h)
    for hh in heads_c:
        for g, dil in enumerate(C_DILS):
            off = g * 256 + hh * 64
            load_kv(ROW_CK + off, ROW_CV + off, dil)
            S.dma("act", lambda e, off=off: e.dma_start(out=stg[:], in_=pT[ROW_CQ + off:ROW_CQ + off + 64, :]),
                  reads=["pT"], writes=["at_stg"])
            S.op("pool", lambda e: e.tensor_copy(out=q_bf[:], in_=stg[:]), reads=["at_stg"], writes=["at_q"])
            load_table(8 + g * 4 + hh)
            run_seq(dil, g == 0)
        normalize(YROW_C + 64 * hh, None)


def build_attn_test(T, heads_a, heads_c):
    nc = bass.Bass("TRN2", target_bir_lowering=False)
    pT = nc.dram_tensor("pT", [NP_ROWS, T], F32, kind="ExternalInput").ap()
    tg = nc.dram_tensor("tabs_g", [20, 128, 256], F32, kind="ExternalInput").ap()
    tm = nc.dram_tensor("tabs_m", [20, 128, 256], F32, kind="ExternalInput").ap()
    ta = nc.dram_tensor("tabs_a", [20, 128, 256], F32, kind="ExternalInput").ap()
    sk = nc.dram_tensor("sinks", [64, 8], F32, kind="ExternalInput").ap()
    idd = nc.dram_tensor("ident", [128, 128], F32, kind="ExternalInput").ap()
    yT = nc.dram_tensor("yT", [1536, T], F32, kind="ExternalOutput").ap()
    import contextlib
    with contextlib.ExitStack() as st:
        pss = [st.enter_context(nc.psum_tensor(uname("ps%d" % i), [128, 512], F32)) for i in range(8)]
        _PH[0] += 1
        S = Sched(nc)
        emit_attention(S, nc, st, T, pT, yT, tg, tm, ta, sk, idd, pss, heads_a, heads_c)
        S.finish_waits("sp")
        S.emit()
    return nc


CH = 64
B_GN_EPS = 64e-5


def rwkv_consts():
    s_ = np.arange(64)[:, None]; t_ = np.arange(64)[None, :]
    lt = (s_ < t_).astype(np.float32); le = (s_ <= t_).astype(np.float32); gt = (s_ > t_).astype(np.float32)
    m_lt2 = np.ascontiguousarray(np.broadcast_to(np.concatenate([lt, lt], 1)[:, None, :], (64, 3, 128)))
    m_le2 = np.ascontiguousarray(np.broadcast_to(np.concatenate([le, le], 1)[:, None, :], (64, 3, 128)))
    m_gt = np.ascontiguousarray(np.broadcast_to(gt[:, None, :], (64, 3, 64)))
    rst = np.ones((64, 512), np.float32); rst[:, ::64] = 0.0
    return dict(m_lt2=m_lt2, m_le2=m_le2, m_gt=m_gt, rst=rst)


def phase_rwkv(nc, T, pT, yT, prm_d, lmu_d, wup_d, aup_d, gup_d, ident_d, mlt_d, mle_d, mgt_d, rst_d, heads, dbg=None, stop=None):
    import contextlib
    HG = 3
    TT = 512
    NCK = TT // CH
    NH = len(heads)
    with contextlib.ExitStack() as st:
        sb = lambda name, shape, dt=F32: st.enter_context(nc.sbuf_tensor(uname(name), shape, dt))
        pss = [st.enter_context(nc.psum_tensor(uname("rps%d" % i), [128, 512], F32)) for i in range(8)]
        _PH[0] += 1
        S = Sched(nc)
        ident = sb("rw_ident", [128, 128]); m_lt2 = sb("rw_mlt", [64, 3, 128]); m_le2 = sb("rw_mle", [64, 3, 128])
        m_gt = sb("rw_mgt", [64, 3, 64]); rst = sb("rw_rst", [64, 512])
        prm = sb("rw_prm", [64, NH, 10]); lmu = sb("rw_lmu", [128, 4])
        wup = sb("rw_wup", [96, NH * 64]); aup = sb("rw_aup", [96, NH * 64]); gup = sb("rw_gup", [128, 2, NH * 64])
        ones64 = sb("rw_ones", [64, 64]); avg64 = sb("rw_avg", [64, 64]); rkb = sb("rw_rkb", [64, NH, 64])
        for t_, d_ in ((ident, ident_d), (m_lt2, mlt_d), (m_le2, mle_d), (m_gt, mgt_d), (rst, rst_d), (prm, prm_d),
                       (lmu, lmu_d), (wup, wup_d), (aup, aup_d)):
            S.dma("sp", lambda e, t_=t_, d_=d_: e.dma_start(out=t_[:], in_=d_), writes=["const"])
        S.dma("sp", lambda e: e.dma_start(out=gup[:], in_=gup_d.rearrange("(c p) n -> p c n", p=128)), writes=["const"])
        S.op("pool", lambda e: e.memset(ones64[:], 1.0), writes=["const"])
        S.op("pool", lambda e: e.memset(avg64[:], 1.0 / 64), writes=["const"])
        for hi in range(NH):
            S.op("dve", lambda e, hi=hi: e.tensor_scalar(out=rkb[:, hi, :], in0=ones64[:], scalar1=prm[:, hi, 7:8],
                                                        scalar2=None, op0=ALU.mult), reads=["const"], writes=["const"])
        lin = [sb("rw_lin%d" % i, [128, TT + 1]) for i in range(4)]
        ltmp = sb("rw_ltmp", [128, TT])
        th = sb("rw_th", [96, TT]); adm = sb("rw_adm", [96, TT]); sg = sb("rw_sg", [128, 2, TT])
        xin = [sb("rw_xin%d" % i, [64, HG, TT + 1]) for i in range(3)]
        names = ["rm", "km", "vm", "logw", "iclr", "g", "kkn", "k2", "sbon", "Lc", "G", "t0", "t1", "t2",
                 "at"]
        B = {n: sb("rw_" + n, [64, HG, TT]) for n in names}
        for n in ("rt", "bt", "kt", "atb"):
            B[n] = sb("rw_" + n, [64, HG, TT], BF16)
        B["bh"] = B["iclr"]; B["kh"] = B["kkn"]; B["y"] = B["logw"]
        S.alias.update({"bh": "iclr", "kh": "kkn", "y": "logw", ("yo", 0): "Lc", ("yo", 1): "Lc"})
        RhT = sb("rw_RhT", [64, NCK, HG, 64]); Y0T = sb("rw_Y0T", [64, NCK, HG, 64])
        MTa = sb("rw_MT", [64, NCK, HG, 64]); Na = sb("rw_N", [64, NCK, HG, 64])
        Wp = [sb("rw_W%d" % p, [64, HG, 128], BF16) for p in range(2)]; Vtokp = [sb("rw_Vtok%d" % p, [64, HG, 64], BF16) for p in range(2)]
        BKp = [sb("rw_BK%d" % p, [64, HG, 128], BF16) for p in range(2)]; AQp = [sb("rw_AQ%d" % p, [64, HG, 128], BF16) for p in range(2)]
        QPp = [[sb("rw_QP%d_%d" % (p, i), [64, HG, 128], BF16) for i in range(2)] for p in range(2)]
        ARKp = [sb("rw_ARK%d" % p, [64, HG, 128], BF16) for p in range(2)]
        Hs = [sb("rw_H%d" % i, [64, HG, 64]) for i in range(2)]
        yout = [B["Lc"], B["Lc"]]
        cnt = dict(l=0, m=0, y=0)

        def ps_l():
            i = cnt["l"] % 2; cnt["l"] += 1
            return pss[i], ("ps", i)

        def ps_m():
            i = 4 + cnt["m"] % 2; cnt["m"] += 1
            return pss[i], ("ps", i)

        def dve(fn, r, w): S.op("dve", fn, reads=r, writes=w)
        def act(fn, r, w): S.op("act", fn, reads=r, writes=w)
        def pool(fn, r, w): S.op("pool", fn, reads=r, writes=w)
        def pe(fn, r, w): S.op("pe", fn, reads=r, writes=w)

        for g0 in range(0, NH, HG):
            hs = heads[g0:g0 + HG]
            pool(lambda e: e.memset(Hs[0][:], 0.0), [], ["H0"])
            st_h = dict(hcur=0)

            def do_tile(ti, g0=g0, hs=hs, st_h=st_h):
                t0 = ti * TT
                srcs = [(ROW_WD, 96), (ROW_AD, 96), (ROW_GD, 128), (ROW_GD + 128, 128)]
                for i, (row, n) in enumerate(srcs):
                    if t0 == 0:
                        pool(lambda e, i=i, n=n: e.memset(lin[i][0:n, 0:1], 0.0), [], ["lin%d" % i])
                        S.dma("sp", lambda e, i=i, row=row, n=n: e.dma_start(out=lin[i][0:n, 1:TT + 1],
                                                                          in_=pT[row:row + n, 0:TT]),
                              reads=["pT"], writes=["lin%d" % i])
                    else:
                        S.dma("sp", lambda e, i=i, row=row, n=n: e.dma_start(out=lin[i][0:n, :],
                                                                          in_=pT[row:row + n, t0 - 1:t0 + TT]),
                              reads=["pT"], writes=["lin%d" % i])
                for i, row0 in enumerate((ROW_BR, ROW_BK, ROW_BV)):
                    for j, h in enumerate(hs):
                        row = row0 + 64 * h
                        if t0 == 0:
                            pool(lambda e, i=i, j=j: e.memset(xin[i][:, j, 0:1], 0.0), [], ["xin%d" % i])
                            S.dma("act", lambda e, i=i, j=j, row=row: e.dma_start(out=xin[i][:, j, 1:TT + 1],
                                                                              in_=pT[row:row + 64, 0:TT]),
                                  reads=["pT"], writes=["xin%d" % i])
                        else:
                            S.dma("act", lambda e, i=i, j=j, row=row: e.dma_start(out=xin[i][:, j, :],
                                                                              in_=pT[row:row + 64, t0 - 1:t0 + TT]),
                                  reads=["pT"], writes=["xin%d" % i])
                outs = [th, adm, sg[:, 0, :], sg[:, 1, :]]
                for i, (row, n) in enumerate(srcs):
                    pool(lambda e, i=i, n=n: e.tensor_tensor(out=ltmp[0:n, :], in0=lin[i][0:n, 0:TT], in1=lin[i][0:n, 1:TT + 1],
                                                           op=ALU.subtract), ["lin%d" % i], ["ltmp"])
                    o = outs[i]
                    dve(lambda e, i=i, n=n, o=o: e.scalar_tensor_tensor(out=o[0:n, :] if i < 2 else o, in0=ltmp[0:n, :],
                                                                       scalar=lmu[0:n, i:i + 1], in1=lin[i][0:n, 1:TT + 1],
                                                                       op0=ALU.mult, op1=ALU.add),
                        ["ltmp", "lin%d" % i, "const"], ["lo%d" % i])
                act(lambda e: e.activation(out=th[:], in_=th[:], func=AF.Tanh), ["lo0"], ["lo0"])
                act(lambda e: e.activation(out=sg[:, 0, :], in_=sg[:, 0, :], func=AF.Sigmoid), ["lo2"], ["lo2"])
                act(lambda e: e.activation(out=sg[:, 1, :], in_=sg[:, 1, :], func=AF.Sigmoid), ["lo3"], ["lo3"])
                for i, nm in enumerate(("rm", "km", "vm")):
                    pool(lambda e, i=i: e.tensor_tensor(out=B["t0"][:], in0=xin[i][:, :, 0:TT], in1=xin[i][:, :, 1:TT + 1],
                                                      op=ALU.subtract), ["xin%d" % i], ["t0"])
                    for j in range(HG):
                        dve(lambda e, i=i, j=j, nm=nm: e.scalar_tensor_tensor(
                            out=B[nm][:, j, :], in0=B["t0"][:, j, :], scalar=prm[:, g0 + j, i:i + 1],
                            in1=xin[i][:, j, 1:TT + 1], op0=ALU.mult, op1=ALU.add),
                            ["t0", "xin%d" % i, "const"], [nm])
                for j in range(HG):
                    hj = g0 + j
                    cs = slice(hj * 64, hj * 64 + 64)
                    p1, k1 = ps_l()
                    pe(lambda e, p1=p1, cs=cs: e.matmul(p1[0:64, :], wup[:, cs], th[:], start=True, stop=True),
                       ["lo0", "const"], [k1])
                    act(lambda e, p1=p1, j=j, hj=hj: e.activation(out=B["logw"][:, j, :], in_=p1[0:64, :], func=AF.Sigmoid,
                                                               bias=prm[:, hj, 3:4]), [k1, "const"], ["logw"])
                    p2, k2_ = ps_l()
                    pe(lambda e, p2=p2, cs=cs: e.matmul(p2[0:64, :], aup[:, cs], adm[:], start=True, stop=True),
                       ["lo1", "const"], [k2_])
                    act(lambda e, p2=p2, j=j, hj=hj: e.activation(out=B["iclr"][:, j, :], in_=p2[0:64, :], func=AF.Sigmoid,
                                                               bias=prm[:, hj, 4:5]), [k2_, "const"], ["iclr"])
                    p3, k3 = ps_l()
                    for c in range(2):
                        pe(lambda e, p3=p3, cs=cs, c=c: e.matmul(p3[0:64, :], gup[:, c, cs], sg[:, c, :], start=(c == 0),
                                                               stop=(c == 1)), ["lo2", "lo3", "const"], [k3])
                    act(lambda e, p3=p3, j=j: e.copy(out=B["g"][:, j, :], in_=p3[0:64, :]), [k3], ["g"])
                    dve(lambda e, j=j, hj=hj: e.tensor_scalar(out=B["kkn"][:, j, :], in0=B["km"][:, j, :],
                                                             scalar1=prm[:, hj, 5:6], scalar2=None, op0=ALU.mult),
                        ["km", "const"], ["kkn"])
                    pool(lambda e, j=j: e.tensor_tensor(out=B["t1"][:, j, :], in0=B["kkn"][:, j, :], in1=B["kkn"][:, j, :],
                                                      op=ALU.mult), ["kkn"], ["t1"])
                    p4, k4 = ps_l()
                    pe(lambda e, p4=p4, j=j: e.matmul(p4[0:64, :], ones64[:], B["t1"][:, j, :], start=True, stop=True),
                       ["t1", "const"], [k4])
                    act(lambda e, p4=p4, j=j: e.activation(out=B["t2"][:, j, :], in_=p4[0:64, :], func=AF.Sqrt), [k4], ["t2"])
                    dve(lambda e, j=j: e.tensor_scalar(out=B["t2"][:, j, :], in0=B["t2"][:, j, :], scalar1=1e-12, scalar2=None,
                                                      op0=ALU.max), ["t2"], ["t2"])
                    dve(lambda e, j=j: e.reciprocal(out=B["t2"][:, j, :], in_=B["t2"][:, j, :]), ["t2"], ["t2"])
                    dve(lambda e, j=j, hj=hj: e.tensor_scalar(out=B["k2"][:, j, :], in0=B["iclr"][:, j, :], scalar1=-1.0,
                                                             scalar2=prm[:, hj, 6:7], op0=ALU.add, op1=ALU.mult),
                        ["iclr", "const"], ["k2"])
                dve(lambda e: e.tensor_tensor(out=B["kkn"][:], in0=B["kkn"][:], in1=B["t2"][:], op=ALU.mult),
                    ["kkn", "t2"], ["kkn"])
                dve(lambda e: e.scalar_tensor_tensor(out=B["k2"][:], in0=B["k2"][:], scalar=1.0, in1=B["km"][:],
                                                     op0=ALU.add, op1=ALU.mult), ["k2", "km"], ["k2"])
                pool(lambda e: e.tensor_tensor(out=B["t1"][:], in0=B["rm"][:], in1=B["k2"][:], op=ALU.mult),
                     ["rm", "k2"], ["t1"])
                for j in range(HG):
                    p5, k5 = ps_l()
                    pe(lambda e, p5=p5, j=j: e.matmul(p5[0:64, :], rkb[:, g0 + j, :], B["t1"][:, j, :], start=True, stop=True),
                       ["t1", "const"], [k5])
                    act(lambda e, p5=p5, j=j: e.copy(out=B["sbon"][:, j, :], in_=p5[0:64, :]), [k5], ["sbon"])
                dve(lambda e: e.tensor_scalar(out=B["logw"][:], in0=B["logw"][:], scalar1=-math.exp(-0.5), scalar2=None,
                                              op0=ALU.mult), ["logw"], ["logw"])
                for j in range(HG):
                    dve(lambda e, j=j: e.tensor_tensor_scan(out=B["Lc"][:, j, :], data0=rst[:], data1=B["logw"][:, j, :],
                                                           initial=0.0, op0=ALU.mult, op1=ALU.add),
                        ["logw", "const"], ["Lc"])
                act(lambda e: e.activation(out=B["G"][:], in_=B["Lc"][:], func=AF.Exp), ["Lc"], ["G"])
                pool(lambda e: e.tensor_tensor(out=B["rt"][:], in0=B["rm"][:], in1=B["G"][:], op=ALU.mult), ["rm", "G"], ["rt"])
                dve(lambda e: e.tensor_tensor(out=B["t0"][:], in0=B["Lc"][:], in1=B["logw"][:], op=ALU.subtract),
                    ["Lc", "logw"], ["t0"])
                act(lambda e: e.activation(out=B["t0"][:], in_=B["t0"][:], func=AF.Exp), ["t0"], ["t0"])
                dve(lambda e: e.scalar_tensor_tensor(out=B["at"][:], in0=B["kkn"][:], scalar=-1.0, in1=B["t0"][:],
                                                     op0=ALU.mult, op1=ALU.mult), ["kkn", "t0"], ["at"])
                pool(lambda e: e.tensor_copy(out=B["atb"][:], in_=B["at"][:]), ["at"], ["atb"])
                pool(lambda e: e.tensor_tensor(out=B["t2"][:], in0=B["kkn"][:], in1=B["iclr"][:], op=ALU.mult),
                     ["kkn", "iclr"], ["t2"])
                act(lambda e: e.activation(out=B["t1"][:], in_=B["Lc"][:], func=AF.Exp, scale=-1.0), ["Lc", "t1"], ["t1"])
                dve(lambda e: e.tensor_tensor(out=B["bt"][:], in0=B["t2"][:], in1=B["t1"][:], op=ALU.mult), ["t2", "t1"], ["bt"])
                pool(lambda e: e.tensor_tensor(out=B["kt"][:], in0=B["k2"][:], in1=B["t1"][:], op=ALU.mult), ["k2", "t1"], ["kt"])
                for j in range(HG):
                    lc3 = B["Lc"][:, j, :].rearrange("p (c t) -> p c t", t=CH)
                    o3 = B["t0"][:, j, :].rearrange("p (c t) -> p c t", t=CH)
                    dve(lambda e, lc3=lc3, o3=o3: e.tensor_tensor(out=o3, in0=lc3[:, :, CH - 1:CH].to_broadcast([64, NCK, CH]),
                                                                 in1=lc3, op=ALU.subtract), ["Lc", "at"], ["t0"])
                act(lambda e: e.activation(out=B["t0"][:], in_=B["t0"][:], func=AF.Exp), ["t0"], ["t0"])
                dve(lambda e: e.tensor_tensor(out=B["bh"][:], in0=B["t2"][:], in1=B["t0"][:], op=ALU.mult), ["t2", "t0"], ["bh"])
                pool(lambda e: e.tensor_tensor(out=B["kh"][:], in0=B["k2"][:], in1=B["t0"][:], op=ALU.mult), ["k2", "t0"], ["kh"])

                if dbg is not None and ti == 0 and g0 == 0:
                    for di_, nm_ in enumerate(["rm", "km", "vm", "logw", "iclr", "g", "kkn", "k2", "sbon", "Lc", "G", "rt", "at", "bt", "kt", "bh", "kh"]):
                        S.dma("sp", lambda e, di_=di_, nm_=nm_: e.dma_start(out=dbg[di_], in_=B[nm_][:]), reads=[nm_], writes=["dbg"])
                if stop == "prep":
                    return
                def do_chunk(c, pb):
                    W, Vtok, BK, AQ, QP, ARK = Wp[pb], Vtokp[pb], BKp[pb], AQp[pb], QPp[pb], ARKp[pb]
                    kW, kV, kBK, kAQ, kARK = 'W%d' % pb, 'Vtok%d' % pb, 'BK%d' % pb, 'AQ%d' % pb, 'ARK%d' % pb
                    pw_i, pq_i = (6, 0) if pb == 0 else (7, 1)
                    csl = slice(c * CH, (c + 1) * CH)
                    tp1, ktp1 = pss[2], ("ps", 2)
                    tp2, ktp2 = pss[3], ("ps", 3)
                    for j in range(HG):
                        pe(lambda e, j=j: e.transpose(tp1[0:64, j * 128:j * 128 + 64], B["at"][:, j, csl], ident[0:64, 0:64]),
                           ["at", "const"], [ktp1])
                        pe(lambda e, j=j: e.transpose(tp1[0:64, j * 128 + 64:j * 128 + 128], B["vm"][:, j, csl], ident[0:64, 0:64]),
                           ["vm", "const"], [ktp1])
                        pe(lambda e, j=j: e.transpose(tp2[0:64, j * 128:j * 128 + 64], B["bh"][:, j, csl], ident[0:64, 0:64]),
                           ["bh", "const"], [ktp2])
                        pe(lambda e, j=j: e.transpose(tp2[0:64, j * 128 + 64:j * 128 + 128], B["kh"][:, j, csl], ident[0:64, 0:64]),
                           ["kh", "const"], [ktp2])
                    tp1v = tp1[0:64, 0:HG * 128].rearrange("p (h x) -> p h x", x=128)
                    tp2v = tp2[0:64, 0:HG * 128].rearrange("p (h x) -> p h x", x=128)
                    act(lambda e, tp1v=tp1v: e.copy(out=W[:, :, 0:64], in_=tp1v[:, :, 0:64]), [ktp1], [kW])
                    act(lambda e, tp1v=tp1v: e.copy(out=Vtok[:], in_=tp1v[:, :, 64:128]), [ktp1], [kV])
                    act(lambda e, tp2v=tp2v: e.copy(out=BK[:], in_=tp2v), [ktp2], [kBK])
                    yield
                    if stop == "c1":
                        return
                    m1, km1 = ps_m()
                    for j in range(HG):
                        pe(lambda e, m1=m1, j=j: e.matmul(m1[0:64, j * 128:j * 128 + 64], B["kt"][:, j, csl], B["atb"][:, j, csl],
                                                        start=True, stop=True), ["kt", "atb"], [km1])
                        pe(lambda e, m1=m1, j=j: e.matmul(m1[0:64, j * 128 + 64:j * 128 + 128], B["bt"][:, j, csl], B["atb"][:, j, csl],
                                                        start=True, stop=True), ["bt", "atb"], [km1])
                    m1v = m1[0:64, 0:HG * 128].rearrange("p (h x) -> p h x", x=128)
                    dve(lambda e, m1v=m1v: e.tensor_tensor(out=AQ[:], in0=m1v, in1=m_lt2[:], op=ALU.mult), [km1, "const"], [kAQ])
                    m2, km2 = ps_m()
                    for j in range(HG):
                        pe(lambda e, m2=m2, j=j: e.matmul(m2[0:64, j * 64:j * 64 + 64], B["atb"][:, j, csl], B["bt"][:, j, csl],
                                                        start=True, stop=True), ["bt", "atb"], [km2])
                    m2v = m2[0:64, 0:HG * 64].rearrange("p (h x) -> p h x", x=64)
                    qp = 0
                    dve(lambda e: e.tensor_copy(out=QP[0][:, :, 0:64], in_=AQ[:, :, 64:128]), [kAQ], ["QP%d_0" % pb])
                    dve(lambda e, m2v=m2v: e.tensor_tensor(out=QP[0][:, :, 64:128], in0=m2v, in1=m_gt[:], op=ALU.mult),
                        [km2, "const"], ["QP%d_0" % pb])
                    m3, km3 = ps_m()
                    for j in range(HG):
                        pe(lambda e, m3=m3, j=j: e.matmul(m3[0:64, j * 128:j * 128 + 64], B["bt"][:, j, csl], B["rt"][:, j, csl],
                                                        start=True, stop=True), ["bt", "rt"], [km3])
                        pe(lambda e, m3=m3, j=j: e.matmul(m3[0:64, j * 128 + 64:j * 128 + 128], B["kt"][:, j, csl], B["rt"][:, j, csl],
                                                        start=True, stop=True), ["kt", "rt"], [km3])
                    m3v = m3[0:64, 0:HG * 128].rearrange("p (h x) -> p h x", x=128)
                    dve(lambda e, m3v=m3v: e.tensor_tensor(out=ARK[:], in0=m3v, in1=m_le2[:], op=ALU.mult), [km3, "const"], [kARK])
                    yield
                    if stop == "c2":
                        return
                    m4, km4 = ps_m()
                    for j in range(HG):
                        pe(lambda e, m4=m4, j=j: e.matmul(m4[0:64, j * 64:j * 64 + 64], AQ[:, j, 0:64], Vtok[:, j, :],
                                                        start=True, stop=True), [kAQ, kV], [km4])
                    m4v = m4[0:64, 0:HG * 64].rearrange("p (h x) -> p h x", x=64)
                    act(lambda e, m4v=m4v: e.copy(out=W[:, :, 64:128], in_=m4v), [km4], [kW])
                    yield
                    for it in range(6):
                        Qb = QP[qp]
                        kq = "QP%d_%d" % (pb, qp)
                        pw, kpw = pss[pw_i], ("ps", pw_i)
                        for j in range(HG):
                            pe(lambda e, j=j, Qb=Qb: e.matmul(pw[0:64, j * 128:j * 128 + 128], Qb[:, j, 0:64], W[:, j, :],
                                                             start=True, stop=True), [kq, kW], [kpw])
                        pwv = pw[0:64, 0:HG * 128].rearrange("p (h x) -> p h x", x=128)
                        dve(lambda e, pwv=pwv: e.tensor_tensor(out=W[:], in0=pwv, in1=W[:], op=ALU.add), [kpw, kW], [kW])
                        yield
                        if it < 5:
                            pq, kpq = pss[pq_i], ("ps", pq_i)
                            for j in range(HG):
                                pe(lambda e, j=j, Qb=Qb: e.matmul(pq[0:64, j * 128:j * 128 + 64], Qb[:, j, 64:128], Qb[:, j, 0:64],
                                                                 start=True, stop=True), [kq], [kpq])
                                pe(lambda e, j=j, Qb=Qb: e.matmul(pq[0:64, j * 128 + 64:j * 128 + 128], Qb[:, j, 0:64], Qb[:, j, 64:128],
                                                                 start=True, stop=True), [kq], [kpq])
                            pqv = pq[0:64, 0:HG * 128].rearrange("p (h x) -> p h x", x=128)
                            qn = 1 - qp
                            act(lambda e, pqv=pqv, qn=qn: e.copy(out=QP[qn][:], in_=pqv), [kpq], ["QP%d_%d" % (pb, qn)])
                            qp = qn
                            yield
                    if stop == "c3":
                        return
                    m5, km5 = ps_m()
                    for j in range(HG):
                        pe(lambda e, m5=m5, j=j: e.matmul(m5[0:64, j * 64:j * 64 + 64], W[:, j, 0:64], ARK[:, j, 0:64],
                                                        start=True, stop=True), [kW, kARK], [km5])
                    m5b, km5b = ps_m()
                    for j in range(HG):
                        pe(lambda e, m5b=m5b, j=j: e.matmul(m5b[0:64, j * 64:j * 64 + 64], W[:, j, 64:128], ARK[:, j, 0:64],
                                                          start=True, stop=False), [kW, kARK], [km5b])
                        pe(lambda e, m5b=m5b, j=j: e.matmul(m5b[0:64, j * 64:j * 64 + 64], Vtok[:, j, :], ARK[:, j, 64:128],
                                                          start=False, stop=True), [kV, kARK], [km5b])
                    m5v = m5[0:64, 0:HG * 64].rearrange("p (h x) -> p h x", x=64)
                    m5bv = m5b[0:64, 0:HG * 64].rearrange("p (h x) -> p h x", x=64)
                    dve(lambda e, m5v=m5v, c=c: e.tensor_tensor(out=RhT[:, c, :, :], in0=m5v, in1=B["rt"][:, :, csl],
                                                              op=ALU.add), [km5, "rt"], ["RhT"])
                    act(lambda e, m5bv=m5bv, c=c: e.copy(out=Y0T[:, c, :, :], in_=m5bv), [km5b], ["Y0T"])
                    if stop == "c4":
                        return
                    m6, km6 = ps_m()
                    for j in range(HG):
                        pe(lambda e, m6=m6, j=j: e.matmul(m6[0:64, j * 64:j * 64 + 64], W[:, j, 0:64], BK[:, j, 0:64],
                                                        start=True, stop=True), [kW, kBK], [km6])
                    m6b, km6b = ps_m()
                    for j in range(HG):
                        pe(lambda e, m6b=m6b, j=j: e.matmul(m6b[0:64, j * 64:j * 64 + 64], BK[:, j, 0:64], W[:, j, 64:128],
                                                          start=True, stop=False), [kW, kBK], [km6b])
                        pe(lambda e, m6b=m6b, j=j: e.matmul(m6b[0:64, j * 64:j * 64 + 64], BK[:, j, 64:128], Vtok[:, j, :],
                                                          start=False, stop=True), [kV, kBK], [km6b])
                    m6v = m6[0:64, 0:HG * 64].rearrange("p (h x) -> p h x", x=64)
                    m6bv = m6b[0:64, 0:HG * 64].rearrange("p (h x) -> p h x", x=64)
                    for j in range(HG):
                        gc = B["G"][:, j, c * CH + CH - 1:c * CH + CH]
                        dve(lambda e, m6v=m6v, j=j, c=c, gc=gc: e.scalar_tensor_tensor(
                            out=MTa[:, c, j, :], in0=ident[0:64, 0:64], scalar=gc, in1=m6v[:, j, :],
                            op0=ALU.mult, op1=ALU.add), [km6, "G", "const"], ["MT"])
                    act(lambda e, m6bv=m6bv, c=c: e.copy(out=Na[:, c, :, :], in_=m6bv), [km6b], ["N"])

                for c in range(0, NCK, 2):
                    alive = [do_chunk(c, 0), do_chunk(c + 1, 1)]
                    while alive:
                        for g_ in list(alive):
                            try:
                                next(g_)
                            except StopIteration:
                                alive.remove(g_)
                if stop in ("chunk", "c1", "c2", "c3", "c4"):
                    return

                def do_seq(c):
                    hcur = st_h["hcur"]
                    Hc = Hs[hcur]; Hn = Hs[1 - hcur]
                    kh_, kn_ = "H%d" % hcur, "H%d" % (1 - hcur)
                    py, kpy = ps_m()
                    for j in range(HG):
                        pe(lambda e, py=py, j=j, Hc=Hc, c=c: e.matmul(py[0:64, j * 64:j * 64 + 64], Hc[:, j, :], RhT[:, c, j, :],
                                                                    start=True, stop=True), [kh_, "RhT"], [kpy])
                    pyv = py[0:64, 0:HG * 64].rearrange("p (h x) -> p h x", x=64)
                    dve(lambda e, pyv=pyv, c=c: e.tensor_tensor(out=B["y"][:, :, c * CH:(c + 1) * CH], in0=pyv, in1=Y0T[:, c, :, :],
                                                              op=ALU.add), [kpy, "Y0T"], ["y"])
                    ph, kph = ps_m()
                    for j in range(HG):
                        pe(lambda e, ph=ph, j=j, Hc=Hc, c=c: e.matmul(ph[0:64, j * 64:j * 64 + 64], MTa[:, c, j, :], Hc[:, j, :],
                                                                    start=True, stop=True), [kh_, "MT"], [kph])
                    phv = ph[0:64, 0:HG * 64].rearrange("p (h x) -> p h x", x=64)
                    dve(lambda e, phv=phv, c=c, Hn=Hn: e.tensor_tensor(out=Hn[:], in0=phv, in1=Na[:, c, :, :], op=ALU.add),
                        [kph, "N"], [kn_])
                    st_h["hcur"] = 1 - hcur

                for c in range(NCK):
                    do_seq(c)
                if dbg is not None and ti == 0 and g0 == 0:
                    S.dma("sp", lambda e: e.dma_start(out=dbg[17], in_=B["y"][:]), reads=["y"], writes=["dbg"])
                    for di_, nm_ in enumerate([RhT, Y0T, MTa, Na]):
                        S.dma("sp", lambda e, di_=di_, nm_=nm_: e.dma_start(out=dbg[18 + di_].rearrange("p h x -> p (h x)"), in_=nm_[:].rearrange("p c h x -> p (c h x)")),
                              reads=["RhT", "Y0T", "MT", "N"], writes=["dbg"])
                yi = cnt["y"] % 2; cnt["y"] += 1
                yb = yout[yi]
                for j in range(HG):
                    hj = g0 + j
                    p6, k6 = ps_l()
                    pe(lambda e, p6=p6, j=j: e.matmul(p6[0:64, :], avg64[:], B["y"][:, j, :], start=True, stop=True),
                       ["y", "const"], [k6])
                    dve(lambda e, p6=p6, j=j: e.tensor_tensor(out=B["t0"][:, j, :], in0=B["y"][:, j, :], in1=p6[0:64, :],
                                                            op=ALU.subtract), [k6, "y"], ["t0"])
                    pool(lambda e, j=j: e.tensor_tensor(out=B["t1"][:, j, :], in0=B["t0"][:, j, :], in1=B["t0"][:, j, :],
                                                      op=ALU.mult), ["t0"], ["t1"])
                    p7, k7 = ps_l()
                    pe(lambda e, p7=p7, j=j: e.matmul(p7[0:64, :], avg64[:], B["t1"][:, j, :], start=True, stop=True),
                       ["t1", "const"], [k7])
                    dve(lambda e, p7=p7, j=j: e.tensor_scalar(out=B["t2"][:, j, :], in0=p7[0:64, :], scalar1=B_GN_EPS, scalar2=None,
                                                            op0=ALU.add), [k7], ["t2"])
                    act(lambda e, j=j: e.activation(out=B["t2"][:, j, :], in_=B["t2"][:, j, :], func=AF.Sqrt), ["t2"], ["t2"])
                    dve(lambda e, j=j: e.reciprocal(out=B["t2"][:, j, :], in_=B["t2"][:, j, :]), ["t2"], ["t2"])
                    dve(lambda e, j=j: e.tensor_tensor(out=B["t0"][:, j, :], in0=B["t0"][:, j, :], in1=B["t2"][:, j, :],
                                                      op=ALU.mult), ["t0", "t2"], ["t0"])
                    dve(lambda e, j=j, hj=hj: e.tensor_scalar(out=B["t0"][:, j, :], in0=B["t0"][:, j, :], scalar1=prm[:, hj, 8:9],
                                                             scalar2=prm[:, hj, 9:10], op0=ALU.mult, op1=ALU.add),
                        ["t0", "const"], ["t0"])
                pool(lambda e: e.tensor_tensor(out=B["t1"][:], in0=B["sbon"][:], in1=B["vm"][:], op=ALU.mult),
                     ["sbon", "vm", "t1"], ["t1"])
                dve(lambda e: e.tensor_tensor(out=B["t0"][:], in0=B["t0"][:], in1=B["t1"][:], op=ALU.add), ["t0", "t1"], ["t0"])
                dve(lambda e, yb=yb: e.tensor_tensor(out=yb[:], in0=B["t0"][:], in1=B["g"][:], op=ALU.mult),
                    ["t0", "g"], [("yo", yi)])
                for j, h in enumerate(hs):
                    S.dma("sp", lambda e, yb=yb, j=j, h=h: e.dma_start(out=yT[YROW_B + 64 * h:YROW_B + 64 * h + 64, t0:t0 + TT],
                                                                     in_=yb[:, j, :]), reads=[("yo", yi)], writes=["yT"])

            for ti in range(T // TT):
                do_tile(ti)
        S.barrier_all()
        S.emit()


def build_rwkv_test(T, heads, debug=False, stop=None):
    nc = bass.Bass("TRN2", target_bir_lowering=False)
    NH = len(heads)
    di = lambda n, s: nc.dram_tensor(n, s, F32, kind="ExternalInput").ap()
    pT = di("pT", [NP_ROWS, T]); prm = di("prm", [64, NH, 10]); lmu = di("lmu", [128, 4])
    wup = di("wup", [96, NH * 64]); aup = di("aup", [96, NH * 64]); gup = di("gup", [256, NH * 64])
    ident = di("ident", [128, 128]); mlt = di("m_lt2", [64, 3, 128]); mle = di("m_le2", [64, 3, 128])
    mgt = di("m_gt", [64, 3, 64]); rst = di("rst", [64, 512])
    yT = nc.dram_tensor("yT", [1536, T], F32, kind="ExternalOutput").ap()
    dbg = nc.dram_tensor("dbg", [22, 64, 3, 512], F32, kind="ExternalOutput").ap() if debug else None
    phase_rwkv(nc, T, pT, yT, prm, lmu, wup, aup, gup, ident, mlt, mle, mgt, rst, heads, dbg=dbg, stop=stop)
    return nc


def rwkv_host_params(heads, mu, w0, w_up, a0, a_up, g_up, k_k, k_a, r_k, lnx_g, lnx_b):
    NH = len(heads)
    prm = np.zeros((64, NH, 10), np.float32)
    cols = np.concatenate([np.arange(64 * h, 64 * h + 64) for h in heads])
    for i, h in enumerate(heads):
        sl = slice(64 * h, 64 * h + 64)
        prm[:, i, 0] = mu[0:768][sl]; prm[:, i, 1] = mu[768:1536][sl]; prm[:, i, 2] = mu[1536:2304][sl]
        prm[:, i, 3] = w0[sl]; prm[:, i, 4] = a0[sl]; prm[:, i, 5] = k_k[sl]; prm[:, i, 6] = k_a[sl]
        prm[:, i, 7] = r_k.reshape(-1)[sl]; prm[:, i, 8] = lnx_g[sl]; prm[:, i, 9] = lnx_b[sl]
    lmu = np.zeros((128, 4), np.float32)
    lmu[0:96, 0] = mu[2304:2400]; lmu[0:96, 1] = mu[2400:2496]; lmu[:, 2] = mu[2496:2624]; lmu[:, 3] = mu[2624:2752]
    return dict(prm=prm, lmu=lmu, wup=np.ascontiguousarray(w_up[:, cols]), aup=np.ascontiguousarray(a_up[:, cols]),
                gup=np.ascontiguousarray(g_up[:, cols]))


class WeightStream:
    def __init__(self, S, nc, st, name, max_elems, n_stage=2, n_bf=2, direct=False):
        self.S, self.nc, self.name = S, nc, name
        if direct:
            n_stage = 0
        self.stage = [st.enter_context(nc.sbuf_tensor(uname("%s_st%d" % (name, i)), [128, max_elems], F32)) for i in range(n_stage)]
        self.bf = [st.enter_context(nc.sbuf_tensor(uname("%s_bf%d" % (name, i)), [128, max_elems], BF16)) for i in range(n_bf)]
        self.i = 0
        self.q = 0

    def load_bf(self, scr, shapes, dram_key):
        S = self.S
        bi = self.i % len(self.bf); self.i += 1
        bfb = self.bf[bi]
        n_tot = sum(int(np.prod(shp[1:])) for shp in shapes)
        q = ("sp", "act")[self.q % 2]; self.q += 1
        S.dma(q, lambda e, bfb=bfb, n_tot=n_tot: e.dma_start(out=bfb[:, 0:n_tot], in_=scr[:, 0:n_tot]), reads=[dram_key],
              writes=[(self.name, "bf", bi)])
        off = 0
        outs = []
        for shp in shapes:
            n = int(np.prod(shp[1:]))
            pat = "p (a b) -> p a b" if len(shp) == 3 else "p (a b c) -> p a b c"
            kw = dict(a=shp[1], b=shp[2]) if len(shp) == 3 else dict(a=shp[1], b=shp[2], c=shp[3])
            outs.append(bfb[:, off:off + n].rearrange(pat, **kw))
            off += n
        return outs, (self.name, "bf", bi)

    def load(self, src_views, shapes, dram_key):
        S = self.S
        si = self.i % len(self.stage); bi = self.i % len(self.bf); self.i += 1
        stg, bfb = self.stage[si], self.bf[bi]
        off = 0
        outs = []
        for v, shp in zip(src_views, shapes):
            n = int(np.prod(shp[1:]))
            pat = "p (a b) -> p a b" if len(shp) == 3 else "p (a b c) -> p a b c"
            kw = dict(a=shp[1], b=shp[2]) if len(shp) == 3 else dict(a=shp[1], b=shp[2], c=shp[3])
            dst = stg[:, off:off + n].rearrange(pat, **kw)
            q = ("sp", "act")[self.q % 2]; self.q += 1
            S.dma(q, lambda e, dst=dst, v=v: e.dma_start(out=dst, in_=v), reads=[dram_key],
                  writes=[(self.name, "st", si)])
            outs.append(bfb[:, off:off + n].rearrange(pat, **kw))
            off += n
        S.op("pool", lambda e, stg=stg, bfb=bfb, off=off: e.tensor_copy(out=bfb[:, 0:off], in_=stg[:, 0:off]),
             reads=[(self.name, "st", si)], writes=[(self.name, "bf", bi)])
        return outs, (self.name, "bf", bi)


def phase_precast(nc, panels, scr, max_elems):
    import contextlib
    with contextlib.ExitStack() as st:
        _PH[0] += 1
        S = Sched(nc)
        ws = WeightStream(S, nc, st, "pc_w", max_elems)
        for i, (views, shapes) in enumerate(panels):
            outs, kw = ws.load(views, shapes, "Wsrc")
            n_tot = sum(int(np.prod(shp[1:])) for shp in shapes)
            bfb = ws.bf[(ws.i - 1) % len(ws.bf)]
            S.dma("sp", lambda e, i=i, bfb=bfb, n_tot=n_tot: e.dma_start(out=scr[i][:, 0:n_tot], in_=bfb[:, 0:n_tot]),
                  reads=[kw], writes=["scr"])
        S.barrier_all()
        S.emit()


def emit_norm_T(S, nc, x_sb, kx, h_out, kh, g_col, ones_bf, sq, rstd, ps, kps, n):
    for c in range(KC):
        S.op("act", lambda e, c=c: e.activation(out=sq[:, c, :n], in_=x_sb[:, c, :], func=AF.Square), reads=[kx], writes=["nrm_sq"])
    for c in range(KC):
        S.op("pe", lambda e, c=c: e.matmul(ps[:, :n], ones_bf[:], sq[:, c, :n], start=(c == 0), stop=(c == KC - 1)),
             reads=["nrm_sq", "ones"], writes=[kps])
    S.op("dve", lambda e: e.tensor_scalar(out=rstd[:, :n], in0=ps[:, :n], scalar1=1.0 / D_MODEL, scalar2=NORM_EPS,
                                          op0=ALU.mult, op1=ALU.add), reads=[kps], writes=["nrm_rstd"])
    S.op("act", lambda e: e.activation(out=rstd[:, :n], in_=rstd[:, :n], func=AF.Sqrt), reads=["nrm_rstd"], writes=["nrm_rstd"])
    S.op("dve", lambda e: e.reciprocal(out=rstd[:, :n], in_=rstd[:, :n]), reads=["nrm_rstd"], writes=["nrm_rstd"])
    for c in range(KC):
        S.op("dve", lambda e, c=c: e.scalar_tensor_tensor(out=h_out[:, c, :], in0=x_sb[:, c, :], scalar=g_col[:, c:c + 1],
                                                         in1=rstd[:, :n], op0=ALU.mult, op1=ALU.mult),
             reads=[kx, "nrm_rstd", "gcol"], writes=[kh])


def phase_proj(nc, TL, xT, g_d, W, pT, ncols, scr=None):
    import contextlib
    TS = min(2048, TL); TT = 512; CB = 256
    Wv0 = W.rearrange("(c p) n -> p c n", p=128)
    if scr is not None:
        panels = []
        for cb0 in range(0, ncols, CB):
            cw = min(CB, ncols - cb0)
            panels.append(([Wv0[:, :, cb0:cb0 + cw]], [(128, KC, cw)]))
        phase_precast(nc, panels, scr, KC * CB)
    with contextlib.ExitStack() as st:
        sb = lambda name, shape, dt=F32: st.enter_context(nc.sbuf_tensor(uname(name), shape, dt))
        pss = [st.enter_context(nc.psum_tensor(uname("pps%d" % i), [128, 512], F32)) for i in range(8)]
        _PH[0] += 1
        S = Sched(nc)
        ones_bf = sb("pj_ones", [128, 128], BF16); g_sb = sb("pj_g", [128, KC])
        hT = sb("pj_h", [128, KC, TS], BF16)
        xin = [sb("pj_x%d" % i, [128, KC, TT]) for i in range(1)]
        sq = sb("pj_sq", [128, KC, TT], BF16); rstd = sb("pj_rstd", [128, TT])
        ot = [sb("pj_o%d" % i, [128, TT]) for i in range(4)]
        ws = WeightStream(S, nc, st, "pj_w", KC * CB, direct=scr is not None)
        S.op("pool", lambda e: e.memset(ones_bf[:], 1.0), writes=["ones"])
        S.dma("sp", lambda e: e.dma_start(out=g_sb[:], in_=g_d), writes=["gcol"])
        xTv = xT.rearrange("(c p) t -> p c t", p=128)
        Wv = W.rearrange("(c p) n -> p c n", p=128)
        cnt = dict(o=0, ps=0)
        for s0 in range(0, TL, TS):
            for tt in range(TS // TT):
                c0 = s0 + tt * TT
                S.dma("sp", lambda e, c0=c0: e.dma_start(out=xin[0][:], in_=xTv[:, :, c0:c0 + TT]), reads=["xT"], writes=["pj_x"])
                emit_norm_T(S, nc, xin[0], "pj_x", hT[:, :, tt * TT:(tt + 1) * TT], ("pj_h", tt), g_sb, ones_bf, sq, rstd,
                            pss[0], ("ps", 0), TT)
            for cb0 in range(0, ncols, CB):
                cw = min(CB, ncols - cb0)
                if scr is not None:
                    (wv,), kw = ws.load_bf(scr[cb0 // CB], [(128, KC, cw)], "Wscr")
                else:
                    (wv,), kw = ws.load([Wv[:, :, cb0:cb0 + cw]], [(128, KC, cw)], "W")
                for m0 in range(0, cw, 128):
                    mw = min(128, cw - m0)
                    for tt in range(TS // TT):
                        pi = 1 + cnt["ps"] % 7; cnt["ps"] += 1
                        ps = pss[pi]
                        for c in range(KC):
                            S.op("pe", lambda e, ps=ps, wv=wv, c=c, m0=m0, mw=mw, tt=tt:
                                 e.matmul(ps[:mw, :TT], wv[:, c, m0:m0 + mw], hT[:, c, tt * TT:(tt + 1) * TT],
                                          start=(c == 0), stop=(c == KC - 1)),
                                 reads=[kw, ("pj_h", tt)], writes=[("ps", pi)])
                        oi = cnt["o"] % 4; cnt["o"] += 1
                        ob = ot[oi]
                        if oi % 2:
                            S.op("act", lambda e, ob=ob, ps=ps, mw=mw: e.copy(out=ob[:mw, :], in_=ps[:mw, :TT]),
                                 reads=[("ps", pi)], writes=[("pj_o", oi)])
                        else:
                            S.op("dve", lambda e, ob=ob, ps=ps, mw=mw: e.tensor_copy(out=ob[:mw, :], in_=ps[:mw, :TT]),
                                 reads=[("ps", pi)], writes=[("pj_o", oi)])
                        r0 = cb0 + m0; t0 = s0 + tt * TT
                        S.dma("sp", lambda e, ob=ob, mw=mw, r0=r0, t0=t0: e.dma_start(out=pT[r0:r0 + mw, t0:t0 + TT], in_=ob[:mw, :]),
                              reads=[("pj_o", oi)], writes=["pT"])
        S.barrier_all()
        S.emit()


def phase_merge(nc, TL, xT, g_d, Wg, gate_col0, yT, projw, wout, x1T, scr=None, scr_o=None):
    import contextlib
    TT = 512
    YK = (4, 6, 2)
    YO = (0, 4, 10)
    Wgv0 = Wg.rearrange("(c p) n -> p c n", p=128)
    Pv0 = projw.rearrange("(c p) n -> p c n", p=128)
    Wov0 = wout.rearrange("(c p) n -> p c n", p=128)

    def mg_views(m):
        views = [Wgv0[:, :, gate_col0 + i * D_MODEL + m * 128:gate_col0 + i * D_MODEL + m * 128 + 128] for i in range(3)]
        views.append(Pv0[:, :, m * 128:(m + 1) * 128])
        return views, [(128, KC, 128)] * 3 + [(128, 12, 128)]

    if scr is not None:
        phase_precast(nc, [mg_views(m) for m in range(KC)], scr, KC * 3 * 128 + 12 * 128)
        phase_precast(nc, [([Wov0[:, :, m * 128:(m + 1) * 128]], [(128, KC, 128)]) for m in range(KC)], scr_o, KC * 128)
    with contextlib.ExitStack() as st:
        sb = lambda name, shape, dt=F32: st.enter_context(nc.sbuf_tensor(uname(name), shape, dt))
        pss = [st.enter_context(nc.psum_tensor(uname("mps%d" % i), [128, 512], F32)) for i in range(8)]
        _PH[0] += 1
        S = Sched(nc)
        ones_bf = sb("mg_ones", [128, 128], BF16); g_sb = sb("mg_g", [128, KC])
        xin = sb("mg_x", [128, KC, TT]); hT = sb("mg_h", [128, KC, TT], BF16)
        rstd = sb("mg_rstd", [128, TT])
        yst = sb("mg_yst", [128, 12, TT]); ybf = sb("mg_ybf", [128, 12, TT], BF16)
        mrgb = sb("mg_mrgb", [128, KC, TT], BF16)
        sq = mrgb
        S.alias["nrm_sq"] = "mg_mrgb"
        sig = [sb("mg_sig%d" % i, [128, TT]) for i in range(2)]
        tmp = sb("mg_tmp", [128, TT]); mrg = sb("mg_mrg", [128, TT])
        xo = [sb("mg_xo%d" % i, [128, TT]) for i in range(2)]
        ws = WeightStream(S, nc, st, "mg_w", KC * 3 * 128 + 12 * 128, direct=scr is not None)
        S.op("pool", lambda e: e.memset(ones_bf[:], 1.0), writes=["ones"])
        S.dma("sp", lambda e: e.dma_start(out=g_sb[:], in_=g_d), writes=["gcol"])
        xTv = xT.rearrange("(c p) t -> p c t", p=128)
        yTv = yT.rearrange("(c p) t -> p c t", p=128)
        Wgv = Wg.rearrange("(c p) n -> p c n", p=128)
        Pv = projw.rearrange("(c p) n -> p c n", p=128)
        Wov = wout.rearrange("(c p) n -> p c n", p=128)
        cnt = dict(ps=0, s=0, o=0)

        def nps():
            i = 1 + cnt["ps"] % 7; cnt["ps"] += 1
            return pss[i], ("ps", i)

        for t0 in range(0, TL, TT):
            S.dma("sp", lambda e, t0=t0: e.dma_start(out=xin[:], in_=xTv[:, :, t0:t0 + TT]), reads=["xT"], writes=["mg_x"])
            S.dma("act", lambda e, t0=t0: e.dma_start(out=yst[:], in_=yTv[:, :, t0:t0 + TT]), reads=["yT"], writes=["mg_yst"])
            S.op("pool", lambda e: e.tensor_copy(out=ybf[:], in_=yst[:]), reads=["mg_yst"], writes=["mg_ybf"])
            emit_norm_T(S, nc, xin, "mg_x", hT, "mg_h", g_sb, ones_bf, sq, rstd, pss[0], ("ps", 0), TT)
            for m in range(KC):
                gsrc = Wgv[:, :, gate_col0 + m * 128:gate_col0 + m * 128 + 128]
                views = [Wgv[:, :, gate_col0 + i * D_MODEL + m * 128:gate_col0 + i * D_MODEL + m * 128 + 128] for i in range(3)]
                views.append(Pv[:, :, m * 128:(m + 1) * 128])
                shapes = [(128, KC, 128)] * 3 + [(128, 12, 128)]
                if scr is not None:
                    (w0v, w1v, w2v, pv), kw = ws.load_bf(scr[m], shapes, "Wmgs")
                else:
                    (w0v, w1v, w2v, pv), kw = ws.load(views, shapes, "Wmg")
                wgs = (w0v, w1v, w2v)
                for i in range(3):
                    pg, kpg = nps()
                    for c in range(KC):
                        S.op("pe", lambda e, pg=pg, i=i, c=c, wgs=wgs: e.matmul(pg[:, :TT], wgs[i][:, c, :], hT[:, c, :], start=(c == 0),
                                                                            stop=(c == KC - 1)), reads=[kw, "mg_h"], writes=[kpg])
                    pz, kpz = nps()
                    for u in range(YK[i]):
                        S.op("pe", lambda e, pz=pz, i=i, u=u, pv=pv: e.matmul(pz[:, :TT], pv[:, YO[i] + u, :], ybf[:, YO[i] + u, :],
                                                                           start=(u == 0), stop=(u == YK[i] - 1)),
                             reads=[kw, "mg_ybf"], writes=[kpz])
                    si = cnt["s"] % 2; cnt["s"] += 1
                    sg = sig[si]
                    S.op("act", lambda e, sg=sg, pg=pg: e.activation(out=sg[:], in_=pg[:, :TT], func=AF.Sigmoid), reads=[kpg],
                         writes=[("mg_sig", si)])
                    if i == 0:
                        S.op("dve", lambda e, sg=sg, pz=pz: e.tensor_tensor(out=mrg[:], in0=pz[:, :TT], in1=sg[:], op=ALU.mult),
                             reads=[kpz, ("mg_sig", si)], writes=["mg_mrg"])
                    else:
                        S.op("dve", lambda e, sg=sg, pz=pz: e.tensor_tensor(out=tmp[:], in0=pz[:, :TT], in1=sg[:], op=ALU.mult),
                             reads=[kpz, ("mg_sig", si)], writes=["mg_tmp"])
                        if i == 1:
                            S.op("pool", lambda e: e.tensor_tensor(out=mrg[:], in0=mrg[:], in1=tmp[:], op=ALU.add),
                                 reads=["mg_mrg", "mg_tmp"], writes=["mg_mrg"])
                        else:
                            S.op("pool", lambda e, m=m: e.tensor_tensor(out=mrgb[:, m, :], in0=mrg[:], in1=tmp[:], op=ALU.add),
                                 reads=["mg_mrg", "mg_tmp"], writes=["mg_mrgb"])
            for m in range(KC):
                if scr is not None:
                    (wov,), kw = ws.load_bf(scr_o[m], [(128, KC, 128)], "Wouts")
                else:
                    (wov,), kw = ws.load([Wov[:, :, m * 128:(m + 1) * 128]], [(128, KC, 128)], "Wout")
                po, kpo = nps()
                for c in range(KC):
                    S.op("pe", lambda e, po=po, c=c, wov=wov: e.matmul(po[:, :TT], wov[:, c, :], mrgb[:, c, :], start=(c == 0),
                                                                     stop=(c == KC - 1)), reads=[kw, "mg_mrgb"], writes=[kpo])
                oi = cnt["o"] % 2; cnt["o"] += 1
                ob = xo[oi]
                S.op("dve", lambda e, ob=ob, po=po, m=m: e.tensor_tensor(out=ob[:], in0=po[:, :TT], in1=xin[:, m, :], op=ALU.add),
                     reads=[kpo, "mg_x"], writes=[("mg_xo", oi)])
                S.dma("sp", lambda e, ob=ob, m=m, t0=t0: e.dma_start(out=x1T[m * 128:(m + 1) * 128, t0:t0 + TT], in_=ob[:]),
                      reads=[("mg_xo", oi)], writes=["x1T"])
        S.barrier_all()
        S.emit()


def phase_ffn(nc, TL, x1T, g_d, wup, conv_d, wdown, xoT, d_ff, halo_d=None, scr_u=None, scr_d=None):
    import contextlib
    TT = 512
    NF = d_ff // 128
    NHALF = 2 if (scr_u is not None and TL % (2 * TT) == 0) else 1
    TB = TT * NHALF
    Wuv0 = wup.rearrange("(c p) n -> p c n", p=128)
    Wdv0 = wdown.rearrange("(f p) n -> p f n", p=128)
    if scr_u is not None:
        phase_precast(nc, [([Wuv0[:, :, f * 128:(f + 1) * 128], Wuv0[:, :, d_ff + f * 128:d_ff + (f + 1) * 128]], [(128, KC, 128)] * 2)
                           for f in range(NF)], scr_u, KC * 256)
        phase_precast(nc, [([Wdv0[:, :, m * 128:(m + 1) * 128]], [(128, NF, 128)]) for m in range(KC)], scr_d, NF * 128)
    with contextlib.ExitStack() as st:
        sb = lambda name, shape, dt=F32: st.enter_context(nc.sbuf_tensor(uname(name), shape, dt))
        pss = [st.enter_context(nc.psum_tensor(uname("fps%d" % i), [128, 512], F32)) for i in range(8)]
        _PH[0] += 1
        S = Sched(nc)
        ones_bf = sb("ff_ones", [128, 128], BF16); g_sb = sb("ff_g", [128, KC]); cw = sb("ff_cw", [128, NF, 3])
        xin = sb("ff_x", [128, KC, TT]); hT = sb("ff_h", [128, KC, TB], BF16)
        rstd = sb("ff_rstd", [128, TT])
        actb = sb("ff_act", [128, max(NF, KC), TB], BF16)
        sq = actb[:, 0:KC, 0:TT]
        S.alias["nrm_sq"] = "ff_act"
        xr = [sb("ff_xr%d" % i, [128, TT]) for i in range(2)]
        carry = sb("ff_carry", [128, NF, 2])
        gbuf = [sb("ff_gb%d" % i, [128, TT + 2]) for i in range(2)]
        cv = [sb("ff_cv%d" % i, [128, TT]) for i in range(2)]
        xo = [sb("ff_xo%d" % i, [128, TT]) for i in range(2)]
        ws = WeightStream(S, nc, st, "ff_w", max(KC * 256, NF * 128), n_stage=2, n_bf=3 if scr_u is not None else 2,
                          direct=scr_u is not None)
        S.op("pool", lambda e: e.memset(ones_bf[:], 1.0), writes=["ones"])
        S.dma("sp", lambda e: e.dma_start(out=g_sb[:], in_=g_d), writes=["gcol"])
        S.dma("sp", lambda e: e.dma_start(out=cw[:], in_=conv_d), writes=["ff_cw"])
        if halo_d is None:
            S.op("pool", lambda e: e.memset(carry[:], 0.0), writes=["ff_carry"])
        else:
            S.dma("sp", lambda e: e.dma_start(out=carry[:], in_=halo_d), writes=["ff_carry"])
        xv = x1T.rearrange("(c p) t -> p c t", p=128)
        Wuv = wup.rearrange("(c p) n -> p c n", p=128)
        Wdv = wdown.rearrange("(f p) n -> p f n", p=128)
        cnt = dict(ps=0, g=0, o=0)

        def nps():
            i = 1 + cnt["ps"] % 7; cnt["ps"] += 1
            return pss[i], ("ps", i)

        for t0 in range(0, TL, TB):
            for h in range(NHALF):
                S.dma("sp", lambda e, t0=t0, h=h: e.dma_start(out=xin[:], in_=xv[:, :, t0 + h * TT:t0 + (h + 1) * TT]),
                      reads=["x1T"], writes=["ff_x"])
                emit_norm_T(S, nc, xin, "ff_x", hT[:, :, h * TT:(h + 1) * TT], ("ff_h", h), g_sb, ones_bf, sq, rstd, pss[0],
                            ("ps", 0), TT)
            for f in range(NF):
                views = [Wuv[:, :, f * 128:(f + 1) * 128], Wuv[:, :, d_ff + f * 128:d_ff + (f + 1) * 128]]
                if scr_u is not None:
                    (wg, wv), kw = ws.load_bf(scr_u[f], [(128, KC, 128)] * 2, "Wups")
                else:
                    (wg, wv), kw = ws.load(views, [(128, KC, 128)] * 2, "Wup")
                for h in range(NHALF):
                    hs = slice(h * TT, (h + 1) * TT)
                    pg, kpg = nps()
                    for c in range(KC):
                        S.op("pe", lambda e, pg=pg, c=c, wg=wg, hs=hs: e.matmul(pg[:, :TT], wg[:, c, :], hT[:, c, hs], start=(c == 0),
                                                                             stop=(c == KC - 1)), reads=[kw, ("ff_h", h)], writes=[kpg])
                    pv, kpv = nps()
                    for c in range(KC):
                        S.op("pe", lambda e, pv=pv, c=c, wv=wv, hs=hs: e.matmul(pv[:, :TT], wv[:, c, :], hT[:, c, hs], start=(c == 0),
                                                                             stop=(c == KC - 1)), reads=[kw, ("ff_h", h)], writes=[kpv])
                    gi = cnt["g"] % 2; cnt["g"] += 1
                    gb = gbuf[gi]; cb = cv[gi]
                    kgb, kcb = ("ff_gb", gi), ("ff_cv", gi)
                    S.op("pool", lambda e, gb=gb, f=f: e.tensor_copy(out=gb[:, 0:2], in_=carry[:, f, :]), reads=["ff_carry"], writes=[kgb])
                    S.op("act", lambda e, gb=gb, pg=pg: e.copy(out=gb[:, 2:TT + 2], in_=pg[:, :TT]), reads=[kpg], writes=[kgb])
                    S.op("pool", lambda e, gb=gb, f=f: e.tensor_copy(out=carry[:, f, :], in_=gb[:, TT:TT + 2]), reads=[kgb],
                         writes=["ff_carry"])
                    S.op("dve", lambda e, gb=gb, cb=cb, f=f: e.tensor_scalar(out=cb[:], in0=gb[:, 0:TT], scalar1=cw[:, f, 0:1], scalar2=None,
                                                                           op0=ALU.mult), reads=[kgb, "ff_cw"], writes=[kcb])
                    S.op("dve", lambda e, gb=gb, cb=cb, f=f: e.scalar_tensor_tensor(out=cb[:], in0=gb[:, 1:TT + 1], scalar=cw[:, f, 1:2],
                                                                                  in1=cb[:], op0=ALU.mult, op1=ALU.add),
                         reads=[kgb, "ff_cw", kcb], writes=[kcb])
                    S.op("dve", lambda e, gb=gb, cb=cb, f=f: e.scalar_tensor_tensor(out=cb[:], in0=gb[:, 2:TT + 2], scalar=cw[:, f, 2:3],
                                                                                  in1=cb[:], op0=ALU.mult, op1=ALU.add),
                         reads=[kgb, "ff_cw", kcb], writes=[kcb])
                    S.op("act", lambda e, cb=cb: e.activation(out=cb[:], in_=cb[:], func=AF.Silu), reads=[kcb], writes=[kcb])
                    S.op("dve", lambda e, cb=cb, pv=pv, f=f, hs=hs: e.tensor_tensor(out=actb[:, f, hs], in0=pv[:, :TT], in1=cb[:], op=ALU.mult),
                         reads=[kpv, kcb], writes=["ff_act"])
            for m in range(KC):
                if scr_d is not None:
                    (wd,), kw = ws.load_bf(scr_d[m], [(128, NF, 128)], "Wdowns")
                else:
                    (wd,), kw = ws.load([Wdv[:, :, m * 128:(m + 1) * 128]], [(128, NF, 128)], "Wdown")
                for h in range(NHALF):
                    hs = slice(h * TT, (h + 1) * TT)
                    tk = t0 + h * TT
                    po, kpo = nps()
                    for f in range(NF):
                        S.op("pe", lambda e, po=po, f=f, wd=wd, hs=hs: e.matmul(po[:, :TT], wd[:, f, :], actb[:, f, hs], start=(f == 0),
                                                                             stop=(f == NF - 1)), reads=[kw, "ff_act"], writes=[kpo])
                    oi = cnt["o"] % 2; cnt["o"] += 1
                    ob = xo[oi]; xrb = xr[oi]
                    S.dma("act", lambda e, xrb=xrb, m=m, tk=tk: e.dma_start(out=xrb[:], in_=x1T[m * 128:(m + 1) * 128, tk:tk + TT]),
                          reads=["x1T"], writes=[("ff_xr", oi)])
                    S.op("dve", lambda e, ob=ob, po=po, xrb=xrb: e.tensor_tensor(out=ob[:], in0=po[:, :TT], in1=xrb[:], op=ALU.add),
                         reads=[kpo, ("ff_xr", oi)], writes=[("ff_xo", oi)])
                    S.dma("sp", lambda e, ob=ob, m=m, tk=tk: e.dma_start(out=xoT[m * 128:(m + 1) * 128, tk:tk + TT], in_=ob[:]),
                          reads=[("ff_xo", oi)], writes=["xoT"])
        S.barrier_all()
        S.emit()


def phase_final_norm(nc, TL, xT, g_d, outT):
    import contextlib
    TT = 512
    with contextlib.ExitStack() as st:
        sb = lambda name, shape, dt=F32: st.enter_context(nc.sbuf_tensor(uname(name), shape, dt))
        pss = [st.enter_context(nc.psum_tensor(uname("nps%d" % i), [128, 512], F32)) for i in range(2)]
        _PH[0] += 1
        S = Sched(nc)
        ones_bf = sb("fn_ones", [128, 128], BF16); g_sb = sb("fn_g", [128, KC])
        xin = [sb("fn_x%d" % i, [128, KC, TT]) for i in range(2)]
        ho = [sb("fn_h%d" % i, [128, KC, TT]) for i in range(2)]
        sq = sb("fn_sq", [128, KC, TT], BF16); rstd = sb("fn_rstd", [128, TT])
        S.op("pool", lambda e: e.memset(ones_bf[:], 1.0), writes=["ones"])
        S.dma("sp", lambda e: e.dma_start(out=g_sb[:], in_=g_d), writes=["gcol"])
        xv = xT.rearrange("(c p) t -> p c t", p=128)
        ov = outT.rearrange("(c p) t -> p c t", p=128)
        for i, t0 in enumerate(range(0, TL, TT)):
            b = i % 2
            S.dma("sp", lambda e, t0=t0, b=b: e.dma_start(out=xin[b][:], in_=xv[:, :, t0:t0 + TT]), reads=["xT"], writes=[("fn_x", b)])
            emit_norm_T(S, nc, xin[b], ("fn_x", b), ho[b], ("fn_h", b), g_sb, ones_bf, sq, rstd, pss[0], ("ps", 0), TT)
            S.dma("act", lambda e, t0=t0, b=b: e.dma_start(out=ov[:, :, t0:t0 + TT], in_=ho[b][:]), reads=[("fn_h", b)], writes=["outT"])
        S.barrier_all()
        S.emit()


def build_dense_test(TL, d_ff, ncols_p):
    nc = bass.Bass("TRN2", target_bir_lowering=False)
    di = lambda n, s: nc.dram_tensor(n, s, F32, kind="ExternalInput").ap()
    xT = di("xT", [D_MODEL, TL]); g1 = di("g1", [128, KC]); g2 = di("g2", [128, KC]); gf = di("gf", [128, KC])
    w_in = di("w_in", [D_MODEL, ncols_p + 6144]); yT = di("yT", [1536, TL]); projw = di("projw", [1536, D_MODEL])
    wout = di("wout", [D_MODEL, D_MODEL]); wup = di("wup", [D_MODEL, 2 * d_ff]); conv = di("conv", [128, d_ff // 128, 3])
    wdown = di("wdown", [d_ff, D_MODEL])
    pT = nc.dram_tensor("pT", [ncols_p, TL], F32, kind="ExternalOutput").ap()
    x1T = nc.dram_tensor("x1T", [D_MODEL, TL], F32, kind="ExternalOutput").ap()
    x2T = nc.dram_tensor("x2T", [D_MODEL, TL], F32, kind="ExternalOutput").ap()
    outT = nc.dram_tensor("outT", [D_MODEL, TL], F32, kind="ExternalOutput").ap()
    phase_proj(nc, TL, xT, g1, w_in, pT, ncols_p)
    phase_merge(nc, TL, xT, g1, w_in, ncols_p, yT, projw, wout, x1T)
    phase_ffn(nc, TL, x1T, g2, wup, conv, wdown, x2T, d_ff)
    phase_final_norm(nc, TL, x2T, gf, outT)
    return nc


def build_full(TL, n_layers, d_ff, heads_a, heads_c, heads_b, final=True):
    nc = bass.Bass("TRN2", target_bir_lowering=False)
    di = lambda n, s: nc.dram_tensor(n, list(s), F32, kind="ExternalInput").ap()
    L = n_layers
    NHB = len(heads_b)
    xT = di("xT", [D_MODEL, TL])
    g1 = di("g1", [L, 128, KC]); g2 = di("g2", [L, 128, KC]); gf = di("gf", [128, KC])
    w_in = di("w_in", [L, D_MODEL, NP_ROWS + 3 * D_MODEL])
    projw = di("projw", [L, 1536, D_MODEL]); wout = di("w_out", [L, D_MODEL, D_MODEL])
    wup = di("ffn_up", [L, D_MODEL, 2 * d_ff]); conv = di("conv", [L, 128, d_ff // 128, 3]); wdown = di("ffn_down", [L, d_ff, D_MODEL])
    tabs_g = di("tabs_g", [20, 128, 256]); tabs_m = di("tabs_m", [20, 128, 256]); tabs_a = di("tabs_a", [20, 128, 256])
    sinks = di("sinks", [L, 64, 8]); ident = di("ident", [128, 128])
    prm = di("prm", [L, 64, NHB, 10]); lmu = di("lmu", [L, 128, 4])
    rwup = di("rw_up", [L, 96, NHB * 64]); raup = di("ra_up", [L, 96, NHB * 64]); rgup = di("rg_up", [L, 256, NHB * 64])
    mlt = di("m_lt2", [64, 3, 128]); mle = di("m_le2", [64, 3, 128]); mgt = di("m_gt", [64, 3, 64]); rst = di("rst", [64, 512])
    outT = nc.dram_tensor("outT", [D_MODEL, TL], F32, kind="ExternalOutput").ap()
    pT = nc.dram_tensor("pT_s", [NP_ROWS, TL], F32).ap()
    yT = nc.dram_tensor("yT_s", [1536, TL], F32).ap()
    x1T = nc.dram_tensor("x1T_s", [D_MODEL, TL], F32).ap()
    xs = [nc.dram_tensor("xs%d" % i, [D_MODEL, TL], F32).ap() for i in range(2)]
    NF = d_ff // 128
    sc_pj = nc.dram_tensor("sc_pj", [(NP_ROWS + 255) // 256, 128, KC * 256], BF16).ap()
    sc_mg = nc.dram_tensor("sc_mg", [KC, 128, KC * 3 * 128 + 12 * 128], BF16).ap()
    sc_wo = nc.dram_tensor("sc_wo", [KC, 128, KC * 128], BF16).ap()
    sc_up = nc.dram_tensor("sc_up", [NF, 128, KC * 256], BF16).ap()
    sc_dn = nc.dram_tensor("sc_dn", [KC, 128, NF * 128], BF16).ap()
    cur = xT
    for l in range(L):
        phase_proj(nc, TL, cur, g1[l], w_in[l], pT, NP_ROWS, scr=sc_pj)
        phase_attn(nc, TL, pT, yT, tabs_g, tabs_m, tabs_a, sinks[l], ident, heads_a, heads_c)
        phase_rwkv(nc, TL, pT, yT, prm[l], lmu[l], rwup[l], raup[l], rgup[l], ident, mlt, mle, mgt, rst, heads_b)
        phase_merge(nc, TL, cur, g1[l], w_in[l], NP_ROWS, yT, projw[l], wout[l], x1T, scr=sc_mg, scr_o=sc_wo)
        nxt = outT if (l == L - 1 and not final) else xs[l % 2]
        phase_ffn(nc, TL, x1T, g2[l], wup[l], conv[l], wdown[l], nxt, d_ff, scr_u=sc_up, scr_d=sc_dn)
        cur = nxt
    if final:
        phase_final_norm(nc, TL, cur, gf, outT)
    return nc


def phase_attn(nc, T, pT, yT, tg, tm, ta, sk, idd, heads_a, heads_c):
    import contextlib
    with contextlib.ExitStack() as st:
        pss = [st.enter_context(nc.psum_tensor(uname("aps%d" % i), [128, 512], F32)) for i in range(8)]
        _PH[0] += 1
        S = Sched(nc)
        emit_attention(S, nc, st, T, pT, yT, tg, tm, ta, sk, idd, pss, heads_a, heads_c)
        S.barrier_all()
        S.emit()


def host_inputs(inp, n_layers, d_ff, heads_b):
    L = n_layers
    f32 = lambda a: np.ascontiguousarray(np.asarray(a, dtype=np.float32))
    gl = lambda g: np.ascontiguousarray(np.asarray(g, np.float32).reshape(-1, KC, 128).transpose(0, 2, 1))
    tg, tm, ta = attn_tables(np.asarray(inp["rel_bias"], np.float32))
    d = dict(g1=gl(inp["norm1_g"][:L]), g2=gl(inp["norm2_g"][:L]), gf=gl(inp["final_g"])[0],
             w_in=f32(inp["w_in"][:L]),
             projw=f32(np.concatenate([np.asarray(inp["proj_a"][:L]), np.asarray(inp["proj_b"][:L]), np.asarray(inp["proj_c"][:L])], axis=1)),
             w_out=f32(inp["w_out"][:L]), ffn_up=f32(inp["ffn_up"][:L]), ffn_down=f32(inp["ffn_down"][:L]),
             conv=f32(np.asarray(inp["ffn_conv"][:L]).transpose(0, 2, 1).reshape(L, d_ff // 128, 128, 3).transpose(0, 2, 1, 3)),
             tabs_g=tg, tabs_m=tm, tabs_a=ta,
             sinks=f32(np.broadcast_to(np.asarray(inp["attn_sinks"][:L])[:, None, :], (L, 64, 8))),
             ident=np.eye(128, dtype=np.float32))
    prm, lmu, wu, au, gu = [], [], [], [], []
    for l in range(L):
        hp = rwkv_host_params(heads_b, *[np.asarray(inp[k][l], np.float32) for k in
                                         ("rwkv_mu", "rwkv_w0", "rwkv_w_up", "rwkv_a0", "rwkv_a_up", "rwkv_g_up", "rwkv_k_k",
                                          "rwkv_k_a", "rwkv_r_k", "rwkv_lnx_g", "rwkv_lnx_b")])
        prm.append(hp["prm"]); lmu.append(hp["lmu"]); wu.append(hp["wup"]); au.append(hp["aup"]); gu.append(hp["gup"])
    d.update(prm=np.stack(prm), lmu=np.stack(lmu), rw_up=np.stack(wu), ra_up=np.stack(au), rg_up=np.stack(gu))
    d.update(rwkv_consts())
    return d


_NC_CACHE = {}
N_LAUNCH = 1


def kernel(**inp):
    x = np.asarray(inp["x"], np.float32)
    Bn, Sq, Dm = x.shape
    L = np.asarray(inp["w_in"]).shape[0]
    d_ff = np.asarray(inp["ffn_down"]).shape[1]
    heads_a, heads_c, heads_b = list(range(8)), list(range(4)), list(range(12))
    nl = N_LAUNCH if L % N_LAUNCH == 0 else 1
    Lp = L // nl
    xTs = [np.ascontiguousarray(x[b].T) for b in range(Bn)]
    per_layer = ("norm1_g", "w_in", "attn_sinks", "rwkv_mu", "rwkv_w0", "rwkv_w_up", "rwkv_a0", "rwkv_a_up", "rwkv_g_up", "rwkv_k_k",
                 "rwkv_k_a", "rwkv_r_k", "rwkv_lnx_g", "rwkv_lnx_b", "proj_a", "proj_b", "proj_c", "w_out", "norm2_g", "ffn_up",
                 "ffn_conv", "ffn_down")
    for li in range(nl):
        final = (li == nl - 1)
        key = (Sq, Lp, d_ff, final)
        if key not in _NC_CACHE:
            _NC_CACHE[key] = build_full(Sq, Lp, d_ff, heads_a, heads_c, heads_b, final=final)
        nc = _NC_CACHE[key]
        sub = dict(inp)
        for k in per_layer:
            sub[k] = np.asarray(inp[k])[li * Lp:(li + 1) * Lp]
        shared = host_inputs(sub, Lp, d_ff, heads_b)
        in_maps = []
        for b in range(Bn):
            m = dict(shared)
            m["xT"] = xTs[b]
            in_maps.append(m)
        res = run_bass_kernel_spmd(nc, in_maps, core_ids=list(range(Bn)))
        xTs = [np.ascontiguousarray(r["outT"]) for r in res.results]
    out = np.stack([np.ascontiguousarray(t.T) for t in xTs], axis=0)
    return out.astype(np.float32)
```

```python
import concourse.bass as bass
import concourse.mybir as mybir

ENGS = ("pe", "dve", "act", "pool", "sp")


_PH = [0]


def uname(n):
    return "%s_%d" % (n, _PH[0])


class Sched:
    _uid = 0

    def __init__(self, nc, n_dma_sems=24):
        self.nc = nc
        self.streams = {e: [] for e in ENGS}
        self.seq = {e: 0 for e in ENGS}
        self.waited = {}
        self.lastw = {}
        self.readers = {}
        self.n_dma = n_dma_sems
        self.dma_cnt = [0] * n_dma_sems
        self.dma_rr = 0
        self.sems = {}
        self.n_wait = 0
        self.alias = {}

    def canon(self, keys):
        return [self.alias.get(k, k) for k in keys]

    def eng(self, e):
        nc = self.nc
        return {"pe": nc.tensor, "dve": nc.vector, "act": nc.scalar, "pool": nc.gpsimd, "sp": nc.sync}[e]

    def _need(self, cons, deps):
        best = {}
        for p, v in deps:
            if p is None:
                continue
            if v > best.get(p, 0):
                best[p] = v
        out = []
        for p, v in best.items():
            if self.waited.get((cons, p), 0) >= v:
                continue
            self.waited[(cons, p)] = v
            out.append((p, v))
        return out

    def _deps(self, e, reads, writes, same_engine_war=False):
        deps = []
        me = ("eng", e)
        for k in reads:
            w = self.lastw.get(k)
            if w is not None:
                deps.append(w)
        for k in writes:
            w = self.lastw.get(k)
            if w is not None:
                deps.append(w)
            for p, v in self.readers.get(k, {}).items():
                deps.append((p, v))
        return deps

    def _record(self, prod, val, reads, writes):
        for k in reads:
            d = self.readers.setdefault(k, {})
            if d.get(prod, 0) < val:
                d[prod] = val
        for k in writes:
            self.lastw[k] = (prod, val)
            self.readers[k] = {}

    def op(self, e, fn, reads=(), writes=()):
        reads = self.canon(reads); writes = self.canon(writes)
        deps = self._deps(e, reads, writes)
        if e == "pe":
            deps = [d for d in deps if d[0] != ("eng", "pe")]
        waits = self._need(e, deps)
        self.seq[e] += 1
        val = self.seq[e]
        self.streams[e].append(("op", fn, waits, None))
        self._record(("eng", e), val, reads, writes)
        return val

    def dma(self, q, fn, reads=(), writes=()):
        reads = self.canon(reads); writes = self.canon(writes)
        deps = self._deps(q, reads, writes, same_engine_war=True)
        i = self.dma_rr
        self.dma_rr = (self.dma_rr + 1) % self.n_dma
        prod = ("dma", i)
        if self.dma_cnt[i] > 0:
            deps.append((prod, self.dma_cnt[i]))
        waits = self._need(q, deps)
        self.dma_cnt[i] += 16
        val = self.dma_cnt[i]
        self.streams[q].append(("dma", fn, waits, (i, 16)))
        self._record(prod, val, reads, writes)
        return prod, val

    def finish_waits(self, e="sp"):
        deps = [(("dma", i), c) for i, c in enumerate(self.dma_cnt) if c > 0]
        deps += [(("eng", x), self.seq[x]) for x in ENGS if self.seq[x] > 0 and x != e]
        waits = self._need(e, deps)
        self.streams[e].append(("wait", None, waits, None))

    def barrier_all(self):
        for e in ENGS:
            deps = [(("dma", i), c) for i, c in enumerate(self.dma_cnt) if c > 0]
            deps += [(("eng", x), self.seq[x]) for x in ENGS if self.seq[x] > 0 and x != e]
            waits = self._need(e, deps)
            self.streams[e].append(("wait", None, waits, None))

    def emit(self):
        nc = self.nc
        Sched._uid += 1
        u = Sched._uid
        esem = {e: nc.alloc_semaphore("s%d_%s" % (u, e)) for e in ENGS}
        dsem = [nc.alloc_semaphore("d%d_%d" % (u, i)) for i in range(self.n_dma)]

        def semof(p):
            return esem[p[1]] if p[0] == "eng" else dsem[p[1]]

        def run(e):
            def body(engine):
                for kind, fn, waits, dinfo in self.streams[e]:
                    for p, v in waits:
                        engine.wait_ge(semof(p), v)
                        self.n_wait += 1
                    if kind == "op":
                        fn(engine).then_inc(esem[e], 1)
                    elif kind == "dma":
                        fn(engine).then_inc(dsem[dinfo[0]], dinfo[1])
            return body

        with nc.Block() as block:
            block.tensor(run("pe"))
            block.vector(run("dve"))
            block.scalar(run("act"))
            block.gpsimd(run("pool"))
            block.sync(run("sp"))
        nc.all_engine_barrier()
        nc.clear_and_free_semaphores(list(esem.values()) + dsem)
        nc.all_engine_barrier()


import numpy as np
from concourse.bass_utils import run_bass_kernel_spmd

F32 = mybir.dt.float32
BF16 = mybir.dt.bfloat16
ALU = mybir.AluOpType
AF = mybir.ActivationFunctionType
AX = mybir.AxisListType

D_MODEL = 2048
NORM_EPS = 1e-5
KC = D_MODEL // 128


def emit_rmsnorm_T(S, nc, xT, hT, g_sb, ones_bf, sq, ps, rstd, ntok, keys):
    kx, kh, ksq, kps, krs = keys["x"], keys["h"], keys["sq"], keys["ps"], keys["rstd"]
    for c in range(KC):
        S.op("act", lambda e, c=c: e.activation(out=sq[:, c, :], in_=xT[:, c, :], func=AF.Square),
             reads=[kx], writes=[(ksq, c)])
    for c in range(KC):
        S.op("pe", lambda e, c=c: e.matmul(ps, ones_bf, sq[:, c, :], start=(c == 0), stop=(c == KC - 1)),
             reads=[(ksq, c)], writes=[kps])
    S.op("dve", lambda e: e.tensor_scalar(out=rstd, in0=ps, scalar1=1.0 / D_MODEL, scalar2=NORM_EPS,
                                          op0=ALU.mult, op1=ALU.add), reads=[kps], writes=[krs])
    S.op("act", lambda e: e.activation(out=rstd, in_=rstd, func=AF.Sqrt), reads=[krs], writes=[krs])
    S.op("dve", lambda e: e.reciprocal(out=rstd, in_=rstd), reads=[krs], writes=[krs])
    for c in range(KC):
        S.op("dve", lambda e, c=c: e.scalar_tensor_tensor(out=hT[:, c, :], in0=xT[:, c, :], scalar=g_sb[:, c:c + 1],
                                                         in1=rstd, op0=ALU.mult, op1=ALU.mult),
             reads=[kx, krs], writes=[kh])


def build_proj(T, ncols, TT=512):
    nc = bass.Bass("TRN2", target_bir_lowering=False)
    xT = nc.dram_tensor("xT", [D_MODEL, T], F32, kind="ExternalInput").ap()
    g = nc.dram_tensor("g", [128, KC], F32, kind="ExternalInput").ap()
    W = nc.dram_tensor("W", [D_MODEL, ncols], F32, kind="ExternalInput").ap()
    pT = nc.dram_tensor("pT", [ncols, T], F32, kind="ExternalOutput").ap()
    ntt = T // TT
    CB = 512
    ncb = (ncols + CB - 1) // CB
    import contextlib
    with contextlib.ExitStack() as st:
        sb = lambda name, shape, dt: st.enter_context(nc.sbuf_tensor(uname(name), shape, dt))
        ones_bf = sb("ones", [128, 128], BF16)
        g_sb = sb("g_sb", [128, KC], F32)
        hT = sb("hT", [128, KC, T], BF16)
        xin = [sb("xin%d" % i, [128, KC, TT], F32) for i in range(2)]
        sq = sb("sq", [128, KC, TT], BF16)
        rstd = sb("rstd", [128, TT], F32)
        wt = [sb("wt%d" % i, [128, KC, CB], BF16) for i in range(2)]
        ot = [sb("ot%d" % i, [128, TT], F32) for i in range(4)]
        pss = [st.enter_context(nc.psum_tensor(uname("ps%d" % i), [128, 512], F32)) for i in range(8)]
        _PH[0] += 1
        S = Sched(nc)
        S.op("pool", lambda e: e.memset(ones_bf[:], 1.0), writes=["ones"])
        S.dma("sp", lambda e: e.dma_start(out=g_sb[:], in_=g), writes=["g"])
        xTv = xT.rearrange("(c p) t -> p c t", p=128)
        Wv = W.rearrange("(c p) n -> p c n", p=128)
        for tt in range(ntt):
            xb = xin[tt % 2]
            S.dma("sp", lambda e, xb=xb, tt=tt: e.dma_start(out=xb[:], in_=xTv[:, :, tt * TT:(tt + 1) * TT]),
                  writes=[("xin", tt % 2)])
            keys = dict(x=("xin", tt % 2), h=("h", tt), sq="sq", ps=("ps", 0), rstd="rstd")
            emit_rmsnorm_T(S, nc, xb[:], hT[:, :, tt * TT:(tt + 1) * TT], g_sb[:], ones_bf[:], sq[:], pss[0][:, :TT],
                           rstd[:], TT, keys)
        n_o = 0
        n_ps = 0
        for cb in range(ncb):
            c0 = cb * CB
            cw = min(CB, ncols - c0)
            wb = wt[cb % 2]
            S.dma("pool", lambda e, wb=wb, c0=c0, cw=cw: e.dma_start(out=wb[:, :, :cw], in_=Wv[:, :, c0:c0 + cw]),
                  writes=[("wt", cb % 2)])
            for m0 in range(0, cw, 128):
                mw = min(128, cw - m0)
                for tt in range(ntt):
                    pi = 1 + (n_ps % 7); n_ps += 1
                    ps = pss[pi]
                    for c in range(KC):
                        S.op("pe", lambda e, ps=ps, wb=wb, c=c, m0=m0, mw=mw, tt=tt:
                             e.matmul(ps[:mw, :TT], wb[:, c, m0:m0 + mw], hT[:, c, tt * TT:(tt + 1) * TT],
                                      start=(c == 0), stop=(c == KC - 1)),
                             reads=[("wt", cb % 2), ("h", tt), "ones", "g"], writes=[("ps", pi)])
                    oi = n_o % 4; n_o += 1
                    ob = ot[oi]
                    eng = "act" if (n_o % 2) else "dve"
                    if eng == "act":
                        S.op("act", lambda e, ob=ob, ps=ps, mw=mw: e.copy(out=ob[:mw, :], in_=ps[:mw, :TT]),
                             reads=[("ps", pi)], writes=[("ot", oi)])
                    else:
                        S.op("dve", lambda e, ob=ob, ps=ps, mw=mw: e.tensor_copy(out=ob[:mw, :], in_=ps[:mw, :TT]),
                             reads=[("ps", pi)], writes=[("ot", oi)])
                    S.dma("sp", lambda e, ob=ob, mw=mw, r0=c0 + m0, tt=tt:
                          e.dma_start(out=pT[r0:r0 + mw, tt * TT:(tt + 1) * TT], in_=ob[:mw, :]),
                          reads=[("ot", oi)])
        S.finish_waits("sp")
        S.emit()
    return nc


import math

ROW_AQ, ROW_AK, ROW_AV = 0, 512, 640
ROW_BR, ROW_BK, ROW_BV, ROW_WD, ROW_AD, ROW_GD = 768, 1536, 2304, 3072, 3168, 3264
ROW_CQ, ROW_CK, ROW_CV = 3520, 4288, 5056
NP_ROWS = 5824
YROW_A, YROW_B, YROW_C = 0, 512, 1280
C_DILS = (1, 4, 16)


def ssl(c0, n, d):
    return slice(c0, c0 + d * (n - 1) + 1, d)


def t5_bucket_np(dist):
    dist = np.asarray(dist, np.int64)
    nf = np.maximum(dist, 1).astype(np.float32)
    large = 16 + (np.log(nf / np.float32(16)) / np.float32(math.log(2048 / 16)) * np.float32(16)).astype(np.int32)
    return np.where(dist < 16, dist, np.minimum(large, 31))


def attn_tables(rel_bias):
    i = np.arange(128)[None, :]
    j = np.arange(128)[:, None]
    dists = (i - j, i + 128 - j)
    specs = [(h, 1, 127) for h in range(8)] + [(8 + g * 4 + hh, dil, 128) for g, dil in enumerate(C_DILS)
                                              for hh in range(4)]
    gathered = np.zeros((20, 128, 256), np.float32)
    mul = np.zeros((20, 128, 256), np.float32)
    add = np.zeros((20, 128, 256), np.float32)
    for n, (col, dil, ms) in enumerate(specs):
        for half, d in enumerate(dists):
            valid = (d >= 0) & (d <= ms)
            idx = t5_bucket_np(np.maximum(d, 0) * dil)
            gathered[n, :, half * 128:(half + 1) * 128] = rel_bias[idx, col]
            mul[n, :, half * 128:(half + 1) * 128] = np.where(valid, 8.0, 0.0)
            add[n, :, half * 128:(half + 1) * 128] = np.where(valid, 0.0, -240000.0)
    return gathered, mul, add


def emit_attention(S, nc, st, T, pT, yT, tabs_g, tabs_m, tabs_a, sinks_rep, ident_d, pss, heads_a, heads_c):
    sb = lambda name, shape, dt: st.enter_context(nc.sbuf_tensor(uname(name), shape, dt))
    NB = T // 128
    q_bf = sb("at_q", [64, T], BF16)
    k_bf = sb("at_k", [64, T], BF16)
    v_f = sb("at_v", [64, T], F32)
    stg = sb("at_stg", [64, T], F32)
    vaug = sb("at_vaug", [128, NB, 65], BF16)
    acc = sb("at_acc", [65, T], F32)
    tb_g = sb("at_tbg", [128, 256], F32)
    tb_m = sb("at_tbm", [128, 256], F32)
    tb_a = sb("at_tba", [128, 256], F32)
    tb = sb("at_tb", [128, 256], BF16)
    pt_sb = [sb("at_pt%d" % i, [128, 512], BF16) for i in range(2)]
    ident_f = sb("at_identf", [128, 128], F32)
    ident_b = sb("at_identb", [128, 128], BF16)
    sel = sb("at_sel", [65, 64], F32)
    esink = sb("at_esink", [64, 8], F32)
    den = sb("at_den", [64, 512], F32)
    yo = [sb("at_yo%d" % i, [64, 512], F32) for i in range(2)]

    S.dma("sp", lambda e: e.dma_start(out=ident_f[:], in_=ident_d), writes=["at_identf"])
    S.op("dve", lambda e: e.tensor_copy(out=ident_b[:], in_=ident_f[:]), reads=["at_identf"], writes=["at_identb"])
    S.op("pool", lambda e: e.memset(sel[:], 0.0), writes=["at_sel"])
    S.op("pool", lambda e: e.memset(sel[64:65, :], 1.0), writes=["at_sel"])
    S.op("pool", lambda e: e.memset(vaug[:, :, 64:65], 1.0), writes=["at_vaug1"])
    S.dma("sp", lambda e: e.dma_start(out=esink[:], in_=sinks_rep), writes=["at_esink"])
    S.op("act", lambda e: e.activation(out=esink[:], in_=esink[:], func=AF.Exp), reads=["at_esink"],
         writes=["at_esink"])

    ps_s = [pss[0], pss[1]]
    ps_o = [pss[2], pss[3]]
    ps_t = pss[4]
    ps_d = pss[5]
    cnt = dict(s=0, o=0, y=0)

    def load_table(n):
        S.dma("sp", lambda e: e.dma_start(out=tb_g[:], in_=tabs_g[n]), writes=["at_tbg"])
        S.dma("sp", lambda e: e.dma_start(out=tb_m[:], in_=tabs_m[n]), writes=["at_tbm"])
        S.dma("sp", lambda e: e.dma_start(out=tb_a[:], in_=tabs_a[n]), writes=["at_tba"])
        S.op("pool", lambda e: e.tensor_tensor(out=tb_g[:], in0=tb_g[:], in1=tb_m[:], op=ALU.mult),
             reads=["at_tbg", "at_tbm"], writes=["at_tbg"])
        S.op("pool", lambda e: e.tensor_tensor(out=tb[:], in0=tb_g[:], in1=tb_a[:], op=ALU.add),
             reads=["at_tbg", "at_tba"], writes=["at_tb"])

    def load_kv(krow, vrow, dil):
        S.dma("act", lambda e: e.dma_start(out=stg[:], in_=pT[krow:krow + 64, :]), reads=["pT"], writes=["at_stg"])
        S.op("pool", lambda e: e.tensor_copy(out=k_bf[:], in_=stg[:]), reads=["at_stg"], writes=["at_k"])
        S.dma("sp", lambda e: e.dma_start(out=v_f[:], in_=pT[vrow:vrow + 64, :]), reads=["pT"], writes=["at_v"])
        Lf = T // dil
        bps = Lf // 128
        for vb0 in range(0, NB, 8):
            for u in range(8):
                vb = vb0 + u
                s_, jb = vb // bps, vb % bps
                c0 = s_ + dil * 128 * jb
                src = v_f[0:64, ssl(c0, 128, dil)]
                S.op("pe", lambda e, u=u, src=src: e.transpose(ps_t[:, u * 64:(u + 1) * 64], src, ident_f[0:64, 0:64]),
                     reads=["at_v", "at_identf"], writes=["ps_t"])
            S.op("dve", lambda e, vb0=vb0: e.tensor_copy(out=vaug[:, vb0:vb0 + 8, 0:64],
                                                        in_=ps_t[:, :].rearrange("p (u d) -> p u d", d=64)),
                 reads=["ps_t"], writes=["at_vaug"])

    def run_seq(dil, first_group):
        Lf = T // dil
        bps = Lf // 128
        nbt = min(4, bps)
        for s_ in range(dil):
            for jb0 in range(0, bps, nbt):
                oi = cnt["o"] % 2; cnt["o"] += 1
                po = ps_o[oi]
                for half in range(0, nbt, 2):
                    si = cnt["s"] % 2; cnt["s"] += 1
                    pst = ps_s[si]
                    ptb = pt_sb[si]
                    nb2 = min(2, nbt - half)
                    lo = 512
                    for r in range(nb2):
                        jb = jb0 + half + r
                        qs = s_ + dil * 128 * jb
                        qv = q_bf[:, ssl(qs, 128, dil)]
                        kv = k_bf[:, ssl(qs, 128, dil)]
                        sl_prev = slice((2 * r) * 128, (2 * r + 1) * 128)
                        sl_cur = slice((2 * r + 1) * 128, (2 * r + 2) * 128)
                        if jb > 0:
                            ks = s_ + dil * 128 * (jb - 1)
                            kpv = k_bf[:, ssl(ks, 128, dil)]
                            S.op("pe", lambda e, pst=pst, sl=sl_prev, kpv=kpv, qv=qv:
                                 e.matmul(pst[:, sl], kpv, qv, start=True, stop=False),
                                 reads=["at_k", "at_q"], writes=[("ps_s", si)])
                            S.op("pe", lambda e, pst=pst, sl=sl_prev:
                                 e.matmul(pst[:, sl], ident_b[:], tb[:, 128:256], start=False, stop=True),
                                 reads=["at_tb", "at_identb"], writes=[("ps_s", si)])
                            lo = min(lo, sl_prev.start)
                        S.op("pe", lambda e, pst=pst, sl=sl_cur, kv=kv, qv=qv:
                             e.matmul(pst[:, sl], kv, qv, start=True, stop=False),
                             reads=["at_k", "at_q"], writes=[("ps_s", si)])
                        S.op("pe", lambda e, pst=pst, sl=sl_cur:
                             e.matmul(pst[:, sl], ident_b[:], tb[:, 0:128], start=False, stop=True),
                             reads=["at_tb", "at_identb"], writes=[("ps_s", si)])
                        lo = min(lo, sl_cur.start)
                    hi = nb2 * 256
                    S.op("act", lambda e, ptb=ptb, pst=pst, lo=lo, hi=hi:
                         e.activation(out=ptb[:, lo:hi], in_=pst[:, lo:hi], func=AF.Exp, scale=0.125),
                         reads=[("ps_s", si)], writes=[("at_pt", si)])
                    for r in range(nb2):
                        jb = jb0 + half + r
                        vb = s_ * bps + jb
                        osl = slice((half + r) * 128, (half + r + 1) * 128)
                        S.op("pe", lambda e, po=po, osl=osl, vb=vb, ptb=ptb, r=r, last=(jb == 0):
                             e.matmul(po[0:65, osl], vaug[:, vb, :], ptb[:, (2 * r + 1) * 128:(2 * r + 2) * 128],
                                      start=True, stop=last),
                             reads=["at_vaug", "at_vaug1", ("at_pt", si)], writes=[("ps_o", oi)])
                        if jb > 0:
                            S.op("pe", lambda e, po=po, osl=osl, vb=vb, ptb=ptb, r=r:
                                 e.matmul(po[0:65, osl], vaug[:, vb - 1, :], ptb[:, (2 * r) * 128:(2 * r + 1) * 128],
                                          start=False, stop=True),
                                 reads=["at_vaug", "at_vaug1", ("at_pt", si)], writes=[("ps_o", oi)])
                t0 = s_ + dil * 128 * jb0
                n_el = nbt * 128
                av = acc[:, ssl(t0, n_el, dil)]
                if first_group:
                    S.op("dve", lambda e, av=av, po=po, n_el=n_el: e.tensor_copy(out=av, in_=po[0:65, 0:n_el]),
                         reads=[("ps_o", oi)], writes=["at_acc"])
                else:
                    S.op("dve", lambda e, av=av, po=po, n_el=n_el:
                         e.tensor_tensor(out=av, in0=po[0:65, 0:n_el], in1=av, op=ALU.add),
                         reads=[("ps_o", oi), "at_acc"], writes=["at_acc"])

    def normalize(yrow, sink_col):
        for t0 in range(0, T, 512):
            S.op("pe", lambda e, t0=t0: e.matmul(ps_d[0:64, :], sel[:], acc[:, t0:t0 + 512], start=True, stop=True),
                 reads=["at_acc", "at_sel"], writes=["ps_d"])
            if sink_col is not None:
                S.op("dve", lambda e: e.tensor_scalar(out=den[:], in0=ps_d[0:64, :],
                                                      scalar1=esink[:, sink_col:sink_col + 1], scalar2=None,
                                                      op0=ALU.add),
                     reads=["ps_d", "at_esink"], writes=["at_den"])
                S.op("dve", lambda e: e.reciprocal(out=den[:], in_=den[:]), reads=["at_den"], writes=["at_den"])
            else:
                S.op("dve", lambda e: e.reciprocal(out=den[:], in_=ps_d[0:64, :]), reads=["ps_d"], writes=["at_den"])
            yi = cnt["y"] % 2; cnt["y"] += 1
            yb = yo[yi]
            S.op("dve", lambda e, yb=yb, t0=t0: e.tensor_tensor(out=yb[:], in0=acc[0:64, t0:t0 + 512], in1=den[:],
                                                                op=ALU.mult),
                 reads=["at_acc", "at_den"], writes=[("at_yo", yi)])
            S.dma("sp", lambda e, yb=yb, t0=t0: e.dma_start(out=yT[yrow:yrow + 64, t0:t0 + 512], in_=yb[:]),
                  reads=[("at_yo", yi)], writes=["yT"])

    last_kv = None
    for h in heads_a:
        kvh = h // 4
        if last_kv != kvh:
            load_kv(ROW_AK + 64 * kvh, ROW_AV + 64 * kvh, 1)
            last_kv = kvh
        S.dma("act", lambda e, h=h: e.dma_start(out=stg[:], in_=pT[ROW_AQ + 64 * h:ROW_AQ + 64 * h + 64, :]),
              reads=["pT"], writes=["at_stg"])
        S.op("pool", lambda e: e.tensor_copy(out=q_bf[:], in_=stg[:]), reads=["at_stg"], writes=["at_q"])
        load_table(h)
        run_seq(1, True)
        normalize(YROW_A + 64 * h, h)
    for hh in heads_c:
        for g, dil in enumerate(C_DILS):
            off = g * 256 + hh * 64
            load_kv(ROW_CK + off, ROW_CV + off, dil)
            S.dma("act", lambda e, off=off: e.dma_start(out=stg[:], in_=pT[ROW_CQ + off:ROW_CQ + off + 64, :]),
                  reads=["pT"], writes=["at_stg"])
            S.op("pool", lambda e: e.tensor_copy(out=q_bf[:], in_=stg[:]), reads=["at_stg"], writes=["at_q"])
            load_table(8 + g * 4 + hh)
            run_seq(dil, g == 0)
        normalize(YROW_C + 64 * hh, None)


def build_attn_test(T, heads_a, heads_c):
    nc = bass.Bass("TRN2", target_bir_lowering=False)
    pT = nc.dram_tensor("pT", [NP_ROWS, T], F32, kind="ExternalInput").ap()
    tg = nc.dram_tensor("tabs_g", [20, 128, 256], F32, kind="ExternalInput").ap()
    tm = nc.dram_tensor("tabs_m", [20, 128, 256], F32, kind="ExternalInput").ap()
    ta = nc.dram_tensor("tabs_a", [20, 128, 256], F32, kind="ExternalInput").ap()
    sk = nc.dram_tensor("sinks", [64, 8], F32, kind="ExternalInput").ap()
    idd = nc.dram_tensor("ident", [128, 128], F32, kind="ExternalInput").ap()
    yT = nc.dram_tensor("yT", [1536, T], F32, kind="ExternalOutput").ap()
    import contextlib
    with contextlib.ExitStack() as st:
        pss = [st.enter_context(nc.psum_tensor(uname("ps%d" % i), [128, 512], F32)) for i in range(8)]
        _PH[0] += 1
        S = Sched(nc)
        emit_attention(S, nc, st, T, pT, yT, tg, tm, ta, sk, idd, pss, heads_a, heads_c)
        S.finish_waits("sp")
        S.emit()
    return nc


CH = 64
B_GN_EPS = 64e-5


def rwkv_consts():
    s_ = np.arange(64)[:, None]; t_ = np.arange(64)[None, :]
    lt = (s_ < t_).astype(np.float32); le = (s_ <= t_).astype(np.float32); gt = (s_ > t_).astype(np.float32)
    m_lt2 = np.ascontiguousarray(np.broadcast_to(np.concatenate([lt, lt], 1)[:, None, :], (64, 3, 128)))
    m_le2 = np.ascontiguousarray(np.broadcast_to(np.concatenate([le, le], 1)[:, None, :], (64, 3, 128)))
    m_gt = np.ascontiguousarray(np.broadcast_to(gt[:, None, :], (64, 3, 64)))
    rst = np.ones((64, 512), np.float32); rst[:, ::64] = 0.0
    return dict(m_lt2=m_lt2, m_le2=m_le2, m_gt=m_gt, rst=rst)


def phase_rwkv(nc, T, pT, yT, prm_d, lmu_d, wup_d, aup_d, gup_d, ident_d, mlt_d, mle_d, mgt_d, rst_d, heads, dbg=None, stop=None):
    import contextlib
    HG = 3
    TT = 512
    NCK = TT // CH
    NH = len(heads)
    with contextlib.ExitStack() as st:
        sb = lambda name, shape, dt=F32: st.enter_context(nc.sbuf_tensor(uname(name), shape, dt))
        pss = [st.enter_context(nc.psum_tensor(uname("rps%d" % i), [128, 512], F32)) for i in range(8)]
        _PH[0] += 1
        S = Sched(nc)
        ident = sb("rw_ident", [128, 128]); m_lt2 = sb("rw_mlt", [64, 3, 128]); m_le2 = sb("rw_mle", [64, 3, 128])
        m_gt = sb("rw_mgt", [64, 3, 64]); rst = sb("rw_rst", [64, 512])
        prm = sb("rw_prm", [64, NH, 10]); lmu = sb("rw_lmu", [128, 4])
        wup = sb("rw_wup", [96, NH * 64]); aup = sb("rw_aup", [96, NH * 64]); gup = sb("rw_gup", [128, 2, NH * 64])
        ones64 = sb("rw_ones", [64, 64]); avg64 = sb("rw_avg", [64, 64]); rkb = sb("rw_rkb", [64, NH, 64])
        for t_, d_ in ((ident, ident_d), (m_lt2, mlt_d), (m_le2, mle_d), (m_gt, mgt_d), (rst, rst_d), (prm, prm_d),
                       (lmu, lmu_d), (wup, wup_d), (aup, aup_d)):
            S.dma("sp", lambda e, t_=t_, d_=d_: e.dma_start(out=t_[:], in_=d_), writes=["const"])
        S.dma("sp", lambda e: e.dma_start(out=gup[:], in_=gup_d.rearrange("(c p) n -> p c n", p=128)), writes=["const"])
        S.op("pool", lambda e: e.memset(ones64[:], 1.0), writes=["const"])
        S.op("pool", lambda e: e.memset(avg64[:], 1.0 / 64), writes=["const"])
        for hi in range(NH):
            S.op("dve", lambda e, hi=hi: e.tensor_scalar(out=rkb[:, hi, :], in0=ones64[:], scalar1=prm[:, hi, 7:8],
                                                        scalar2=None, op0=ALU.mult), reads=["const"], writes=["const"])
        lin = [sb("rw_lin%d" % i, [128, TT + 1]) for i in range(4)]
        ltmp = sb("rw_ltmp", [128, TT])
        th = sb("rw_th", [96, TT]); adm = sb("rw_adm", [96, TT]); sg = sb("rw_sg", [128, 2, TT])
        xin = [sb("rw_xin%d" % i, [64, HG, TT + 1]) for i in range(3)]
        names = ["rm", "km", "vm", "logw", "iclr", "g", "kkn", "k2", "sbon", "Lc", "G", "t0", "t1", "t2",
                 "at"]
        B = {n: sb("rw_" + n, [64, HG, TT]) for n in names}
        for n in ("rt", "bt", "kt", "atb"):
            B[n] = sb("rw_" + n, [64, HG, TT], BF16)
        B["bh"] = B["iclr"]; B["kh"] = B["kkn"]; B["y"] = B["logw"]
        S.alias.update({"bh": "iclr", "kh": "kkn", "y": "logw", ("yo", 0): "Lc", ("yo", 1): "Lc"})
        RhT = sb("rw_RhT", [64, NCK, HG, 64]); Y0T = sb("rw_Y0T", [64, NCK, HG, 64])
        MTa = sb("rw_MT", [64, NCK, HG, 64]); Na = sb("rw_N", [64, NCK, HG, 64])
        Wp = [sb("rw_W%d" % p, [64, HG, 128], BF16) for p in range(2)]; Vtokp = [sb("rw_Vtok%d" % p, [64, HG, 64], BF16) for p in range(2)]
        BKp = [sb("rw_BK%d" % p, [64, HG, 128], BF16) for p in range(2)]; AQp = [sb("rw_AQ%d" % p, [64, HG, 128], BF16) for p in range(2)]
        QPp = [[sb("rw_QP%d_%d" % (p, i), [64, HG, 128], BF16) for i in range(2)] for p in range(2)]
        ARKp = [sb("rw_ARK%d" % p, [64, HG, 128], BF16) for p in range(2)]
        Hs = [sb("rw_H%d" % i, [64, HG, 64]) for i in range(2)]
        yout = [B["Lc"], B["Lc"]]
        cnt = dict(l=0, m=0, y=0)

        def ps_l():
            i = cnt["l"] % 2; cnt["l"] += 1
            return pss[i], ("ps", i)

        def ps_m():
            i = 4 + cnt["m"] % 2; cnt["m"] += 1
            return pss[i], ("ps", i)

        def dve(fn, r, w): S.op("dve", fn, reads=r, writes=w)
        def act(fn, r, w): S.op("act", fn, reads=r, writes=w)
        def pool(fn, r, w): S.op("pool", fn, reads=r, writes=w)
        def pe(fn, r, w): S.op("pe", fn, reads=r, writes=w)

        for g0 in range(0, NH, HG):
            hs = heads[g0:g0 + HG]
            pool(lambda e: e.memset(Hs[0][:], 0.0), [], ["H0"])
            st_h = dict(hcur=0)

            def do_tile(ti, g0=g0, hs=hs, st_h=st_h):
                t0 = ti * TT
                srcs = [(ROW_WD, 96), (ROW_AD, 96), (ROW_GD, 128), (ROW_GD + 128, 128)]
                for i, (row, n) in enumerate(srcs):
                    if t0 == 0:
                        pool(lambda e, i=i, n=n: e.memset(lin[i][0:n, 0:1], 0.0), [], ["lin%d" % i])
                        S.dma("sp", lambda e, i=i, row=row, n=n: e.dma_start(out=lin[i][0:n, 1:TT + 1],
                                                                          in_=pT[row:row + n, 0:TT]),
                              reads=["pT"], writes=["lin%d" % i])
                    else:
                        S.dma("sp", lambda e, i=i, row=row, n=n: e.dma_start(out=lin[i][0:n, :],
                                                                          in_=pT[row:row + n, t0 - 1:t0 + TT]),
                              reads=["pT"], writes=["lin%d" % i])
                for i, row0 in enumerate((ROW_BR, ROW_BK, ROW_BV)):
                    for j, h in enumerate(hs):
                        row = row0 + 64 * h
                        if t0 == 0:
                            pool(lambda e, i=i, j=j: e.memset(xin[i][:, j, 0:1], 0.0), [], ["xin%d" % i])
                            S.dma("act", lambda e, i=i, j=j, row=row: e.dma_start(out=xin[i][:, j, 1:TT + 1],
                                                                              in_=pT[row:row + 64, 0:TT]),
                                  reads=["pT"], writes=["xin%d" % i])
                        else:
                            S.dma("act", lambda e, i=i, j=j, row=row: e.dma_start(out=xin[i][:, j, :],
                                                                              in_=pT[row:row + 64, t0 - 1:t0 + TT]),
                                  reads=["pT"], writes=["xin%d" % i])
                outs = [th, adm, sg[:, 0, :], sg[:, 1, :]]
                for i, (row, n) in enumerate(srcs):
                    pool(lambda e, i=i, n=n: e.tensor_tensor(out=ltmp[0:n, :], in0=lin[i][0:n, 0:TT], in1=lin[i][0:n, 1:TT + 1],
                                                           op=ALU.subtract), ["lin%d" % i], ["ltmp"])
                    o = outs[i]
                    dve(lambda e, i=i, n=n, o=o: e.scalar_tensor_tensor(out=o[0:n, :] if i < 2 else o, in0=ltmp[0:n, :],
                                                                       scalar=lmu[0:n, i:i + 1], in1=lin[i][0:n, 1:TT + 1],
                                                                       op0=ALU.mult, op1=ALU.add),
                        ["ltmp", "lin%d" % i, "const"], ["lo%d" % i])
                act(lambda e: e.activation(out=th[:], in_=th[:], func=AF.Tanh), ["lo0"], ["lo0"])
                act(lambda e: e.activation(out=sg[:, 0, :], in_=sg[:, 0, :], func=AF.Sigmoid), ["lo2"], ["lo2"])
                act(lambda e: e.activation(out=sg[:, 1, :], in_=sg[:, 1, :], func=AF.Sigmoid), ["lo3"], ["lo3"])
                for i, nm in enumerate(("rm", "km", "vm")):
                    pool(lambda e, i=i: e.tensor_tensor(out=B["t0"][:], in0=xin[i][:, :, 0:TT], in1=xin[i][:, :, 1:TT + 1],
                                                      op=ALU.subtract), ["xin%d" % i], ["t0"])
                    for j in range(HG):
                        dve(lambda e, i=i, j=j, nm=nm: e.scalar_tensor_tensor(
                            out=B[nm][:, j, :], in0=B["t0"][:, j, :], scalar=prm[:, g0 + j, i:i + 1],
                            in1=xin[i][:, j, 1:TT + 1], op0=ALU.mult, op1=ALU.add),
                            ["t0", "xin%d" % i, "const"], [nm])
                for j in range(HG):
                    hj = g0 + j
                    cs = slice(hj * 64, hj * 64 + 64)
                    p1, k1 = ps_l()
                    pe(lambda e, p1=p1, cs=cs: e.matmul(p1[0:64, :], wup[:, cs], th[:], start=True, stop=True),
                       ["lo0", "const"], [k1])
                    act(lambda e, p1=p1, j=j, hj=hj: e.activation(out=B["logw"][:, j, :], in_=p1[0:64, :], func=AF.Sigmoid,
                                                               bias=prm[:, hj, 3:4]), [k1, "const"], ["logw"])
                    p2, k2_ = ps_l()
                    pe(lambda e, p2=p2, cs=cs: e.matmul(p2[0:64, :], aup[:, cs], adm[:], start=True, stop=True),
                       ["lo1", "const"], [k2_])
                    act(lambda e, p2=p2, j=j, hj=hj: e.activation(out=B["iclr"][:, j, :], in_=p2[0:64, :], func=AF.Sigmoid,
                                                               bias=prm[:, hj, 4:5]), [k2_, "const"], ["iclr"])
                    p3, k3 = ps_l()
                    for c in range(2):
                        pe(lambda e, p3=p3, cs=cs, c=c: e.matmul(p3[0:64, :], gup[:, c, cs], sg[:, c, :], start=(c == 0),
                                                               stop=(c == 1)), ["lo2", "lo3", "const"], [k3])
                    act(lambda e, p3=p3, j=j: e.copy(out=B["g"][:, j, :], in_=p3[0:64, :]), [k3], ["g"])
                    dve(lambda e, j=j, hj=hj: e.tensor_scalar(out=B["kkn"][:, j, :], in0=B["km"][:, j, :],
                                                             scalar1=prm[:, hj, 5:6], scalar2=None, op0=ALU.mult),
                        ["km", "const"], ["kkn"])
                    pool(lambda e, j=j: e.tensor_tensor(out=B["t1"][:, j, :], in0=B["kkn"][:, j, :], in1=B["kkn"][:, j, :],
                                                      op=ALU.mult), ["kkn"], ["t1"])
                    p4, k4 = ps_l()
                    pe(lambda e, p4=p4, j=j: e.matmul(p4[0:64, :], ones64[:], B["t1"][:, j, :], start=True, stop=True),
                       ["t1", "const"], [k4])
                    act(lambda e, p4=p4, j=j: e.activation(out=B["t2"][:, j, :], in_=p4[0:64, :], func=AF.Sqrt), [k4], ["t2"])
                    dve(lambda e, j=j: e.tensor_scalar(out=B["t2"][:, j, :], in0=B["t2"][:, j, :], scalar1=1e-12, scalar2=None,
                                                      op0=ALU.max), ["t2"], ["t2"])
                    dve(lambda e, j=j: e.reciprocal(out=B["t2"][:, j, :], in_=B["t2"][:, j, :]), ["t2"], ["t2"])
                    dve(lambda e, j=j, hj=hj: e.tensor_scalar(out=B["k2"][:, j, :], in0=B["iclr"][:, j, :], scalar1=-1.0,
                                                             scalar2=prm[:, hj, 6:7], op0=ALU.add, op1=ALU.mult),
                        ["iclr", "const"], ["k2"])
                dve(lambda e: e.tensor_tensor(out=B["kkn"][:], in0=B["kkn"][:], in1=B["t2"][:], op=ALU.mult),
                    ["kkn", "t2"], ["kkn"])
                dve(lambda e: e.scalar_tensor_tensor(out=B["k2"][:], in0=B["k2"][:], scalar=1.0, in1=B["km"][:],
                                                     op0=ALU.add, op1=ALU.mult), ["k2", "km"], ["k2"])
                pool(lambda e: e.tensor_tensor(out=B["t1"][:], in0=B["rm"][:], in1=B["k2"][:], op=ALU.mult),
                     ["rm", "k2"], ["t1"])
                for j in range(HG):
                    p5, k5 = ps_l()
                    pe(lambda e, p5=p5, j=j: e.matmul(p5[0:64, :], rkb[:, g0 + j, :], B["t1"][:, j, :], start=True, stop=True),
                       ["t1", "const"], [k5])
                    act(lambda e, p5=p5, j=j: e.copy(out=B["sbon"][:, j, :], in_=p5[0:64, :]), [k5], ["sbon"])
                dve(lambda e: e.tensor_scalar(out=B["logw"][:], in0=B["logw"][:], scalar1=-math.exp(-0.5), scalar2=None,
                                              op0=ALU.mult), ["logw"], ["logw"])
                for j in range(HG):
                    dve(lambda e, j=j: e.tensor_tensor_scan(out=B["Lc"][:, j, :], data0=rst[:], data1=B["logw"][:, j, :],
                                                           initial=0.0, op0=ALU.mult, op1=ALU.add),
                        ["logw", "const"], ["Lc"])
                act(lambda e: e.activation(out=B["G"][:], in_=B["Lc"][:], func=AF.Exp), ["Lc"], ["G"])
                pool(lambda e: e.tensor_tensor(out=B["rt"][:], in0=B["rm"][:], in1=B["G"][:], op=ALU.mult), ["rm", "G"], ["rt"])
                dve(lambda e: e.tensor_tensor(out=B["t0"][:], in0=B["Lc"][:], in1=B["logw"][:], op=ALU.subtract),
                    ["Lc", "logw"], ["t0"])
                act(lambda e: e.activation(out=B["t0"][:], in_=B["t0"][:], func=AF.Exp), ["t0"], ["t0"])
                dve(lambda e: e.scalar_tensor_tensor(out=B["at"][:], in0=B["kkn"][:], scalar=-1.0, in1=B["t0"][:],
                                                     op0=ALU.mult, op1=ALU.mult), ["kkn", "t0"], ["at"])
                pool(lambda e: e.tensor_copy(out=B["atb"][:], in_=B["at"][:]), ["at"], ["atb"])
                pool(lambda e: e.tensor_tensor(out=B["t2"][:], in0=B["kkn"][:], in1=B["iclr"][:], op=ALU.mult),
                     ["kkn", "iclr"], ["t2"])
                act(lambda e: e.activation(out=B["t1"][:], in_=B["Lc"][:], func=AF.Exp, scale=-1.0), ["Lc", "t1"], ["t1"])
                dve(lambda e: e.tensor_tensor(out=B["bt"][:], in0=B["t2"][:], in1=B["t1"][:], op=ALU.mult), ["t2", "t1"], ["bt"])
                pool(lambda e: e.tensor_tensor(out=B["kt"][:], in0=B["k2"][:], in1=B["t1"][:], op=ALU.mult), ["k2", "t1"], ["kt"])
                for j in range(HG):
                    lc3 = B["Lc"][:, j, :].rearrange("p (c t) -> p c t", t=CH)
                    o3 = B["t0"][:, j, :].rearrange("p (c t) -> p c t", t=CH)
                    dve(lambda e, lc3=lc3, o3=o3: e.tensor_tensor(out=o3, in0=lc3[:, :, CH - 1:CH].to_broadcast([64, NCK, CH]),
                                                                 in1=lc3, op=ALU.subtract), ["Lc", "at"], ["t0"])
                act(lambda e: e.activation(out=B["t0"][:], in_=B["t0"][:], func=AF.Exp), ["t0"], ["t0"])
                dve(lambda e: e.tensor_tensor(out=B["bh"][:], in0=B["t2"][:], in1=B["t0"][:], op=ALU.mult), ["t2", "t0"], ["bh"])
                pool(lambda e: e.tensor_tensor(out=B["kh"][:], in0=B["k2"][:], in1=B["t0"][:], op=ALU.mult), ["k2", "t0"], ["kh"])

                if dbg is not None and ti == 0 and g0 == 0:
                    for di_, nm_ in enumerate(["rm", "km", "vm", "logw", "iclr", "g", "kkn", "k2", "sbon", "Lc", "G", "rt", "at", "bt", "kt", "bh", "kh"]):
                        S.dma("sp", lambda e, di_=di_, nm_=nm_: e.dma_start(out=dbg[di_], in_=B[nm_][:]), reads=[nm_], writes=["dbg"])
                if stop == "prep":
                    return
                def do_chunk(c, pb):
                    W, Vtok, BK, AQ, QP, ARK = Wp[pb], Vtokp[pb], BKp[pb], AQp[pb], QPp[pb], ARKp[pb]
                    kW, kV, kBK, kAQ, kARK = 'W%d' % pb, 'Vtok%d' % pb, 'BK%d' % pb, 'AQ%d' % pb, 'ARK%d' % pb
                    pw_i, pq_i = (6, 0) if pb == 0 else (7, 1)
                    csl = slice(c * CH, (c + 1) * CH)
                    tp1, ktp1 = pss[2], ("ps", 2)
                    tp2, ktp2 = pss[3], ("ps", 3)
                    for j in range(HG):
                        pe(lambda e, j=j: e.transpose(tp1[0:64, j * 128:j * 128 + 64], B["at"][:, j, csl], ident[0:64, 0:64]),
                           ["at", "const"], [ktp1])
                        pe(lambda e, j=j: e.transpose(tp1[0:64, j * 128 + 64:j * 128 + 128], B["vm"][:, j, csl], ident[0:64, 0:64]),
                           ["vm", "const"], [ktp1])
                        pe(lambda e, j=j: e.transpose(tp2[0:64, j * 128:j * 128 + 64], B["bh"][:, j, csl], ident[0:64, 0:64]),
                           ["bh", "const"], [ktp2])
                        pe(lambda e, j=j: e.transpose(tp2[0:64, j * 128 + 64:j * 128 + 128], B["kh"][:, j, csl], ident[0:64, 0:64]),
                           ["kh", "const"], [ktp2])
                    tp1v = tp1[0:64, 0:HG * 128].rearrange("p (h x) -> p h x", x=128)
                    tp2v = tp2[0:64, 0:HG * 128].rearrange("p (h x) -> p h x", x=128)
                    act(lambda e, tp1v=tp1v: e.copy(out=W[:, :, 0:64], in_=tp1v[:, :, 0:64]), [ktp1], [kW])
                    act(lambda e, tp1v=tp1v: e.copy(out=Vtok[:], in_=tp1v[:, :, 64:128]), [ktp1], [kV])
                    act(lambda e, tp2v=tp2v: e.copy(out=BK[:], in_=tp2v), [ktp2], [kBK])
                    yield
                    if stop == "c1":
                        return
                    m1, km1 = ps_m()
                    for j in range(HG):
                        pe(lambda e, m1=m1, j=j: e.matmul(m1[0:64, j * 128:j * 128 + 64], B["kt"][:, j, csl], B["atb"][:, j, csl],
                                                        start=True, stop=True), ["kt", "atb"], [km1])
                        pe(lambda e, m1=m1, j=j: e.matmul(m1[0:64, j * 128 + 64:j * 128 + 128], B["bt"][:, j, csl], B["atb"][:, j, csl],
                                                        start=True, stop=True), ["bt", "atb"], [km1])
                    m1v = m1[0:64, 0:HG * 128].rearrange("p (h x) -> p h x", x=128)
                    dve(lambda e, m1v=m1v: e.tensor_tensor(out=AQ[:], in0=m1v, in1=m_lt2[:], op=ALU.mult), [km1, "const"], [kAQ])
                    m2, km2 = ps_m()
                    for j in range(HG):
                        pe(lambda e, m2=m2, j=j: e.matmul(m2[0:64, j * 64:j * 64 + 64], B["atb"][:, j, csl], B["bt"][:, j, csl],
                                                        start=True, stop=True), ["bt", "atb"], [km2])
                    m2v = m2[0:64, 0:HG * 64].rearrange("p (h x) -> p h x", x=64)
                    qp = 0
                    dve(lambda e: e.tensor_copy(out=QP[0][:, :, 0:64], in_=AQ[:, :, 64:128]), [kAQ], ["QP%d_0" % pb])
                    dve(lambda e, m2v=m2v: e.tensor_tensor(out=QP[0][:, :, 64:128], in0=m2v, in1=m_gt[:], op=ALU.mult),
                        [km2, "const"], ["QP%d_0" % pb])
                    m3, km3 = ps_m()
                    for j in range(HG):
                        pe(lambda e, m3=m3, j=j: e.matmul(m3[0:64, j * 128:j * 128 + 64], B["bt"][:, j, csl], B["rt"][:, j, csl],
                                                        start=True, stop=True), ["bt", "rt"], [km3])
                        pe(lambda e, m3=m3, j=j: e.matmul(m3[0:64, j * 128 + 64:j * 128 + 128], B["kt"][:, j, csl], B["rt"][:, j, csl],
                                                        start=True, stop=True), ["kt", "rt"], [km3])
                    m3v = m3[0:64, 0:HG * 128].rearrange("p (h x) -> p h x", x=128)
                    dve(lambda e, m3v=m3v: e.tensor_tensor(out=ARK[:], in0=m3v, in1=m_le2[:], op=ALU.mult), [km3, "const"], [kARK])
                    yield
                    if stop == "c2":
                        return
                    m4, km4 = ps_m()
                    for j in range(HG):
                        pe(lambda e, m4=m4, j=j: e.matmul(m4[0:64, j * 64:j * 64 + 64], AQ[:, j, 0:64], Vtok[:, j, :],
                                                        start=True, stop=True), [kAQ, kV], [km4])
                    m4v = m4[0:64, 0:HG * 64].rearrange("p (h x) -> p h x", x=64)
                    act(lambda e, m4v=m4v: e.copy(out=W[:, :, 64:128], in_=m4v), [km4], [kW])
                    yield
                    for it in range(6):
                        Qb = QP[qp]
                        kq = "QP%d_%d" % (pb, qp)
                        pw, kpw = pss[pw_i], ("ps", pw_i)
                        for j in range(HG):
                            pe(lambda e, j=j, Qb=Qb: e.matmul(pw[0:64, j * 128:j * 128 + 128], Qb[:, j, 0:64], W[:, j, :],
                                                             start=True, stop=True), [kq, kW], [kpw])
                        pwv = pw[0:64, 0:HG * 128].rearrange("p (h x) -> p h x", x=128)
                        dve(lambda e, pwv=pwv: e.tensor_tensor(out=W[:], in0=pwv, in1=W[:], op=ALU.add), [kpw, kW], [kW])
                        yield
                        if it < 5:
                            pq, kpq = pss[pq_i], ("ps", pq_i)
                            for j in range(HG):
                                pe(lambda e, j=j, Qb=Qb: e.matmul(pq[0:64, j * 128:j * 128 + 64], Qb[:, j, 64:128], Qb[:, j, 0:64],
                                                                 start=True, stop=True), [kq], [kpq])
                                pe(lambda e, j=j, Qb=Qb: e.matmul(pq[0:64, j * 128 + 64:j * 128 + 128], Qb[:, j, 0:64], Qb[:, j, 64:128],
                                                                 start=True, stop=True), [kq], [kpq])
                            pqv = pq[0:64, 0:HG * 128].rearrange("p (h x) -> p h x", x=128)
                            qn = 1 - qp
                            act(lambda e, pqv=pqv, qn=qn: e.copy(out=QP[qn][:], in_=pqv), [kpq], ["QP%d_%d" % (pb, qn)])
                            qp = qn
                            yield
                    if stop == "c3":
                        return
                    m5, km5 = ps_m()
                    for j in range(HG):
                        pe(lambda e, m5=m5, j=j: e.matmul(m5[0:64, j * 64:j * 64 + 64], W[:, j, 0:64], ARK[:, j, 0:64],
                                                        start=True, stop=True), [kW, kARK], [km5])
                    m5b, km5b = ps_m()
                    for j in range(HG):
                        pe(lambda e, m5b=m5b, j=j: e.matmul(m5b[0:64, j * 64:j * 64 + 64], W[:, j, 64:128], ARK[:, j, 0:64],
                                                          start=True, stop=False), [kW, kARK], [km5b])
                        pe(lambda e, m5b=m5b, j=j: e.matmul(m5b[0:64, j * 64:j * 64 + 64], Vtok[:, j, :], ARK[:, j, 64:128],
                                                          start=False, stop=True), [kV, kARK], [km5b])
                    m5v = m5[0:64, 0:HG * 64].rearrange("p (h x) -> p h x", x=64)
                    m5bv = m5b[0:64, 0:HG * 64].rearrange("p (h x) -> p h x", x=64)
                    dve(lambda e, m5v=m5v, c=c: e.tensor_tensor(out=RhT[:, c, :, :], in0=m5v, in1=B["rt"][:, :, csl],
                                                              op=ALU.add), [km5, "rt"], ["RhT"])
                    act(lambda e, m5bv=m5bv, c=c: e.copy(out=Y0T[:, c, :, :], in_=m5bv), [km5b], ["Y0T"])
                    if stop == "c4":
                        return
                    m6, km6 = ps_m()
                    for j in range(HG):
                        pe(lambda e, m6=m6, j=j: e.matmul(m6[0:64, j * 64:j * 64 + 64], W[:, j, 0:64], BK[:, j, 0:64],
                                                        start=True, stop=True), [kW, kBK], [km6])
                    m6b, km6b = ps_m()
                    for j in range(HG):
                        pe(lambda e, m6b=m6b, j=j: e.matmul(m6b[0:64, j * 64:j * 64 + 64], BK[:, j, 0:64], W[:, j, 64:128],
                                                          start=True, stop=False), [kW, kBK], [km6b])
                        pe(lambda e, m6b=m6b, j=j: e.matmul(m6b[0:64, j * 64:j * 64 + 64], BK[:, j, 64:128], Vtok[:, j, :],
                                                          start=False, stop=True), [kV, kBK], [km6b])
                    m6v = m6[0:64, 0:HG * 64].rearrange("p (h x) -> p h x", x=64)
                    m6bv = m6b[0:64, 0:HG * 64].rearrange("p (h x) -> p h x", x=64)
                    for j in range(HG):
                        gc = B["G"][:, j, c * CH + CH - 1:c * CH + CH]
                        dve(lambda e, m6v=m6v, j=j, c=c, gc=gc: e.scalar_tensor_tensor(
                            out=MTa[:, c, j, :], in0=ident[0:64, 0:64], scalar=gc, in1=m6v[:, j, :],
                            op0=ALU.mult, op1=ALU.add), [km6, "G", "const"], ["MT"])
                    act(lambda e, m6bv=m6bv, c=c: e.copy(out=Na[:, c, :, :], in_=m6bv), [km6b], ["N"])

                for c in range(0, NCK, 2):
                    alive = [do_chunk(c, 0), do_chunk(c + 1, 1)]
                    while alive:
                        for g_ in list(alive):
                            try:
                                next(g_)
                            except StopIteration:
                                alive.remove(g_)
                if stop in ("chunk", "c1", "c2", "c3", "c4"):
                    return

                def do_seq(c):
                    hcur = st_h["hcur"]
                    Hc = Hs[hcur]; Hn = Hs[1 - hcur]
                    kh_, kn_ = "H%d" % hcur, "H%d" % (1 - hcur)
                    py, kpy = ps_m()
                    for j in range(HG):
                        pe(lambda e, py=py, j=j, Hc=Hc, c=c: e.matmul(py[0:64, j * 64:j * 64 + 64], Hc[:, j, :], RhT[:, c, j, :],
                                                                    start=True, stop=True), [kh_, "RhT"], [kpy])
                    pyv = py[0:64, 0:HG * 64].rearrange("p (h x) -> p h x", x=64)
                    dve(lambda e, pyv=pyv, c=c: e.tensor_tensor(out=B["y"][:, :, c * CH:(c + 1) * CH], in0=pyv, in1=Y0T[:, c, :, :],
                                                              op=ALU.add), [kpy, "Y0T"], ["y"])
                    ph, kph = ps_m()
                    for j in range(HG):
                        pe(lambda e, ph=ph, j=j, Hc=Hc, c=c: e.matmul(ph[0:64, j * 64:j * 64 + 64], MTa[:, c, j, :], Hc[:, j, :],
                                                                    start=True, stop=True), [kh_, "MT"], [kph])
                    phv = ph[0:64, 0:HG * 64].rearrange("p (h x) -> p h x", x=64)
                    dve(lambda e, phv=phv, c=c, Hn=Hn: e.tensor_tensor(out=Hn[:], in0=phv, in1=Na[:, c, :, :], op=ALU.add),
                        [kph, "N"], [kn_])
                    st_h["hcur"] = 1 - hcur

                for c in range(NCK):
                    do_seq(c)
                if dbg is not None and ti == 0 and g0 == 0:
                    S.dma("sp", lambda e: e.dma_start(out=dbg[17], in_=B["y"][:]), reads=["y"], writes=["dbg"])
                    for di_, nm_ in enumerate([RhT, Y0T, MTa, Na]):
                        S.dma("sp", lambda e, di_=di_, nm_=nm_: e.dma_start(out=dbg[18 + di_].rearrange("p h x -> p (h x)"), in_=nm_[:].rearrange("p c h x -> p (c h x)")),
                              reads=["RhT", "Y0T", "MT", "N"], writes=["dbg"])
                yi = cnt["y"] % 2; cnt["y"] += 1
                yb = yout[yi]
                for j in range(HG):
                    hj = g0 + j
                    p6, k6 = ps_l()
                    pe(lambda e, p6=p6, j=j: e.matmul(p6[0:64, :], avg64[:], B["y"][:, j, :], start=True, stop=True),
                       ["y", "const"], [k6])
                    dve(lambda e, p6=p6, j=j: e.tensor_tensor(out=B["t0"][:, j, :], in0=B["y"][:, j, :], in1=p6[0:64, :],
                                                            op=ALU.subtract), [k6, "y"], ["t0"])
                    pool(lambda e, j=j: e.tensor_tensor(out=B["t1"][:, j, :], in0=B["t0"][:, j, :], in1=B["t0"][:, j, :],
                                                      op=ALU.mult), ["t0"], ["t1"])
                    p7, k7 = ps_l()
                    pe(lambda e, p7=p7, j=j: e.matmul(p7[0:64, :], avg64[:], B["t1"][:, j, :], start=True, stop=True),
                       ["t1", "const"], [k7])
                    dve(lambda e, p7=p7, j=j: e.tensor_scalar(out=B["t2"][:, j, :], in0=p7[0:64, :], scalar1=B_GN_EPS, scalar2=None,
                                                            op0=ALU.add), [k7], ["t2"])
                    act(lambda e, j=j: e.activation(out=B["t2"][:, j, :], in_=B["t2"][:, j, :], func=AF.Sqrt), ["t2"], ["t2"])
                    dve(lambda e, j=j: e.reciprocal(out=B["t2"][:, j, :], in_=B["t2"][:, j, :]), ["t2"], ["t2"])
                    dve(lambda e, j=j: e.tensor_tensor(out=B["t0"][:, j, :], in0=B["t0"][:, j, :], in1=B["t2"][:, j, :],
                                                      op=ALU.mult), ["t0", "t2"], ["t0"])
                    dve(lambda e, j=j, hj=hj: e.tensor_scalar(out=B["t0"][:, j, :], in0=B["t0"][:, j, :], scalar1=prm[:, hj, 8:9],
                                                             scalar2=prm[:, hj, 9:10], op0=ALU.mult, op1=ALU.add),
                        ["t0", "const"], ["t0"])
                pool(lambda e: e.tensor_tensor(out=B["t1"][:], in0=B["sbon"][:], in1=B["vm"][:], op=ALU.mult),
                     ["sbon", "vm", "t1"], ["t1"])
                dve(lambda e: e.tensor_tensor(out=B["t0"][:], in0=B["t0"][:], in1=B["t1"][:], op=ALU.add), ["t0", "t1"], ["t0"])
                dve(lambda e, yb=yb: e.tensor_tensor(out=yb[:], in0=B["t0"][:], in1=B["g"][:], op=ALU.mult),
                    ["t0", "g"], [("yo", yi)])
                for j, h in enumerate(hs):
                    S.dma("sp", lambda e, yb=yb, j=j, h=h: e.dma_start(out=yT[YROW_B + 64 * h:YROW_B + 64 * h + 64, t0:t0 + TT],
                                                                     in_=yb[:, j, :]), reads=[("yo", yi)], writes=["yT"])

            for ti in range(T // TT):
                do_tile(ti)
        S.barrier_all()
        S.emit()


def build_rwkv_test(T, heads, debug=False, stop=None):
    nc = bass.Bass("TRN2", target_bir_lowering=False)
    NH = len(heads)
    di = lambda n, s: nc.dram_tensor(n, s, F32, kind="ExternalInput").ap()
    pT = di("pT", [NP_ROWS, T]); prm = di("prm", [64, NH, 10]); lmu = di("lmu", [128, 4])
    wup = di("wup", [96, NH * 64]); aup = di("aup", [96, NH * 64]); gup = di("gup", [256, NH * 64])
    ident = di("ident", [128, 128]); mlt = di("m_lt2", [64, 3, 128]); mle = di("m_le2", [64, 3, 128])
    mgt = di("m_gt", [64, 3, 64]); rst = di("rst", [64, 512])
    yT = nc.dram_tensor("yT", [1536, T], F32, kind="ExternalOutput").ap()
    dbg = nc.dram_tensor("dbg", [22, 64, 3, 512], F32, kind="ExternalOutput").ap() if debug else None
    phase_rwkv(nc, T, pT, yT, prm, lmu, wup, aup, gup, ident, mlt, mle, mgt, rst, heads, dbg=dbg, stop=stop)
    return nc


def rwkv_host_params(heads, mu, w0, w_up, a0, a_up, g_up, k_k, k_a, r_k, lnx_g, lnx_b):
    NH = len(heads)
    prm = np.zeros((64, NH, 10), np.float32)
    cols = np.concatenate([np.arange(64 * h, 64 * h + 64) for h in heads])
    for i, h in enumerate(heads):
        sl = slice(64 * h, 64 * h + 64)
        prm[:, i, 0] = mu[0:768][sl]; prm[:, i, 1] = mu[768:1536][sl]; prm[:, i, 2] = mu[1536:2304][sl]
        prm[:, i, 3] = w0[sl]; prm[:, i, 4] = a0[sl]; prm[:, i, 5] = k_k[sl]; prm[:, i, 6] = k_a[sl]
        prm[:, i, 7] = r_k.reshape(-1)[sl]; prm[:, i, 8] = lnx_g[sl]; prm[:, i, 9] = lnx_b[sl]
    lmu = np.zeros((128, 4), np.float32)
    lmu[0:96, 0] = mu[2304:2400]; lmu[0:96, 1] = mu[2400:2496]; lmu[:, 2] = mu[2496:2624]; lmu[:, 3] = mu[2624:2752]
    return dict(prm=prm, lmu=lmu, wup=np.ascontiguousarray(w_up[:, cols]), aup=np.ascontiguousarray(a_up[:, cols]),
                gup=np.ascontiguousarray(g_up[:, cols]))


class WeightStream:
    def __init__(self, S, nc, st, name, max_elems, n_stage=2, n_bf=2, direct=False):
        self.S, self.nc, self.name = S, nc, name
        if direct:
            n_stage = 0
        self.stage = [st.enter_context(nc.sbuf_tensor(uname("%s_st%d" % (name, i)), [128, max_elems], F32)) for i in range(n_stage)]
        self.bf = [st.enter_context(nc.sbuf_tensor(uname("%s_bf%d" % (name, i)), [128, max_elems], BF16)) for i in range(n_bf)]
        self.i = 0
        self.q = 0

    def load_bf(self, scr, shapes, dram_key):
        S = self.S
        bi = self.i % len(self.bf); self.i += 1
        bfb = self.bf[bi]
        n_tot = sum(int(np.prod(shp[1:])) for shp in shapes)
        q = ("sp", "act")[self.q % 2]; self.q += 1
        S.dma(q, lambda e, bfb=bfb, n_tot=n_tot: e.dma_start(out=bfb[:, 0:n_tot], in_=scr[:, 0:n_tot]), reads=[dram_key],
              writes=[(self.name, "bf", bi)])
        off = 0
        outs = []
        for shp in shapes:
            n = int(np.prod(shp[1:]))
            pat = "p (a b) -> p a b" if len(shp) == 3 else "p (a b c) -> p a b c"
            kw = dict(a=shp[1], b=shp[2]) if len(shp) == 3 else dict(a=shp[1], b=shp[2], c=shp[3])
            outs.append(bfb[:, off:off + n].rearrange(pat, **kw))
            off += n
        return outs, (self.name, "bf", bi)

    def load(self, src_views, shapes, dram_key):
        S = self.S
        si = self.i % len(self.stage); bi = self.i % len(self.bf); self.i += 1
        stg, bfb = self.stage[si], self.bf[bi]
        off = 0
        outs = []
        for v, shp in zip(src_views, shapes):
            n = int(np.prod(shp[1:]))
            pat = "p (a b) -> p a b" if len(shp) == 3 else "p (a b c) -> p a b c"
            kw = dict(a=shp[1], b=shp[2]) if len(shp) == 3 else dict(a=shp[1], b=shp[2], c=shp[3])
            dst = stg[:, off:off + n].rearrange(pat, **kw)
            q = ("sp", "act")[self.q % 2]; self.q += 1
            S.dma(q, lambda e, dst=dst, v=v: e.dma_start(out=dst, in_=v), reads=[dram_key],
                  writes=[(self.name, "st", si)])
            outs.append(bfb[:, off:off + n].rearrange(pat, **kw))
            off += n
        S.op("pool", lambda e, stg=stg, bfb=bfb, off=off: e.tensor_copy(out=bfb[:, 0:off], in_=stg[:, 0:off]),
             reads=[(self.name, "st", si)], writes=[(self.name, "bf", bi)])
        return outs, (self.name, "bf", bi)


def phase_precast(nc, panels, scr, max_elems):
    import contextlib
    with contextlib.ExitStack() as st:
        _PH[0] += 1
        S = Sched(nc)
        ws = WeightStream(S, nc, st, "pc_w", max_elems)
        for i, (views, shapes) in enumerate(panels):
            outs, kw = ws.load(views, shapes, "Wsrc")
            n_tot = sum(int(np.prod(shp[1:])) for shp in shapes)
            bfb = ws.bf[(ws.i - 1) % len(ws.bf)]
            S.dma("sp", lambda e, i=i, bfb=bfb, n_tot=n_tot: e.dma_start(out=scr[i][:, 0:n_tot], in_=bfb[:, 0:n_tot]),
                  reads=[kw], writes=["scr"])
        S.barrier_all()
        S.emit()


def emit_norm_T(S, nc, x_sb, kx, h_out, kh, g_col, ones_bf, sq, rstd, ps, kps, n):
    for c in range(KC):
        S.op("act", lambda e, c=c: e.activation(out=sq[:, c, :n], in_=x_sb[:, c, :], func=AF.Square), reads=[kx], writes=["nrm_sq"])
    for c in range(KC):
        S.op("pe", lambda e, c=c: e.matmul(ps[:, :n], ones_bf[:], sq[:, c, :n], start=(c == 0), stop=(c == KC - 1)),
             reads=["nrm_sq", "ones"], writes=[kps])
    S.op("dve", lambda e: e.tensor_scalar(out=rstd[:, :n], in0=ps[:, :n], scalar1=1.0 / D_MODEL, scalar2=NORM_EPS,
                                          op0=ALU.mult, op1=ALU.add), reads=[kps], writes=["nrm_rstd"])
    S.op("act", lambda e: e.activation(out=rstd[:, :n], in_=rstd[:, :n], func=AF.Sqrt), reads=["nrm_rstd"], writes=["nrm_rstd"])
    S.op("dve", lambda e: e.reciprocal(out=rstd[:, :n], in_=rstd[:, :n]), reads=["nrm_rstd"], writes=["nrm_rstd"])
    for c in range(KC):
        S.op("dve", lambda e, c=c: e.scalar_tensor_tensor(out=h_out[:, c, :], in0=x_sb[:, c, :], scalar=g_col[:, c:c + 1],
                                                         in1=rstd[:, :n], op0=ALU.mult, op1=ALU.mult),
             reads=[kx, "nrm_rstd", "gcol"], writes=[kh])


def phase_proj(nc, TL, xT, g_d, W, pT, ncols, scr=None):
    import contextlib
    TS = min(2048, TL); TT = 512; CB = 256
    Wv0 = W.rearrange("(c p) n -> p c n", p=128)
    if scr is not None:
        panels = []
        for cb0 in range(0, ncols, CB):
            cw = min(CB, ncols - cb0)
            panels.append(([Wv0[:, :, cb0:cb0 + cw]], [(128, KC, cw)]))
        phase_precast(nc, panels, scr, KC * CB)
    with contextlib.ExitStack() as st:
        sb = lambda name, shape, dt=F32: st.enter_context(nc.sbuf_tensor(uname(name), shape, dt))
        pss = [st.enter_context(nc.psum_tensor(uname("pps%d" % i), [128, 512], F32)) for i in range(8)]
        _PH[0] += 1
        S = Sched(nc)
        ones_bf = sb("pj_ones", [128, 128], BF16); g_sb = sb("pj_g", [128, KC])
        hT = sb("pj_h", [128, KC, TS], BF16)
        xin = [sb("pj_x%d" % i, [128, KC, TT]) for i in range(1)]
        sq = sb("pj_sq", [128, KC, TT], BF16); rstd = sb("pj_rstd", [128, TT])
        ot = [sb("pj_o%d" % i, [128, TT]) for i in range(4)]
        ws = WeightStream(S, nc, st, "pj_w", KC * CB, direct=scr is not None)
        S.op("pool", lambda e: e.memset(ones_bf[:], 1.0), writes=["ones"])
        S.dma("sp", lambda e: e.dma_start(out=g_sb[:], in_=g_d), writes=["gcol"])
        xTv = xT.rearrange("(c p) t -> p c t", p=128)
        Wv = W.rearrange("(c p) n -> p c n", p=128)
        cnt = dict(o=0, ps=0)
        for s0 in range(0, TL, TS):
            for tt in range(TS // TT):
                c0 = s0 + tt * TT
                S.dma("sp", lambda e, c0=c0: e.dma_start(out=xin[0][:], in_=xTv[:, :, c0:c0 + TT]), reads=["xT"], writes=["pj_x"])
                emit_norm_T(S, nc, xin[0], "pj_x", hT[:, :, tt * TT:(tt + 1) * TT], ("pj_h", tt), g_sb, ones_bf, sq, rstd,
                            pss[0], ("ps", 0), TT)
            for cb0 in range(0, ncols, CB):
                cw = min(CB, ncols - cb0)
                if scr is not None:
                    (wv,), kw = ws.load_bf(scr[cb0 // CB], [(128, KC, cw)], "Wscr")
                else:
                    (wv,), kw = ws.load([Wv[:, :, cb0:cb0 + cw]], [(128, KC, cw)], "W")
                for m0 in range(0, cw, 128):
                    mw = min(128, cw - m0)
                    for tt in range(TS // TT):
                        pi = 1 + cnt["ps"] % 7; cnt["ps"] += 1
                        ps = pss[pi]
                        for c in range(KC):
                            S.op("pe", lambda e, ps=ps, wv=wv, c=c, m0=m0, mw=mw, tt=tt:
                                 e.matmul(ps[:mw, :TT], wv[:, c, m0:m0 + mw], hT[:, c, tt * TT:(tt + 1) * TT],
                                          start=(c == 0), stop=(c == KC - 1)),
                                 reads=[kw, ("pj_h", tt)], writes=[("ps", pi)])
                        oi = cnt["o"] % 4; cnt["o"] += 1
                        ob = ot[oi]
                        if oi % 2:
                            S.op("act", lambda e, ob=ob, ps=ps, mw=mw: e.copy(out=ob[:mw, :], in_=ps[:mw, :TT]),
                                 reads=[("ps", pi)], writes=[("pj_o", oi)])
                        else:
                            S.op("dve", lambda e, ob=ob, ps=ps, mw=mw: e.tensor_copy(out=ob[:mw, :], in_=ps[:mw, :TT]),
                                 reads=[("ps", pi)], writes=[("pj_o", oi)])
                        r0 = cb0 + m0; t0 = s0 + tt * TT
                        S.dma("sp", lambda e, ob=ob, mw=mw, r0=r0, t0=t0: e.dma_start(out=pT[r0:r0 + mw, t0:t0 + TT], in_=ob[:mw, :]),
                              reads=[("pj_o", oi)], writes=["pT"])
        S.barrier_all()
        S.emit()


def phase_merge(nc, TL, xT, g_d, Wg, gate_col0, yT, projw, wout, x1T, scr=None, scr_o=None):
    import contextlib
    TT = 512
    YK = (4, 6, 2)
    YO = (0, 4, 10)
    NHALF = 2 if (scr is not None and TL % (2 * TT) == 0) else 1
    TB = TT * NHALF
    Wgv0 = Wg.rearrange("(c p) n -> p c n", p=128)
    Pv0 = projw.rearrange("(c p) n -> p c n", p=128)
    Wov0 = wout.rearrange("(c p) n -> p c n", p=128)

    def mg_views(m):
        views = [Wgv0[:, :, gate_col0 + i * D_MODEL + m * 128:gate_col0 + i * D_MODEL + m * 128 + 128] for i in range(3)]
        views.append(Pv0[:, :, m * 128:(m + 1) * 128])
        return views, [(128, KC, 128)] * 3 + [(128, 12, 128)]

    if scr is not None:
        phase_precast(nc, [mg_views(m) for m in range(KC)], scr, KC * 3 * 128 + 12 * 128)
        phase_precast(nc, [([Wov0[:, :, m * 128:(m + 1) * 128]], [(128, KC, 128)]) for m in range(KC)], scr_o, KC * 128)
    with contextlib.ExitStack() as st:
        sb = lambda name, shape, dt=F32: st.enter_context(nc.sbuf_tensor(uname(name), shape, dt))
        pss = [st.enter_context(nc.psum_tensor(uname("mps%d" % i), [128, 512], F32)) for i in range(8)]
        _PH[0] += 1
        S = Sched(nc)
        ones_bf = sb("mg_ones", [128, 128], BF16); g_sb = sb("mg_g", [128, KC])
        xin = sb("mg_x", [128, KC, TT]); hT = sb("mg_h", [128, KC, TB], BF16)
        xr = [sb("mg_xr%d" % i, [128, TT]) for i in range(2)]
        rstd = sb("mg_rstd", [128, TT])
        yst = sb("mg_yst", [128, 12, TT]); ybf = sb("mg_ybf", [128, 12, TB], BF16)
        mrgb = sb("mg_mrgb", [128, KC, TB], BF16)
        sq = mrgb[:, :, 0:TT]
        S.alias["nrm_sq"] = "mg_mrgb"
        sig = [sb("mg_sig%d" % i, [128, TT]) for i in range(2)]
        tmp = sb("mg_tmp", [128, TT]); mrg = sb("mg_mrg", [128, TT])
        xo = [sb("mg_xo%d" % i, [128, TT]) for i in range(2)]
        ws = WeightStream(S, nc, st, "mg_w", KC * 3 * 128 + 12 * 128, n_stage=1, direct=scr is not None)
        S.op("pool", lambda e: e.memset(ones_bf[:], 1.0), writes=["ones"])
        S.dma("sp", lambda e: e.dma_start(out=g_sb[:], in_=g_d), writes=["gcol"])
        xTv = xT.rearrange("(c p) t -> p c t", p=128)
        yTv = yT.rearrange("(c p) t -> p c t", p=128)
        Wgv = Wg.rearrange("(c p) n -> p c n", p=128)
        Pv = projw.rearrange("(c p) n -> p c n", p=128)
        Wov = wout.rearrange("(c p) n -> p c n", p=128)
        cnt = dict(ps=0, s=0, o=0)

        def nps():
            i = 1 + cnt["ps"] % 7; cnt["ps"] += 1
            return pss[i], ("ps", i)

        for t0 in range(0, TL, TB):
            for h in range(NHALF):
                hs = slice(h * TT, (h + 1) * TT)
                tk = t0 + h * TT
                S.dma("sp", lambda e, tk=tk: e.dma_start(out=xin[:], in_=xTv[:, :, tk:tk + TT]), reads=["xT"], writes=["mg_x"])
                S.dma("act", lambda e, tk=tk: e.dma_start(out=yst[:], in_=yTv[:, :, tk:tk + TT]), reads=["yT"], writes=["mg_yst"])
                S.op("pool", lambda e, hs=hs: e.tensor_copy(out=ybf[:, :, hs], in_=yst[:]), reads=["mg_yst"], writes=[("mg_ybf", h)])
                emit_norm_T(S, nc, xin, "mg_x", hT[:, :, hs], ("mg_h", h), g_sb, ones_bf, sq, rstd, pss[0], ("ps", 0), TT)
            for m in range(KC):
                views = [Wgv[:, :, gate_col0 + i * D_MODEL + m * 128:gate_col0 + i * D_MODEL + m * 128 + 128] for i in range(3)]
                views.append(Pv[:, :, m * 128:(m + 1) * 128])
                shapes = [(128, KC, 128)] * 3 + [(128, 12, 128)]
                if scr is not None:
                    (w0v, w1v, w2v, pv), kw = ws.load_bf(scr[m], shapes, "Wmgs")
                else:
                    (w0v, w1v, w2v, pv), kw = ws.load(views, shapes, "Wmg")
                wgs = (w0v, w1v, w2v)
                for h in range(NHALF):
                    hs = slice(h * TT, (h + 1) * TT)
                    for i in range(3):
                        pg, kpg = nps()
                        for c in range(KC):
                            S.op("pe", lambda e, pg=pg, i=i, c=c, wgs=wgs, hs=hs: e.matmul(pg[:, :TT], wgs[i][:, c, :], hT[:, c, hs],
                                                                                       start=(c == 0), stop=(c == KC - 1)),
                                 reads=[kw, ("mg_h", h)], writes=[kpg])
                        pz, kpz = nps()
                        for u in range(YK[i]):
                            S.op("pe", lambda e, pz=pz, i=i, u=u, pv=pv, hs=hs: e.matmul(pz[:, :TT], pv[:, YO[i] + u, :], ybf[:, YO[i] + u, hs],
                                                                                      start=(u == 0), stop=(u == YK[i] - 1)),
                                 reads=[kw, ("mg_ybf", h)], writes=[kpz])
                        si = cnt["s"] % 2; cnt["s"] += 1
                        sg = sig[si]
                        S.op("act", lambda e, sg=sg, pg=pg: e.activation(out=sg[:], in_=pg[:, :TT], func=AF.Sigmoid), reads=[kpg],
                             writes=[("mg_sig", si)])
                        if i == 0:
                            S.op("dve", lambda e, sg=sg, pz=pz: e.tensor_tensor(out=mrg[:], in0=pz[:, :TT], in1=sg[:], op=ALU.mult),
                                 reads=[kpz, ("mg_sig", si)], writes=["mg_mrg"])
                        else:
                            S.op("dve", lambda e, sg=sg, pz=pz: e.tensor_tensor(out=tmp[:], in0=pz[:, :TT], in1=sg[:], op=ALU.mult),
                                 reads=[kpz, ("mg_sig", si)], writes=["mg_tmp"])
                            if i == 1:
                                S.op("pool", lambda e: e.tensor_tensor(out=mrg[:], in0=mrg[:], in1=tmp[:], op=ALU.add),
                                     reads=["mg_mrg", "mg_tmp"], writes=["mg_mrg"])
                            else:
                                S.op("pool", lambda e, m=m, hs=hs: e.tensor_tensor(out=mrgb[:, m, hs], in0=mrg[:], in1=tmp[:], op=ALU.add),
                                     reads=["mg_mrg", "mg_tmp"], writes=["mg_mrgb"])
            for m in range(KC):
                if scr is not None:
                    (wov,), kw = ws.load_bf(scr_o[m], [(128, KC, 128)], "Wouts")
                else:
                    (wov,), kw = ws.load([Wov[:, :, m * 128:(m + 1) * 128]], [(128, KC, 128)], "Wout")
                for h in range(NHALF):
                    hs = slice(h * TT, (h + 1) * TT)
                    tk = t0 + h * TT
                    po, kpo = nps()
                    for c in range(KC):
                        S.op("pe", lambda e, po=po, c=c, wov=wov, hs=hs: e.matmul(po[:, :TT], wov[:, c, :], mrgb[:, c, hs], start=(c == 0),
                                                                               stop=(c == KC - 1)), reads=[kw, "mg_mrgb"], writes=[kpo])
                    oi = cnt["o"] % 2; cnt["o"] += 1
                    ob = xo[oi]; xrb = xr[oi]
                    S.dma("act", lambda e, xrb=xrb, m=m, tk=tk: e.dma_start(out=xrb[:], in_=xT[m * 128:(m + 1) * 128, tk:tk + TT]),
                          reads=["xT"], writes=[("mg_xr", oi)])
                    S.op("dve", lambda e, ob=ob, po=po, xrb=xrb: e.tensor_tensor(out=ob[:], in0=po[:, :TT], in1=xrb[:], op=ALU.add),
                         reads=[kpo, ("mg_xr", oi)], writes=[("mg_xo", oi)])
                    S.dma("sp", lambda e, ob=ob, m=m, tk=tk: e.dma_start(out=x1T[m * 128:(m + 1) * 128, tk:tk + TT], in_=ob[:]),
                          reads=[("mg_xo", oi)], writes=["x1T"])
        S.barrier_all()
        S.emit()


def phase_ffn(nc, TL, x1T, g_d, wup, conv_d, wdown, xoT, d_ff, halo_d=None, scr_u=None, scr_d=None):
    import contextlib
    TT = 512
    NF = d_ff // 128
    NHALF = 2 if (scr_u is not None and TL % (2 * TT) == 0) else 1
    TB = TT * NHALF
    Wuv0 = wup.rearrange("(c p) n -> p c n", p=128)
    Wdv0 = wdown.rearrange("(f p) n -> p f n", p=128)
    if scr_u is not None:
        phase_precast(nc, [([Wuv0[:, :, f * 128:(f + 1) * 128], Wuv0[:, :, d_ff + f * 128:d_ff + (f + 1) * 128]], [(128, KC, 128)] * 2)
                           for f in range(NF)], scr_u, KC * 256)
        phase_precast(nc, [([Wdv0[:, :, m * 128:(m + 1) * 128]], [(128, NF, 128)]) for m in range(KC)], scr_d, NF * 128)
    with contextlib.ExitStack() as st:
        sb = lambda name, shape, dt=F32: st.enter_context(nc.sbuf_tensor(uname(name), shape, dt))
        pss = [st.enter_context(nc.psum_tensor(uname("fps%d" % i), [128, 512], F32)) for i in range(8)]
        _PH[0] += 1
        S = Sched(nc)
        ones_bf = sb("ff_ones", [128, 128], BF16); g_sb = sb("ff_g", [128, KC]); cw = sb("ff_cw", [128, NF, 3])
        xin = sb("ff_x", [128, KC, TT]); hT = sb("ff_h", [128, KC, TB], BF16)
        rstd = sb("ff_rstd", [128, TT])
        actb = sb("ff_act", [128, max(NF, KC), TB], BF16)
        sq = actb[:, 0:KC, 0:TT]
        S.alias["nrm_sq"] = "ff_act"
        xr = [sb("ff_xr%d" % i, [128, TT]) for i in range(2)]
        carry = sb("ff_carry", [128, NF, 2])
        gbuf = [sb("ff_gb%d" % i, [128, TT + 2]) for i in range(2)]
        cv = [sb("ff_cv%d" % i, [128, TT]) for i in range(2)]
        xo = [sb("ff_xo%d" % i, [128, TT]) for i in range(2)]
        ws = WeightStream(S, nc, st, "ff_w", max(KC * 256, NF * 128), n_stage=2, n_bf=3 if scr_u is not None else 2,
                          direct=scr_u is not None)
        S.op("pool", lambda e: e.memset(ones_bf[:], 1.0), writes=["ones"])
        S.dma("sp", lambda e: e.dma_start(out=g_sb[:], in_=g_d), writes=["gcol"])
        S.dma("sp", lambda e: e.dma_start(out=cw[:], in_=conv_d), writes=["ff_cw"])
        if halo_d is None:
            S.op("pool", lambda e: e.memset(carry[:], 0.0), writes=["ff_carry"])
        else:
            S.dma("sp", lambda e: e.dma_start(out=carry[:], in_=halo_d), writes=["ff_carry"])
        xv = x1T.rearrange("(c p) t -> p c t", p=128)
        Wuv = wup.rearrange("(c p) n -> p c n", p=128)
        Wdv = wdown.rearrange("(f p) n -> p f n", p=128)
        cnt = dict(ps=0, g=0, o=0)

        def nps():
            i = 1 + cnt["ps"] % 7; cnt["ps"] += 1
            return pss[i], ("ps", i)

        for t0 in range(0, TL, TB):
            for h in range(NHALF):
                S.dma("sp", lambda e, t0=t0, h=h: e.dma_start(out=xin[:], in_=xv[:, :, t0 + h * TT:t0 + (h + 1) * TT]),
                      reads=["x1T"], writes=["ff_x"])
                emit_norm_T(S, nc, xin, "ff_x", hT[:, :, h * TT:(h + 1) * TT], ("ff_h", h), g_sb, ones_bf, sq, rstd, pss[0],
                            ("ps", 0), TT)
            for f in range(NF):
                views = [Wuv[:, :, f * 128:(f + 1) * 128], Wuv[:, :, d_ff + f * 128:d_ff + (f + 1) * 128]]
                if scr_u is not None:
                    (wg, wv), kw = ws.load_bf(scr_u[f], [(128, KC, 128)] * 2, "Wups")
                else:
                    (wg, wv), kw = ws.load(views, [(128, KC, 128)] * 2, "Wup")
                for h in range(NHALF):
                    hs = slice(h * TT, (h + 1) * TT)
                    pg, kpg = nps()
                    for c in range(KC):
                        S.op("pe", lambda e, pg=pg, c=c, wg=wg, hs=hs: e.matmul(pg[:, :TT], wg[:, c, :], hT[:, c, hs], start=(c == 0),
                                                                             stop=(c == KC - 1)), reads=[kw, ("ff_h", h)], writes=[kpg])
                    pv, kpv = nps()
                    for c in range(KC):
                        S.op("pe", lambda e, pv=pv, c=c, wv=wv, hs=hs: e.matmul(pv[:, :TT], wv[:, c, :], hT[:, c, hs], start=(c == 0),
                                                                             stop=(c == KC - 1)), reads=[kw, ("ff_h", h)], writes=[kpv])
                    gi = cnt["g"] % 2; cnt["g"] += 1
                    gb = gbuf[gi]; cb = cv[gi]
                    kgb, kcb = ("ff_gb", gi), ("ff_cv", gi)
                    S.op("pool", lambda e, gb=gb, f=f: e.tensor_copy(out=gb[:, 0:2], in_=carry[:, f, :]), reads=["ff_carry"], writes=[kgb])
                    S.op("act", lambda e, gb=gb, pg=pg: e.copy(out=gb[:, 2:TT + 2], in_=pg[:, :TT]), reads=[kpg], writes=[kgb])
                    S.op("pool", lambda e, gb=gb, f=f: e.tensor_copy(out=carry[:, f, :], in_=gb[:, TT:TT + 2]), reads=[kgb],
                         writes=["ff_carry"])
                    S.op("dve", lambda e, gb=gb, cb=cb, f=f: e.tensor_scalar(out=cb[:], in0=gb[:, 0:TT], scalar1=cw[:, f, 0:1], scalar2=None,
                                                                           op0=ALU.mult), reads=[kgb, "ff_cw"], writes=[kcb])
                    S.op("dve", lambda e, gb=gb, cb=cb, f=f: e.scalar_tensor_tensor(out=cb[:], in0=gb[:, 1:TT + 1], scalar=cw[:, f, 1:2],
                                                                                  in1=cb[:], op0=ALU.mult, op1=ALU.add),
                         reads=[kgb, "ff_cw", kcb], writes=[kcb])
                    S.op("dve", lambda e, gb=gb, cb=cb, f=f: e.scalar_tensor_tensor(out=cb[:], in0=gb[:, 2:TT + 2], scalar=cw[:, f, 2:3],
                                                                                  in1=cb[:], op0=ALU.mult, op1=ALU.add),
                         reads=[kgb, "ff_cw", kcb], writes=[kcb])
                    S.op("act", lambda e, cb=cb: e.activation(out=cb[:], in_=cb[:], func=AF.Silu), reads=[kcb], writes=[kcb])
                    S.op("dve", lambda e, cb=cb, pv=pv, f=f, hs=hs: e.tensor_tensor(out=actb[:, f, hs], in0=pv[:, :TT], in1=cb[:], op=ALU.mult),
                         reads=[kpv, kcb], writes=["ff_act"])
            for m in range(KC):
                if scr_d is not None:
                    (wd,), kw = ws.load_bf(scr_d[m], [(128, NF, 128)], "Wdowns")
                else:
                    (wd,), kw = ws.load([Wdv[:, :, m * 128:(m + 1) * 128]], [(128, NF, 128)], "Wdown")
                for h in range(NHALF):
                    hs = slice(h * TT, (h + 1) * TT)
                    tk = t0 + h * TT
                    po, kpo = nps()
                    for f in range(NF):
                        S.op("pe", lambda e, po=po, f=f, wd=wd, hs=hs: e.matmul(po[:, :TT], wd[:, f, :], actb[:, f, hs], start=(f == 0),
                                                                             stop=(f == NF - 1)), reads=[kw, "ff_act"], writes=[kpo])
                    oi = cnt["o"] % 2; cnt["o"] += 1
                    ob = xo[oi]; xrb = xr[oi]
                    S.dma("act", lambda e, xrb=xrb, m=m, tk=tk: e.dma_start(out=xrb[:], in_=x1T[m * 128:(m + 1) * 128, tk:tk + TT]),
                          reads=["x1T"], writes=[("ff_xr", oi)])
                    S.op("dve", lambda e, ob=ob, po=po, xrb=xrb: e.tensor_tensor(out=ob[:], in0=po[:, :TT], in1=xrb[:], op=ALU.add),
                         reads=[kpo, ("ff_xr", oi)], writes=[("ff_xo", oi)])
                    S.dma("sp", lambda e, ob=ob, m=m, tk=tk: e.dma_start(out=xoT[m * 128:(m + 1) * 128, tk:tk + TT], in_=ob[:]),
                          reads=[("ff_xo", oi)], writes=["xoT"])
        S.barrier_all()
        S.emit()


def phase_final_norm(nc, TL, xT, g_d, outT):
    import contextlib
    TT = 512
    with contextlib.ExitStack() as st:
        sb = lambda name, shape, dt=F32: st.enter_context(nc.sbuf_tensor(uname(name), shape, dt))
        pss = [st.enter_context(nc.psum_tensor(uname("nps%d" % i), [128, 512], F32)) for i in range(2)]
        _PH[0] += 1
        S = Sched(nc)
        ones_bf = sb("fn_ones", [128, 128], BF16); g_sb = sb("fn_g", [128, KC])
        xin = [sb("fn_x%d" % i, [128, KC, TT]) for i in range(2)]
        ho = [sb("fn_h%d" % i, [128, KC, TT]) for i in range(2)]
        sq = sb("fn_sq", [128, KC, TT], BF16); rstd = sb("fn_rstd", [128, TT])
        S.op("pool", lambda e: e.memset(ones_bf[:], 1.0), writes=["ones"])
        S.dma("sp", lambda e: e.dma_start(out=g_sb[:], in_=g_d), writes=["gcol"])
        xv = xT.rearrange("(c p) t -> p c t", p=128)
        ov = outT.rearrange("(c p) t -> p c t", p=128)
        for i, t0 in enumerate(range(0, TL, TT)):
            b = i % 2
            S.dma("sp", lambda e, t0=t0, b=b: e.dma_start(out=xin[b][:], in_=xv[:, :, t0:t0 + TT]), reads=["xT"], writes=[("fn_x", b)])
            emit_norm_T(S, nc, xin[b], ("fn_x", b), ho[b], ("fn_h", b), g_sb, ones_bf, sq, rstd, pss[0], ("ps", 0), TT)
            S.dma("act", lambda e, t0=t0, b=b: e.dma_start(out=ov[:, :, t0:t0 + TT], in_=ho[b][:]), reads=[("fn_h", b)], writes=["outT"])
        S.barrier_all()
        S.emit()


def build_dense_test(TL, d_ff, ncols_p):
    nc = bass.Bass("TRN2", target_bir_lowering=False)
    di = lambda n, s: nc.dram_tensor(n, s, F32, kind="ExternalInput").ap()
    xT = di("xT", [D_MODEL, TL]); g1 = di("g1", [128, KC]); g2 = di("g2", [128, KC]); gf = di("gf", [128, KC])
    w_in = di("w_in", [D_MODEL, ncols_p + 6144]); yT = di("yT", [1536, TL]); projw = di("projw", [1536, D_MODEL])
    wout = di("wout", [D_MODEL, D_MODEL]); wup = di("wup", [D_MODEL, 2 * d_ff]); conv = di("conv", [128, d_ff // 128, 3])
    wdown = di("wdown", [d_ff, D_MODEL])
    pT = nc.dram_tensor("pT", [ncols_p, TL], F32, kind="ExternalOutput").ap()
    x1T = nc.dram_tensor("x1T", [D_MODEL, TL], F32, kind="ExternalOutput").ap()
    x2T = nc.dram_tensor("x2T", [D_MODEL, TL], F32, kind="ExternalOutput").ap()
    outT = nc.dram_tensor("outT", [D_MODEL, TL], F32, kind="ExternalOutput").ap()
    phase_proj(nc, TL, xT, g1, w_in, pT, ncols_p)
    phase_merge(nc, TL, xT, g1, w_in, ncols_p, yT, projw, wout, x1T)
    phase_ffn(nc, TL, x1T, g2, wup, conv, wdown, x2T, d_ff)
    phase_final_norm(nc, TL, x2T, gf, outT)
    return nc


def build_full(TL, n_layers, d_ff, heads_a, heads_c, heads_b, final=True):
    nc = bass.Bass("TRN2", target_bir_lowering=False)
    di = lambda n, s: nc.dram_tensor(n, list(s), F32, kind="ExternalInput").ap()
    L = n_layers
    NHB = len(heads_b)
    xT = di("xT", [D_MODEL, TL])
    g1 = di("g1", [L, 128, KC]); g2 = di("g2", [L, 128, KC]); gf = di("gf", [128, KC])
    w_in = di("w_in", [L, D_MODEL, NP_ROWS + 3 * D_MODEL])
    projw = di("projw", [L, 1536, D_MODEL]); wout = di("w_out", [L, D_MODEL, D_MODEL])
    wup = di("ffn_up", [L, D_MODEL, 2 * d_ff]); conv = di("conv", [L, 128, d_ff // 128, 3]); wdown = di("ffn_down", [L, d_ff, D_MODEL])
    tabs_g = di("tabs_g", [20, 128, 256]); tabs_m = di("tabs_m", [20, 128, 256]); tabs_a = di("tabs_a", [20, 128, 256])
    sinks = di("sinks", [L, 64, 8]); ident = di("ident", [128, 128])
    prm = di("prm", [L, 64, NHB, 10]); lmu = di("lmu", [L, 128, 4])
    rwup = di("rw_up", [L, 96, NHB * 64]); raup = di("ra_up", [L, 96, NHB * 64]); rgup = di("rg_up", [L, 256, NHB * 64])
    mlt = di("m_lt2", [64, 3, 128]); mle = di("m_le2", [64, 3, 128]); mgt = di("m_gt", [64, 3, 64]); rst = di("rst", [64, 512])
    outT = nc.dram_tensor("outT", [D_MODEL, TL], F32, kind="ExternalOutput").ap()
    pT = nc.dram_tensor("pT_s", [NP_ROWS, TL], F32).ap()
    yT = nc.dram_tensor("yT_s", [1536, TL], F32).ap()
    x1T = nc.dram_tensor("x1T_s", [D_MODEL, TL], F32).ap()
    xs = [nc.dram_tensor("xs%d" % i, [D_MODEL, TL], F32).ap() for i in range(2)]
    NF = d_ff // 128
    sc_pj = nc.dram_tensor("sc_pj", [(NP_ROWS + 255) // 256, 128, KC * 256], BF16).ap()
    sc_mg = nc.dram_tensor("sc_mg", [KC, 128, KC * 3 * 128 + 12 * 128], BF16).ap()
    sc_wo = nc.dram_tensor("sc_wo", [KC, 128, KC * 128], BF16).ap()
    sc_up = nc.dram_tensor("sc_up", [NF, 128, KC * 256], BF16).ap()
    sc_dn = nc.dram_tensor("sc_dn", [KC, 128, NF * 128], BF16).ap()
    cur = xT
    for l in range(L):
        phase_proj(nc, TL, cur, g1[l], w_in[l], pT, NP_ROWS, scr=sc_pj)
        phase_attn(nc, TL, pT, yT, tabs_g, tabs_m, tabs_a, sinks[l], ident, heads_a, heads_c)
        phase_rwkv(nc, TL, pT, yT, prm[l], lmu[l], rwup[l], raup[l], rgup[l], ident, mlt, mle, mgt, rst, heads_b)
        phase_merge(nc, TL, cur, g1[l], w_in[l], NP_ROWS, yT, projw[l], wout[l], x1T, scr=sc_mg, scr_o=sc_wo)
        nxt = outT if (l == L - 1 and not final) else xs[l % 2]
        phase_ffn(nc, TL, x1T, g2[l], wup[l], conv[l], wdown[l], nxt, d_ff, scr_u=sc_up, scr_d=sc_dn)
        cur = nxt
    if final:
        phase_final_norm(nc, TL, cur, gf, outT)
    return nc


def phase_attn(nc, T, pT, yT, tg, tm, ta, sk, idd, heads_a, heads_c):
    import contextlib
    with contextlib.ExitStack() as st:
        pss = [st.enter_context(nc.psum_tensor(uname("aps%d" % i), [128, 512], F32)) for i in range(8)]
        _PH[0] += 1
        S = Sched(nc)
        emit_attention(S, nc, st, T, pT, yT, tg, tm, ta, sk, idd, pss, heads_a, heads_c)
        S.barrier_all()
        S.emit()


def host_inputs(inp, n_layers, d_ff, heads_b):
    L = n_layers
    f32 = lambda a: np.ascontiguousarray(np.asarray(a, dtype=np.float32))
    gl = lambda g: np.ascontiguousarray(np.asarray(g, np.float32).reshape(-1, KC, 128).transpose(0, 2, 1))
    tg, tm, ta = attn_tables(np.asarray(inp["rel_bias"], np.float32))
    d = dict(g1=gl(inp["norm1_g"][:L]), g2=gl(inp["norm2_g"][:L]), gf=gl(inp["final_g"])[0],
             w_in=f32(inp["w_in"][:L]),
             projw=f32(np.concatenate([np.asarray(inp["proj_a"][:L]), np.asarray(inp["proj_b"][:L]), np.asarray(inp["proj_c"][:L])], axis=1)),
             w_out=f32(inp["w_out"][:L]), ffn_up=f32(inp["ffn_up"][:L]), ffn_down=f32(inp["ffn_down"][:L]),
             conv=f32(np.asarray(inp["ffn_conv"][:L]).transpose(0, 2, 1).reshape(L, d_ff // 128, 128, 3).transpose(0, 2, 1, 3)),
             tabs_g=tg, tabs_m=tm, tabs_a=ta,
             sinks=f32(np.broadcast_to(np.asarray(inp["attn_sinks"][:L])[:, None, :], (L, 64, 8))),
             ident=np.eye(128, dtype=np.float32))
    prm, lmu, wu, au, gu = [], [], [], [], []
    for l in range(L):
        hp = rwkv_host_params(heads_b, *[np.asarray(inp[k][l], np.float32) for k in
                                         ("rwkv_mu", "rwkv_w0", "rwkv_w_up", "rwkv_a0", "rwkv_a_up", "rwkv_g_up", "rwkv_k_k",
                                          "rwkv_k_a", "rwkv_r_k", "rwkv_lnx_g", "rwkv_lnx_b")])
        prm.append(hp["prm"]); lmu.append(hp["lmu"]); wu.append(hp["wup"]); au.append(hp["aup"]); gu.append(hp["gup"])
    d.update(prm=np.stack(prm), lmu=np.stack(lmu), rw_up=np.stack(wu), ra_up=np.stack(au), rg_up=np.stack(gu))
    d.update(rwkv_consts())
    return d


_NC_CACHE = {}
N_LAUNCH = 1


def kernel(**inp):
    x = np.asarray(inp["x"], np.float32)
    Bn, Sq, Dm = x.shape
    L = np.asarray(inp["w_in"]).shape[0]
    d_ff = np.asarray(inp["ffn_down"]).shape[1]
    heads_a, heads_c, heads_b = list(range(8)), list(range(4)), list(range(12))
    nl = N_LAUNCH if L % N_LAUNCH == 0 else 1
    Lp = L // nl
    xTs = [np.ascontiguousarray(x[b].T) for b in range(Bn)]
    per_layer = ("norm1_g", "w_in", "attn_sinks", "rwkv_mu", "rwkv_w0", "rwkv_w_up", "rwkv_a0", "rwkv_a_up", "rwkv_g_up", "rwkv_k_k",
                 "rwkv_k_a", "rwkv_r_k", "rwkv_lnx_g", "rwkv_lnx_b", "proj_a", "proj_b", "proj_c", "w_out", "norm2_g", "ffn_up",
                 "ffn_conv", "ffn_down")
    for li in range(nl):
        final = (li == nl - 1)
        key = (Sq, Lp, d_ff, final)
        if key not in _NC_CACHE:
            _NC_CACHE[key] = build_full(Sq, Lp, d_ff, heads_a, heads_c, heads_b, final=final)
        nc = _NC_CACHE[key]
        sub = dict(inp)
        for k in per_layer:
            sub[k] = np.asarray(inp[k])[li * Lp:(li + 1) * Lp]
        shared = host_inputs(sub, Lp, d_ff, heads_b)
        in_maps = []
        for b in range(Bn):
            m = dict(shared)
            m["xT"] = xTs[b]
            in_maps.append(m)
        res = run_bass_kernel_spmd(nc, in_maps, core_ids=list(range(Bn)))
        xTs = [np.ascontiguousarray(r["outT"]) for r in res.results]
    out = np.stack([np.ascontiguousarray(t.T) for t in xTs], axis=0)
    return out.astype(np.float32)
```

```python
import concourse.bass as bass
import concourse.mybir as mybir

ENGS = ("pe", "dve", "act", "pool", "sp")


_PH = [0]


def uname(n):
    return "%s_%d" % (n, _PH[0])


class Sched:
    _uid = 0

    def __init__(self, nc, n_dma_sems=24):
        self.nc = nc
        self.streams = {e: [] for e in ENGS}
        self.seq = {e: 0 for e in ENGS}
        self.waited = {}
        self.lastw = {}
        self.readers = {}
        self.n_dma = n_dma_sems
        self.dma_cnt = [0] * n_dma_sems
        self.dma_rr = 0
        self.sems = {}
        self.n_wait = 0
        self.alias = {}
        self.hist = {e: [] for e in ENGS}
        self.dma_snap = {}

    def _clock(self, e):
        return {p: v for (c, p), v in self.waited.items() if c == e}

    def _merge(self, cons, snap):
        for p, v in snap.items():
            if p == ("eng", cons):
                continue
            if self.waited.get((cons, p), 0) < v:
                self.waited[(cons, p)] = v

    def _absorb(self, cons, waits):
        import bisect
        for p, v in waits:
            if p[0] == "eng":
                h = self.hist[p[1]]
                if h:
                    i = bisect.bisect_right(h, v, key=lambda t: t[0]) - 1
                    if i >= 0:
                        self._merge(cons, h[i][1])
            else:
                snap = self.dma_snap.get((p[1], v))
                if snap:
                    self._merge(cons, snap)

    def canon(self, keys):
        return [self.alias.get(k, k) for k in keys]

    def eng(self, e):
        nc = self.nc
        return {"pe": nc.tensor, "dve": nc.vector, "act": nc.scalar, "pool": nc.gpsimd, "sp": nc.sync}[e]

    def _need(self, cons, deps):
        best = {}
        for p, v in deps:
            if p is None:
                continue
            if v > best.get(p, 0):
                best[p] = v
        out = []
        for p, v in sorted(best.items(), key=lambda kv: (kv[0][0] != "eng", -kv[1])):
            if self.waited.get((cons, p), 0) >= v:
                continue
            self.waited[(cons, p)] = v
            out.append((p, v))
            self._absorb(cons, [(p, v)])
        return out

    def _deps(self, e, reads, writes, same_engine_war=False):
        deps = []
        me = ("eng", e)
        for k in reads:
            w = self.lastw.get(k)
            if w is not None:
                deps.append(w)
        for k in writes:
            w = self.lastw.get(k)
            if w is not None:
                deps.append(w)
            for p, v in self.readers.get(k, {}).items():
                deps.append((p, v))
        return deps

    def _record(self, prod, val, reads, writes):
        for k in reads:
            d = self.readers.setdefault(k, {})
            if d.get(prod, 0) < val:
                d[prod] = val
        for k in writes:
            self.lastw[k] = (prod, val)
            self.readers[k] = {}

    def op(self, e, fn, reads=(), writes=()):
        reads = self.canon(reads); writes = self.canon(writes)
        deps = self._deps(e, reads, writes)
        if e == "pe":
            deps = [d for d in deps if d[0] != ("eng", "pe")]
        waits = self._need(e, deps)
        self.seq[e] += 1
        val = self.seq[e]
        if waits or not self.hist[e]:
            self.hist[e].append((val, self._clock(e)))
        self.streams[e].append(("op", fn, waits, None))
        self._record(("eng", e), val, reads, writes)
        return val

    def dma(self, q, fn, reads=(), writes=()):
        reads = self.canon(reads); writes = self.canon(writes)
        deps = self._deps(q, reads, writes, same_engine_war=True)
        i = self.dma_rr
        self.dma_rr = (self.dma_rr + 1) % self.n_dma
        prod = ("dma", i)
        if self.dma_cnt[i] > 0:
            deps.append((prod, self.dma_cnt[i]))
        waits = self._need(q, deps)
        self.dma_cnt[i] += 16
        val = self.dma_cnt[i]
        snap = self._clock(q)
        if q != "sp" and self.seq[q] > 0:
            snap[("eng", q)] = max(snap.get(("eng", q), 0), 0)
        self.dma_snap[(i, val)] = snap
        self.streams[q].append(("dma", fn, waits, (i, 16)))
        self._record(prod, val, reads, writes)
        return prod, val

    def finish_waits(self, e="sp"):
        deps = [(("dma", i), c) for i, c in enumerate(self.dma_cnt) if c > 0]
        deps += [(("eng", x), self.seq[x]) for x in ENGS if self.seq[x] > 0 and x != e]
        waits = self._need(e, deps)
        self.streams[e].append(("wait", None, waits, None))

    def barrier_all(self):
        for e in ENGS:
            deps = [(("dma", i), c) for i, c in enumerate(self.dma_cnt) if c > 0]
            deps += [(("eng", x), self.seq[x]) for x in ENGS if self.seq[x] > 0 and x != e]
            waits = self._need(e, deps)
            self.streams[e].append(("wait", None, waits, None))

    def emit(self):
        nc = self.nc
        Sched._uid += 1
        u = Sched._uid
        esem = {e: nc.alloc_semaphore("s%d_%s" % (u, e)) for e in ENGS}
        dsem = [nc.alloc_semaphore("d%d_%d" % (u, i)) for i in range(self.n_dma)]

        def semof(p):
            return esem[p[1]] if p[0] == "eng" else dsem[p[1]]

        def run(e):
            def body(engine):
                for kind, fn, waits, dinfo in self.streams[e]:
                    for p, v in waits:
                        engine.wait_ge(semof(p), v)
                        self.n_wait += 1
                    if kind == "op":
                        fn(engine).then_inc(esem[e], 1)
                    elif kind == "dma":
                        fn(engine).then_inc(dsem[dinfo[0]], dinfo[1])
            return body

        with nc.Block() as block:
            block.tensor(run("pe"))
            block.vector(run("dve"))
            block.scalar(run("act"))
            block.gpsimd(run("pool"))
            block.sync(run("sp"))
        nc.all_engine_barrier()
        nc.clear_and_free_semaphores(list(esem.values()) + dsem)
        nc.all_engine_barrier()


import numpy as np
from concourse.bass_utils import run_bass_kernel_spmd

F32 = mybir.dt.float32
BF16 = mybir.dt.bfloat16
ALU = mybir.AluOpType
AF = mybir.ActivationFunctionType
AX = mybir.AxisListType

D_MODEL = 2048
NORM_EPS = 1e-5
KC = D_MODEL // 128


def emit_rmsnorm_T(S, nc, xT, hT, g_sb, ones_bf, sq, ps, rstd, ntok, keys):
    kx, kh, ksq, kps, krs = keys["x"], keys["h"], keys["sq"], keys["ps"], keys["rstd"]
    for c in range(KC):
        S.op("act", lambda e, c=c: e.activation(out=sq[:, c, :], in_=xT[:, c, :], func=AF.Square),
             reads=[kx], writes=[(ksq, c)])
    for c in range(KC):
        S.op("pe", lambda e, c=c: e.matmul(ps, ones_bf, sq[:, c, :], start=(c == 0), stop=(c == KC - 1)),
             reads=[(ksq, c)], writes=[kps])
    S.op("dve", lambda e: e.tensor_scalar(out=rstd, in0=ps, scalar1=1.0 / D_MODEL, scalar2=NORM_EPS,
                                          op0=ALU.mult, op1=ALU.add), reads=[kps], writes=[krs])
    S.op("act", lambda e: e.activation(out=rstd, in_=rstd, func=AF.Sqrt), reads=[krs], writes=[krs])
    S.op("dve", lambda e: e.reciprocal(out=rstd, in_=rstd), reads=[krs], writes=[krs])
    for c in range(KC):
        S.op("dve", lambda e, c=c: e.scalar_tensor_tensor(out=hT[:, c, :], in0=xT[:, c, :], scalar=g_sb[:, c:c + 1],
                                                         in1=rstd, op0=ALU.mult, op1=ALU.mult),
             reads=[kx, krs], writes=[kh])


def build_proj(T, ncols, TT=512):
    nc = bass.Bass("TRN2", target_bir_lowering=False)
    xT = nc.dram_tensor("xT", [D_MODEL, T], F32, kind="ExternalInput").ap()
    g = nc.dram_tensor("g", [128, KC], F32, kind="ExternalInput").ap()
    W = nc.dram_tensor("W", [D_MODEL, ncols], F32, kind="ExternalInput").ap()
    pT = nc.dram_tensor("pT", [ncols, T], F32, kind="ExternalOutput").ap()
    ntt = T // TT
    CB = 512
    ncb = (ncols + CB - 1) // CB
    import contextlib
    with contextlib.ExitStack() as st:
        sb = lambda name, shape, dt: st.enter_context(nc.sbuf_tensor(uname(name), shape, dt))
        ones_bf = sb("ones", [128, 128], BF16)
        g_sb = sb("g_sb", [128, KC], F32)
        hT = sb("hT", [128, KC, T], BF16)
        xin = [sb("xin%d" % i, [128, KC, TT], F32) for i in range(2)]
        sq = sb("sq", [128, KC, TT], BF16)
        rstd = sb("rstd", [128, TT], F32)
        wt = [sb("wt%d" % i, [128, KC, CB], BF16) for i in range(2)]
        ot = [sb("ot%d" % i, [128, TT], F32) for i in range(4)]
        pss = [st.enter_context(nc.psum_tensor(uname("ps%d" % i), [128, 512], F32)) for i in range(8)]
        _PH[0] += 1
        S = Sched(nc)
        S.op("pool", lambda e: e.memset(ones_bf[:], 1.0), writes=["ones"])
        S.dma("sp", lambda e: e.dma_start(out=g_sb[:], in_=g), writes=["g"])
        xTv = xT.rearrange("(c p) t -> p c t", p=128)
        Wv = W.rearrange("(c p) n -> p c n", p=128)
        for tt in range(ntt):
            xb = xin[tt % 2]
            S.dma("sp", lambda e, xb=xb, tt=tt: e.dma_start(out=xb[:], in_=xTv[:, :, tt * TT:(tt + 1) * TT]),
                  writes=[("xin", tt % 2)])
            keys = dict(x=("xin", tt % 2), h=("h", tt), sq="sq", ps=("ps", 0), rstd="rstd")
            emit_rmsnorm_T(S, nc, xb[:], hT[:, :, tt * TT:(tt + 1) * TT], g_sb[:], ones_bf[:], sq[:], pss[0][:, :TT],
                           rstd[:], TT, keys)
        n_o = 0
        n_ps = 0
        for cb in range(ncb):
            c0 = cb * CB
            cw = min(CB, ncols - c0)
            wb = wt[cb % 2]
            S.dma("pool", lambda e, wb=wb, c0=c0, cw=cw: e.dma_start(out=wb[:, :, :cw], in_=Wv[:, :, c0:c0 + cw]),
                  writes=[("wt", cb % 2)])
            for m0 in range(0, cw, 128):
                mw = min(128, cw - m0)
                for tt in range(ntt):
                    pi = 1 + (n_ps % 7); n_ps += 1
                    ps = pss[pi]
                    for c in range(KC):
                        S.op("pe", lambda e, ps=ps, wb=wb, c=c, m0=m0, mw=mw, tt=tt:
                             e.matmul(ps[:mw, :TT], wb[:, c, m0:m0 + mw], hT[:, c, tt * TT:(tt + 1) * TT],
                                      start=(c == 0), stop=(c == KC - 1)),
                             reads=[("wt", cb % 2), ("h", tt), "ones", "g"], writes=[("ps", pi)])
                    oi = n_o % 4; n_o += 1
                    ob = ot[oi]
                    eng = "act" if (n_o % 2) else "dve"
                    if eng == "act":
                        S.op("act", lambda e, ob=ob, ps=ps, mw=mw: e.copy(out=ob[:mw, :], in_=ps[:mw, :TT]),
                             reads=[("ps", pi)], writes=[("ot", oi)])
                    else:
                        S.op("dve", lambda e, ob=ob, ps=ps, mw=mw: e.tensor_copy(out=ob[:mw, :], in_=ps[:mw, :TT]),
                             reads=[("ps", pi)], writes=[("ot", oi)])
                    S.dma("sp", lambda e, ob=ob, mw=mw, r0=c0 + m0, tt=tt:
                          e.dma_start(out=pT[r0:r0 + mw, tt * TT:(tt + 1) * TT], in_=ob[:mw, :]),
                          reads=[("ot", oi)])
        S.finish_waits("sp")
        S.emit()
    return nc


import math

ROW_AQ, ROW_AK, ROW_AV = 0, 512, 640
ROW_BR, ROW_BK, ROW_BV, ROW_WD, ROW_AD, ROW_GD = 768, 1536, 2304, 3072, 3168, 3264
ROW_CQ, ROW_CK, ROW_CV = 3520, 4288, 5056
NP_ROWS = 5824
YROW_A, YROW_B, YROW_C = 0, 512, 1280
C_DILS = (1, 4, 16)


def ssl(c0, n, d):
    return slice(c0, c0 + d * (n - 1) + 1, d)


def t5_bucket_np(dist):
    dist = np.asarray(dist, np.int64)
    nf = np.maximum(dist, 1).astype(np.float32)
    large = 16 + (np.log(nf / np.float32(16)) / np.float32(math.log(2048 / 16)) * np.float32(16)).astype(np.int32)
    return np.where(dist < 16, dist, np.minimum(large, 31))


def attn_tables(rel_bias):
    i = np.arange(128)[None, :]
    j = np.arange(128)[:, None]
    dists = (i - j, i + 128 - j)
    specs = [(h, 1, 127) for h in range(8)] + [(8 + g * 4 + hh, dil, 128) for g, dil in enumerate(C_DILS)
                                              for hh in range(4)]
    gathered = np.zeros((20, 128, 256), np.float32)
    mul = np.zeros((20, 128, 256), np.float32)
    add = np.zeros((20, 128, 256), np.float32)
    for n, (col, dil, ms) in enumerate(specs):
        for half, d in enumerate(dists):
            valid = (d >= 0) & (d <= ms)
            idx = t5_bucket_np(np.maximum(d, 0) * dil)
            gathered[n, :, half * 128:(half + 1) * 128] = rel_bias[idx, col]
            mul[n, :, half * 128:(half + 1) * 128] = np.where(valid, 8.0, 0.0)
            add[n, :, half * 128:(half + 1) * 128] = np.where(valid, 0.0, -240000.0)
    return gathered, mul, add


def emit_attention(S, nc, st, T, pT, yT, tabs_g, tabs_m, tabs_a, sinks_rep, ident_d, pss, heads_a, heads_c):
    sb = lambda name, shape, dt: st.enter_context(nc.sbuf_tensor(uname(name), shape, dt))
    NB = T // 128
    q_bf = sb("at_q", [64, T], BF16)
    k_bf = sb("at_k", [64, T], BF16)
    v_f = sb("at_v", [64, T], F32)
    stg = sb("at_stg", [64, T], F32)
    vaug = sb("at_vaug", [128, NB, 65], BF16)
    acc = sb("at_acc", [65, T], F32)
    tb_g = sb("at_tbg", [128, 256], F32)
    tb_m = sb("at_tbm", [128, 256], F32)
    tb_a = sb("at_tba", [128, 256], F32)
    tb = sb("at_tb", [128, 256], BF16)
    pt_sb = [sb("at_pt%d" % i, [128, 512], BF16) for i in range(2)]
    ident_f = sb("at_identf", [128, 128], F32)
    ident_b = sb("at_identb", [128, 128], BF16)
    sel = sb("at_sel", [65, 64], F32)
    esink = sb("at_esink", [64, 8], F32)
    den = sb("at_den", [64, 512], F32)
    yo = [sb("at_yo%d" % i, [64, 512], F32) for i in range(2)]

    S.dma("sp", lambda e: e.dma_start(out=ident_f[:], in_=ident_d), writes=["at_identf"])
    S.op("dve", lambda e: e.tensor_copy(out=ident_b[:], in_=ident_f[:]), reads=["at_identf"], writes=["at_identb"])
    S.op("pool", lambda e: e.memset(sel[:], 0.0), writes=["at_sel"])
    S.op("pool", lambda e: e.memset(sel[64:65, :], 1.0), writes=["at_sel"])
    S.op("pool", lambda e: e.memset(vaug[:, :, 64:65], 1.0), writes=["at_vaug1"])
    S.dma("sp", lambda e: e.dma_start(out=esink[:], in_=sinks_rep), writes=["at_esink"])
    S.op("act", lambda e: e.activation(out=esink[:], in_=esink[:], func=AF.Exp), reads=["at_esink"],
         writes=["at_esink"])

    ps_s = [pss[0], pss[1]]
    ps_o = [pss[2], pss[3]]
    ps_t = pss[4]
    ps_d = pss[5]
    cnt = dict(s=0, o=0, y=0)

    def load_table(n):
        S.dma("sp", lambda e: e.dma_start(out=tb_g[:], in_=tabs_g[n]), writes=["at_tbg"])
        S.dma("sp", lambda e: e.dma_start(out=tb_m[:], in_=tabs_m[n]), writes=["at_tbm"])
        S.dma("sp", lambda e: e.dma_start(out=tb_a[:], in_=tabs_a[n]), writes=["at_tba"])
        S.op("pool", lambda e: e.tensor_tensor(out=tb_g[:], in0=tb_g[:], in1=tb_m[:], op=ALU.mult),
             reads=["at_tbg", "at_tbm"], writes=["at_tbg"])
        S.op("pool", lambda e: e.tensor_tensor(out=tb[:], in0=tb_g[:], in1=tb_a[:], op=ALU.add),
             reads=["at_tbg", "at_tba"], writes=["at_tb"])

    def load_kv(krow, vrow, dil):
        S.dma("act", lambda e: e.dma_start(out=stg[:], in_=pT[krow:krow + 64, :]), reads=["pT"], writes=["at_stg"])
        S.op("pool", lambda e: e.tensor_copy(out=k_bf[:], in_=stg[:]), reads=["at_stg"], writes=["at_k"])
        S.dma("sp", lambda e: e.dma_start(out=v_f[:], in_=pT[vrow:vrow + 64, :]), reads=["pT"], writes=["at_v"])
        Lf = T // dil
        bps = Lf // 128
        for vb0 in range(0, NB, 8):
            for u in range(8):
                vb = vb0 + u
                s_, jb = vb // bps, vb % bps
                c0 = s_ + dil * 128 * jb
                src = v_f[0:64, ssl(c0, 128, dil)]
                S.op("pe", lambda e, u=u, src=src: e.transpose(ps_t[:, u * 64:(u + 1) * 64], src, ident_f[0:64, 0:64]),
                     reads=["at_v", "at_identf"], writes=["ps_t"])
            S.op("dve", lambda e, vb0=vb0: e.tensor_copy(out=vaug[:, vb0:vb0 + 8, 0:64],
                                                        in_=ps_t[:, :].rearrange("p (u d) -> p u d", d=64)),
                 reads=["ps_t"], writes=["at_vaug"])

    def run_seq(dil, first_group):
        Lf = T // dil
        bps = Lf // 128
        nbt = min(4, bps)
        for s_ in range(dil):
            for jb0 in range(0, bps, nbt):
                oi = cnt["o"] % 2; cnt["o"] += 1
                po = ps_o[oi]
                for half in range(0, nbt, 2):
                    si = cnt["s"] % 2; cnt["s"] += 1
                    pst = ps_s[si]
                    ptb = pt_sb[si]
                    nb2 = min(2, nbt - half)
                    lo = 512
                    for r in range(nb2):
                        jb = jb0 + half + r
                        qs = s_ + dil * 128 * jb
                        qv = q_bf[:, ssl(qs, 128, dil)]
                        kv = k_bf[:, ssl(qs, 128, dil)]
                        sl_prev = slice((2 * r) * 128, (2 * r + 1) * 128)
                        sl_cur = slice((2 * r + 1) * 128, (2 * r + 2) * 128)
                        if jb > 0:
                            ks = s_ + dil * 128 * (jb - 1)
                            kpv = k_bf[:, ssl(ks, 128, dil)]
                            S.op("pe", lambda e, pst=pst, sl=sl_prev, kpv=kpv, qv=qv:
                                 e.matmul(pst[:, sl], kpv, qv, start=True, stop=False),
                                 reads=["at_k", "at_q"], writes=[("ps_s", si)])
                            S.op("pe", lambda e, pst=pst, sl=sl_prev:
                                 e.matmul(pst[:, sl], ident_b[:], tb[:, 128:256], start=False, stop=True),
                                 reads=["at_tb", "at_identb"], writes=[("ps_s", si)])
                            lo = min(lo, sl_prev.start)
                        S.op("pe", lambda e, pst=pst, sl=sl_cur, kv=kv, qv=qv:
                             e.matmul(pst[:, sl], kv, qv, start=True, stop=False),
                             reads=["at_k", "at_q"], writes=[("ps_s", si)])
                        S.op("pe", lambda e, pst=pst, sl=sl_cur:
                             e.matmul(pst[:, sl], ident_b[:], tb[:, 0:128], start=False, stop=True),
                             reads=["at_tb", "at_identb"], writes=[("ps_s", si)])
                        lo = min(lo, sl_cur.start)
                    hi = nb2 * 256
                    S.op("act", lambda e, ptb=ptb, pst=pst, lo=lo, hi=hi:
                         e.activation(out=ptb[:, lo:hi], in_=pst[:, lo:hi], func=AF.Exp, scale=0.125),
                         reads=[("ps_s", si)], writes=[("at_pt", si)])
                    for r in range(nb2):
                        jb = jb0 + half + r
                        vb = s_ * bps + jb
                        osl = slice((half + r) * 128, (half + r + 1) * 128)
                        S.op("pe", lambda e, po=po, osl=osl, vb=vb, ptb=ptb, r=r, last=(jb == 0):
                             e.matmul(po[0:65, osl], vaug[:, vb, :], ptb[:, (2 * r + 1) * 128:(2 * r + 2) * 128],
                                      start=True, stop=last),
                             reads=["at_vaug", "at_vaug1", ("at_pt", si)], writes=[("ps_o", oi)])
                        if jb > 0:
                            S.op("pe", lambda e, po=po, osl=osl, vb=vb, ptb=ptb, r=r:
                                 e.matmul(po[0:65, osl], vaug[:, vb - 1, :], ptb[:, (2 * r) * 128:(2 * r + 1) * 128],
                                          start=False, stop=True),
                                 reads=["at_vaug", "at_vaug1", ("at_pt", si)], writes=[("ps_o", oi)])
                t0 = s_ + dil * 128 * jb0
                n_el = nbt * 128
                av = acc[:, ssl(t0, n_el, dil)]
                if first_group:
                    S.op("dve", lambda e, av=av, po=po, n_el=n_el: e.tensor_copy(out=av, in_=po[0:65, 0:n_el]),
                         reads=[("ps_o", oi)], writes=["at_acc"])
                else:
                    S.op("dve", lambda e, av=av, po=po, n_el=n_el:
                         e.tensor_tensor(out=av, in0=po[0:65, 0:n_el], in1=av, op=ALU.add),
                         reads=[("ps_o", oi), "at_acc"], writes=["at_acc"])

    def normalize(yrow, sink_col):
        for t0 in range(0, T, 512):
            S.op("pe", lambda e, t0=t0: e.matmul(ps_d[0:64, :], sel[:], acc[:, t0:t0 + 512], start=True, stop=True),
                 reads=["at_acc", "at_sel"], writes=["ps_d"])
            if sink_col is not None:
                S.op("dve", lambda e: e.tensor_scalar(out=den[:], in0=ps_d[0:64, :],
                                                      scalar1=esink[:, sink_col:sink_col + 1], scalar2=None,
                                                      op0=ALU.add),
                     reads=["ps_d", "at_esink"], writes=["at_den"])
                S.op("dve", lambda e: e.reciprocal(out=den[:], in_=den[:]), reads=["at_den"], writes=["at_den"])
            else:
                S.op("dve", lambda e: e.reciprocal(out=den[:], in_=ps_d[0:64, :]), reads=["ps_d"], writes=["at_den"])
            yi = cnt["y"] % 2; cnt["y"] += 1
            yb = yo[yi]
            S.op("dve", lambda e, yb=yb, t0=t0: e.tensor_tensor(out=yb[:], in0=acc[0:64, t0:t0 + 512], in1=den[:],
                                                                op=ALU.mult),
                 reads=["at_acc", "at_den"], writes=[("at_yo", yi)])
            S.dma("sp", lambda e, yb=yb, t0=t0: e.dma_start(out=yT[yrow:yrow + 64, t0:t0 + 512], in_=yb[:]),
                  reads=[("at_yo", yi)], writes=["yT"])

    last_kv = None
    for h in heads_a:
        kvh = h // 4
        if last_kv != kvh:
            load_kv(ROW_AK + 64 * kvh, ROW_AV + 64 * kvh, 1)
            last_kv = kvh
        S.dma("act", lambda e, h=h: e.dma_start(out=stg[:], in_=pT[ROW_AQ + 64 * h:ROW_AQ + 64 * h + 64, :]),
              reads=["pT"], writes=["at_stg"])
        S.op("pool", lambda e: e.tensor_copy(out=q_bf[:], in_=stg[:]), reads=["at_stg"], writes=["at_q"])
        load_table(h)
        run_seq(1, True)
        normalize(YROW_A + 64 * h, h)
    for hh in heads_c:
        for g, dil in enumerate(C_DILS):
            off = g * 256 + hh * 64
            load_kv(ROW_CK + off, ROW_CV + off, dil)
            S.dma("act", lambda e, off=off: e.dma_start(out=stg[:], in_=pT[ROW_CQ + off:ROW_CQ + off + 64, :]),
                  reads=["pT"], writes=["at_stg"])
            S.op("pool", lambda e: e.tensor_copy(out=q_bf[:], in_=stg[:]), reads=["at_stg"], writes=["at_q"])
            load_table(8 + g * 4 + hh)
            run_seq(dil, g == 0)
        normalize(YROW_C + 64 * hh, None)


def build_attn_test(T, heads_a, heads_c):
    nc = bass.Bass("TRN2", target_bir_lowering=False)
    pT = nc.dram_tensor("pT", [NP_ROWS, T], F32, kind="ExternalInput").ap()
    tg = nc.dram_tensor("tabs_g", [20, 128, 256], F32, kind="ExternalInput").ap()
    tm = nc.dram_tensor("tabs_m", [20, 128, 256], F32, kind="ExternalInput").ap()
    ta = nc.dram_tensor("tabs_a", [20, 128, 256], F32, kind="ExternalInput").ap()
    sk = nc.dram_tensor("sinks", [64, 8], F32, kind="ExternalInput").ap()
    idd = nc.dram_tensor("ident", [128, 128], F32, kind="ExternalInput").ap()
    yT = nc.dram_tensor("yT", [1536, T], F32, kind="ExternalOutput").ap()
    import contextlib
    with contextlib.ExitStack() as st:
        pss = [st.enter_context(nc.psum_tensor(uname("ps%d" % i), [128, 512], F32)) for i in range(8)]
        _PH[0] += 1
        S = Sched(nc)
        emit_attention(S, nc, st, T, pT, yT, tg, tm, ta, sk, idd, pss, heads_a, heads_c)
        S.finish_waits("sp")
        S.emit()
    return nc


CH = 64
B_GN_EPS = 64e-5


def rwkv_consts():
    s_ = np.arange(64)[:, None]; t_ = np.arange(64)[None, :]
    lt = (s_ < t_).astype(np.float32); le = (s_ <= t_).astype(np.float32); gt = (s_ > t_).astype(np.float32)
    m_lt2 = np.ascontiguousarray(np.broadcast_to(np.concatenate([lt, lt], 1)[:, None, :], (64, 3, 128)))
    m_le2 = np.ascontiguousarray(np.broadcast_to(np.concatenate([le, le], 1)[:, None, :], (64, 3, 128)))
    m_gt = np.ascontiguousarray(np.broadcast_to(gt[:, None, :], (64, 3, 64)))
    rst = np.ones((64, 512), np.float32); rst[:, ::64] = 0.0
    return dict(m_lt2=m_lt2, m_le2=m_le2, m_gt=m_gt, rst=rst)


def phase_rwkv(nc, T, pT, yT, prm_d, lmu_d, wup_d, aup_d, gup_d, ident_d, mlt_d, mle_d, mgt_d, rst_d, heads, dbg=None, stop=None):
    import contextlib
    HG = 3
    TT = 512
    NCK = TT // CH
    NH = len(heads)
    with contextlib.ExitStack() as st:
        sb = lambda name, shape, dt=F32: st.enter_context(nc.sbuf_tensor(uname(name), shape, dt))
        pss = [st.enter_context(nc.psum_tensor(uname("rps%d" % i), [128, 512], F32)) for i in range(8)]
        _PH[0] += 1
        S = Sched(nc)
        ident = sb("rw_ident", [128, 128]); m_lt2 = sb("rw_mlt", [64, 3, 128]); m_le2 = sb("rw_mle", [64, 3, 128])
        m_gt = sb("rw_mgt", [64, 3, 64]); rst = sb("rw_rst", [64, 512])
        prm = sb("rw_prm", [64, NH, 10]); lmu = sb("rw_lmu", [128, 4])
        wup = sb("rw_wup", [96, NH * 64]); aup = sb("rw_aup", [96, NH * 64]); gup = sb("rw_gup", [128, 2, NH * 64])
        ones64 = sb("rw_ones", [64, 64]); avg64 = sb("rw_avg", [64, 64]); rkb = sb("rw_rkb", [64, NH, 64])
        for t_, d_ in ((ident, ident_d), (m_lt2, mlt_d), (m_le2, mle_d), (m_gt, mgt_d), (rst, rst_d), (prm, prm_d),
                       (lmu, lmu_d), (wup, wup_d), (aup, aup_d)):
            S.dma("sp", lambda e, t_=t_, d_=d_: e.dma_start(out=t_[:], in_=d_), writes=["const"])
        S.dma("sp", lambda e: e.dma_start(out=gup[:], in_=gup_d.rearrange("(c p) n -> p c n", p=128)), writes=["const"])
        S.op("pool", lambda e: e.memset(ones64[:], 1.0), writes=["const"])
        S.op("pool", lambda e: e.memset(avg64[:], 1.0 / 64), writes=["const"])
        for hi in range(NH):
            S.op("dve", lambda e, hi=hi: e.tensor_scalar(out=rkb[:, hi, :], in0=ones64[:], scalar1=prm[:, hi, 7:8],
                                                        scalar2=None, op0=ALU.mult), reads=["const"], writes=["const"])
        lin = [sb("rw_lin%d" % i, [128, TT + 1]) for i in range(4)]
        ltmp = sb("rw_ltmp", [128, TT])
        th = sb("rw_th", [96, TT]); adm = sb("rw_adm", [96, TT]); sg = sb("rw_sg", [128, 2, TT])
        xin = [sb("rw_xin%d" % i, [64, HG, TT + 1]) for i in range(3)]
        names = ["rm", "km", "vm", "logw", "iclr", "g", "kkn", "k2", "sbon", "Lc", "G", "t0", "t1", "t2",
                 "at"]
        B = {n: sb("rw_" + n, [64, HG, TT]) for n in names}
        for n in ("rt", "bt", "kt", "atb"):
            B[n] = sb("rw_" + n, [64, HG, TT], BF16)
        B["bh"] = B["iclr"]; B["kh"] = B["kkn"]; B["y"] = B["logw"]
        S.alias.update({"bh": "iclr", "kh": "kkn", "y": "logw", ("yo", 0): "Lc", ("yo", 1): "Lc"})
        RhT = sb("rw_RhT", [64, NCK, HG, 64]); Y0T = sb("rw_Y0T", [64, NCK, HG, 64])
        MTa = sb("rw_MT", [64, NCK, HG, 64]); Na = sb("rw_N", [64, NCK, HG, 64])
        Wp = [sb("rw_W%d" % p, [64, HG, 128], BF16) for p in range(2)]; Vtokp = [sb("rw_Vtok%d" % p, [64, HG, 64], BF16) for p in range(2)]
        BKp = [sb("rw_BK%d" % p, [64, HG, 128], BF16) for p in range(2)]; AQp = [sb("rw_AQ%d" % p, [64, HG, 128], BF16) for p in range(2)]
        QPp = [[sb("rw_QP%d_%d" % (p, i), [64, HG, 128], BF16) for i in range(2)] for p in range(2)]
        ARKp = [sb("rw_ARK%d" % p, [64, HG, 128], BF16) for p in range(2)]
        Hs = [sb("rw_H%d" % i, [64, HG, 64]) for i in range(2)]
        yout = [B["Lc"], B["Lc"]]
        cnt = dict(l=0, m=0, y=0)

        def ps_l():
            i = cnt["l"] % 2; cnt["l"] += 1
            return pss[i], ("ps", i)

        def ps_m():
            i = 4 + cnt["m"] % 2; cnt["m"] += 1
            return pss[i], ("ps", i)

        def dve(fn, r, w): S.op("dve", fn, reads=r, writes=w)
        def act(fn, r, w): S.op("act", fn, reads=r, writes=w)
        def pool(fn, r, w): S.op("pool", fn, reads=r, writes=w)
        def pe(fn, r, w): S.op("pe", fn, reads=r, writes=w)

        for g0 in range(0, NH, HG):
            hs = heads[g0:g0 + HG]
            pool(lambda e: e.memset(Hs[0][:], 0.0), [], ["H0"])
            st_h = dict(hcur=0)

            def do_tile(ti, g0=g0, hs=hs, st_h=st_h):
                t0 = ti * TT
                srcs = [(ROW_WD, 96), (ROW_AD, 96), (ROW_GD, 128), (ROW_GD + 128, 128)]
                for i, (row, n) in enumerate(srcs):
                    if t0 == 0:
                        pool(lambda e, i=i, n=n: e.memset(lin[i][0:n, 0:1], 0.0), [], ["lin%d" % i])
                        S.dma("sp", lambda e, i=i, row=row, n=n: e.dma_start(out=lin[i][0:n, 1:TT + 1],
                                                                          in_=pT[row:row + n, 0:TT]),
                              reads=["pT"], writes=["lin%d" % i])
                    else:
                        S.dma("sp", lambda e, i=i, row=row, n=n: e.dma_start(out=lin[i][0:n, :],
                                                                          in_=pT[row:row + n, t0 - 1:t0 + TT]),
                              reads=["pT"], writes=["lin%d" % i])
                for i, row0 in enumerate((ROW_BR, ROW_BK, ROW_BV)):
                    for j, h in enumerate(hs):
                        row = row0 + 64 * h
                        if t0 == 0:
                            pool(lambda e, i=i, j=j: e.memset(xin[i][:, j, 0:1], 0.0), [], ["xin%d" % i])
                            S.dma("act", lambda e, i=i, j=j, row=row: e.dma_start(out=xin[i][:, j, 1:TT + 1],
                                                                              in_=pT[row:row + 64, 0:TT]),
                                  reads=["pT"], writes=["xin%d" % i])
                        else:
                            S.dma("act", lambda e, i=i, j=j, row=row: e.dma_start(out=xin[i][:, j, :],
                                                                              in_=pT[row:row + 64, t0 - 1:t0 + TT]),
                                  reads=["pT"], writes=["xin%d" % i])
                outs = [th, adm, sg[:, 0, :], sg[:, 1, :]]
                for i, (row, n) in enumerate(srcs):
                    pool(lambda e, i=i, n=n: e.tensor_tensor(out=ltmp[0:n, :], in0=lin[i][0:n, 0:TT], in1=lin[i][0:n, 1:TT + 1],
                                                           op=ALU.subtract), ["lin%d" % i], ["ltmp"])
                    o = outs[i]
                    dve(lambda e, i=i, n=n, o=o: e.scalar_tensor_tensor(out=o[0:n, :] if i < 2 else o, in0=ltmp[0:n, :],
                                                                       scalar=lmu[0:n, i:i + 1], in1=lin[i][0:n, 1:TT + 1],
                                                                       op0=ALU.mult, op1=ALU.add),
                        ["ltmp", "lin%d" % i, "const"], ["lo%d" % i])
                act(lambda e: e.activation(out=th[:], in_=th[:], func=AF.Tanh), ["lo0"], ["lo0"])
                act(lambda e: e.activation(out=sg[:, 0, :], in_=sg[:, 0, :], func=AF.Sigmoid), ["lo2"], ["lo2"])
                act(lambda e: e.activation(out=sg[:, 1, :], in_=sg[:, 1, :], func=AF.Sigmoid), ["lo3"], ["lo3"])
                for i, nm in enumerate(("rm", "km", "vm")):
                    pool(lambda e, i=i: e.tensor_tensor(out=B["t0"][:], in0=xin[i][:, :, 0:TT], in1=xin[i][:, :, 1:TT + 1],
                                                      op=ALU.subtract), ["xin%d" % i], ["t0"])
                    for j in range(HG):
                        dve(lambda e, i=i, j=j, nm=nm: e.scalar_tensor_tensor(
                            out=B[nm][:, j, :], in0=B["t0"][:, j, :], scalar=prm[:, g0 + j, i:i + 1],
                            in1=xin[i][:, j, 1:TT + 1], op0=ALU.mult, op1=ALU.add),
                            ["t0", "xin%d" % i, "const"], [nm])
                for j in range(HG):
                    hj = g0 + j
                    cs = slice(hj * 64, hj * 64 + 64)
                    p1, k1 = ps_l()
                    pe(lambda e, p1=p1, cs=cs: e.matmul(p1[0:64, :], wup[:, cs], th[:], start=True, stop=True),
                       ["lo0", "const"], [k1])
                    act(lambda e, p1=p1, j=j, hj=hj: e.activation(out=B["logw"][:, j, :], in_=p1[0:64, :], func=AF.Sigmoid,
                                                               bias=prm[:, hj, 3:4]), [k1, "const"], ["logw"])
                    p2, k2_ = ps_l()
                    pe(lambda e, p2=p2, cs=cs: e.matmul(p2[0:64, :], aup[:, cs], adm[:], start=True, stop=True),
                       ["lo1", "const"], [k2_])
                    act(lambda e, p2=p2, j=j, hj=hj: e.activation(out=B["iclr"][:, j, :], in_=p2[0:64, :], func=AF.Sigmoid,
                                                               bias=prm[:, hj, 4:5]), [k2_, "const"], ["iclr"])
                    p3, k3 = ps_l()
                    for c in range(2):
                        pe(lambda e, p3=p3, cs=cs, c=c: e.matmul(p3[0:64, :], gup[:, c, cs], sg[:, c, :], start=(c == 0),
                                                               stop=(c == 1)), ["lo2", "lo3", "const"], [k3])
                    act(lambda e, p3=p3, j=j: e.copy(out=B["g"][:, j, :], in_=p3[0:64, :]), [k3], ["g"])
                    dve(lambda e, j=j, hj=hj: e.tensor_scalar(out=B["kkn"][:, j, :], in0=B["km"][:, j, :],
                                                             scalar1=prm[:, hj, 5:6], scalar2=None, op0=ALU.mult),
                        ["km", "const"], ["kkn"])
                    pool(lambda e, j=j: e.tensor_tensor(out=B["t1"][:, j, :], in0=B["kkn"][:, j, :], in1=B["kkn"][:, j, :],
                                                      op=ALU.mult), ["kkn"], ["t1"])
                    p4, k4 = ps_l()
                    pe(lambda e, p4=p4, j=j: e.matmul(p4[0:64, :], ones64[:], B["t1"][:, j, :], start=True, stop=True),
                       ["t1", "const"], [k4])
                    act(lambda e, p4=p4, j=j: e.activation(out=B["t2"][:, j, :], in_=p4[0:64, :], func=AF.Sqrt), [k4], ["t2"])
                    dve(lambda e, j=j: e.tensor_scalar(out=B["t2"][:, j, :], in0=B["t2"][:, j, :], scalar1=1e-12, scalar2=None,
                                                      op0=ALU.max), ["t2"], ["t2"])
                    dve(lambda e, j=j: e.reciprocal(out=B["t2"][:, j, :], in_=B["t2"][:, j, :]), ["t2"], ["t2"])
                    dve(lambda e, j=j, hj=hj: e.tensor_scalar(out=B["k2"][:, j, :], in0=B["iclr"][:, j, :], scalar1=-1.0,
                                                             scalar2=prm[:, hj, 6:7], op0=ALU.add, op1=ALU.mult),
                        ["iclr", "const"], ["k2"])
                dve(lambda e: e.tensor_tensor(out=B["kkn"][:], in0=B["kkn"][:], in1=B["t2"][:], op=ALU.mult),
                    ["kkn", "t2"], ["kkn"])
                dve(lambda e: e.scalar_tensor_tensor(out=B["k2"][:], in0=B["k2"][:], scalar=1.0, in1=B["km"][:],
                                                     op0=ALU.add, op1=ALU.mult), ["k2", "km"], ["k2"])
                pool(lambda e: e.tensor_tensor(out=B["t1"][:], in0=B["rm"][:], in1=B["k2"][:], op=ALU.mult),
                     ["rm", "k2"], ["t1"])
                for j in range(HG):
                    p5, k5 = ps_l()
                    pe(lambda e, p5=p5, j=j: e.matmul(p5[0:64, :], rkb[:, g0 + j, :], B["t1"][:, j, :], start=True, stop=True),
                       ["t1", "const"], [k5])
                    act(lambda e, p5=p5, j=j: e.copy(out=B["sbon"][:, j, :], in_=p5[0:64, :]), [k5], ["sbon"])
                dve(lambda e: e.tensor_scalar(out=B["logw"][:], in0=B["logw"][:], scalar1=-math.exp(-0.5), scalar2=None,
                                              op0=ALU.mult), ["logw"], ["logw"])
                for j in range(HG):
                    dve(lambda e, j=j: e.tensor_tensor_scan(out=B["Lc"][:, j, :], data0=rst[:], data1=B["logw"][:, j, :],
                                                           initial=0.0, op0=ALU.mult, op1=ALU.add),
                        ["logw", "const"], ["Lc"])
                act(lambda e: e.activation(out=B["G"][:], in_=B["Lc"][:], func=AF.Exp), ["Lc"], ["G"])
                pool(lambda e: e.tensor_tensor(out=B["rt"][:], in0=B["rm"][:], in1=B["G"][:], op=ALU.mult), ["rm", "G"], ["rt"])
                dve(lambda e: e.tensor_tensor(out=B["t0"][:], in0=B["Lc"][:], in1=B["logw"][:], op=ALU.subtract),
                    ["Lc", "logw"], ["t0"])
                act(lambda e: e.activation(out=B["t0"][:], in_=B["t0"][:], func=AF.Exp), ["t0"], ["t0"])
                dve(lambda e: e.scalar_tensor_tensor(out=B["at"][:], in0=B["kkn"][:], scalar=-1.0, in1=B["t0"][:],
                                                     op0=ALU.mult, op1=ALU.mult), ["kkn", "t0"], ["at"])
                pool(lambda e: e.tensor_copy(out=B["atb"][:], in_=B["at"][:]), ["at"], ["atb"])
                pool(lambda e: e.tensor_tensor(out=B["t2"][:], in0=B["kkn"][:], in1=B["iclr"][:], op=ALU.mult),
                     ["kkn", "iclr"], ["t2"])
                act(lambda e: e.activation(out=B["t1"][:], in_=B["Lc"][:], func=AF.Exp, scale=-1.0), ["Lc", "t1"], ["t1"])
                dve(lambda e: e.tensor_tensor(out=B["bt"][:], in0=B["t2"][:], in1=B["t1"][:], op=ALU.mult), ["t2", "t1"], ["bt"])
                pool(lambda e: e.tensor_tensor(out=B["kt"][:], in0=B["k2"][:], in1=B["t1"][:], op=ALU.mult), ["k2", "t1"], ["kt"])
                for j in range(HG):
                    lc3 = B["Lc"][:, j, :].rearrange("p (c t) -> p c t", t=CH)
                    o3 = B["t0"][:, j, :].rearrange("p (c t) -> p c t", t=CH)
                    dve(lambda e, lc3=lc3, o3=o3: e.tensor_tensor(out=o3, in0=lc3[:, :, CH - 1:CH].to_broadcast([64, NCK, CH]),
                                                                 in1=lc3, op=ALU.subtract), ["Lc", "at"], ["t0"])
                act(lambda e: e.activation(out=B["t0"][:], in_=B["t0"][:], func=AF.Exp), ["t0"], ["t0"])
                dve(lambda e: e.tensor_tensor(out=B["bh"][:], in0=B["t2"][:], in1=B["t0"][:], op=ALU.mult), ["t2", "t0"], ["bh"])
                pool(lambda e: e.tensor_tensor(out=B["kh"][:], in0=B["k2"][:], in1=B["t0"][:], op=ALU.mult), ["k2", "t0"], ["kh"])

                if dbg is not None and ti == 0 and g0 == 0:
                    for di_, nm_ in enumerate(["rm", "km", "vm", "logw", "iclr", "g", "kkn", "k2", "sbon", "Lc", "G", "rt", "at", "bt", "kt", "bh", "kh"]):
                        S.dma("sp", lambda e, di_=di_, nm_=nm_: e.dma_start(out=dbg[di_], in_=B[nm_][:]), reads=[nm_], writes=["dbg"])
                if stop == "prep":
                    return
                def do_chunk(c, pb):
                    W, Vtok, BK, AQ, QP, ARK = Wp[pb], Vtokp[pb], BKp[pb], AQp[pb], QPp[pb], ARKp[pb]
                    kW, kV, kBK, kAQ, kARK = 'W%d' % pb, 'Vtok%d' % pb, 'BK%d' % pb, 'AQ%d' % pb, 'ARK%d' % pb
                    pw_i, pq_i = (6, 0) if pb == 0 else (7, 1)
                    csl = slice(c * CH, (c + 1) * CH)
                    tp1, ktp1 = pss[2], ("ps", 2)
                    tp2, ktp2 = pss[3], ("ps", 3)
                    for j in range(HG):
                        pe(lambda e, j=j: e.transpose(tp1[0:64, j * 128:j * 128 + 64], B["at"][:, j, csl], ident[0:64, 0:64]),
                           ["at", "const"], [ktp1])
                        pe(lambda e, j=j: e.transpose(tp1[0:64, j * 128 + 64:j * 128 + 128], B["vm"][:, j, csl], ident[0:64, 0:64]),
                           ["vm", "const"], [ktp1])
                        pe(lambda e, j=j: e.transpose(tp2[0:64, j * 128:j * 128 + 64], B["bh"][:, j, csl], ident[0:64, 0:64]),
                           ["bh", "const"], [ktp2])
                        pe(lambda e, j=j: e.transpose(tp2[0:64, j * 128 + 64:j * 128 + 128], B["kh"][:, j, csl], ident[0:64, 0:64]),
                           ["kh", "const"], [ktp2])
                    tp1v = tp1[0:64, 0:HG * 128].rearrange("p (h x) -> p h x", x=128)
                    tp2v = tp2[0:64, 0:HG * 128].rearrange("p (h x) -> p h x", x=128)
                    act(lambda e, tp1v=tp1v: e.copy(out=W[:, :, 0:64], in_=tp1v[:, :, 0:64]), [ktp1], [kW])
                    act(lambda e, tp1v=tp1v: e.copy(out=Vtok[:], in_=tp1v[:, :, 64:128]), [ktp1], [kV])
                    act(lambda e, tp2v=tp2v: e.copy(out=BK[:], in_=tp2v), [ktp2], [kBK])
                    yield
                    if stop == "c1":
                        return
                    m1, km1 = ps_m()
                    for j in range(HG):
                        pe(lambda e, m1=m1, j=j: e.matmul(m1[0:64, j * 128:j * 128 + 64], B["kt"][:, j, csl], B["atb"][:, j, csl],
                                                        start=True, stop=True), ["kt", "atb"], [km1])
                        pe(lambda e, m1=m1, j=j: e.matmul(m1[0:64, j * 128 + 64:j * 128 + 128], B["bt"][:, j, csl], B["atb"][:, j, csl],
                                                        start=True, stop=True), ["bt", "atb"], [km1])
                    m1v = m1[0:64, 0:HG * 128].rearrange("p (h x) -> p h x", x=128)
                    dve(lambda e, m1v=m1v: e.tensor_tensor(out=AQ[:], in0=m1v, in1=m_lt2[:], op=ALU.mult), [km1, "const"], [kAQ])
                    m2, km2 = ps_m()
                    for j in range(HG):
                        pe(lambda e, m2=m2, j=j: e.matmul(m2[0:64, j * 64:j * 64 + 64], B["atb"][:, j, csl], B["bt"][:, j, csl],
                                                        start=True, stop=True), ["bt", "atb"], [km2])
                    m2v = m2[0:64, 0:HG * 64].rearrange("p (h x) -> p h x", x=64)
                    qp = 0
                    dve(lambda e: e.tensor_copy(out=QP[0][:, :, 0:64], in_=AQ[:, :, 64:128]), [kAQ], ["QP%d_0" % pb])
                    dve(lambda e, m2v=m2v: e.tensor_tensor(out=QP[0][:, :, 64:128], in0=m2v, in1=m_gt[:], op=ALU.mult),
                        [km2, "const"], ["QP%d_0" % pb])
                    m3, km3 = ps_m()
                    for j in range(HG):
                        pe(lambda e, m3=m3, j=j: e.matmul(m3[0:64, j * 128:j * 128 + 64], B["bt"][:, j, csl], B["rt"][:, j, csl],
                                                        start=True, stop=True), ["bt", "rt"], [km3])
                        pe(lambda e, m3=m3, j=j: e.matmul(m3[0:64, j * 128 + 64:j * 128 + 128], B["kt"][:, j, csl], B["rt"][:, j, csl],
                                                        start=True, stop=True), ["kt", "rt"], [km3])
                    m3v = m3[0:64, 0:HG * 128].rearrange("p (h x) -> p h x", x=128)
                    dve(lambda e, m3v=m3v: e.tensor_tensor(out=ARK[:], in0=m3v, in1=m_le2[:], op=ALU.mult), [km3, "const"], [kARK])
                    yield
                    if stop == "c2":
                        return
                    m4, km4 = ps_m()
                    for j in range(HG):
                        pe(lambda e, m4=m4, j=j: e.matmul(m4[0:64, j * 64:j * 64 + 64], AQ[:, j, 0:64], Vtok[:, j, :],
                                                        start=True, stop=True), [kAQ, kV], [km4])
                    m4v = m4[0:64, 0:HG * 64].rearrange("p (h x) -> p h x", x=64)
                    act(lambda e, m4v=m4v: e.copy(out=W[:, :, 64:128], in_=m4v), [km4], [kW])
                    yield
                    for it in range(6):
                        Qb = QP[qp]
                        kq = "QP%d_%d" % (pb, qp)
                        pw, kpw = pss[pw_i], ("ps", pw_i)
                        for j in range(HG):
                            pe(lambda e, j=j, Qb=Qb: e.matmul(pw[0:64, j * 128:j * 128 + 128], Qb[:, j, 0:64], W[:, j, :],
                                                             start=True, stop=True), [kq, kW], [kpw])
                        pwv = pw[0:64, 0:HG * 128].rearrange("p (h x) -> p h x", x=128)
                        dve(lambda e, pwv=pwv: e.tensor_tensor(out=W[:], in0=pwv, in1=W[:], op=ALU.add), [kpw, kW], [kW])
                        yield
                        if it < 5:
                            pq, kpq = pss[pq_i], ("ps", pq_i)
                            for j in range(HG):
                                pe(lambda e, j=j, Qb=Qb: e.matmul(pq[0:64, j * 128:j * 128 + 64], Qb[:, j, 64:128], Qb[:, j, 0:64],
                                                                 start=True, stop=True), [kq], [kpq])
                                pe(lambda e, j=j, Qb=Qb: e.matmul(pq[0:64, j * 128 + 64:j * 128 + 128], Qb[:, j, 0:64], Qb[:, j, 64:128],
                                                                 start=True, stop=True), [kq], [kpq])
                            pqv = pq[0:64, 0:HG * 128].rearrange("p (h x) -> p h x", x=128)
                            qn = 1 - qp
                            act(lambda e, pqv=pqv, qn=qn: e.copy(out=QP[qn][:], in_=pqv), [kpq], ["QP%d_%d" % (pb, qn)])
                            qp = qn
                            yield
                    if stop == "c3":
                        return
                    m5, km5 = ps_m()
                    for j in range(HG):
                        pe(lambda e, m5=m5, j=j: e.matmul(m5[0:64, j * 64:j * 64 + 64], W[:, j, 0:64], ARK[:, j, 0:64],
                                                        start=True, stop=True), [kW, kARK], [km5])
                    m5b, km5b = ps_m()
                    for j in range(HG):
                        pe(lambda e, m5b=m5b, j=j: e.matmul(m5b[0:64, j * 64:j * 64 + 64], W[:, j, 64:128], ARK[:, j, 0:64],
                                                          start=True, stop=False), [kW, kARK], [km5b])
                        pe(lambda e, m5b=m5b, j=j: e.matmul(m5b[0:64, j * 64:j * 64 + 64], Vtok[:, j, :], ARK[:, j, 64:128],
                                                          start=False, stop=True), [kV, kARK], [km5b])
                    m5v = m5[0:64, 0:HG * 64].rearrange("p (h x) -> p h x", x=64)
                    m5bv = m5b[0:64, 0:HG * 64].rearrange("p (h x) -> p h x", x=64)
                    dve(lambda e, m5v=m5v, c=c: e.tensor_tensor(out=RhT[:, c, :, :], in0=m5v, in1=B["rt"][:, :, csl],
                                                              op=ALU.add), [km5, "rt"], ["RhT"])
                    act(lambda e, m5bv=m5bv, c=c: e.copy(out=Y0T[:, c, :, :], in_=m5bv), [km5b], ["Y0T"])
                    if stop == "c4":
                        return
                    m6, km6 = ps_m()
                    for j in range(HG):
                        pe(lambda e, m6=m6, j=j: e.matmul(m6[0:64, j * 64:j * 64 + 64], W[:, j, 0:64], BK[:, j, 0:64],
                                                        start=True, stop=True), [kW, kBK], [km6])
                    m6b, km6b = ps_m()
                    for j in range(HG):
                        pe(lambda e, m6b=m6b, j=j: e.matmul(m6b[0:64, j * 64:j * 64 + 64], BK[:, j, 0:64], W[:, j, 64:128],
                                                          start=True, stop=False), [kW, kBK], [km6b])
                        pe(lambda e, m6b=m6b, j=j: e.matmul(m6b[0:64, j * 64:j * 64 + 64], BK[:, j, 64:128], Vtok[:, j, :],
                                                          start=False, stop=True), [kV, kBK], [km6b])
                    m6v = m6[0:64, 0:HG * 64].rearrange("p (h x) -> p h x", x=64)
                    m6bv = m6b[0:64, 0:HG * 64].rearrange("p (h x) -> p h x", x=64)
                    for j in range(HG):
                        gc = B["G"][:, j, c * CH + CH - 1:c * CH + CH]
                        dve(lambda e, m6v=m6v, j=j, c=c, gc=gc: e.scalar_tensor_tensor(
                            out=MTa[:, c, j, :], in0=ident[0:64, 0:64], scalar=gc, in1=m6v[:, j, :],
                            op0=ALU.mult, op1=ALU.add), [km6, "G", "const"], ["MT"])
                    act(lambda e, m6bv=m6bv, c=c: e.copy(out=Na[:, c, :, :], in_=m6bv), [km6b], ["N"])

                for c in range(0, NCK, 2):
                    alive = [do_chunk(c, 0), do_chunk(c + 1, 1)]
                    while alive:
                        for g_ in list(alive):
                            try:
                                next(g_)
                            except StopIteration:
                                alive.remove(g_)
                if stop in ("chunk", "c1", "c2", "c3", "c4"):
                    return

                def do_seq(c):
                    hcur = st_h["hcur"]
                    Hc = Hs[hcur]; Hn = Hs[1 - hcur]
                    kh_, kn_ = "H%d" % hcur, "H%d" % (1 - hcur)
                    py, kpy = ps_m()
                    for j in range(HG):
                        pe(lambda e, py=py, j=j, Hc=Hc, c=c: e.matmul(py[0:64, j * 64:j * 64 + 64], Hc[:, j, :], RhT[:, c, j, :],
                                                                    start=True, stop=True), [kh_, "RhT"], [kpy])
                    pyv = py[0:64, 0:HG * 64].rearrange("p (h x) -> p h x", x=64)
                    dve(lambda e, pyv=pyv, c=c: e.tensor_tensor(out=B["y"][:, :, c * CH:(c + 1) * CH], in0=pyv, in1=Y0T[:, c, :, :],
                                                              op=ALU.add), [kpy, "Y0T"], ["y"])
                    ph, kph = ps_m()
                    for j in range(HG):
                        pe(lambda e, ph=ph, j=j, Hc=Hc, c=c: e.matmul(ph[0:64, j * 64:j * 64 + 64], MTa[:, c, j, :], Hc[:, j, :],
                                                                    start=True, stop=True), [kh_, "MT"], [kph])
                    phv = ph[0:64, 0:HG * 64].rearrange("p (h x) -> p h x", x=64)
                    dve(lambda e, phv=phv, c=c, Hn=Hn: e.tensor_tensor(out=Hn[:], in0=phv, in1=Na[:, c, :, :], op=ALU.add),
                        [kph, "N"], [kn_])
                    st_h["hcur"] = 1 - hcur

                for c in range(NCK):
                    do_seq(c)
                if dbg is not None and ti == 0 and g0 == 0:
                    S.dma("sp", lambda e: e.dma_start(out=dbg[17], in_=B["y"][:]), reads=["y"], writes=["dbg"])
                    for di_, nm_ in enumerate([RhT, Y0T, MTa, Na]):
                        S.dma("sp", lambda e, di_=di_, nm_=nm_: e.dma_start(out=dbg[18 + di_].rearrange("p h x -> p (h x)"), in_=nm_[:].rearrange("p c h x -> p (c h x)")),
                              reads=["RhT", "Y0T", "MT", "N"], writes=["dbg"])
                yi = cnt["y"] % 2; cnt["y"] += 1
                yb = yout[yi]
                for j in range(HG):
                    hj = g0 + j
                    p6, k6 = ps_l()
                    pe(lambda e, p6=p6, j=j: e.matmul(p6[0:64, :], avg64[:], B["y"][:, j, :], start=True, stop=True),
                       ["y", "const"], [k6])
                    dve(lambda e, p6=p6, j=j: e.tensor_tensor(out=B["t0"][:, j, :], in0=B["y"][:, j, :], in1=p6[0:64, :],
                                                            op=ALU.subtract), [k6, "y"], ["t0"])
                    pool(lambda e, j=j: e.tensor_tensor(out=B["t1"][:, j, :], in0=B["t0"][:, j, :], in1=B["t0"][:, j, :],
                                                      op=ALU.mult), ["t0"], ["t1"])
                    p7, k7 = ps_l()
                    pe(lambda e, p7=p7, j=j: e.matmul(p7[0:64, :], avg64[:], B["t1"][:, j, :], start=True, stop=True),
                       ["t1", "const"], [k7])
                    dve(lambda e, p7=p7, j=j: e.tensor_scalar(out=B["t2"][:, j, :], in0=p7[0:64, :], scalar1=B_GN_EPS, scalar2=None,
                                                            op0=ALU.add), [k7], ["t2"])
                    act(lambda e, j=j: e.activation(out=B["t2"][:, j, :], in_=B["t2"][:, j, :], func=AF.Sqrt), ["t2"], ["t2"])
                    dve(lambda e, j=j: e.reciprocal(out=B["t2"][:, j, :], in_=B["t2"][:, j, :]), ["t2"], ["t2"])
                    dve(lambda e, j=j: e.tensor_tensor(out=B["t0"][:, j, :], in0=B["t0"][:, j, :], in1=B["t2"][:, j, :],
                                                      op=ALU.mult), ["t0", "t2"], ["t0"])
                    dve(lambda e, j=j, hj=hj: e.tensor_scalar(out=B["t0"][:, j, :], in0=B["t0"][:, j, :], scalar1=prm[:, hj, 8:9],
                                                             scalar2=prm[:, hj, 9:10], op0=ALU.mult, op1=ALU.add),
                        ["t0", "const"], ["t0"])
                pool(lambda e: e.tensor_tensor(out=B["t1"][:], in0=B["sbon"][:], in1=B["vm"][:], op=ALU.mult),
                     ["sbon", "vm", "t1"], ["t1"])
                dve(lambda e: e.tensor_tensor(out=B["t0"][:], in0=B["t0"][:], in1=B["t1"][:], op=ALU.add), ["t0", "t1"], ["t0"])
                dve(lambda e, yb=yb: e.tensor_tensor(out=yb[:], in0=B["t0"][:], in1=B["g"][:], op=ALU.mult),
                    ["t0", "g"], [("yo", yi)])
                for j, h in enumerate(hs):
                    S.dma("sp", lambda e, yb=yb, j=j, h=h: e.dma_start(out=yT[YROW_B + 64 * h:YROW_B + 64 * h + 64, t0:t0 + TT],
                                                                     in_=yb[:, j, :]), reads=[("yo", yi)], writes=["yT"])

            for ti in range(T // TT):
                do_tile(ti)
        S.barrier_all()
        S.emit()


def build_rwkv_test(T, heads, debug=False, stop=None):
    nc = bass.Bass("TRN2", target_bir_lowering=False)
    NH = len(heads)
    di = lambda n, s: nc.dram_tensor(n, s, F32, kind="ExternalInput").ap()
    pT = di("pT", [NP_ROWS, T]); prm = di("prm", [64, NH, 10]); lmu = di("lmu", [128, 4])
    wup = di("wup", [96, NH * 64]); aup = di("aup", [96, NH * 64]); gup = di("gup", [256, NH * 64])
    ident = di("ident", [128, 128]); mlt = di("m_lt2", [64, 3, 128]); mle = di("m_le2", [64, 3, 128])
    mgt = di("m_gt", [64, 3, 64]); rst = di("rst", [64, 512])
    yT = nc.dram_tensor("yT", [1536, T], F32, kind="ExternalOutput").ap()
    dbg = nc.dram_tensor("dbg", [22, 64, 3, 512], F32, kind="ExternalOutput").ap() if debug else None
    phase_rwkv(nc, T, pT, yT, prm, lmu, wup, aup, gup, ident, mlt, mle, mgt, rst, heads, dbg=dbg, stop=stop)
    return nc


def rwkv_host_params(heads, mu, w0, w_up, a0, a_up, g_up, k_k, k_a, r_k, lnx_g, lnx_b):
    NH = len(heads)
    prm = np.zeros((64, NH, 10), np.float32)
    cols = np.concatenate([np.arange(64 * h, 64 * h + 64) for h in heads])
    for i, h in enumerate(heads):
        sl = slice(64 * h, 64 * h + 64)
        prm[:, i, 0] = mu[0:768][sl]; prm[:, i, 1] = mu[768:1536][sl]; prm[:, i, 2] = mu[1536:2304][sl]
        prm[:, i, 3] = w0[sl]; prm[:, i, 4] = a0[sl]; prm[:, i, 5] = k_k[sl]; prm[:, i, 6] = k_a[sl]
        prm[:, i, 7] = r_k.reshape(-1)[sl]; prm[:, i, 8] = lnx_g[sl]; prm[:, i, 9] = lnx_b[sl]
    lmu = np.zeros((128, 4), np.float32)
    lmu[0:96, 0] = mu[2304:2400]; lmu[0:96, 1] = mu[2400:2496]; lmu[:, 2] = mu[2496:2624]; lmu[:, 3] = mu[2624:2752]
    return dict(prm=prm, lmu=lmu, wup=np.ascontiguousarray(w_up[:, cols]), aup=np.ascontiguousarray(a_up[:, cols]),
                gup=np.ascontiguousarray(g_up[:, cols]))


class WeightStream:
    def __init__(self, S, nc, st, name, max_elems, n_stage=2, n_bf=2, direct=False):
        self.S, self.nc, self.name = S, nc, name
        if direct:
            n_stage = 0
        self.stage = [st.enter_context(nc.sbuf_tensor(uname("%s_st%d" % (name, i)), [128, max_elems], F32)) for i in range(n_stage)]
        self.bf = [st.enter_context(nc.sbuf_tensor(uname("%s_bf%d" % (name, i)), [128, max_elems], BF16)) for i in range(n_bf)]
        self.i = 0
        self.q = 0

    def load_bf(self, scr, shapes, dram_key):
        S = self.S
        bi = self.i % len(self.bf); self.i += 1
        bfb = self.bf[bi]
        n_tot = sum(int(np.prod(shp[1:])) for shp in shapes)
        q = ("sp", "act")[self.q % 2]; self.q += 1
        S.dma(q, lambda e, bfb=bfb, n_tot=n_tot: e.dma_start(out=bfb[:, 0:n_tot], in_=scr[:, 0:n_tot]), reads=[dram_key],
              writes=[(self.name, "bf", bi)])
        off = 0
        outs = []
        for shp in shapes:
            n = int(np.prod(shp[1:]))
            pat = "p (a b) -> p a b" if len(shp) == 3 else "p (a b c) -> p a b c"
            kw = dict(a=shp[1], b=shp[2]) if len(shp) == 3 else dict(a=shp[1], b=shp[2], c=shp[3])
            outs.append(bfb[:, off:off + n].rearrange(pat, **kw))
            off += n
        return outs, (self.name, "bf", bi)

    def load(self, src_views, shapes, dram_key):
        S = self.S
        si = self.i % len(self.stage); bi = self.i % len(self.bf); self.i += 1
        stg, bfb = self.stage[si], self.bf[bi]
        off = 0
        outs = []
        for v, shp in zip(src_views, shapes):
            n = int(np.prod(shp[1:]))
            pat = "p (a b) -> p a b" if len(shp) == 3 else "p (a b c) -> p a b c"
            kw = dict(a=shp[1], b=shp[2]) if len(shp) == 3 else dict(a=shp[1], b=shp[2], c=shp[3])
            dst = stg[:, off:off + n].rearrange(pat, **kw)
            q = ("sp", "act")[self.q % 2]; self.q += 1
            S.dma(q, lambda e, dst=dst, v=v: e.dma_start(out=dst, in_=v), reads=[dram_key],
                  writes=[(self.name, "st", si)])
            outs.append(bfb[:, off:off + n].rearrange(pat, **kw))
            off += n
        S.op("pool", lambda e, stg=stg, bfb=bfb, off=off: e.tensor_copy(out=bfb[:, 0:off], in_=stg[:, 0:off]),
             reads=[(self.name, "st", si)], writes=[(self.name, "bf", bi)])
        return outs, (self.name, "bf", bi)


def phase_precast(nc, panels, scr, max_elems):
    import contextlib
    with contextlib.ExitStack() as st:
        _PH[0] += 1
        S = Sched(nc)
        ws = WeightStream(S, nc, st, "pc_w", max_elems)
        for i, (views, shapes) in enumerate(panels):
            outs, kw = ws.load(views, shapes, "Wsrc")
            n_tot = sum(int(np.prod(shp[1:])) for shp in shapes)
            bfb = ws.bf[(ws.i - 1) % len(ws.bf)]
            S.dma("sp", lambda e, i=i, bfb=bfb, n_tot=n_tot: e.dma_start(out=scr[i][:, 0:n_tot], in_=bfb[:, 0:n_tot]),
                  reads=[kw], writes=["scr"])
        S.barrier_all()
        S.emit()


def emit_norm_T(S, nc, x_sb, kx, h_out, kh, g_col, ones_bf, sq, rstd, ps, kps, n):
    for c in range(KC):
        S.op("act", lambda e, c=c: e.activation(out=sq[:, c, :n], in_=x_sb[:, c, :], func=AF.Square), reads=[kx], writes=["nrm_sq"])
    for c in range(KC):
        S.op("pe", lambda e, c=c: e.matmul(ps[:, :n], ones_bf[:], sq[:, c, :n], start=(c == 0), stop=(c == KC - 1)),
             reads=["nrm_sq", "ones"], writes=[kps])
    S.op("dve", lambda e: e.tensor_scalar(out=rstd[:, :n], in0=ps[:, :n], scalar1=1.0 / D_MODEL, scalar2=NORM_EPS,
                                          op0=ALU.mult, op1=ALU.add), reads=[kps], writes=["nrm_rstd"])
    S.op("act", lambda e: e.activation(out=rstd[:, :n], in_=rstd[:, :n], func=AF.Sqrt), reads=["nrm_rstd"], writes=["nrm_rstd"])
    S.op("dve", lambda e: e.reciprocal(out=rstd[:, :n], in_=rstd[:, :n]), reads=["nrm_rstd"], writes=["nrm_rstd"])
    for c in range(KC):
        S.op("dve", lambda e, c=c: e.scalar_tensor_tensor(out=h_out[:, c, :], in0=x_sb[:, c, :], scalar=g_col[:, c:c + 1],
                                                         in1=rstd[:, :n], op0=ALU.mult, op1=ALU.mult),
             reads=[kx, "nrm_rstd", "gcol"], writes=[kh])


def phase_proj(nc, TL, xT, g_d, W, pT, ncols, scr=None):
    import contextlib
    TS = min(2048, TL); TT = 512; CB = 256
    Wv0 = W.rearrange("(c p) n -> p c n", p=128)
    if scr is not None:
        panels = []
        for cb0 in range(0, ncols, CB):
            cw = min(CB, ncols - cb0)
            panels.append(([Wv0[:, :, cb0:cb0 + cw]], [(128, KC, cw)]))
        phase_precast(nc, panels, scr, KC * CB)
    with contextlib.ExitStack() as st:
        sb = lambda name, shape, dt=F32: st.enter_context(nc.sbuf_tensor(uname(name), shape, dt))
        pss = [st.enter_context(nc.psum_tensor(uname("pps%d" % i), [128, 512], F32)) for i in range(8)]
        _PH[0] += 1
        S = Sched(nc)
        ones_bf = sb("pj_ones", [128, 128], BF16); g_sb = sb("pj_g", [128, KC])
        hT = sb("pj_h", [128, KC, TS], BF16)
        xin = [sb("pj_x%d" % i, [128, KC, TT]) for i in range(1)]
        sq = sb("pj_sq", [128, KC, TT], BF16); rstd = sb("pj_rstd", [128, TT])
        ot = [sb("pj_o%d" % i, [128, TT]) for i in range(4)]
        ws = WeightStream(S, nc, st, "pj_w", KC * CB, direct=scr is not None)
        S.op("pool", lambda e: e.memset(ones_bf[:], 1.0), writes=["ones"])
        S.dma("sp", lambda e: e.dma_start(out=g_sb[:], in_=g_d), writes=["gcol"])
        xTv = xT.rearrange("(c p) t -> p c t", p=128)
        Wv = W.rearrange("(c p) n -> p c n", p=128)
        cnt = dict(o=0, ps=0)
        for s0 in range(0, TL, TS):
            for tt in range(TS // TT):
                c0 = s0 + tt * TT
                S.dma("sp", lambda e, c0=c0: e.dma_start(out=xin[0][:], in_=xTv[:, :, c0:c0 + TT]), reads=["xT"], writes=["pj_x"])
                emit_norm_T(S, nc, xin[0], "pj_x", hT[:, :, tt * TT:(tt + 1) * TT], ("pj_h", tt), g_sb, ones_bf, sq, rstd,
                            pss[0], ("ps", 0), TT)
            for cb0 in range(0, ncols, CB):
                cw = min(CB, ncols - cb0)
                if scr is not None:
                    (wv,), kw = ws.load_bf(scr[cb0 // CB], [(128, KC, cw)], "Wscr")
                else:
                    (wv,), kw = ws.load([Wv[:, :, cb0:cb0 + cw]], [(128, KC, cw)], "W")
                for m0 in range(0, cw, 128):
                    mw = min(128, cw - m0)
                    for tt in range(TS // TT):
                        pi = 1 + cnt["ps"] % 7; cnt["ps"] += 1
                        ps = pss[pi]
                        for c in range(KC):
                            S.op("pe", lambda e, ps=ps, wv=wv, c=c, m0=m0, mw=mw, tt=tt:
                                 e.matmul(ps[:mw, :TT], wv[:, c, m0:m0 + mw], hT[:, c, tt * TT:(tt + 1) * TT],
                                          start=(c == 0), stop=(c == KC - 1)),
                                 reads=[kw, ("pj_h", tt)], writes=[("ps", pi)])
                        oi = cnt["o"] % 4; cnt["o"] += 1
                        ob = ot[oi]
                        if oi % 2:
                            S.op("act", lambda e, ob=ob, ps=ps, mw=mw: e.copy(out=ob[:mw, :], in_=ps[:mw, :TT]),
                                 reads=[("ps", pi)], writes=[("pj_o", oi)])
                        else:
                            S.op("dve", lambda e, ob=ob, ps=ps, mw=mw: e.tensor_copy(out=ob[:mw, :], in_=ps[:mw, :TT]),
                                 reads=[("ps", pi)], writes=[("pj_o", oi)])
                        r0 = cb0 + m0; t0 = s0 + tt * TT
                        S.dma("sp", lambda e, ob=ob, mw=mw, r0=r0, t0=t0: e.dma_start(out=pT[r0:r0 + mw, t0:t0 + TT], in_=ob[:mw, :]),
                              reads=[("pj_o", oi)], writes=["pT"])
        S.barrier_all()
        S.emit()


def phase_merge(nc, TL, xT, g_d, Wg, gate_col0, yT, projw, wout, x1T, scr=None, scr_o=None):
    import contextlib
    TT = 512
    YK = (4, 6, 2)
    YO = (0, 4, 10)
    NHALF = 2 if (scr is not None and TL % (2 * TT) == 0) else 1
    TB = TT * NHALF
    Wgv0 = Wg.rearrange("(c p) n -> p c n", p=128)
    Pv0 = projw.rearrange("(c p) n -> p c n", p=128)
    Wov0 = wout.rearrange("(c p) n -> p c n", p=128)

    def mg_views(m):
        views = [Wgv0[:, :, gate_col0 + i * D_MODEL + m * 128:gate_col0 + i * D_MODEL + m * 128 + 128] for i in range(3)]
        views.append(Pv0[:, :, m * 128:(m + 1) * 128])
        return views, [(128, KC, 128)] * 3 + [(128, 12, 128)]

    if scr is not None:
        phase_precast(nc, [mg_views(m) for m in range(KC)], scr, KC * 3 * 128 + 12 * 128)
        phase_precast(nc, [([Wov0[:, :, m * 128:(m + 1) * 128]], [(128, KC, 128)]) for m in range(KC)], scr_o, KC * 128)
    with contextlib.ExitStack() as st:
        sb = lambda name, shape, dt=F32: st.enter_context(nc.sbuf_tensor(uname(name), shape, dt))
        pss = [st.enter_context(nc.psum_tensor(uname("mps%d" % i), [128, 512], F32)) for i in range(8)]
        _PH[0] += 1
        S = Sched(nc)
        ones_bf = sb("mg_ones", [128, 128], BF16); g_sb = sb("mg_g", [128, KC])
        xin = sb("mg_x", [128, KC, TT]); hT = sb("mg_h", [128, KC, TB], BF16)
        xr = [sb("mg_xr%d" % i, [128, TT]) for i in range(2)]
        rstd = sb("mg_rstd", [128, TT])
        yst = sb("mg_yst", [128, 12, TT]); ybf = sb("mg_ybf", [128, 12, TB], BF16)
        mrgb = sb("mg_mrgb", [128, KC, TB], BF16)
        sq = mrgb[:, :, 0:TT]
        S.alias["nrm_sq"] = "mg_mrgb"
        sig = [sb("mg_sig%d" % i, [128, TT]) for i in range(2)]
        tmp = sb("mg_tmp", [128, TT]); mrg = sb("mg_mrg", [128, TT])
        xo = [sb("mg_xo%d" % i, [128, TT]) for i in range(2)]
        ws = WeightStream(S, nc, st, "mg_w", KC * 3 * 128 + 12 * 128, n_stage=1, direct=scr is not None)
        S.op("pool", lambda e: e.memset(ones_bf[:], 1.0), writes=["ones"])
        S.dma("sp", lambda e: e.dma_start(out=g_sb[:], in_=g_d), writes=["gcol"])
        xTv = xT.rearrange("(c p) t -> p c t", p=128)
        yTv = yT.rearrange("(c p) t -> p c t", p=128)
        Wgv = Wg.rearrange("(c p) n -> p c n", p=128)
        Pv = projw.rearrange("(c p) n -> p c n", p=128)
        Wov = wout.rearrange("(c p) n -> p c n", p=128)
        cnt = dict(ps=0, s=0, o=0)

        def nps():
            i = 1 + cnt["ps"] % 7; cnt["ps"] += 1
            return pss[i], ("ps", i)

        for t0 in range(0, TL, TB):
            for h in range(NHALF):
                hs = slice(h * TT, (h + 1) * TT)
                tk = t0 + h * TT
                S.dma("sp", lambda e, tk=tk: e.dma_start(out=xin[:], in_=xTv[:, :, tk:tk + TT]), reads=["xT"], writes=["mg_x"])
                S.dma("act", lambda e, tk=tk: e.dma_start(out=yst[:], in_=yTv[:, :, tk:tk + TT]), reads=["yT"], writes=["mg_yst"])
                S.op("pool", lambda e, hs=hs: e.tensor_copy(out=ybf[:, :, hs], in_=yst[:]), reads=["mg_yst"], writes=[("mg_ybf", h)])
                emit_norm_T(S, nc, xin, "mg_x", hT[:, :, hs], ("mg_h", h), g_sb, ones_bf, sq, rstd, pss[0], ("ps", 0), TT)
            for m in range(KC):
                views = [Wgv[:, :, gate_col0 + i * D_MODEL + m * 128:gate_col0 + i * D_MODEL + m * 128 + 128] for i in range(3)]
                views.append(Pv[:, :, m * 128:(m + 1) * 128])
                shapes = [(128, KC, 128)] * 3 + [(128, 12, 128)]
                if scr is not None:
                    (w0v, w1v, w2v, pv), kw = ws.load_bf(scr[m], shapes, "Wmgs")
                else:
                    (w0v, w1v, w2v, pv), kw = ws.load(views, shapes, "Wmg")
                wgs = (w0v, w1v, w2v)
                for h in range(NHALF):
                    hs = slice(h * TT, (h + 1) * TT)
                    for i in range(3):
                        pg, kpg = nps()
                        for c in range(KC):
                            S.op("pe", lambda e, pg=pg, i=i, c=c, wgs=wgs, hs=hs: e.matmul(pg[:, :TT], wgs[i][:, c, :], hT[:, c, hs],
                                                                                       start=(c == 0), stop=(c == KC - 1)),
                                 reads=[kw, ("mg_h", h)], writes=[kpg])
                        pz, kpz = nps()
                        for u in range(YK[i]):
                            S.op("pe", lambda e, pz=pz, i=i, u=u, pv=pv, hs=hs: e.matmul(pz[:, :TT], pv[:, YO[i] + u, :], ybf[:, YO[i] + u, hs],
                                                                                      start=(u == 0), stop=(u == YK[i] - 1)),
                                 reads=[kw, ("mg_ybf", h)], writes=[kpz])
                        si = cnt["s"] % 2; cnt["s"] += 1
                        sg = sig[si]
                        S.op("act", lambda e, sg=sg, pg=pg: e.activation(out=sg[:], in_=pg[:, :TT], func=AF.Sigmoid), reads=[kpg],
                             writes=[("mg_sig", si)])
                        if i == 0:
                            S.op("dve", lambda e, sg=sg, pz=pz: e.tensor_tensor(out=mrg[:], in0=pz[:, :TT], in1=sg[:], op=ALU.mult),
                                 reads=[kpz, ("mg_sig", si)], writes=["mg_mrg"])
                        else:
                            S.op("dve", lambda e, sg=sg, pz=pz: e.tensor_tensor(out=tmp[:], in0=pz[:, :TT], in1=sg[:], op=ALU.mult),
                                 reads=[kpz, ("mg_sig", si)], writes=["mg_tmp"])
                            if i == 1:
                                S.op("pool", lambda e: e.tensor_tensor(out=mrg[:], in0=mrg[:], in1=tmp[:], op=ALU.add),
                                     reads=["mg_mrg", "mg_tmp"], writes=["mg_mrg"])
                            else:
                                S.op("pool", lambda e, m=m, hs=hs: e.tensor_tensor(out=mrgb[:, m, hs], in0=mrg[:], in1=tmp[:], op=ALU.add),
                                     reads=["mg_mrg", "mg_tmp"], writes=["mg_mrgb"])
            for m in range(KC):
                if scr is not None:
                    (wov,), kw = ws.load_bf(scr_o[m], [(128, KC, 128)], "Wouts")
                else:
                    (wov,), kw = ws.load([Wov[:, :, m * 128:(m + 1) * 128]], [(128, KC, 128)], "Wout")
                for h in range(NHALF):
                    hs = slice(h * TT, (h + 1) * TT)
                    tk = t0 + h * TT
                    po, kpo = nps()
                    for c in range(KC):
                        S.op("pe", lambda e, po=po, c=c, wov=wov, hs=hs: e.matmul(po[:, :TT], wov[:, c, :], mrgb[:, c, hs], start=(c == 0),
                                                                               stop=(c == KC - 1)), reads=[kw, "mg_mrgb"], writes=[kpo])
                    oi = cnt["o"] % 2; cnt["o"] += 1
                    ob = xo[oi]; xrb = xr[oi]
                    S.dma("act", lambda e, xrb=xrb, m=m, tk=tk: e.dma_start(out=xrb[:], in_=xT[m * 128:(m + 1) * 128, tk:tk + TT]),
                          reads=["xT"], writes=[("mg_xr", oi)])
                    S.op("dve", lambda e, ob=ob, po=po, xrb=xrb: e.tensor_tensor(out=ob[:], in0=po[:, :TT], in1=xrb[:], op=ALU.add),
                         reads=[kpo, ("mg_xr", oi)], writes=[("mg_xo", oi)])
                    S.dma("sp", lambda e, ob=ob, m=m, tk=tk: e.dma_start(out=x1T[m * 128:(m + 1) * 128, tk:tk + TT], in_=ob[:]),
                          reads=[("mg_xo", oi)], writes=["x1T"])
        S.barrier_all()
        S.emit()


def phase_ffn(nc, TL, x1T, g_d, wup, conv_d, wdown, xoT, d_ff, halo_d=None, scr_u=None, scr_d=None):
    import contextlib
    TT = 512
    NF = d_ff // 128
    NHALF = 2 if (scr_u is not None and TL % (2 * TT) == 0) else 1
    TB = TT * NHALF
    Wuv0 = wup.rearrange("(c p) n -> p c n", p=128)
    Wdv0 = wdown.rearrange("(f p) n -> p f n", p=128)
    if scr_u is not None:
        phase_precast(nc, [([Wuv0[:, :, f * 128:(f + 1) * 128], Wuv0[:, :, d_ff + f * 128:d_ff + (f + 1) * 128]], [(128, KC, 128)] * 2)
                           for f in range(NF)], scr_u, KC * 256)
        phase_precast(nc, [([Wdv0[:, :, m * 128:(m + 1) * 128]], [(128, NF, 128)]) for m in range(KC)], scr_d, NF * 128)
    with contextlib.ExitStack() as st:
        sb = lambda name, shape, dt=F32: st.enter_context(nc.sbuf_tensor(uname(name), shape, dt))
        pss = [st.enter_context(nc.psum_tensor(uname("fps%d" % i), [128, 512], F32)) for i in range(8)]
        _PH[0] += 1
        S = Sched(nc)
        ones_bf = sb("ff_ones", [128, 128], BF16); g_sb = sb("ff_g", [128, KC]); cw = sb("ff_cw", [128, NF, 3])
        xin = sb("ff_x", [128, KC, TT]); hT = sb("ff_h", [128, KC, TB], BF16)
        rstd = sb("ff_rstd", [128, TT])
        actb = sb("ff_act", [128, max(NF, KC), TB], BF16)
        sq = actb[:, 0:KC, 0:TT]
        S.alias["nrm_sq"] = "ff_act"
        xr = [sb("ff_xr%d" % i, [128, TT]) for i in range(2)]
        carry = sb("ff_carry", [128, NF, 2])
        gbuf = [sb("ff_gb%d" % i, [128, TT + 2]) for i in range(2)]
        cv = [sb("ff_cv%d" % i, [128, TT]) for i in range(2)]
        xo = [sb("ff_xo%d" % i, [128, TT]) for i in range(2)]
        ws = WeightStream(S, nc, st, "ff_w", max(KC * 256, NF * 128), n_stage=2, n_bf=3 if scr_u is not None else 2,
                          direct=scr_u is not None)
        S.op("pool", lambda e: e.memset(ones_bf[:], 1.0), writes=["ones"])
        S.dma("sp", lambda e: e.dma_start(out=g_sb[:], in_=g_d), writes=["gcol"])
        S.dma("sp", lambda e: e.dma_start(out=cw[:], in_=conv_d), writes=["ff_cw"])
        if halo_d is None:
            S.op("pool", lambda e: e.memset(carry[:], 0.0), writes=["ff_carry"])
        else:
            S.dma("sp", lambda e: e.dma_start(out=carry[:], in_=halo_d), writes=["ff_carry"])
        xv = x1T.rearrange("(c p) t -> p c t", p=128)
        Wuv = wup.rearrange("(c p) n -> p c n", p=128)
        Wdv = wdown.rearrange("(f p) n -> p f n", p=128)
        cnt = dict(ps=0, g=0, o=0)

        def nps():
            i = 1 + cnt["ps"] % 7; cnt["ps"] += 1
            return pss[i], ("ps", i)

        for t0 in range(0, TL, TB):
            for h in range(NHALF):
                S.dma("sp", lambda e, t0=t0, h=h: e.dma_start(out=xin[:], in_=xv[:, :, t0 + h * TT:t0 + (h + 1) * TT]),
                      reads=["x1T"], writes=["ff_x"])
                emit_norm_T(S, nc, xin, "ff_x", hT[:, :, h * TT:(h + 1) * TT], ("ff_h", h), g_sb, ones_bf, sq, rstd, pss[0],
                            ("ps", 0), TT)
            for f in range(NF):
                views = [Wuv[:, :, f * 128:(f + 1) * 128], Wuv[:, :, d_ff + f * 128:d_ff + (f + 1) * 128]]
                if scr_u is not None:
                    (wg, wv), kw = ws.load_bf(scr_u[f], [(128, KC, 128)] * 2, "Wups")
                else:
                    (wg, wv), kw = ws.load(views, [(128, KC, 128)] * 2, "Wup")
                for h in range(NHALF):
                    hs = slice(h * TT, (h + 1) * TT)
                    pg, kpg = nps()
                    for c in range(KC):
                        S.op("pe", lambda e, pg=pg, c=c, wg=wg, hs=hs: e.matmul(pg[:, :TT], wg[:, c, :], hT[:, c, hs], start=(c == 0),
                                                                             stop=(c == KC - 1)), reads=[kw, ("ff_h", h)], writes=[kpg])
                    pv, kpv = nps()
                    for c in range(KC):
                        S.op("pe", lambda e, pv=pv, c=c, wv=wv, hs=hs: e.matmul(pv[:, :TT], wv[:, c, :], hT[:, c, hs], start=(c == 0),
                                                                             stop=(c == KC - 1)), reads=[kw, ("ff_h", h)], writes=[kpv])
                    gi = cnt["g"] % 2; cnt["g"] += 1
                    gb = gbuf[gi]; cb = cv[gi]
                    kgb, kcb = ("ff_gb", gi), ("ff_cv", gi)
                    S.op("pool", lambda e, gb=gb, f=f: e.tensor_copy(out=gb[:, 0:2], in_=carry[:, f, :]), reads=["ff_carry"], writes=[kgb])
                    S.op("act", lambda e, gb=gb, pg=pg: e.copy(out=gb[:, 2:TT + 2], in_=pg[:, :TT]), reads=[kpg], writes=[kgb])
                    S.op("pool", lambda e, gb=gb, f=f: e.tensor_copy(out=carry[:, f, :], in_=gb[:, TT:TT + 2]), reads=[kgb],
                         writes=["ff_carry"])
                    S.op("dve", lambda e, gb=gb, cb=cb, f=f: e.tensor_scalar(out=cb[:], in0=gb[:, 0:TT], scalar1=cw[:, f, 0:1], scalar2=None,
                                                                           op0=ALU.mult), reads=[kgb, "ff_cw"], writes=[kcb])
                    S.op("dve", lambda e, gb=gb, cb=cb, f=f: e.scalar_tensor_tensor(out=cb[:], in0=gb[:, 1:TT + 1], scalar=cw[:, f, 1:2],
                                                                                  in1=cb[:], op0=ALU.mult, op1=ALU.add),
                         reads=[kgb, "ff_cw", kcb], writes=[kcb])
                    S.op("dve", lambda e, gb=gb, cb=cb, f=f: e.scalar_tensor_tensor(out=cb[:], in0=gb[:, 2:TT + 2], scalar=cw[:, f, 2:3],
                                                                                  in1=cb[:], op0=ALU.mult, op1=ALU.add),
                         reads=[kgb, "ff_cw", kcb], writes=[kcb])
                    S.op("act", lambda e, cb=cb: e.activation(out=cb[:], in_=cb[:], func=AF.Silu), reads=[kcb], writes=[kcb])
                    S.op("dve", lambda e, cb=cb, pv=pv, f=f, hs=hs: e.tensor_tensor(out=actb[:, f, hs], in0=pv[:, :TT], in1=cb[:], op=ALU.mult),
                         reads=[kpv, kcb], writes=["ff_act"])
            for m in range(KC):
                if scr_d is not None:
                    (wd,), kw = ws.load_bf(scr_d[m], [(128, NF, 128)], "Wdowns")
                else:
                    (wd,), kw = ws.load([Wdv[:, :, m * 128:(m + 1) * 128]], [(128, NF, 128)], "Wdown")
                for h in range(NHALF):
                    hs = slice(h * TT, (h + 1) * TT)
                    tk = t0 + h * TT
                    po, kpo = nps()
                    for f in range(NF):
                        S.op("pe", lambda e, po=po, f=f, wd=wd, hs=hs: e.matmul(po[:, :TT], wd[:, f, :], actb[:, f, hs], start=(f == 0),
                                                                             stop=(f == NF - 1)), reads=[kw, "ff_act"], writes=[kpo])
                    oi = cnt["o"] % 2; cnt["o"] += 1
                    ob = xo[oi]; xrb = xr[oi]
                    S.dma("act", lambda e, xrb=xrb, m=m, tk=tk: e.dma_start(out=xrb[:], in_=x1T[m * 128:(m + 1) * 128, tk:tk + TT]),
                          reads=["x1T"], writes=[("ff_xr", oi)])
                    S.op("dve", lambda e, ob=ob, po=po, xrb=xrb: e.tensor_tensor(out=ob[:], in0=po[:, :TT], in1=xrb[:], op=ALU.add),
                         reads=[kpo, ("ff_xr", oi)], writes=[("ff_xo", oi)])
                    S.dma("sp", lambda e, ob=ob, m=m, tk=tk: e.dma_start(out=xoT[m * 128:(m + 1) * 128, tk:tk + TT], in_=ob[:]),
                          reads=[("ff_xo", oi)], writes=["xoT"])
        S.barrier_all()
        S.emit()


def phase_final_norm(nc, TL, xT, g_d, outT):
    import contextlib
    TT = 512
    with contextlib.ExitStack() as st:
        sb = lambda name, shape, dt=F32: st.enter_context(nc.sbuf_tensor(uname(name), shape, dt))
        pss = [st.enter_context(nc.psum_tensor(uname("nps%d" % i), [128, 512], F32)) for i in range(2)]
        _PH[0] += 1
        S = Sched(nc)
        ones_bf = sb("fn_ones", [128, 128], BF16); g_sb = sb("fn_g", [128, KC])
        xin = [sb("fn_x%d" % i, [128, KC, TT]) for i in range(2)]
        ho = [sb("fn_h%d" % i, [128, KC, TT]) for i in range(2)]
        sq = sb("fn_sq", [128, KC, TT], BF16); rstd = sb("fn_rstd", [128, TT])
        S.op("pool", lambda e: e.memset(ones_bf[:], 1.0), writes=["ones"])
        S.dma("sp", lambda e: e.dma_start(out=g_sb[:], in_=g_d), writes=["gcol"])
        xv = xT.rearrange("(c p) t -> p c t", p=128)
        ov = outT.rearrange("(c p) t -> p c t", p=128)
        for i, t0 in enumerate(range(0, TL, TT)):
            b = i % 2
            S.dma("sp", lambda e, t0=t0, b=b: e.dma_start(out=xin[b][:], in_=xv[:, :, t0:t0 + TT]), reads=["xT"], writes=[("fn_x", b)])
            emit_norm_T(S, nc, xin[b], ("fn_x", b), ho[b], ("fn_h", b), g_sb, ones_bf, sq, rstd, pss[0], ("ps", 0), TT)
            S.dma("act", lambda e, t0=t0, b=b: e.dma_start(out=ov[:, :, t0:t0 + TT], in_=ho[b][:]), reads=[("fn_h", b)], writes=["outT"])
        S.barrier_all()
        S.emit()


def build_dense_test(TL, d_ff, ncols_p):
    nc = bass.Bass("TRN2", target_bir_lowering=False)
    di = lambda n, s: nc.dram_tensor(n, s, F32, kind="ExternalInput").ap()
    xT = di("xT", [D_MODEL, TL]); g1 = di("g1", [128, KC]); g2 = di("g2", [128, KC]); gf = di("gf", [128, KC])
    w_in = di("w_in", [D_MODEL, ncols_p + 6144]); yT = di("yT", [1536, TL]); projw = di("projw", [1536, D_MODEL])
    wout = di("wout", [D_MODEL, D_MODEL]); wup = di("wup", [D_MODEL, 2 * d_ff]); conv = di("conv", [128, d_ff // 128, 3])
    wdown = di("wdown", [d_ff, D_MODEL])
    pT = nc.dram_tensor("pT", [ncols_p, TL], F32, kind="ExternalOutput").ap()
    x1T = nc.dram_tensor("x1T", [D_MODEL, TL], F32, kind="ExternalOutput").ap()
    x2T = nc.dram_tensor("x2T", [D_MODEL, TL], F32, kind="ExternalOutput").ap()
    outT = nc.dram_tensor("outT", [D_MODEL, TL], F32, kind="ExternalOutput").ap()
    phase_proj(nc, TL, xT, g1, w_in, pT, ncols_p)
    phase_merge(nc, TL, xT, g1, w_in, ncols_p, yT, projw, wout, x1T)
    phase_ffn(nc, TL, x1T, g2, wup, conv, wdown, x2T, d_ff)
    phase_final_norm(nc, TL, x2T, gf, outT)
    return nc


def build_full(TL, n_layers, d_ff, heads_a, heads_c, heads_b, final=True):
    nc = bass.Bass("TRN2", target_bir_lowering=False)
    di = lambda n, s: nc.dram_tensor(n, list(s), F32, kind="ExternalInput").ap()
    L = n_layers
    NHB = len(heads_b)
    xT = di("xT", [D_MODEL, TL])
    g1 = di("g1", [L, 128, KC]); g2 = di("g2", [L, 128, KC]); gf = di("gf", [128, KC])
    w_in = di("w_in", [L, D_MODEL, NP_ROWS + 3 * D_MODEL])
    projw = di("projw", [L, 1536, D_MODEL]); wout = di("w_out", [L, D_MODEL, D_MODEL])
    wup = di("ffn_up", [L, D_MODEL, 2 * d_ff]); conv = di("conv", [L, 128, d_ff // 128, 3]); wdown = di("ffn_down", [L, d_ff, D_MODEL])
    tabs_g = di("tabs_g", [20, 128, 256]); tabs_m = di("tabs_m", [20, 128, 256]); tabs_a = di("tabs_a", [20, 128, 256])
    sinks = di("sinks", [L, 64, 8]); ident = di("ident", [128, 128])
    prm = di("prm", [L, 64, NHB, 10]); lmu = di("lmu", [L, 128, 4])
    rwup = di("rw_up", [L, 96, NHB * 64]); raup = di("ra_up", [L, 96, NHB * 64]); rgup = di("rg_up", [L, 256, NHB * 64])
    mlt = di("m_lt2", [64, 3, 128]); mle = di("m_le2", [64, 3, 128]); mgt = di("m_gt", [64, 3, 64]); rst = di("rst", [64, 512])
    outT = nc.dram_tensor("outT", [D_MODEL, TL], F32, kind="ExternalOutput").ap()
    pT = nc.dram_tensor("pT_s", [NP_ROWS, TL], F32).ap()
    yT = nc.dram_tensor("yT_s", [1536, TL], F32).ap()
    x1T = nc.dram_tensor("x1T_s", [D_MODEL, TL], F32).ap()
    xs = [nc.dram_tensor("xs%d" % i, [D_MODEL, TL], F32).ap() for i in range(2)]
    NF = d_ff // 128
    sc_pj = nc.dram_tensor("sc_pj", [(NP_ROWS + 255) // 256, 128, KC * 256], BF16).ap()
    sc_mg = nc.dram_tensor("sc_mg", [KC, 128, KC * 3 * 128 + 12 * 128], BF16).ap()
    sc_wo = nc.dram_tensor("sc_wo", [KC, 128, KC * 128], BF16).ap()
    sc_up = nc.dram_tensor("sc_up", [NF, 128, KC * 256], BF16).ap()
    sc_dn = nc.dram_tensor("sc_dn", [KC, 128, NF * 128], BF16).ap()
    cur = xT
    for l in range(L):
        phase_proj(nc, TL, cur, g1[l], w_in[l], pT, NP_ROWS, scr=sc_pj)
        phase_attn(nc, TL, pT, yT, tabs_g, tabs_m, tabs_a, sinks[l], ident, heads_a, heads_c)
        phase_rwkv(nc, TL, pT, yT, prm[l], lmu[l], rwup[l], raup[l], rgup[l], ident, mlt, mle, mgt, rst, heads_b)
        phase_merge(nc, TL, cur, g1[l], w_in[l], NP_ROWS, yT, projw[l], wout[l], x1T, scr=sc_mg, scr_o=sc_wo)
        nxt = outT if (l == L - 1 and not final) else xs[l % 2]
        phase_ffn(nc, TL, x1T, g2[l], wup[l], conv[l], wdown[l], nxt, d_ff, scr_u=sc_up, scr_d=sc_dn)
        cur = nxt
    if final:
        phase_final_norm(nc, TL, cur, gf, outT)
    return nc


def phase_attn(nc, T, pT, yT, tg, tm, ta, sk, idd, heads_a, heads_c):
    import contextlib
    with contextlib.ExitStack() as st:
        pss = [st.enter_context(nc.psum_tensor(uname("aps%d" % i), [128, 512], F32)) for i in range(8)]
        _PH[0] += 1
        S = Sched(nc)
        emit_attention(S, nc, st, T, pT, yT, tg, tm, ta, sk, idd, pss, heads_a, heads_c)
        S.barrier_all()
        S.emit()


def host_inputs(inp, n_layers, d_ff, heads_b):
    L = n_layers
    f32 = lambda a: np.ascontiguousarray(np.asarray(a, dtype=np.float32))
    gl = lambda g: np.ascontiguousarray(np.asarray(g, np.float32).reshape(-1, KC, 128).transpose(0, 2, 1))
    tg, tm, ta = attn_tables(np.asarray(inp["rel_bias"], np.float32))
    d = dict(g1=gl(inp["norm1_g"][:L]), g2=gl(inp["norm2_g"][:L]), gf=gl(inp["final_g"])[0],
             w_in=f32(inp["w_in"][:L]),
             projw=f32(np.concatenate([np.asarray(inp["proj_a"][:L]), np.asarray(inp["proj_b"][:L]), np.asarray(inp["proj_c"][:L])], axis=1)),
             w_out=f32(inp["w_out"][:L]), ffn_up=f32(inp["ffn_up"][:L]), ffn_down=f32(inp["ffn_down"][:L]),
             conv=f32(np.asarray(inp["ffn_conv"][:L]).transpose(0, 2, 1).reshape(L, d_ff // 128, 128, 3).transpose(0, 2, 1, 3)),
             tabs_g=tg, tabs_m=tm, tabs_a=ta,
             sinks=f32(np.broadcast_to(np.asarray(inp["attn_sinks"][:L])[:, None, :], (L, 64, 8))),
             ident=np.eye(128, dtype=np.float32))
    prm, lmu, wu, au, gu = [], [], [], [], []
    for l in range(L):
        hp = rwkv_host_params(heads_b, *[np.asarray(inp[k][l], np.float32) for k in
                                         ("rwkv_mu", "rwkv_w0", "rwkv_w_up", "rwkv_a0", "rwkv_a_up", "rwkv_g_up", "rwkv_k_k",
                                          "rwkv_k_a", "rwkv_r_k", "rwkv_lnx_g", "rwkv_lnx_b")])
        prm.append(hp["prm"]); lmu.append(hp["lmu"]); wu.append(hp["wup"]); au.append(hp["aup"]); gu.append(hp["gup"])
    d.update(prm=np.stack(prm), lmu=np.stack(lmu), rw_up=np.stack(wu), ra_up=np.stack(au), rg_up=np.stack(gu))
    d.update(rwkv_consts())
    return d


_NC_CACHE = {}
N_LAUNCH = 1


def kernel(**inp):
    x = np.asarray(inp["x"], np.float32)
    Bn, Sq, Dm = x.shape
    L = np.asarray(inp["w_in"]).shape[0]
    d_ff = np.asarray(inp["ffn_down"]).shape[1]
    heads_a, heads_c, heads_b = list(range(8)), list(range(4)), list(range(12))
    nl = N_LAUNCH if L % N_LAUNCH == 0 else 1
    Lp = L // nl
    xTs = [np.ascontiguousarray(x[b].T) for b in range(Bn)]
    per_layer = ("norm1_g", "w_in", "attn_sinks", "rwkv_mu", "rwkv_w0", "rwkv_w_up", "rwkv_a0", "rwkv_a_up", "rwkv_g_up", "rwkv_k_k",
                 "rwkv_k_a", "rwkv_r_k", "rwkv_lnx_g", "rwkv_lnx_b", "proj_a", "proj_b", "proj_c", "w_out", "norm2_g", "ffn_up",
                 "ffn_conv", "ffn_down")
    for li in range(nl):
        final = (li == nl - 1)
        key = (Sq, Lp, d_ff, final)
        if key not in _NC_CACHE:
            _NC_CACHE[key] = build_full(Sq, Lp, d_ff, heads_a, heads_c, heads_b, final=final)
        nc = _NC_CACHE[key]
        sub = dict(inp)
        for k in per_layer:
            sub[k] = np.asarray(inp[k])[li * Lp:(li + 1) * Lp]
        shared = host_inputs(sub, Lp, d_ff, heads_b)
        in_maps = []
        for b in range(Bn):
            m = dict(shared)
            m["xT"] = xTs[b]
            in_maps.append(m)
        res = run_bass_kernel_spmd(nc, in_maps, core_ids=list(range(Bn)))
        xTs = [np.ascontiguousarray(r["outT"]) for r in res.results]
    out = np.stack([np.ascontiguousarray(t.T) for t in xTs], axis=0)
    return out.astype(np.float32)
```

```python
import concourse.bass as bass
import concourse.mybir as mybir

ENGS = ("pe", "dve", "act", "pool", "sp")


_PH = [0]


def uname(n):
    return "%s_%d" % (n, _PH[0])


class Sched:
    _uid = 0

    def __init__(self, nc, n_dma_sems=24):
        self.nc = nc
        self.streams = {e: [] for e in ENGS}
        self.seq = {e: 0 for e in ENGS}
        self.waited = {}
        self.lastw = {}
        self.readers = {}
        self.n_dma = n_dma_sems
        self.dma_cnt = [0] * n_dma_sems
        self.dma_rr = 0
        self.sems = {}
        self.n_wait = 0
        self.alias = {}
        self.hist = {e: [] for e in ENGS}
        self.dma_snap = {}

    def _clock(self, e):
        return {p: v for (c, p), v in self.waited.items() if c == e}

    def _merge(self, cons, snap):
        for p, v in snap.items():
            if p == ("eng", cons):
                continue
            if self.waited.get((cons, p), 0) < v:
                self.waited[(cons, p)] = v

    def _absorb(self, cons, waits):
        import bisect
        for p, v in waits:
            if p[0] == "eng":
                h = self.hist[p[1]]
                if h:
                    i = bisect.bisect_right(h, v, key=lambda t: t[0]) - 1
                    if i >= 0:
                        self._merge(cons, h[i][1])
            else:
                snap = self.dma_snap.get((p[1], v))
                if snap:
                    self._merge(cons, snap)

    def canon(self, keys):
        return [self.alias.get(k, k) for k in keys]

    def eng(self, e):
        nc = self.nc
        return {"pe": nc.tensor, "dve": nc.vector, "act": nc.scalar, "pool": nc.gpsimd, "sp": nc.sync}[e]

    def _need(self, cons, deps):
        best = {}
        for p, v in deps:
            if p is None:
                continue
            if v > best.get(p, 0):
                best[p] = v
        out = []
        for p, v in sorted(best.items(), key=lambda kv: (kv[0][0] != "eng", -kv[1])):
            if self.waited.get((cons, p), 0) >= v:
                continue
            self.waited[(cons, p)] = v
            out.append((p, v))
            self._absorb(cons, [(p, v)])
        return out

    def _deps(self, e, reads, writes, same_engine_war=False):
        deps = []
        me = ("eng", e)
        for k in reads:
            w = self.lastw.get(k)
            if w is not None:
                deps.append(w)
        for k in writes:
            w = self.lastw.get(k)
            if w is not None:
                deps.append(w)
            for p, v in self.readers.get(k, {}).items():
                deps.append((p, v))
        return deps

    def _record(self, prod, val, reads, writes):
        for k in reads:
            d = self.readers.setdefault(k, {})
            if d.get(prod, 0) < val:
                d[prod] = val
        for k in writes:
            self.lastw[k] = (prod, val)
            self.readers[k] = {}

    def op(self, e, fn, reads=(), writes=()):
        reads = self.canon(reads); writes = self.canon(writes)
        deps = self._deps(e, reads, writes)
        if e == "pe":
            deps = [d for d in deps if d[0] != ("eng", "pe")]
        waits = self._need(e, deps)
        self.seq[e] += 1
        val = self.seq[e]
        if waits or not self.hist[e]:
            self.hist[e].append((val, self._clock(e)))
        self.streams[e].append(("op", fn, waits, None))
        self._record(("eng", e), val, reads, writes)
        return val

    def dma(self, q, fn, reads=(), writes=()):
        reads = self.canon(reads); writes = self.canon(writes)
        deps = self._deps(q, reads, writes, same_engine_war=True)
        i = self.dma_rr
        self.dma_rr = (self.dma_rr + 1) % self.n_dma
        prod = ("dma", i)
        if self.dma_cnt[i] > 0:
            deps.append((prod, self.dma_cnt[i]))
        waits = self._need(q, deps)
        self.dma_cnt[i] += 16
        val = self.dma_cnt[i]
        snap = self._clock(q)
        if q != "sp" and self.seq[q] > 0:
            snap[("eng", q)] = max(snap.get(("eng", q), 0), 0)
        self.dma_snap[(i, val)] = snap
        self.streams[q].append(("dma", fn, waits, (i, 16)))
        self._record(prod, val, reads, writes)
        return prod, val

    def finish_waits(self, e="sp"):
        deps = [(("dma", i), c) for i, c in enumerate(self.dma_cnt) if c > 0]
        deps += [(("eng", x), self.seq[x]) for x in ENGS if self.seq[x] > 0 and x != e]
        waits = self._need(e, deps)
        self.streams[e].append(("wait", None, waits, None))

    def barrier_all(self):
        for e in ENGS:
            deps = [(("dma", i), c) for i, c in enumerate(self.dma_cnt) if c > 0]
            deps += [(("eng", x), self.seq[x]) for x in ENGS if self.seq[x] > 0 and x != e]
            waits = self._need(e, deps)
            self.streams[e].append(("wait", None, waits, None))

    def emit(self):
        nc = self.nc
        Sched._uid += 1
        u = Sched._uid
        esem = {e: nc.alloc_semaphore("s%d_%s" % (u, e)) for e in ENGS}
        dsem = [nc.alloc_semaphore("d%d_%d" % (u, i)) for i in range(self.n_dma)]

        def semof(p):
            return esem[p[1]] if p[0] == "eng" else dsem[p[1]]

        def run(e):
            def body(engine):
                for kind, fn, waits, dinfo in self.streams[e]:
                    for p, v in waits:
                        engine.wait_ge(semof(p), v)
                        self.n_wait += 1
                    if kind == "op":
                        fn(engine).then_inc(esem[e], 1)
                    elif kind == "dma":
                        fn(engine).then_inc(dsem[dinfo[0]], dinfo[1])
            return body

        with nc.Block() as block:
            block.tensor(run("pe"))
            block.vector(run("dve"))
            block.scalar(run("act"))
            block.gpsimd(run("pool"))
            block.sync(run("sp"))
        nc.all_engine_barrier()
        nc.clear_and_free_semaphores(list(esem.values()) + dsem)
        nc.all_engine_barrier()


import numpy as np
from concourse.bass_utils import run_bass_kernel_spmd

F32 = mybir.dt.float32
BF16 = mybir.dt.bfloat16
ALU = mybir.AluOpType
AF = mybir.ActivationFunctionType
AX = mybir.AxisListType

D_MODEL = 2048
NORM_EPS = 1e-5
KC = D_MODEL // 128


def emit_rmsnorm_T(S, nc, xT, hT, g_sb, ones_bf, sq, ps, rstd, ntok, keys):
    kx, kh, ksq, kps, krs = keys["x"], keys["h"], keys["sq"], keys["ps"], keys["rstd"]
    for c in range(KC):
        S.op("act", lambda e, c=c: e.activation(out=sq[:, c, :], in_=xT[:, c, :], func=AF.Square),
             reads=[kx], writes=[(ksq, c)])
    for c in range(KC):
        S.op("pe", lambda e, c=c: e.matmul(ps, ones_bf, sq[:, c, :], start=(c == 0), stop=(c == KC - 1)),
             reads=[(ksq, c)], writes=[kps])
    S.op("dve", lambda e: e.tensor_scalar(out=rstd, in0=ps, scalar1=1.0 / D_MODEL, scalar2=NORM_EPS,
                                          op0=ALU.mult, op1=ALU.add), reads=[kps], writes=[krs])
    S.op("act", lambda e: e.activation(out=rstd, in_=rstd, func=AF.Sqrt), reads=[krs], writes=[krs])
    S.op("dve", lambda e: e.reciprocal(out=rstd, in_=rstd), reads=[krs], writes=[krs])
    for c in range(KC):
        S.op("dve", lambda e, c=c: e.scalar_tensor_tensor(out=hT[:, c, :], in0=xT[:, c, :], scalar=g_sb[:, c:c + 1],
                                                         in1=rstd, op0=ALU.mult, op1=ALU.mult),
             reads=[kx, krs], writes=[kh])


def build_proj(T, ncols, TT=512):
    nc = bass.Bass("TRN2", target_bir_lowering=False)
    xT = nc.dram_tensor("xT", [D_MODEL, T], F32, kind="ExternalInput").ap()
    g = nc.dram_tensor("g", [128, KC], F32, kind="ExternalInput").ap()
    W = nc.dram_tensor("W", [D_MODEL, ncols], F32, kind="ExternalInput").ap()
    pT = nc.dram_tensor("pT", [ncols, T], F32, kind="ExternalOutput").ap()
    ntt = T // TT
    CB = 512
    ncb = (ncols + CB - 1) // CB
    import contextlib
    with contextlib.ExitStack() as st:
        sb = lambda name, shape, dt: st.enter_context(nc.sbuf_tensor(uname(name), shape, dt))
        ones_bf = sb("ones", [128, 128], BF16)
        g_sb = sb("g_sb", [128, KC], F32)
        hT = sb("hT", [128, KC, T], BF16)
        xin = [sb("xin%d" % i, [128, KC, TT], F32) for i in range(2)]
        sq = sb("sq", [128, KC, TT], BF16)
        rstd = sb("rstd", [128, TT], F32)
        wt = [sb("wt%d" % i, [128, KC, CB], BF16) for i in range(2)]
        ot = [sb("ot%d" % i, [128, TT], F32) for i in range(4)]
        pss = [st.enter_context(nc.psum_tensor(uname("ps%d" % i), [128, 512], F32)) for i in range(8)]
        _PH[0] += 1
        S = Sched(nc)
        S.op("pool", lambda e: e.memset(ones_bf[:], 1.0), writes=["ones"])
        S.dma("sp", lambda e: e.dma_start(out=g_sb[:], in_=g), writes=["g"])
        xTv = xT.rearrange("(c p) t -> p c t", p=128)
        Wv = W.rearrange("(c p) n -> p c n", p=128)
        for tt in range(ntt):
            xb = xin[tt % 2]
            S.dma("sp", lambda e, xb=xb, tt=tt: e.dma_start(out=xb[:], in_=xTv[:, :, tt * TT:(tt + 1) * TT]),
                  writes=[("xin", tt % 2)])
            keys = dict(x=("xin", tt % 2), h=("h", tt), sq="sq", ps=("ps", 0), rstd="rstd")
            emit_rmsnorm_T(S, nc, xb[:], hT[:, :, tt * TT:(tt + 1) * TT], g_sb[:], ones_bf[:], sq[:], pss[0][:, :TT],
                           rstd[:], TT, keys)
        n_o = 0
        n_ps = 0
        for cb in range(ncb):
            c0 = cb * CB
            cw = min(CB, ncols - c0)
            wb = wt[cb % 2]
            S.dma("pool", lambda e, wb=wb, c0=c0, cw=cw: e.dma_start(out=wb[:, :, :cw], in_=Wv[:, :, c0:c0 + cw]),
                  writes=[("wt", cb % 2)])
            for m0 in range(0, cw, 128):
                mw = min(128, cw - m0)
                for tt in range(ntt):
                    pi = 1 + (n_ps % 7); n_ps += 1
                    ps = pss[pi]
                    for c in range(KC):
                        S.op("pe", lambda e, ps=ps, wb=wb, c=c, m0=m0, mw=mw, tt=tt:
                             e.matmul(ps[:mw, :TT], wb[:, c, m0:m0 + mw], hT[:, c, tt * TT:(tt + 1) * TT],
                                      start=(c == 0), stop=(c == KC - 1)),
                             reads=[("wt", cb % 2), ("h", tt), "ones", "g"], writes=[("ps", pi)])
                    oi = n_o % 4; n_o += 1
                    ob = ot[oi]
                    eng = "act" if (n_o % 2) else "dve"
                    if eng == "act":
                        S.op("act", lambda e, ob=ob, ps=ps, mw=mw: e.copy(out=ob[:mw, :], in_=ps[:mw, :TT]),
                             reads=[("ps", pi)], writes=[("ot", oi)])
                    else:
                        S.op("dve", lambda e, ob=ob, ps=ps, mw=mw: e.tensor_copy(out=ob[:mw, :], in_=ps[:mw, :TT]),
                             reads=[("ps", pi)], writes=[("ot", oi)])
                    S.dma("sp", lambda e, ob=ob, mw=mw, r0=c0 + m0, tt=tt:
                          e.dma_start(out=pT[r0:r0 + mw, tt * TT:(tt + 1) * TT], in_=ob[:mw, :]),
                          reads=[("ot", oi)])
        S.finish_waits("sp")
        S.emit()
    return nc


import math

ROW_AQ, ROW_AK, ROW_AV = 0, 512, 640
ROW_BR, ROW_BK, ROW_BV, ROW_WD, ROW_AD, ROW_GD = 768, 1536, 2304, 3072, 3168, 3264
ROW_CQ, ROW_CK, ROW_CV = 3520, 4288, 5056
NP_ROWS = 5824
YROW_A, YROW_B, YROW_C = 0, 512, 1280
C_DILS = (1, 4, 16)


def ssl(c0, n, d):
    return slice(c0, c0 + d * (n - 1) + 1, d)


def t5_bucket_np(dist):
    dist = np.asarray(dist, np.int64)
    nf = np.maximum(dist, 1).astype(np.float32)
    large = 16 + (np.log(nf / np.float32(16)) / np.float32(math.log(2048 / 16)) * np.float32(16)).astype(np.int32)
    return np.where(dist < 16, dist, np.minimum(large, 31))


def attn_tables(rel_bias):
    i = np.arange(128)[None, :]
    j = np.arange(128)[:, None]
    dists = (i - j, i + 128 - j)
    specs = [(h, 1, 127) for h in range(8)] + [(8 + g * 4 + hh, dil, 128) for g, dil in enumerate(C_DILS)
                                              for hh in range(4)]
    gathered = np.zeros((20, 128, 256), np.float32)
    mul = np.zeros((20, 128, 256), np.float32)
    add = np.zeros((20, 128, 256), np.float32)
    for n, (col, dil, ms) in enumerate(specs):
        for half, d in enumerate(dists):
            valid = (d >= 0) & (d <= ms)
            idx = t5_bucket_np(np.maximum(d, 0) * dil)
            gathered[n, :, half * 128:(half + 1) * 128] = rel_bias[idx, col]
            mul[n, :, half * 128:(half + 1) * 128] = np.where(valid, 8.0, 0.0)
            add[n, :, half * 128:(half + 1) * 128] = np.where(valid, 0.0, -240000.0)
    return gathered, mul, add


def emit_attention(S, nc, st, T, pT, yT, tabs_g, tabs_m, tabs_a, sinks_rep, ident_d, pss, heads_a, heads_c, between=None):
    sb = lambda name, shape, dt: st.enter_context(nc.sbuf_tensor(uname(name), shape, dt))
    NB = T // 128
    q_bf = sb("at_q", [64, T], BF16)
    k_bf = sb("at_k", [64, T], BF16)
    v_f = sb("at_v", [64, T], F32)
    stg = sb("at_stg", [64, T], F32)
    vaug = sb("at_vaug", [128, NB, 65], BF16)
    acc = sb("at_acc", [65, T], F32)
    tb_g = sb("at_tbg", [128, 256], F32)
    tb_m = sb("at_tbm", [128, 256], F32)
    tb_a = sb("at_tba", [128, 256], F32)
    tb = sb("at_tb", [128, 256], BF16)
    pt_sb = [sb("at_pt%d" % i, [128, 512], BF16) for i in range(2)]
    ident_f = sb("at_identf", [128, 128], F32)
    ident_b = sb("at_identb", [128, 128], BF16)
    sel = sb("at_sel", [65, 64], F32)
    esink = sb("at_esink", [64, 8], F32)
    den = sb("at_den", [64, 512], F32)
    yo = [sb("at_yo%d" % i, [64, 512], F32) for i in range(2)]

    S.dma("sp", lambda e: e.dma_start(out=ident_f[:], in_=ident_d), writes=["at_identf"])
    S.op("dve", lambda e: e.tensor_copy(out=ident_b[:], in_=ident_f[:]), reads=["at_identf"], writes=["at_identb"])
    S.op("pool", lambda e: e.memset(sel[:], 0.0), writes=["at_sel"])
    S.op("pool", lambda e: e.memset(sel[64:65, :], 1.0), writes=["at_sel"])
    S.op("pool", lambda e: e.memset(vaug[:, :, 64:65], 1.0), writes=["at_vaug1"])
    S.dma("sp", lambda e: e.dma_start(out=esink[:], in_=sinks_rep), writes=["at_esink"])
    S.op("act", lambda e: e.activation(out=esink[:], in_=esink[:], func=AF.Exp), reads=["at_esink"],
         writes=["at_esink"])

    ps_s = [pss[0], pss[1]]
    ps_o = [pss[2], pss[3]]
    ps_t = pss[4]
    ps_d = pss[5]
    cnt = dict(s=0, o=0, y=0)

    def load_table(n):
        S.dma("sp", lambda e: e.dma_start(out=tb_g[:], in_=tabs_g[n]), writes=["at_tbg"])
        S.dma("sp", lambda e: e.dma_start(out=tb_m[:], in_=tabs_m[n]), writes=["at_tbm"])
        S.dma("sp", lambda e: e.dma_start(out=tb_a[:], in_=tabs_a[n]), writes=["at_tba"])
        S.op("pool", lambda e: e.tensor_tensor(out=tb_g[:], in0=tb_g[:], in1=tb_m[:], op=ALU.mult),
             reads=["at_tbg", "at_tbm"], writes=["at_tbg"])
        S.op("pool", lambda e: e.tensor_tensor(out=tb[:], in0=tb_g[:], in1=tb_a[:], op=ALU.add),
             reads=["at_tbg", "at_tba"], writes=["at_tb"])

    def load_kv(krow, vrow, dil):
        S.dma("act", lambda e: e.dma_start(out=stg[:], in_=pT[krow:krow + 64, :]), reads=["pT"], writes=["at_stg"])
        S.op("pool", lambda e: e.tensor_copy(out=k_bf[:], in_=stg[:]), reads=["at_stg"], writes=["at_k"])
        S.dma("sp", lambda e: e.dma_start(out=v_f[:], in_=pT[vrow:vrow + 64, :]), reads=["pT"], writes=["at_v"])
        Lf = T // dil
        bps = Lf // 128
        for vb0 in range(0, NB, 8):
            for u in range(8):
                vb = vb0 + u
                s_, jb = vb // bps, vb % bps
                c0 = s_ + dil * 128 * jb
                src = v_f[0:64, ssl(c0, 128, dil)]
                S.op("pe", lambda e, u=u, src=src: e.transpose(ps_t[:, u * 64:(u + 1) * 64], src, ident_f[0:64, 0:64]),
                     reads=["at_v", "at_identf"], writes=["ps_t"])
            S.op("dve", lambda e, vb0=vb0: e.tensor_copy(out=vaug[:, vb0:vb0 + 8, 0:64],
                                                        in_=ps_t[:, :].rearrange("p (u d) -> p u d", d=64)),
                 reads=["ps_t"], writes=["at_vaug"])

    def run_seq(dil, first_group):
        Lf = T // dil
        bps = Lf // 128
        nbt = min(4, bps)
        for s_ in range(dil):
            for jb0 in range(0, bps, nbt):
                oi = cnt["o"] % 2; cnt["o"] += 1
                po = ps_o[oi]
                for half in range(0, nbt, 2):
                    si = cnt["s"] % 2; cnt["s"] += 1
                    pst = ps_s[si]
                    ptb = pt_sb[si]
                    nb2 = min(2, nbt - half)
                    lo = 512
                    for r in range(nb2):
                        jb = jb0 + half + r
                        qs = s_ + dil * 128 * jb
                        qv = q_bf[:, ssl(qs, 128, dil)]
                        kv = k_bf[:, ssl(qs, 128, dil)]
                        sl_prev = slice((2 * r) * 128, (2 * r + 1) * 128)
                        sl_cur = slice((2 * r + 1) * 128, (2 * r + 2) * 128)
                        if jb > 0:
                            ks = s_ + dil * 128 * (jb - 1)
                            kpv = k_bf[:, ssl(ks, 128, dil)]
                            S.op("pe", lambda e, pst=pst, sl=sl_prev, kpv=kpv, qv=qv:
                                 e.matmul(pst[:, sl], kpv, qv, start=True, stop=False),
                                 reads=["at_k", "at_q"], writes=[("ps_s", si)])
                            S.op("pe", lambda e, pst=pst, sl=sl_prev:
                                 e.matmul(pst[:, sl], ident_b[:], tb[:, 128:256], start=False, stop=True),
                                 reads=["at_tb", "at_identb"], writes=[("ps_s", si)])
                            lo = min(lo, sl_prev.start)
                        S.op("pe", lambda e, pst=pst, sl=sl_cur, kv=kv, qv=qv:
                             e.matmul(pst[:, sl], kv, qv, start=True, stop=False),
                             reads=["at_k", "at_q"], writes=[("ps_s", si)])
                        S.op("pe", lambda e, pst=pst, sl=sl_cur:
                             e.matmul(pst[:, sl], ident_b[:], tb[:, 0:128], start=False, stop=True),
                             reads=["at_tb", "at_identb"], writes=[("ps_s", si)])
                        lo = min(lo, sl_cur.start)
                    hi = nb2 * 256
                    S.op("act", lambda e, ptb=ptb, pst=pst, lo=lo, hi=hi:
                         e.activation(out=ptb[:, lo:hi], in_=pst[:, lo:hi], func=AF.Exp, scale=0.125),
                         reads=[("ps_s", si)], writes=[("at_pt", si)])
                    for r in range(nb2):
                        jb = jb0 + half + r
                        vb = s_ * bps + jb
                        osl = slice((half + r) * 128, (half + r + 1) * 128)
                        S.op("pe", lambda e, po=po, osl=osl, vb=vb, ptb=ptb, r=r, last=(jb == 0):
                             e.matmul(po[0:65, osl], vaug[:, vb, :], ptb[:, (2 * r + 1) * 128:(2 * r + 2) * 128],
                                      start=True, stop=last),
                             reads=["at_vaug", "at_vaug1", ("at_pt", si)], writes=[("ps_o", oi)])
                        if jb > 0:
                            S.op("pe", lambda e, po=po, osl=osl, vb=vb, ptb=ptb, r=r:
                                 e.matmul(po[0:65, osl], vaug[:, vb - 1, :], ptb[:, (2 * r) * 128:(2 * r + 1) * 128],
                                          start=False, stop=True),
                                 reads=["at_vaug", "at_vaug1", ("at_pt", si)], writes=[("ps_o", oi)])
                t0 = s_ + dil * 128 * jb0
                n_el = nbt * 128
                av = acc[:, ssl(t0, n_el, dil)]
                if first_group:
                    S.op("dve", lambda e, av=av, po=po, n_el=n_el: e.tensor_copy(out=av, in_=po[0:65, 0:n_el]),
                         reads=[("ps_o", oi)], writes=["at_acc"])
                else:
                    S.op("dve", lambda e, av=av, po=po, n_el=n_el:
                         e.tensor_tensor(out=av, in0=po[0:65, 0:n_el], in1=av, op=ALU.add),
                         reads=[("ps_o", oi), "at_acc"], writes=["at_acc"])

    def normalize(yrow, sink_col):
        for t0 in range(0, T, 512):
            S.op("pe", lambda e, t0=t0: e.matmul(ps_d[0:64, :], sel[:], acc[:, t0:t0 + 512], start=True, stop=True),
                 reads=["at_acc", "at_sel"], writes=["ps_d"])
            if sink_col is not None:
                S.op("dve", lambda e: e.tensor_scalar(out=den[:], in0=ps_d[0:64, :],
                                                      scalar1=esink[:, sink_col:sink_col + 1], scalar2=None,
                                                      op0=ALU.add),
                     reads=["ps_d", "at_esink"], writes=["at_den"])
                S.op("dve", lambda e: e.reciprocal(out=den[:], in_=den[:]), reads=["at_den"], writes=["at_den"])
            else:
                S.op("dve", lambda e: e.reciprocal(out=den[:], in_=ps_d[0:64, :]), reads=["ps_d"], writes=["at_den"])
            yi = cnt["y"] % 2; cnt["y"] += 1
            yb = yo[yi]
            S.op("dve", lambda e, yb=yb, t0=t0: e.tensor_tensor(out=yb[:], in0=acc[0:64, t0:t0 + 512], in1=den[:],
                                                                op=ALU.mult),
                 reads=["at_acc", "at_den"], writes=[("at_yo", yi)])
            S.dma("sp", lambda e, yb=yb, t0=t0: e.dma_start(out=yT[yrow:yrow + 64, t0:t0 + 512], in_=yb[:]),
                  reads=[("at_yo", yi)], writes=["yT"])

    last_kv = None
    for h in heads_a:
        kvh = h // 4
        if last_kv != kvh:
            load_kv(ROW_AK + 64 * kvh, ROW_AV + 64 * kvh, 1)
            last_kv = kvh
        S.dma("act", lambda e, h=h: e.dma_start(out=stg[:], in_=pT[ROW_AQ + 64 * h:ROW_AQ + 64 * h + 64, :]),
              reads=["pT"], writes=["at_stg"])
        S.op("pool", lambda e: e.tensor_copy(out=q_bf[:], in_=stg[:]), reads=["at_stg"], writes=["at_q"])
        load_table(h)
        run_seq(1, True)
        normalize(YROW_A + 64 * h, h)
        if between is not None:
            between()
    for hh in heads_c:
        for g, dil in enumerate(C_DILS):
            off = g * 256 + hh * 64
            load_kv(ROW_CK + off, ROW_CV + off, dil)
            S.dma("act", lambda e, off=off: e.dma_start(out=stg[:], in_=pT[ROW_CQ + off:ROW_CQ + off + 64, :]),
                  reads=["pT"], writes=["at_stg"])
            S.op("pool", lambda e: e.tensor_copy(out=q_bf[:], in_=stg[:]), reads=["at_stg"], writes=["at_q"])
            load_table(8 + g * 4 + hh)
            run_seq(dil, g == 0)
            if between is not None:
                between()
        normalize(YROW_C + 64 * hh, None)


def build_attn_test(T, heads_a, heads_c):
    nc = bass.Bass("TRN2", target_bir_lowering=False)
    pT = nc.dram_tensor("pT", [NP_ROWS, T], F32, kind="ExternalInput").ap()
    tg = nc.dram_tensor("tabs_g", [20, 128, 256], F32, kind="ExternalInput").ap()
    tm = nc.dram_tensor("tabs_m", [20, 128, 256], F32, kind="ExternalInput").ap()
    ta = nc.dram_tensor("tabs_a", [20, 128, 256], F32, kind="ExternalInput").ap()
    sk = nc.dram_tensor("sinks", [64, 8], F32, kind="ExternalInput").ap()
    idd = nc.dram_tensor("ident", [128, 128], F32, kind="ExternalInput").ap()
    yT = nc.dram_tensor("yT", [1536, T], F32, kind="ExternalOutput").ap()
    import contextlib
    with contextlib.ExitStack() as st:
        pss = [st.enter_context(nc.psum_tensor(uname("ps%d" % i), [128, 512], F32)) for i in range(8)]
        _PH[0] += 1
        S = Sched(nc)
        emit_attention(S, nc, st, T, pT, yT, tg, tm, ta, sk, idd, pss, heads_a, heads_c)
        S.finish_waits("sp")
        S.emit()
    return nc


CH = 64
B_GN_EPS = 64e-5


def rwkv_consts():
    s_ = np.arange(64)[:, None]; t_ = np.arange(64)[None, :]
    lt = (s_ < t_).astype(np.float32); le = (s_ <= t_).astype(np.float32); gt = (s_ > t_).astype(np.float32)
    m_lt2 = np.ascontiguousarray(np.broadcast_to(np.concatenate([lt, lt], 1)[:, None, :], (64, 3, 128)))
    m_le2 = np.ascontiguousarray(np.broadcast_to(np.concatenate([le, le], 1)[:, None, :], (64, 3, 128)))
    m_gt = np.ascontiguousarray(np.broadcast_to(gt[:, None, :], (64, 3, 64)))
    rst = np.ones((64, 512), np.float32); rst[:, ::64] = 0.0
    return dict(m_lt2=m_lt2, m_le2=m_le2, m_gt=m_gt, rst=rst)


def phase_rwkv(nc, T, pT, yT, prm_d, lmu_d, wup_d, aup_d, gup_d, ident_d, mlt_d, mle_d, mgt_d, rst_d, heads, dbg=None, stop=None):
    import contextlib
    HG = 3
    TT = 512
    NCK = TT // CH
    NH = len(heads)
    with contextlib.ExitStack() as st:
        sb = lambda name, shape, dt=F32: st.enter_context(nc.sbuf_tensor(uname(name), shape, dt))
        pss = [st.enter_context(nc.psum_tensor(uname("rps%d" % i), [128, 512], F32)) for i in range(8)]
        _PH[0] += 1
        S = Sched(nc)
        ident = sb("rw_ident", [128, 128]); m_lt2 = sb("rw_mlt", [64, 3, 128]); m_le2 = sb("rw_mle", [64, 3, 128])
        m_gt = sb("rw_mgt", [64, 3, 64]); rst = sb("rw_rst", [64, 512])
        prm = sb("rw_prm", [64, NH, 10]); lmu = sb("rw_lmu", [128, 4])
        wup = sb("rw_wup", [96, NH * 64]); aup = sb("rw_aup", [96, NH * 64]); gup = sb("rw_gup", [128, 2, NH * 64])
        ones64 = sb("rw_ones", [64, 64]); avg64 = sb("rw_avg", [64, 64]); rkb = sb("rw_rkb", [64, NH, 64])
        for t_, d_ in ((ident, ident_d), (m_lt2, mlt_d), (m_le2, mle_d), (m_gt, mgt_d), (rst, rst_d), (prm, prm_d),
                       (lmu, lmu_d), (wup, wup_d), (aup, aup_d)):
            S.dma("sp", lambda e, t_=t_, d_=d_: e.dma_start(out=t_[:], in_=d_), writes=["const"])
        S.dma("sp", lambda e: e.dma_start(out=gup[:], in_=gup_d.rearrange("(c p) n -> p c n", p=128)), writes=["const"])
        S.op("pool", lambda e: e.memset(ones64[:], 1.0), writes=["const"])
        S.op("pool", lambda e: e.memset(avg64[:], 1.0 / 64), writes=["const"])
        for hi in range(NH):
            S.op("dve", lambda e, hi=hi: e.tensor_scalar(out=rkb[:, hi, :], in0=ones64[:], scalar1=prm[:, hi, 7:8],
                                                        scalar2=None, op0=ALU.mult), reads=["const"], writes=["const"])
        lin = [sb("rw_lin%d" % i, [128, TT + 1]) for i in range(4)]
        ltmp = sb("rw_ltmp", [128, TT])
        th = sb("rw_th", [96, TT]); adm = sb("rw_adm", [96, TT]); sg = sb("rw_sg", [128, 2, TT])
        xin = [sb("rw_xin%d" % i, [64, HG, TT + 1]) for i in range(3)]
        names = ["rm", "km", "vm", "logw", "iclr", "g", "kkn", "k2", "sbon", "Lc", "G", "t0", "t1", "t2",
                 "at"]
        B = {n: sb("rw_" + n, [64, HG, TT]) for n in names}
        for n in ("rt", "bt", "kt", "atb"):
            B[n] = sb("rw_" + n, [64, HG, TT], BF16)
        B["bh"] = B["iclr"]; B["kh"] = B["kkn"]; B["y"] = B["logw"]
        S.alias.update({"bh": "iclr", "kh": "kkn", "y": "logw", ("yo", 0): "Lc", ("yo", 1): "Lc"})
        RhT = sb("rw_RhT", [64, NCK, HG, 64]); Y0T = sb("rw_Y0T", [64, NCK, HG, 64])
        MTa = sb("rw_MT", [64, NCK, HG, 64]); Na = sb("rw_N", [64, NCK, HG, 64])
        Wp = [sb("rw_W%d" % p, [64, HG, 128], BF16) for p in range(2)]; Vtokp = [sb("rw_Vtok%d" % p, [64, HG, 64], BF16) for p in range(2)]
        BKp = [sb("rw_BK%d" % p, [64, HG, 128], BF16) for p in range(2)]; AQp = [sb("rw_AQ%d" % p, [64, HG, 128], BF16) for p in range(2)]
        QPp = [[sb("rw_QP%d_%d" % (p, i), [64, HG, 128], BF16) for i in range(2)] for p in range(2)]
        ARKp = [sb("rw_ARK%d" % p, [64, HG, 128], BF16) for p in range(2)]
        Hs = [sb("rw_H%d" % i, [64, HG, 64]) for i in range(2)]
        yout = [B["Lc"], B["Lc"]]
        cnt = dict(l=0, m=0, y=0)

        def ps_l():
            i = cnt["l"] % 2; cnt["l"] += 1
            return pss[i], ("ps", i)

        def ps_m():
            i = 4 + cnt["m"] % 2; cnt["m"] += 1
            return pss[i], ("ps", i)

        def dve(fn, r, w): S.op("dve", fn, reads=r, writes=w)
        def act(fn, r, w): S.op("act", fn, reads=r, writes=w)
        def pool(fn, r, w): S.op("pool", fn, reads=r, writes=w)
        def pe(fn, r, w): S.op("pe", fn, reads=r, writes=w)

        for g0 in range(0, NH, HG):
            hs = heads[g0:g0 + HG]
            pool(lambda e: e.memset(Hs[0][:], 0.0), [], ["H0"])
            st_h = dict(hcur=0)

            def do_tile(ti, g0=g0, hs=hs, st_h=st_h):
                t0 = ti * TT
                srcs = [(ROW_WD, 96), (ROW_AD, 96), (ROW_GD, 128), (ROW_GD + 128, 128)]
                for i, (row, n) in enumerate(srcs):
                    if t0 == 0:
                        pool(lambda e, i=i, n=n: e.memset(lin[i][0:n, 0:1], 0.0), [], ["lin%d" % i])
                        S.dma("sp", lambda e, i=i, row=row, n=n: e.dma_start(out=lin[i][0:n, 1:TT + 1],
                                                                          in_=pT[row:row + n, 0:TT]),
                              reads=["pT"], writes=["lin%d" % i])
                    else:
                        S.dma("sp", lambda e, i=i, row=row, n=n: e.dma_start(out=lin[i][0:n, :],
                                                                          in_=pT[row:row + n, t0 - 1:t0 + TT]),
                              reads=["pT"], writes=["lin%d" % i])
                for i, row0 in enumerate((ROW_BR, ROW_BK, ROW_BV)):
                    for j, h in enumerate(hs):
                        row = row0 + 64 * h
                        if t0 == 0:
                            pool(lambda e, i=i, j=j: e.memset(xin[i][:, j, 0:1], 0.0), [], ["xin%d" % i])
                            S.dma("act", lambda e, i=i, j=j, row=row: e.dma_start(out=xin[i][:, j, 1:TT + 1],
                                                                              in_=pT[row:row + 64, 0:TT]),
                                  reads=["pT"], writes=["xin%d" % i])
                        else:
                            S.dma("act", lambda e, i=i, j=j, row=row: e.dma_start(out=xin[i][:, j, :],
                                                                              in_=pT[row:row + 64, t0 - 1:t0 + TT]),
                                  reads=["pT"], writes=["xin%d" % i])
                outs = [th, adm, sg[:, 0, :], sg[:, 1, :]]
                for i, (row, n) in enumerate(srcs):
                    pool(lambda e, i=i, n=n: e.tensor_tensor(out=ltmp[0:n, :], in0=lin[i][0:n, 0:TT], in1=lin[i][0:n, 1:TT + 1],
                                                           op=ALU.subtract), ["lin%d" % i], ["ltmp"])
                    o = outs[i]
                    dve(lambda e, i=i, n=n, o=o: e.scalar_tensor_tensor(out=o[0:n, :] if i < 2 else o, in0=ltmp[0:n, :],
                                                                       scalar=lmu[0:n, i:i + 1], in1=lin[i][0:n, 1:TT + 1],
                                                                       op0=ALU.mult, op1=ALU.add),
                        ["ltmp", "lin%d" % i, "const"], ["lo%d" % i])
                act(lambda e: e.activation(out=th[:], in_=th[:], func=AF.Tanh), ["lo0"], ["lo0"])
                act(lambda e: e.activation(out=sg[:, 0, :], in_=sg[:, 0, :], func=AF.Sigmoid), ["lo2"], ["lo2"])
                act(lambda e: e.activation(out=sg[:, 1, :], in_=sg[:, 1, :], func=AF.Sigmoid), ["lo3"], ["lo3"])
                for i, nm in enumerate(("rm", "km", "vm")):
                    pool(lambda e, i=i: e.tensor_tensor(out=B["t0"][:], in0=xin[i][:, :, 0:TT], in1=xin[i][:, :, 1:TT + 1],
                                                      op=ALU.subtract), ["xin%d" % i], ["t0"])
                    for j in range(HG):
                        dve(lambda e, i=i, j=j, nm=nm: e.scalar_tensor_tensor(
                            out=B[nm][:, j, :], in0=B["t0"][:, j, :], scalar=prm[:, g0 + j, i:i + 1],
                            in1=xin[i][:, j, 1:TT + 1], op0=ALU.mult, op1=ALU.add),
                            ["t0", "xin%d" % i, "const"], [nm])
                for j in range(HG):
                    hj = g0 + j
                    cs = slice(hj * 64, hj * 64 + 64)
                    p1, k1 = ps_l()
                    pe(lambda e, p1=p1, cs=cs: e.matmul(p1[0:64, :], wup[:, cs], th[:], start=True, stop=True),
                       ["lo0", "const"], [k1])
                    act(lambda e, p1=p1, j=j, hj=hj: e.activation(out=B["logw"][:, j, :], in_=p1[0:64, :], func=AF.Sigmoid,
                                                               bias=prm[:, hj, 3:4]), [k1, "const"], ["logw"])
                    p2, k2_ = ps_l()
                    pe(lambda e, p2=p2, cs=cs: e.matmul(p2[0:64, :], aup[:, cs], adm[:], start=True, stop=True),
                       ["lo1", "const"], [k2_])
                    act(lambda e, p2=p2, j=j, hj=hj: e.activation(out=B["iclr"][:, j, :], in_=p2[0:64, :], func=AF.Sigmoid,
                                                               bias=prm[:, hj, 4:5]), [k2_, "const"], ["iclr"])
                    p3, k3 = ps_l()
                    for c in range(2):
                        pe(lambda e, p3=p3, cs=cs, c=c: e.matmul(p3[0:64, :], gup[:, c, cs], sg[:, c, :], start=(c == 0),
                                                               stop=(c == 1)), ["lo2", "lo3", "const"], [k3])
                    act(lambda e, p3=p3, j=j: e.copy(out=B["g"][:, j, :], in_=p3[0:64, :]), [k3], ["g"])
                    dve(lambda e, j=j, hj=hj: e.tensor_scalar(out=B["kkn"][:, j, :], in0=B["km"][:, j, :],
                                                             scalar1=prm[:, hj, 5:6], scalar2=None, op0=ALU.mult),
                        ["km", "const"], ["kkn"])
                    pool(lambda e, j=j: e.tensor_tensor(out=B["t1"][:, j, :], in0=B["kkn"][:, j, :], in1=B["kkn"][:, j, :],
                                                      op=ALU.mult), ["kkn"], ["t1"])
                    p4, k4 = ps_l()
                    pe(lambda e, p4=p4, j=j: e.matmul(p4[0:64, :], ones64[:], B["t1"][:, j, :], start=True, stop=True),
                       ["t1", "const"], [k4])
                    act(lambda e, p4=p4, j=j: e.activation(out=B["t2"][:, j, :], in_=p4[0:64, :], func=AF.Sqrt), [k4], ["t2"])
                    dve(lambda e, j=j: e.tensor_scalar(out=B["t2"][:, j, :], in0=B["t2"][:, j, :], scalar1=1e-12, scalar2=None,
                                                      op0=ALU.max), ["t2"], ["t2"])
                    dve(lambda e, j=j: e.reciprocal(out=B["t2"][:, j, :], in_=B["t2"][:, j, :]), ["t2"], ["t2"])
                    dve(lambda e, j=j, hj=hj: e.tensor_scalar(out=B["k2"][:, j, :], in0=B["iclr"][:, j, :], scalar1=-1.0,
                                                             scalar2=prm[:, hj, 6:7], op0=ALU.add, op1=ALU.mult),
                        ["iclr", "const"], ["k2"])
                dve(lambda e: e.tensor_tensor(out=B["kkn"][:], in0=B["kkn"][:], in1=B["t2"][:], op=ALU.mult),
                    ["kkn", "t2"], ["kkn"])
                dve(lambda e: e.scalar_tensor_tensor(out=B["k2"][:], in0=B["k2"][:], scalar=1.0, in1=B["km"][:],
                                                     op0=ALU.add, op1=ALU.mult), ["k2", "km"], ["k2"])
                pool(lambda e: e.tensor_tensor(out=B["t1"][:], in0=B["rm"][:], in1=B["k2"][:], op=ALU.mult),
                     ["rm", "k2"], ["t1"])
                for j in range(HG):
                    p5, k5 = ps_l()
                    pe(lambda e, p5=p5, j=j: e.matmul(p5[0:64, :], rkb[:, g0 + j, :], B["t1"][:, j, :], start=True, stop=True),
                       ["t1", "const"], [k5])
                    act(lambda e, p5=p5, j=j: e.copy(out=B["sbon"][:, j, :], in_=p5[0:64, :]), [k5], ["sbon"])
                dve(lambda e: e.tensor_scalar(out=B["logw"][:], in0=B["logw"][:], scalar1=-math.exp(-0.5), scalar2=None,
                                              op0=ALU.mult), ["logw"], ["logw"])
                for j in range(HG):
                    dve(lambda e, j=j: e.tensor_tensor_scan(out=B["Lc"][:, j, :], data0=rst[:], data1=B["logw"][:, j, :],
                                                           initial=0.0, op0=ALU.mult, op1=ALU.add),
                        ["logw", "const"], ["Lc"])
                act(lambda e: e.activation(out=B["G"][:], in_=B["Lc"][:], func=AF.Exp), ["Lc"], ["G"])
                pool(lambda e: e.tensor_tensor(out=B["rt"][:], in0=B["rm"][:], in1=B["G"][:], op=ALU.mult), ["rm", "G"], ["rt"])
                dve(lambda e: e.tensor_tensor(out=B["t0"][:], in0=B["Lc"][:], in1=B["logw"][:], op=ALU.subtract),
                    ["Lc", "logw"], ["t0"])
                act(lambda e: e.activation(out=B["t0"][:], in_=B["t0"][:], func=AF.Exp), ["t0"], ["t0"])
                dve(lambda e: e.scalar_tensor_tensor(out=B["at"][:], in0=B["kkn"][:], scalar=-1.0, in1=B["t0"][:],
                                                     op0=ALU.mult, op1=ALU.mult), ["kkn", "t0"], ["at"])
                pool(lambda e: e.tensor_copy(out=B["atb"][:], in_=B["at"][:]), ["at"], ["atb"])
                pool(lambda e: e.tensor_tensor(out=B["t2"][:], in0=B["kkn"][:], in1=B["iclr"][:], op=ALU.mult),
                     ["kkn", "iclr"], ["t2"])
                act(lambda e: e.activation(out=B["t1"][:], in_=B["Lc"][:], func=AF.Exp, scale=-1.0), ["Lc", "t1"], ["t1"])
                dve(lambda e: e.tensor_tensor(out=B["bt"][:], in0=B["t2"][:], in1=B["t1"][:], op=ALU.mult), ["t2", "t1"], ["bt"])
                pool(lambda e: e.tensor_tensor(out=B["kt"][:], in0=B["k2"][:], in1=B["t1"][:], op=ALU.mult), ["k2", "t1"], ["kt"])
                for j in range(HG):
                    lc3 = B["Lc"][:, j, :].rearrange("p (c t) -> p c t", t=CH)
                    o3 = B["t0"][:, j, :].rearrange("p (c t) -> p c t", t=CH)
                    dve(lambda e, lc3=lc3, o3=o3: e.tensor_tensor(out=o3, in0=lc3[:, :, CH - 1:CH].to_broadcast([64, NCK, CH]),
                                                                 in1=lc3, op=ALU.subtract), ["Lc", "at"], ["t0"])
                act(lambda e: e.activation(out=B["t0"][:], in_=B["t0"][:], func=AF.Exp), ["t0"], ["t0"])
                dve(lambda e: e.tensor_tensor(out=B["bh"][:], in0=B["t2"][:], in1=B["t0"][:], op=ALU.mult), ["t2", "t0"], ["bh"])
                pool(lambda e: e.tensor_tensor(out=B["kh"][:], in0=B["k2"][:], in1=B["t0"][:], op=ALU.mult), ["k2", "t0"], ["kh"])

                if dbg is not None and ti == 0 and g0 == 0:
                    for di_, nm_ in enumerate(["rm", "km", "vm", "logw", "iclr", "g", "kkn", "k2", "sbon", "Lc", "G", "rt", "at", "bt", "kt", "bh", "kh"]):
                        S.dma("sp", lambda e, di_=di_, nm_=nm_: e.dma_start(out=dbg[di_], in_=B[nm_][:]), reads=[nm_], writes=["dbg"])
                if stop == "prep":
                    return
                def do_chunk(c, pb):
                    W, Vtok, BK, AQ, QP, ARK = Wp[pb], Vtokp[pb], BKp[pb], AQp[pb], QPp[pb], ARKp[pb]
                    kW, kV, kBK, kAQ, kARK = 'W%d' % pb, 'Vtok%d' % pb, 'BK%d' % pb, 'AQ%d' % pb, 'ARK%d' % pb
                    pw_i, pq_i = (6, 0) if pb == 0 else (7, 1)
                    csl = slice(c * CH, (c + 1) * CH)
                    tp1, ktp1 = pss[2], ("ps", 2)
                    tp2, ktp2 = pss[3], ("ps", 3)
                    for j in range(HG):
                        pe(lambda e, j=j: e.transpose(tp1[0:64, j * 128:j * 128 + 64], B["at"][:, j, csl], ident[0:64, 0:64]),
                           ["at", "const"], [ktp1])
                        pe(lambda e, j=j: e.transpose(tp1[0:64, j * 128 + 64:j * 128 + 128], B["vm"][:, j, csl], ident[0:64, 0:64]),
                           ["vm", "const"], [ktp1])
                        pe(lambda e, j=j: e.transpose(tp2[0:64, j * 128:j * 128 + 64], B["bh"][:, j, csl], ident[0:64, 0:64]),
                           ["bh", "const"], [ktp2])
                        pe(lambda e, j=j: e.transpose(tp2[0:64, j * 128 + 64:j * 128 + 128], B["kh"][:, j, csl], ident[0:64, 0:64]),
                           ["kh", "const"], [ktp2])
                    tp1v = tp1[0:64, 0:HG * 128].rearrange("p (h x) -> p h x", x=128)
                    tp2v = tp2[0:64, 0:HG * 128].rearrange("p (h x) -> p h x", x=128)
                    act(lambda e, tp1v=tp1v: e.copy(out=W[:, :, 0:64], in_=tp1v[:, :, 0:64]), [ktp1], [kW])
                    act(lambda e, tp1v=tp1v: e.copy(out=Vtok[:], in_=tp1v[:, :, 64:128]), [ktp1], [kV])
                    act(lambda e, tp2v=tp2v: e.copy(out=BK[:], in_=tp2v), [ktp2], [kBK])
                    yield
                    if stop == "c1":
                        return
                    m1, km1 = ps_m()
                    for j in range(HG):
                        pe(lambda e, m1=m1, j=j: e.matmul(m1[0:64, j * 128:j * 128 + 64], B["kt"][:, j, csl], B["atb"][:, j, csl],
                                                        start=True, stop=True), ["kt", "atb"], [km1])
                        pe(lambda e, m1=m1, j=j: e.matmul(m1[0:64, j * 128 + 64:j * 128 + 128], B["bt"][:, j, csl], B["atb"][:, j, csl],
                                                        start=True, stop=True), ["bt", "atb"], [km1])
                    m1v = m1[0:64, 0:HG * 128].rearrange("p (h x) -> p h x", x=128)
                    dve(lambda e, m1v=m1v: e.tensor_tensor(out=AQ[:], in0=m1v, in1=m_lt2[:], op=ALU.mult), [km1, "const"], [kAQ])
                    m2, km2 = ps_m()
                    for j in range(HG):
                        pe(lambda e, m2=m2, j=j: e.matmul(m2[0:64, j * 64:j * 64 + 64], B["atb"][:, j, csl], B["bt"][:, j, csl],
                                                        start=True, stop=True), ["bt", "atb"], [km2])
                    m2v = m2[0:64, 0:HG * 64].rearrange("p (h x) -> p h x", x=64)
                    qp = 0
                    dve(lambda e: e.tensor_copy(out=QP[0][:, :, 0:64], in_=AQ[:, :, 64:128]), [kAQ], ["QP%d_0" % pb])
                    dve(lambda e, m2v=m2v: e.tensor_tensor(out=QP[0][:, :, 64:128], in0=m2v, in1=m_gt[:], op=ALU.mult),
                        [km2, "const"], ["QP%d_0" % pb])
                    m3, km3 = ps_m()
                    for j in range(HG):
                        pe(lambda e, m3=m3, j=j: e.matmul(m3[0:64, j * 128:j * 128 + 64], B["bt"][:, j, csl], B["rt"][:, j, csl],
                                                        start=True, stop=True), ["bt", "rt"], [km3])
                        pe(lambda e, m3=m3, j=j: e.matmul(m3[0:64, j * 128 + 64:j * 128 + 128], B["kt"][:, j, csl], B["rt"][:, j, csl],
                                                        start=True, stop=True), ["kt", "rt"], [km3])
                    m3v = m3[0:64, 0:HG * 128].rearrange("p (h x) -> p h x", x=128)
                    dve(lambda e, m3v=m3v: e.tensor_tensor(out=ARK[:], in0=m3v, in1=m_le2[:], op=ALU.mult), [km3, "const"], [kARK])
                    yield
                    if stop == "c2":
                        return
                    m4, km4 = ps_m()
                    for j in range(HG):
                        pe(lambda e, m4=m4, j=j: e.matmul(m4[0:64, j * 64:j * 64 + 64], AQ[:, j, 0:64], Vtok[:, j, :],
                                                        start=True, stop=True), [kAQ, kV], [km4])
                    m4v = m4[0:64, 0:HG * 64].rearrange("p (h x) -> p h x", x=64)
                    act(lambda e, m4v=m4v: e.copy(out=W[:, :, 64:128], in_=m4v), [km4], [kW])
                    yield
                    for it in range(6):
                        Qb = QP[qp]
                        kq = "QP%d_%d" % (pb, qp)
                        pw, kpw = pss[pw_i], ("ps", pw_i)
                        for j in range(HG):
                            pe(lambda e, j=j, Qb=Qb: e.matmul(pw[0:64, j * 128:j * 128 + 128], Qb[:, j, 0:64], W[:, j, :],
                                                             start=True, stop=True), [kq, kW], [kpw])
                        pwv = pw[0:64, 0:HG * 128].rearrange("p (h x) -> p h x", x=128)
                        dve(lambda e, pwv=pwv: e.tensor_tensor(out=W[:], in0=pwv, in1=W[:], op=ALU.add), [kpw, kW], [kW])
                        yield
                        if it < 5:
                            pq, kpq = pss[pq_i], ("ps", pq_i)
                            for j in range(HG):
                                pe(lambda e, j=j, Qb=Qb: e.matmul(pq[0:64, j * 128:j * 128 + 64], Qb[:, j, 64:128], Qb[:, j, 0:64],
                                                                 start=True, stop=True), [kq], [kpq])
                                pe(lambda e, j=j, Qb=Qb: e.matmul(pq[0:64, j * 128 + 64:j * 128 + 128], Qb[:, j, 0:64], Qb[:, j, 64:128],
                                                                 start=True, stop=True), [kq], [kpq])
                            pqv = pq[0:64, 0:HG * 128].rearrange("p (h x) -> p h x", x=128)
                            qn = 1 - qp
                            act(lambda e, pqv=pqv, qn=qn: e.copy(out=QP[qn][:], in_=pqv), [kpq], ["QP%d_%d" % (pb, qn)])
                            qp = qn
                            yield
                    if stop == "c3":
                        return
                    m5, km5 = ps_m()
                    for j in range(HG):
                        pe(lambda e, m5=m5, j=j: e.matmul(m5[0:64, j * 64:j * 64 + 64], W[:, j, 0:64], ARK[:, j, 0:64],
                                                        start=True, stop=True), [kW, kARK], [km5])
                    m5b, km5b = ps_m()
                    for j in range(HG):
                        pe(lambda e, m5b=m5b, j=j: e.matmul(m5b[0:64, j * 64:j * 64 + 64], W[:, j, 64:128], ARK[:, j, 0:64],
                                                          start=True, stop=False), [kW, kARK], [km5b])
                        pe(lambda e, m5b=m5b, j=j: e.matmul(m5b[0:64, j * 64:j * 64 + 64], Vtok[:, j, :], ARK[:, j, 64:128],
                                                          start=False, stop=True), [kV, kARK], [km5b])
                    m5v = m5[0:64, 0:HG * 64].rearrange("p (h x) -> p h x", x=64)
                    m5bv = m5b[0:64, 0:HG * 64].rearrange("p (h x) -> p h x", x=64)
                    dve(lambda e, m5v=m5v, c=c: e.tensor_tensor(out=RhT[:, c, :, :], in0=m5v, in1=B["rt"][:, :, csl],
                                                              op=ALU.add), [km5, "rt"], ["RhT"])
                    act(lambda e, m5bv=m5bv, c=c: e.copy(out=Y0T[:, c, :, :], in_=m5bv), [km5b], ["Y0T"])
                    if stop == "c4":
                        return
                    m6, km6 = ps_m()
                    for j in range(HG):
                        pe(lambda e, m6=m6, j=j: e.matmul(m6[0:64, j * 64:j * 64 + 64], W[:, j, 0:64], BK[:, j, 0:64],
                                                        start=True, stop=True), [kW, kBK], [km6])
                    m6b, km6b = ps_m()
                    for j in range(HG):
                        pe(lambda e, m6b=m6b, j=j: e.matmul(m6b[0:64, j * 64:j * 64 + 64], BK[:, j, 0:64], W[:, j, 64:128],
                                                          start=True, stop=False), [kW, kBK], [km6b])
                        pe(lambda e, m6b=m6b, j=j: e.matmul(m6b[0:64, j * 64:j * 64 + 64], BK[:, j, 64:128], Vtok[:, j, :],
                                                          start=False, stop=True), [kV, kBK], [km6b])
                    m6v = m6[0:64, 0:HG * 64].rearrange("p (h x) -> p h x", x=64)
                    m6bv = m6b[0:64, 0:HG * 64].rearrange("p (h x) -> p h x", x=64)
                    for j in range(HG):
                        gc = B["G"][:, j, c * CH + CH - 1:c * CH + CH]
                        dve(lambda e, m6v=m6v, j=j, c=c, gc=gc: e.scalar_tensor_tensor(
                            out=MTa[:, c, j, :], in0=ident[0:64, 0:64], scalar=gc, in1=m6v[:, j, :],
                            op0=ALU.mult, op1=ALU.add), [km6, "G", "const"], ["MT"])
                    act(lambda e, m6bv=m6bv, c=c: e.copy(out=Na[:, c, :, :], in_=m6bv), [km6b], ["N"])

                for c in range(0, NCK, 2):
                    alive = [do_chunk(c, 0), do_chunk(c + 1, 1)]
                    while alive:
                        for g_ in list(alive):
                            try:
                                next(g_)
                            except StopIteration:
                                alive.remove(g_)
                if stop in ("chunk", "c1", "c2", "c3", "c4"):
                    return

                def do_seq(c):
                    hcur = st_h["hcur"]
                    Hc = Hs[hcur]; Hn = Hs[1 - hcur]
                    kh_, kn_ = "H%d" % hcur, "H%d" % (1 - hcur)
                    py, kpy = ps_m()
                    for j in range(HG):
                        pe(lambda e, py=py, j=j, Hc=Hc, c=c: e.matmul(py[0:64, j * 64:j * 64 + 64], Hc[:, j, :], RhT[:, c, j, :],
                                                                    start=True, stop=True), [kh_, "RhT"], [kpy])
                    pyv = py[0:64, 0:HG * 64].rearrange("p (h x) -> p h x", x=64)
                    dve(lambda e, pyv=pyv, c=c: e.tensor_tensor(out=B["y"][:, :, c * CH:(c + 1) * CH], in0=pyv, in1=Y0T[:, c, :, :],
                                                              op=ALU.add), [kpy, "Y0T"], ["y"])
                    ph, kph = ps_m()
                    for j in range(HG):
                        pe(lambda e, ph=ph, j=j, Hc=Hc, c=c: e.matmul(ph[0:64, j * 64:j * 64 + 64], MTa[:, c, j, :], Hc[:, j, :],
                                                                    start=True, stop=True), [kh_, "MT"], [kph])
                    phv = ph[0:64, 0:HG * 64].rearrange("p (h x) -> p h x", x=64)
                    dve(lambda e, phv=phv, c=c, Hn=Hn: e.tensor_tensor(out=Hn[:], in0=phv, in1=Na[:, c, :, :], op=ALU.add),
                        [kph, "N"], [kn_])
                    st_h["hcur"] = 1 - hcur

                for c in range(NCK):
                    do_seq(c)
                if dbg is not None and ti == 0 and g0 == 0:
                    S.dma("sp", lambda e: e.dma_start(out=dbg[17], in_=B["y"][:]), reads=["y"], writes=["dbg"])
                    for di_, nm_ in enumerate([RhT, Y0T, MTa, Na]):
                        S.dma("sp", lambda e, di_=di_, nm_=nm_: e.dma_start(out=dbg[18 + di_].rearrange("p h x -> p (h x)"), in_=nm_[:].rearrange("p c h x -> p (c h x)")),
                              reads=["RhT", "Y0T", "MT", "N"], writes=["dbg"])
                yi = cnt["y"] % 2; cnt["y"] += 1
                yb = yout[yi]
                for j in range(HG):
                    hj = g0 + j
                    p6, k6 = ps_l()
                    pe(lambda e, p6=p6, j=j: e.matmul(p6[0:64, :], avg64[:], B["y"][:, j, :], start=True, stop=True),
                       ["y", "const"], [k6])
                    dve(lambda e, p6=p6, j=j: e.tensor_tensor(out=B["t0"][:, j, :], in0=B["y"][:, j, :], in1=p6[0:64, :],
                                                            op=ALU.subtract), [k6, "y"], ["t0"])
                    pool(lambda e, j=j: e.tensor_tensor(out=B["t1"][:, j, :], in0=B["t0"][:, j, :], in1=B["t0"][:, j, :],
                                                      op=ALU.mult), ["t0"], ["t1"])
                    p7, k7 = ps_l()
                    pe(lambda e, p7=p7, j=j: e.matmul(p7[0:64, :], avg64[:], B["t1"][:, j, :], start=True, stop=True),
                       ["t1", "const"], [k7])
                    dve(lambda e, p7=p7, j=j: e.tensor_scalar(out=B["t2"][:, j, :], in0=p7[0:64, :], scalar1=B_GN_EPS, scalar2=None,
                                                            op0=ALU.add), [k7], ["t2"])
                    act(lambda e, j=j: e.activation(out=B["t2"][:, j, :], in_=B["t2"][:, j, :], func=AF.Sqrt), ["t2"], ["t2"])
                    dve(lambda e, j=j: e.reciprocal(out=B["t2"][:, j, :], in_=B["t2"][:, j, :]), ["t2"], ["t2"])
                    dve(lambda e, j=j: e.tensor_tensor(out=B["t0"][:, j, :], in0=B["t0"][:, j, :], in1=B["t2"][:, j, :],
                                                      op=ALU.mult), ["t0", "t2"], ["t0"])
                    dve(lambda e, j=j, hj=hj: e.tensor_scalar(out=B["t0"][:, j, :], in0=B["t0"][:, j, :], scalar1=prm[:, hj, 8:9],
                                                             scalar2=prm[:, hj, 9:10], op0=ALU.mult, op1=ALU.add),
                        ["t0", "const"], ["t0"])
                pool(lambda e: e.tensor_tensor(out=B["t1"][:], in0=B["sbon"][:], in1=B["vm"][:], op=ALU.mult),
                     ["sbon", "vm", "t1"], ["t1"])
                dve(lambda e: e.tensor_tensor(out=B["t0"][:], in0=B["t0"][:], in1=B["t1"][:], op=ALU.add), ["t0", "t1"], ["t0"])
                dve(lambda e, yb=yb: e.tensor_tensor(out=yb[:], in0=B["t0"][:], in1=B["g"][:], op=ALU.mult),
                    ["t0", "g"], [("yo", yi)])
                for j, h in enumerate(hs):
                    S.dma("sp", lambda e, yb=yb, j=j, h=h: e.dma_start(out=yT[YROW_B + 64 * h:YROW_B + 64 * h + 64, t0:t0 + TT],
                                                                     in_=yb[:, j, :]), reads=[("yo", yi)], writes=["yT"])

            for ti in range(T // TT):
                do_tile(ti)
        S.barrier_all()
        S.emit()


def build_rwkv_test(T, heads, debug=False, stop=None):
    nc = bass.Bass("TRN2", target_bir_lowering=False)
    NH = len(heads)
    di = lambda n, s: nc.dram_tensor(n, s, F32, kind="ExternalInput").ap()
    pT = di("pT", [NP_ROWS, T]); prm = di("prm", [64, NH, 10]); lmu = di("lmu", [128, 4])
    wup = di("wup", [96, NH * 64]); aup = di("aup", [96, NH * 64]); gup = di("gup", [256, NH * 64])
    ident = di("ident", [128, 128]); mlt = di("m_lt2", [64, 3, 128]); mle = di("m_le2", [64, 3, 128])
    mgt = di("m_gt", [64, 3, 64]); rst = di("rst", [64, 512])
    yT = nc.dram_tensor("yT", [1536, T], F32, kind="ExternalOutput").ap()
    dbg = nc.dram_tensor("dbg", [22, 64, 3, 512], F32, kind="ExternalOutput").ap() if debug else None
    phase_rwkv(nc, T, pT, yT, prm, lmu, wup, aup, gup, ident, mlt, mle, mgt, rst, heads, dbg=dbg, stop=stop)
    return nc


def rwkv_host_params(heads, mu, w0, w_up, a0, a_up, g_up, k_k, k_a, r_k, lnx_g, lnx_b):
    NH = len(heads)
    prm = np.zeros((64, NH, 10), np.float32)
    cols = np.concatenate([np.arange(64 * h, 64 * h + 64) for h in heads])
    for i, h in enumerate(heads):
        sl = slice(64 * h, 64 * h + 64)
        prm[:, i, 0] = mu[0:768][sl]; prm[:, i, 1] = mu[768:1536][sl]; prm[:, i, 2] = mu[1536:2304][sl]
        prm[:, i, 3] = w0[sl]; prm[:, i, 4] = a0[sl]; prm[:, i, 5] = k_k[sl]; prm[:, i, 6] = k_a[sl]
        prm[:, i, 7] = r_k.reshape(-1)[sl]; prm[:, i, 8] = lnx_g[sl]; prm[:, i, 9] = lnx_b[sl]
    lmu = np.zeros((128, 4), np.float32)
    lmu[0:96, 0] = mu[2304:2400]; lmu[0:96, 1] = mu[2400:2496]; lmu[:, 2] = mu[2496:2624]; lmu[:, 3] = mu[2624:2752]
    return dict(prm=prm, lmu=lmu, wup=np.ascontiguousarray(w_up[:, cols]), aup=np.ascontiguousarray(a_up[:, cols]),
                gup=np.ascontiguousarray(g_up[:, cols]))


class WeightStream:
    def __init__(self, S, nc, st, name, max_elems, n_stage=2, n_bf=2, direct=False):
        self.S, self.nc, self.name = S, nc, name
        if direct:
            n_stage = 0
        self.stage = [st.enter_context(nc.sbuf_tensor(uname("%s_st%d" % (name, i)), [128, max_elems], F32)) for i in range(n_stage)]
        self.bf = [st.enter_context(nc.sbuf_tensor(uname("%s_bf%d" % (name, i)), [128, max_elems], BF16)) for i in range(n_bf)]
        self.i = 0
        self.q = 0

    def load_bf(self, scr, shapes, dram_key):
        S = self.S
        bi = self.i % len(self.bf); self.i += 1
        bfb = self.bf[bi]
        n_tot = sum(int(np.prod(shp[1:])) for shp in shapes)
        q = ("sp", "act")[self.q % 2]; self.q += 1
        S.dma(q, lambda e, bfb=bfb, n_tot=n_tot: e.dma_start(out=bfb[:, 0:n_tot], in_=scr[:, 0:n_tot]), reads=[dram_key],
              writes=[(self.name, "bf", bi)])
        off = 0
        outs = []
        for shp in shapes:
            n = int(np.prod(shp[1:]))
            pat = "p (a b) -> p a b" if len(shp) == 3 else "p (a b c) -> p a b c"
            kw = dict(a=shp[1], b=shp[2]) if len(shp) == 3 else dict(a=shp[1], b=shp[2], c=shp[3])
            outs.append(bfb[:, off:off + n].rearrange(pat, **kw))
            off += n
        return outs, (self.name, "bf", bi)

    def load(self, src_views, shapes, dram_key):
        S = self.S
        si = self.i % len(self.stage); bi = self.i % len(self.bf); self.i += 1
        stg, bfb = self.stage[si], self.bf[bi]
        off = 0
        outs = []
        for v, shp in zip(src_views, shapes):
            n = int(np.prod(shp[1:]))
            pat = "p (a b) -> p a b" if len(shp) == 3 else "p (a b c) -> p a b c"
            kw = dict(a=shp[1], b=shp[2]) if len(shp) == 3 else dict(a=shp[1], b=shp[2], c=shp[3])
            dst = stg[:, off:off + n].rearrange(pat, **kw)
            q = ("sp", "act")[self.q % 2]; self.q += 1
            S.dma(q, lambda e, dst=dst, v=v: e.dma_start(out=dst, in_=v), reads=[dram_key],
                  writes=[(self.name, "st", si)])
            outs.append(bfb[:, off:off + n].rearrange(pat, **kw))
            off += n
        S.op("pool", lambda e, stg=stg, bfb=bfb, off=off: e.tensor_copy(out=bfb[:, 0:off], in_=stg[:, 0:off]),
             reads=[(self.name, "st", si)], writes=[(self.name, "bf", bi)])
        return outs, (self.name, "bf", bi)


def phase_precast(nc, panels, scr, max_elems):
    import contextlib
    with contextlib.ExitStack() as st:
        _PH[0] += 1
        S = Sched(nc)
        ws = WeightStream(S, nc, st, "pc_w", max_elems)
        for i, (views, shapes) in enumerate(panels):
            outs, kw = ws.load(views, shapes, "Wsrc")
            n_tot = sum(int(np.prod(shp[1:])) for shp in shapes)
            bfb = ws.bf[(ws.i - 1) % len(ws.bf)]
            S.dma("sp", lambda e, i=i, bfb=bfb, n_tot=n_tot: e.dma_start(out=scr[i][:, 0:n_tot], in_=bfb[:, 0:n_tot]),
                  reads=[kw], writes=["scr"])
        S.barrier_all()
        S.emit()


def precast_gen(S, ws, panels, scr, tag):
    for i, (views, shapes) in enumerate(panels):
        outs, kw = ws.load(views, shapes, "Wsrc")
        n_tot = sum(int(np.prod(shp[1:])) for shp in shapes)
        bfb = ws.bf[(ws.i - 1) % len(ws.bf)]
        S.dma("sp", lambda e, i=i, bfb=bfb, n_tot=n_tot: e.dma_start(out=scr[i][:, 0:n_tot], in_=bfb[:, 0:n_tot]),
              reads=[kw], writes=[("scr", tag)])
        yield


def emit_norm_T(S, nc, x_sb, kx, h_out, kh, g_col, ones_bf, sq, rstd, ps, kps, n):
    for c in range(KC):
        S.op("act", lambda e, c=c: e.activation(out=sq[:, c, :n], in_=x_sb[:, c, :], func=AF.Square), reads=[kx], writes=["nrm_sq"])
    for c in range(KC):
        S.op("pe", lambda e, c=c: e.matmul(ps[:, :n], ones_bf[:], sq[:, c, :n], start=(c == 0), stop=(c == KC - 1)),
             reads=["nrm_sq", "ones"], writes=[kps])
    S.op("dve", lambda e: e.tensor_scalar(out=rstd[:, :n], in0=ps[:, :n], scalar1=1.0 / D_MODEL, scalar2=NORM_EPS,
                                          op0=ALU.mult, op1=ALU.add), reads=[kps], writes=["nrm_rstd"])
    S.op("act", lambda e: e.activation(out=rstd[:, :n], in_=rstd[:, :n], func=AF.Sqrt), reads=["nrm_rstd"], writes=["nrm_rstd"])
    S.op("dve", lambda e: e.reciprocal(out=rstd[:, :n], in_=rstd[:, :n]), reads=["nrm_rstd"], writes=["nrm_rstd"])
    for c in range(KC):
        S.op("dve", lambda e, c=c: e.scalar_tensor_tensor(out=h_out[:, c, :], in0=x_sb[:, c, :], scalar=g_col[:, c:c + 1],
                                                         in1=rstd[:, :n], op0=ALU.mult, op1=ALU.mult),
             reads=[kx, "nrm_rstd", "gcol"], writes=[kh])


def phase_proj(nc, TL, xT, g_d, W, pT, ncols, scr=None):
    import contextlib
    TS = min(2048, TL); TT = 512; CB = 256
    Wv0 = W.rearrange("(c p) n -> p c n", p=128)
    if scr is not None:
        panels = []
        for cb0 in range(0, ncols, CB):
            cw = min(CB, ncols - cb0)
            panels.append(([Wv0[:, :, cb0:cb0 + cw]], [(128, KC, cw)]))
        phase_precast(nc, panels, scr, KC * CB)
    with contextlib.ExitStack() as st:
        sb = lambda name, shape, dt=F32: st.enter_context(nc.sbuf_tensor(uname(name), shape, dt))
        pss = [st.enter_context(nc.psum_tensor(uname("pps%d" % i), [128, 512], F32)) for i in range(8)]
        _PH[0] += 1
        S = Sched(nc)
        ones_bf = sb("pj_ones", [128, 128], BF16); g_sb = sb("pj_g", [128, KC])
        hT = sb("pj_h", [128, KC, TS], BF16)
        xin = [sb("pj_x%d" % i, [128, KC, TT]) for i in range(1)]
        sq = sb("pj_sq", [128, KC, TT], BF16); rstd = sb("pj_rstd", [128, TT])
        ot = [sb("pj_o%d" % i, [128, TT]) for i in range(4)]
        ws = WeightStream(S, nc, st, "pj_w", KC * CB, direct=scr is not None)
        S.op("pool", lambda e: e.memset(ones_bf[:], 1.0), writes=["ones"])
        S.dma("sp", lambda e: e.dma_start(out=g_sb[:], in_=g_d), writes=["gcol"])
        xTv = xT.rearrange("(c p) t -> p c t", p=128)
        Wv = W.rearrange("(c p) n -> p c n", p=128)
        cnt = dict(o=0, ps=0)
        for s0 in range(0, TL, TS):
            for tt in range(TS // TT):
                c0 = s0 + tt * TT
                S.dma("sp", lambda e, c0=c0: e.dma_start(out=xin[0][:], in_=xTv[:, :, c0:c0 + TT]), reads=["xT"], writes=["pj_x"])
                emit_norm_T(S, nc, xin[0], "pj_x", hT[:, :, tt * TT:(tt + 1) * TT], ("pj_h", tt), g_sb, ones_bf, sq, rstd,
                            pss[0], ("ps", 0), TT)
            for cb0 in range(0, ncols, CB):
                cw = min(CB, ncols - cb0)
                if scr is not None:
                    (wv,), kw = ws.load_bf(scr[cb0 // CB], [(128, KC, cw)], "Wscr")
                else:
                    (wv,), kw = ws.load([Wv[:, :, cb0:cb0 + cw]], [(128, KC, cw)], "W")
                for m0 in range(0, cw, 128):
                    mw = min(128, cw - m0)
                    for tt in range(TS // TT):
                        pi = 1 + cnt["ps"] % 7; cnt["ps"] += 1
                        ps = pss[pi]
                        for c in range(KC):
                            S.op("pe", lambda e, ps=ps, wv=wv, c=c, m0=m0, mw=mw, tt=tt:
                                 e.matmul(ps[:mw, :TT], wv[:, c, m0:m0 + mw], hT[:, c, tt * TT:(tt + 1) * TT],
                                          start=(c == 0), stop=(c == KC - 1)),
                                 reads=[kw, ("pj_h", tt)], writes=[("ps", pi)])
                        oi = cnt["o"] % 4; cnt["o"] += 1
                        ob = ot[oi]
                        if oi % 2:
                            S.op("act", lambda e, ob=ob, ps=ps, mw=mw: e.copy(out=ob[:mw, :], in_=ps[:mw, :TT]),
                                 reads=[("ps", pi)], writes=[("pj_o", oi)])
                        else:
                            S.op("dve", lambda e, ob=ob, ps=ps, mw=mw: e.tensor_copy(out=ob[:mw, :], in_=ps[:mw, :TT]),
                                 reads=[("ps", pi)], writes=[("pj_o", oi)])
                        r0 = cb0 + m0; t0 = s0 + tt * TT
                        S.dma("sp", lambda e, ob=ob, mw=mw, r0=r0, t0=t0: e.dma_start(out=pT[r0:r0 + mw, t0:t0 + TT], in_=ob[:mw, :]),
                              reads=[("pj_o", oi)], writes=["pT"])
        S.barrier_all()
        S.emit()


def merge_panels(Wg, gate_col0, projw, wout):
    Wgv0 = Wg.rearrange("(c p) n -> p c n", p=128)
    Pv0 = projw.rearrange("(c p) n -> p c n", p=128)
    Wov0 = wout.rearrange("(c p) n -> p c n", p=128)
    pa = []
    for m in range(KC):
        views = [Wgv0[:, :, gate_col0 + i * D_MODEL + m * 128:gate_col0 + i * D_MODEL + m * 128 + 128] for i in range(3)]
        views.append(Pv0[:, :, m * 128:(m + 1) * 128])
        pa.append((views, [(128, KC, 128)] * 3 + [(128, 12, 128)]))
    pb = [([Wov0[:, :, m * 128:(m + 1) * 128]], [(128, KC, 128)]) for m in range(KC)]
    return pa, pb


def ffn_panels(wup, wdown, d_ff):
    NF = d_ff // 128
    Wuv0 = wup.rearrange("(c p) n -> p c n", p=128)
    Wdv0 = wdown.rearrange("(f p) n -> p f n", p=128)
    pu = [([Wuv0[:, :, f * 128:(f + 1) * 128], Wuv0[:, :, d_ff + f * 128:d_ff + (f + 1) * 128]], [(128, KC, 128)] * 2) for f in range(NF)]
    pd = [([Wdv0[:, :, m * 128:(m + 1) * 128]], [(128, NF, 128)]) for m in range(KC)]
    return pu, pd


def phase_merge(nc, TL, xT, g_d, Wg, gate_col0, yT, projw, wout, x1T, scr=None, scr_o=None, precast_done=False):
    import contextlib
    TT = 512
    YK = (4, 6, 2)
    YO = (0, 4, 10)
    NHALF = 2 if (scr is not None and TL % (2 * TT) == 0) else 1
    TB = TT * NHALF
    Wgv0 = Wg.rearrange("(c p) n -> p c n", p=128)
    Pv0 = projw.rearrange("(c p) n -> p c n", p=128)
    Wov0 = wout.rearrange("(c p) n -> p c n", p=128)

    def mg_views(m):
        views = [Wgv0[:, :, gate_col0 + i * D_MODEL + m * 128:gate_col0 + i * D_MODEL + m * 128 + 128] for i in range(3)]
        views.append(Pv0[:, :, m * 128:(m + 1) * 128])
        return views, [(128, KC, 128)] * 3 + [(128, 12, 128)]

    if scr is not None and not precast_done:
        phase_precast(nc, [mg_views(m) for m in range(KC)], scr, KC * 3 * 128 + 12 * 128)
        phase_precast(nc, [([Wov0[:, :, m * 128:(m + 1) * 128]], [(128, KC, 128)]) for m in range(KC)], scr_o, KC * 128)
    with contextlib.ExitStack() as st:
        sb = lambda name, shape, dt=F32: st.enter_context(nc.sbuf_tensor(uname(name), shape, dt))
        pss = [st.enter_context(nc.psum_tensor(uname("mps%d" % i), [128, 512], F32)) for i in range(8)]
        _PH[0] += 1
        S = Sched(nc)
        ones_bf = sb("mg_ones", [128, 128], BF16); g_sb = sb("mg_g", [128, KC])
        xin = sb("mg_x", [128, KC, TT]); hT = sb("mg_h", [128, KC, TB], BF16)
        xr = [sb("mg_xr%d" % i, [128, TT]) for i in range(2)]
        rstd = sb("mg_rstd", [128, TT])
        yst = sb("mg_yst", [128, 12, TT]); ybf = sb("mg_ybf", [128, 12, TB], BF16)
        mrgb = sb("mg_mrgb", [128, KC, TB], BF16)
        sq = mrgb[:, :, 0:TT]
        S.alias["nrm_sq"] = "mg_mrgb"
        sig = [sb("mg_sig%d" % i, [128, TT]) for i in range(2)]
        tmp = sb("mg_tmp", [128, TT]); mrg = sb("mg_mrg", [128, TT])
        xo = [sb("mg_xo%d" % i, [128, TT]) for i in range(2)]
        ws = WeightStream(S, nc, st, "mg_w", KC * 3 * 128 + 12 * 128, n_stage=1, direct=scr is not None)
        S.op("pool", lambda e: e.memset(ones_bf[:], 1.0), writes=["ones"])
        S.dma("sp", lambda e: e.dma_start(out=g_sb[:], in_=g_d), writes=["gcol"])
        xTv = xT.rearrange("(c p) t -> p c t", p=128)
        yTv = yT.rearrange("(c p) t -> p c t", p=128)
        Wgv = Wg.rearrange("(c p) n -> p c n", p=128)
        Pv = projw.rearrange("(c p) n -> p c n", p=128)
        Wov = wout.rearrange("(c p) n -> p c n", p=128)
        cnt = dict(ps=0, s=0, o=0)

        def nps():
            i = 1 + cnt["ps"] % 7; cnt["ps"] += 1
            return pss[i], ("ps", i)

        for t0 in range(0, TL, TB):
            for h in range(NHALF):
                hs = slice(h * TT, (h + 1) * TT)
                tk = t0 + h * TT
                S.dma("sp", lambda e, tk=tk: e.dma_start(out=xin[:], in_=xTv[:, :, tk:tk + TT]), reads=["xT"], writes=["mg_x"])
                S.dma("act", lambda e, tk=tk: e.dma_start(out=yst[:], in_=yTv[:, :, tk:tk + TT]), reads=["yT"], writes=["mg_yst"])
                S.op("pool", lambda e, hs=hs: e.tensor_copy(out=ybf[:, :, hs], in_=yst[:]), reads=["mg_yst"], writes=[("mg_ybf", h)])
                emit_norm_T(S, nc, xin, "mg_x", hT[:, :, hs], ("mg_h", h), g_sb, ones_bf, sq, rstd, pss[0], ("ps", 0), TT)
            for m in range(KC):
                views = [Wgv[:, :, gate_col0 + i * D_MODEL + m * 128:gate_col0 + i * D_MODEL + m * 128 + 128] for i in range(3)]
                views.append(Pv[:, :, m * 128:(m + 1) * 128])
                shapes = [(128, KC, 128)] * 3 + [(128, 12, 128)]
                if scr is not None:
                    (w0v, w1v, w2v, pv), kw = ws.load_bf(scr[m], shapes, "Wmgs")
                else:
                    (w0v, w1v, w2v, pv), kw = ws.load(views, shapes, "Wmg")
                wgs = (w0v, w1v, w2v)
                for h in range(NHALF):
                    hs = slice(h * TT, (h + 1) * TT)
                    for i in range(3):
                        pg, kpg = nps()
                        for c in range(KC):
                            S.op("pe", lambda e, pg=pg, i=i, c=c, wgs=wgs, hs=hs: e.matmul(pg[:, :TT], wgs[i][:, c, :], hT[:, c, hs],
                                                                                       start=(c == 0), stop=(c == KC - 1)),
                                 reads=[kw, ("mg_h", h)], writes=[kpg])
                        pz, kpz = nps()
                        for u in range(YK[i]):
                            S.op("pe", lambda e, pz=pz, i=i, u=u, pv=pv, hs=hs: e.matmul(pz[:, :TT], pv[:, YO[i] + u, :], ybf[:, YO[i] + u, hs],
                                                                                      start=(u == 0), stop=(u == YK[i] - 1)),
                                 reads=[kw, ("mg_ybf", h)], writes=[kpz])
                        si = cnt["s"] % 2; cnt["s"] += 1
                        sg = sig[si]
                        S.op("act", lambda e, sg=sg, pg=pg: e.activation(out=sg[:], in_=pg[:, :TT], func=AF.Sigmoid), reads=[kpg],
                             writes=[("mg_sig", si)])
                        if i == 0:
                            S.op("dve", lambda e, sg=sg, pz=pz: e.tensor_tensor(out=mrg[:], in0=pz[:, :TT], in1=sg[:], op=ALU.mult),
                                 reads=[kpz, ("mg_sig", si)], writes=["mg_mrg"])
                        else:
                            S.op("dve", lambda e, sg=sg, pz=pz: e.tensor_tensor(out=tmp[:], in0=pz[:, :TT], in1=sg[:], op=ALU.mult),
                                 reads=[kpz, ("mg_sig", si)], writes=["mg_tmp"])
                            if i == 1:
                                S.op("pool", lambda e: e.tensor_tensor(out=mrg[:], in0=mrg[:], in1=tmp[:], op=ALU.add),
                                     reads=["mg_mrg", "mg_tmp"], writes=["mg_mrg"])
                            else:
                                S.op("pool", lambda e, m=m, hs=hs: e.tensor_tensor(out=mrgb[:, m, hs], in0=mrg[:], in1=tmp[:], op=ALU.add),
                                     reads=["mg_mrg", "mg_tmp"], writes=["mg_mrgb"])
            for m in range(KC):
                if scr is not None:
                    (wov,), kw = ws.load_bf(scr_o[m], [(128, KC, 128)], "Wouts")
                else:
                    (wov,), kw = ws.load([Wov[:, :, m * 128:(m + 1) * 128]], [(128, KC, 128)], "Wout")
                for h in range(NHALF):
                    hs = slice(h * TT, (h + 1) * TT)
                    tk = t0 + h * TT
                    po, kpo = nps()
                    for c in range(KC):
                        S.op("pe", lambda e, po=po, c=c, wov=wov, hs=hs: e.matmul(po[:, :TT], wov[:, c, :], mrgb[:, c, hs], start=(c == 0),
                                                                               stop=(c == KC - 1)), reads=[kw, "mg_mrgb"], writes=[kpo])
                    oi = cnt["o"] % 2; cnt["o"] += 1
                    ob = xo[oi]; xrb = xr[oi]
                    S.dma("act", lambda e, xrb=xrb, m=m, tk=tk: e.dma_start(out=xrb[:], in_=xT[m * 128:(m + 1) * 128, tk:tk + TT]),
                          reads=["xT"], writes=[("mg_xr", oi)])
                    S.op("dve", lambda e, ob=ob, po=po, xrb=xrb: e.tensor_tensor(out=ob[:], in0=po[:, :TT], in1=xrb[:], op=ALU.add),
                         reads=[kpo, ("mg_xr", oi)], writes=[("mg_xo", oi)])
                    S.dma("sp", lambda e, ob=ob, m=m, tk=tk: e.dma_start(out=x1T[m * 128:(m + 1) * 128, tk:tk + TT], in_=ob[:]),
                          reads=[("mg_xo", oi)], writes=["x1T"])
        S.barrier_all()
        S.emit()


def phase_ffn(nc, TL, x1T, g_d, wup, conv_d, wdown, xoT, d_ff, halo_d=None, scr_u=None, scr_d=None, precast_done=False):
    import contextlib
    TT = 512
    NF = d_ff // 128
    NHALF = 2 if (scr_u is not None and TL % (2 * TT) == 0) else 1
    TB = TT * NHALF
    Wuv0 = wup.rearrange("(c p) n -> p c n", p=128)
    Wdv0 = wdown.rearrange("(f p) n -> p f n", p=128)
    if scr_u is not None and not precast_done:
        phase_precast(nc, [([Wuv0[:, :, f * 128:(f + 1) * 128], Wuv0[:, :, d_ff + f * 128:d_ff + (f + 1) * 128]], [(128, KC, 128)] * 2)
                           for f in range(NF)], scr_u, KC * 256)
        phase_precast(nc, [([Wdv0[:, :, m * 128:(m + 1) * 128]], [(128, NF, 128)]) for m in range(KC)], scr_d, NF * 128)
    with contextlib.ExitStack() as st:
        sb = lambda name, shape, dt=F32: st.enter_context(nc.sbuf_tensor(uname(name), shape, dt))
        pss = [st.enter_context(nc.psum_tensor(uname("fps%d" % i), [128, 512], F32)) for i in range(8)]
        _PH[0] += 1
        S = Sched(nc)
        ones_bf = sb("ff_ones", [128, 128], BF16); g_sb = sb("ff_g", [128, KC]); cw = sb("ff_cw", [128, NF, 3])
        xin = sb("ff_x", [128, KC, TT]); hT = sb("ff_h", [128, KC, TB], BF16)
        rstd = sb("ff_rstd", [128, TT])
        actb = sb("ff_act", [128, max(NF, KC), TB], BF16)
        sq = actb[:, 0:KC, 0:TT]
        S.alias["nrm_sq"] = "ff_act"
        xr = [sb("ff_xr%d" % i, [128, TT]) for i in range(2)]
        carry = sb("ff_carry", [128, NF, 2])
        gbuf = [sb("ff_gb%d" % i, [128, TT + 2]) for i in range(2)]
        cv = [sb("ff_cv%d" % i, [128, TT]) for i in range(2)]
        xo = [sb("ff_xo%d" % i, [128, TT]) for i in range(2)]
        ws = WeightStream(S, nc, st, "ff_w", max(KC * 256, NF * 128), n_stage=2, n_bf=3 if scr_u is not None else 2,
                          direct=scr_u is not None)
        S.op("pool", lambda e: e.memset(ones_bf[:], 1.0), writes=["ones"])
        S.dma("sp", lambda e: e.dma_start(out=g_sb[:], in_=g_d), writes=["gcol"])
        S.dma("sp", lambda e: e.dma_start(out=cw[:], in_=conv_d), writes=["ff_cw"])
        if halo_d is None:
            S.op("pool", lambda e: e.memset(carry[:], 0.0), writes=["ff_carry"])
        else:
            S.dma("sp", lambda e: e.dma_start(out=carry[:], in_=halo_d), writes=["ff_carry"])
        xv = x1T.rearrange("(c p) t -> p c t", p=128)
        Wuv = wup.rearrange("(c p) n -> p c n", p=128)
        Wdv = wdown.rearrange("(f p) n -> p f n", p=128)
        cnt = dict(ps=0, g=0, o=0)

        def nps():
            i = 1 + cnt["ps"] % 7; cnt["ps"] += 1
            return pss[i], ("ps", i)

        for t0 in range(0, TL, TB):
            for h in range(NHALF):
                S.dma("sp", lambda e, t0=t0, h=h: e.dma_start(out=xin[:], in_=xv[:, :, t0 + h * TT:t0 + (h + 1) * TT]),
                      reads=["x1T"], writes=["ff_x"])
                emit_norm_T(S, nc, xin, "ff_x", hT[:, :, h * TT:(h + 1) * TT], ("ff_h", h), g_sb, ones_bf, sq, rstd, pss[0],
                            ("ps", 0), TT)
            for f in range(NF):
                views = [Wuv[:, :, f * 128:(f + 1) * 128], Wuv[:, :, d_ff + f * 128:d_ff + (f + 1) * 128]]
                if scr_u is not None:
                    (wg, wv), kw = ws.load_bf(scr_u[f], [(128, KC, 128)] * 2, "Wups")
                else:
                    (wg, wv), kw = ws.load(views, [(128, KC, 128)] * 2, "Wup")
                for h in range(NHALF):
                    hs = slice(h * TT, (h + 1) * TT)
                    pg, kpg = nps()
                    for c in range(KC):
                        S.op("pe", lambda e, pg=pg, c=c, wg=wg, hs=hs: e.matmul(pg[:, :TT], wg[:, c, :], hT[:, c, hs], start=(c == 0),
                                                                             stop=(c == KC - 1)), reads=[kw, ("ff_h", h)], writes=[kpg])
                    pv, kpv = nps()
                    for c in range(KC):
                        S.op("pe", lambda e, pv=pv, c=c, wv=wv, hs=hs: e.matmul(pv[:, :TT], wv[:, c, :], hT[:, c, hs], start=(c == 0),
                                                                             stop=(c == KC - 1)), reads=[kw, ("ff_h", h)], writes=[kpv])
                    gi = cnt["g"] % 2; cnt["g"] += 1
                    gb = gbuf[gi]; cb = cv[gi]
                    kgb, kcb = ("ff_gb", gi), ("ff_cv", gi)
                    S.op("pool", lambda e, gb=gb, f=f: e.tensor_copy(out=gb[:, 0:2], in_=carry[:, f, :]), reads=["ff_carry"], writes=[kgb])
                    S.op("act", lambda e, gb=gb, pg=pg: e.copy(out=gb[:, 2:TT + 2], in_=pg[:, :TT]), reads=[kpg], writes=[kgb])
                    S.op("pool", lambda e, gb=gb, f=f: e.tensor_copy(out=carry[:, f, :], in_=gb[:, TT:TT + 2]), reads=[kgb],
                         writes=["ff_carry"])
                    S.op("dve", lambda e, gb=gb, cb=cb, f=f: e.tensor_scalar(out=cb[:], in0=gb[:, 0:TT], scalar1=cw[:, f, 0:1], scalar2=None,
                                                                           op0=ALU.mult), reads=[kgb, "ff_cw"], writes=[kcb])
                    S.op("dve", lambda e, gb=gb, cb=cb, f=f: e.scalar_tensor_tensor(out=cb[:], in0=gb[:, 1:TT + 1], scalar=cw[:, f, 1:2],
                                                                                  in1=cb[:], op0=ALU.mult, op1=ALU.add),
                         reads=[kgb, "ff_cw", kcb], writes=[kcb])
                    S.op("dve", lambda e, gb=gb, cb=cb, f=f: e.scalar_tensor_tensor(out=cb[:], in0=gb[:, 2:TT + 2], scalar=cw[:, f, 2:3],
                                                                                  in1=cb[:], op0=ALU.mult, op1=ALU.add),
                         reads=[kgb, "ff_cw", kcb], writes=[kcb])
                    S.op("act", lambda e, cb=cb: e.activation(out=cb[:], in_=cb[:], func=AF.Silu), reads=[kcb], writes=[kcb])
                    S.op("dve", lambda e, cb=cb, pv=pv, f=f, hs=hs: e.tensor_tensor(out=actb[:, f, hs], in0=pv[:, :TT], in1=cb[:], op=ALU.mult),
                         reads=[kpv, kcb], writes=["ff_act"])
            for m in range(KC):
                if scr_d is not None:
                    (wd,), kw = ws.load_bf(scr_d[m], [(128, NF, 128)], "Wdowns")
                else:
                    (wd,), kw = ws.load([Wdv[:, :, m * 128:(m + 1) * 128]], [(128, NF, 128)], "Wdown")
                for h in range(NHALF):
                    hs = slice(h * TT, (h + 1) * TT)
                    tk = t0 + h * TT
                    po, kpo = nps()
                    for f in range(NF):
                        S.op("pe", lambda e, po=po, f=f, wd=wd, hs=hs: e.matmul(po[:, :TT], wd[:, f, :], actb[:, f, hs], start=(f == 0),
                                                                             stop=(f == NF - 1)), reads=[kw, "ff_act"], writes=[kpo])
                    oi = cnt["o"] % 2; cnt["o"] += 1
                    ob = xo[oi]; xrb = xr[oi]
                    S.dma("act", lambda e, xrb=xrb, m=m, tk=tk: e.dma_start(out=xrb[:], in_=x1T[m * 128:(m + 1) * 128, tk:tk + TT]),
                          reads=["x1T"], writes=[("ff_xr", oi)])
                    S.op("dve", lambda e, ob=ob, po=po, xrb=xrb: e.tensor_tensor(out=ob[:], in0=po[:, :TT], in1=xrb[:], op=ALU.add),
                         reads=[kpo, ("ff_xr", oi)], writes=[("ff_xo", oi)])
                    S.dma("sp", lambda e, ob=ob, m=m, tk=tk: e.dma_start(out=xoT[m * 128:(m + 1) * 128, tk:tk + TT], in_=ob[:]),
                          reads=[("ff_xo", oi)], writes=["xoT"])
        S.barrier_all()
        S.emit()


def phase_final_norm(nc, TL, xT, g_d, outT):
    import contextlib
    TT = 512
    with contextlib.ExitStack() as st:
        sb = lambda name, shape, dt=F32: st.enter_context(nc.sbuf_tensor(uname(name), shape, dt))
        pss = [st.enter_context(nc.psum_tensor(uname("nps%d" % i), [128, 512], F32)) for i in range(2)]
        _PH[0] += 1
        S = Sched(nc)
        ones_bf = sb("fn_ones", [128, 128], BF16); g_sb = sb("fn_g", [128, KC])
        xin = [sb("fn_x%d" % i, [128, KC, TT]) for i in range(2)]
        ho = [sb("fn_h%d" % i, [128, KC, TT]) for i in range(2)]
        sq = sb("fn_sq", [128, KC, TT], BF16); rstd = sb("fn_rstd", [128, TT])
        S.op("pool", lambda e: e.memset(ones_bf[:], 1.0), writes=["ones"])
        S.dma("sp", lambda e: e.dma_start(out=g_sb[:], in_=g_d), writes=["gcol"])
        xv = xT.rearrange("(c p) t -> p c t", p=128)
        ov = outT.rearrange("(c p) t -> p c t", p=128)
        for i, t0 in enumerate(range(0, TL, TT)):
            b = i % 2
            S.dma("sp", lambda e, t0=t0, b=b: e.dma_start(out=xin[b][:], in_=xv[:, :, t0:t0 + TT]), reads=["xT"], writes=[("fn_x", b)])
            emit_norm_T(S, nc, xin[b], ("fn_x", b), ho[b], ("fn_h", b), g_sb, ones_bf, sq, rstd, pss[0], ("ps", 0), TT)
            S.dma("act", lambda e, t0=t0, b=b: e.dma_start(out=ov[:, :, t0:t0 + TT], in_=ho[b][:]), reads=[("fn_h", b)], writes=["outT"])
        S.barrier_all()
        S.emit()


def build_dense_test(TL, d_ff, ncols_p):
    nc = bass.Bass("TRN2", target_bir_lowering=False)
    di = lambda n, s: nc.dram_tensor(n, s, F32, kind="ExternalInput").ap()
    xT = di("xT", [D_MODEL, TL]); g1 = di("g1", [128, KC]); g2 = di("g2", [128, KC]); gf = di("gf", [128, KC])
    w_in = di("w_in", [D_MODEL, ncols_p + 6144]); yT = di("yT", [1536, TL]); projw = di("projw", [1536, D_MODEL])
    wout = di("wout", [D_MODEL, D_MODEL]); wup = di("wup", [D_MODEL, 2 * d_ff]); conv = di("conv", [128, d_ff // 128, 3])
    wdown = di("wdown", [d_ff, D_MODEL])
    pT = nc.dram_tensor("pT", [ncols_p, TL], F32, kind="ExternalOutput").ap()
    x1T = nc.dram_tensor("x1T", [D_MODEL, TL], F32, kind="ExternalOutput").ap()
    x2T = nc.dram_tensor("x2T", [D_MODEL, TL], F32, kind="ExternalOutput").ap()
    outT = nc.dram_tensor("outT", [D_MODEL, TL], F32, kind="ExternalOutput").ap()
    phase_proj(nc, TL, xT, g1, w_in, pT, ncols_p)
    phase_merge(nc, TL, xT, g1, w_in, ncols_p, yT, projw, wout, x1T)
    phase_ffn(nc, TL, x1T, g2, wup, conv, wdown, x2T, d_ff)
    phase_final_norm(nc, TL, x2T, gf, outT)
    return nc


def build_full(TL, n_layers, d_ff, heads_a, heads_c, heads_b, final=True):
    nc = bass.Bass("TRN2", target_bir_lowering=False)
    di = lambda n, s: nc.dram_tensor(n, list(s), F32, kind="ExternalInput").ap()
    L = n_layers
    NHB = len(heads_b)
    xT = di("xT", [D_MODEL, TL])
    g1 = di("g1", [L, 128, KC]); g2 = di("g2", [L, 128, KC]); gf = di("gf", [128, KC])
    w_in = di("w_in", [L, D_MODEL, NP_ROWS + 3 * D_MODEL])
    projw = di("projw", [L, 1536, D_MODEL]); wout = di("w_out", [L, D_MODEL, D_MODEL])
    wup = di("ffn_up", [L, D_MODEL, 2 * d_ff]); conv = di("conv", [L, 128, d_ff // 128, 3]); wdown = di("ffn_down", [L, d_ff, D_MODEL])
    tabs_g = di("tabs_g", [20, 128, 256]); tabs_m = di("tabs_m", [20, 128, 256]); tabs_a = di("tabs_a", [20, 128, 256])
    sinks = di("sinks", [L, 64, 8]); ident = di("ident", [128, 128])
    prm = di("prm", [L, 64, NHB, 10]); lmu = di("lmu", [L, 128, 4])
    rwup = di("rw_up", [L, 96, NHB * 64]); raup = di("ra_up", [L, 96, NHB * 64]); rgup = di("rg_up", [L, 256, NHB * 64])
    mlt = di("m_lt2", [64, 3, 128]); mle = di("m_le2", [64, 3, 128]); mgt = di("m_gt", [64, 3, 64]); rst = di("rst", [64, 512])
    outT = nc.dram_tensor("outT", [D_MODEL, TL], F32, kind="ExternalOutput").ap()
    pT = nc.dram_tensor("pT_s", [NP_ROWS, TL], F32).ap()
    yT = nc.dram_tensor("yT_s", [1536, TL], F32).ap()
    x1T = nc.dram_tensor("x1T_s", [D_MODEL, TL], F32).ap()
    xs = [nc.dram_tensor("xs%d" % i, [D_MODEL, TL], F32).ap() for i in range(2)]
    NF = d_ff // 128
    sc_pj = nc.dram_tensor("sc_pj", [(NP_ROWS + 255) // 256, 128, KC * 256], BF16).ap()
    sc_mg = nc.dram_tensor("sc_mg", [KC, 128, KC * 3 * 128 + 12 * 128], BF16).ap()
    sc_wo = nc.dram_tensor("sc_wo", [KC, 128, KC * 128], BF16).ap()
    sc_up = nc.dram_tensor("sc_up", [NF, 128, KC * 256], BF16).ap()
    sc_dn = nc.dram_tensor("sc_dn", [KC, 128, NF * 128], BF16).ap()
    cur = xT
    for l in range(L):
        phase_proj(nc, TL, cur, g1[l], w_in[l], pT, NP_ROWS, scr=sc_pj)
        pa, pb = merge_panels(w_in[l], NP_ROWS, projw[l], wout[l])
        pu, pd = ffn_panels(wup[l], wdown[l], d_ff)
        phase_attn(nc, TL, pT, yT, tabs_g, tabs_m, tabs_a, sinks[l], ident, heads_a, heads_c,
                   precast_jobs=[(pa, sc_mg), (pb, sc_wo), (pu, sc_up), (pd, sc_dn)])
        phase_rwkv(nc, TL, pT, yT, prm[l], lmu[l], rwup[l], raup[l], rgup[l], ident, mlt, mle, mgt, rst, heads_b)
        phase_merge(nc, TL, cur, g1[l], w_in[l], NP_ROWS, yT, projw[l], wout[l], x1T, scr=sc_mg, scr_o=sc_wo, precast_done=True)
        nxt = outT if (l == L - 1 and not final) else xs[l % 2]
        phase_ffn(nc, TL, x1T, g2[l], wup[l], conv[l], wdown[l], nxt, d_ff, scr_u=sc_up, scr_d=sc_dn, precast_done=True)
        cur = nxt
    if final:
        phase_final_norm(nc, TL, cur, gf, outT)
    return nc


def phase_attn(nc, T, pT, yT, tg, tm, ta, sk, idd, heads_a, heads_c, precast_jobs=None):
    import contextlib
    with contextlib.ExitStack() as st:
        pss = [st.enter_context(nc.psum_tensor(uname("aps%d" % i), [128, 512], F32)) for i in range(8)]
        _PH[0] += 1
        S = Sched(nc)
        between = None
        gen = None
        if precast_jobs:
            mx = max(sum(int(np.prod(shp[1:])) for shp in shapes) for panels, _ in precast_jobs for _, shapes in panels)
            ws = WeightStream(S, nc, st, "apc_w", mx, n_stage=1, n_bf=1)
            n_pan = sum(len(p) for p, _ in precast_jobs)
            n_units = len(heads_a) + 3 * len(heads_c)
            per = (n_pan + n_units - 1) // n_units

            def chain():
                for ji, (panels, scr) in enumerate(precast_jobs):
                    yield from precast_gen(S, ws, panels, scr, ji)

            gen = chain()

            def between():
                for _ in range(per):
                    try:
                        next(gen)
                    except StopIteration:
                        return
        emit_attention(S, nc, st, T, pT, yT, tg, tm, ta, sk, idd, pss, heads_a, heads_c, between=between)
        if gen is not None:
            for _ in gen:
                pass
        S.barrier_all()
        S.emit()


def host_inputs(inp, n_layers, d_ff, heads_b):
    L = n_layers
    f32 = lambda a: np.ascontiguousarray(np.asarray(a, dtype=np.float32))
    gl = lambda g: np.ascontiguousarray(np.asarray(g, np.float32).reshape(-1, KC, 128).transpose(0, 2, 1))
    tg, tm, ta = attn_tables(np.asarray(inp["rel_bias"], np.float32))
    d = dict(g1=gl(inp["norm1_g"][:L]), g2=gl(inp["norm2_g"][:L]), gf=gl(inp["final_g"])[0],
             w_in=f32(inp["w_in"][:L]),
             projw=f32(np.concatenate([np.asarray(inp["proj_a"][:L]), np.asarray(inp["proj_b"][:L]), np.asarray(inp["proj_c"][:L])], axis=1)),
             w_out=f32(inp["w_out"][:L]), ffn_up=f32(inp["ffn_up"][:L]), ffn_down=f32(inp["ffn_down"][:L]),
             conv=f32(np.asarray(inp["ffn_conv"][:L]).transpose(0, 2, 1).reshape(L, d_ff // 128, 128, 3).transpose(0, 2, 1, 3)),
             tabs_g=tg, tabs_m=tm, tabs_a=ta,
             sinks=f32(np.broadcast_to(np.asarray(inp["attn_sinks"][:L])[:, None, :], (L, 64, 8))),
             ident=np.eye(128, dtype=np.float32))
    prm, lmu, wu, au, gu = [], [], [], [], []
    for l in range(L):
        hp = rwkv_host_params(heads_b, *[np.asarray(inp[k][l], np.float32) for k in
                                         ("rwkv_mu", "rwkv_w0", "rwkv_w_up", "rwkv_a0", "rwkv_a_up", "rwkv_g_up", "rwkv_k_k",
                                          "rwkv_k_a", "rwkv_r_k", "rwkv_lnx_g", "rwkv_lnx_b")])
        prm.append(hp["prm"]); lmu.append(hp["lmu"]); wu.append(hp["wup"]); au.append(hp["aup"]); gu.append(hp["gup"])
    d.update(prm=np.stack(prm), lmu=np.stack(lmu), rw_up=np.stack(wu), ra_up=np.stack(au), rg_up=np.stack(gu))
    d.update(rwkv_consts())
    return d


_NC_CACHE = {}
N_LAUNCH = 1


def kernel(**inp):
    x = np.asarray(inp["x"], np.float32)
    Bn, Sq, Dm = x.shape
    L = np.asarray(inp["w_in"]).shape[0]
    d_ff = np.asarray(inp["ffn_down"]).shape[1]
    heads_a, heads_c, heads_b = list(range(8)), list(range(4)), list(range(12))
    nl = N_LAUNCH if L % N_LAUNCH == 0 else 1
    Lp = L // nl
    xTs = [np.ascontiguousarray(x[b].T) for b in range(Bn)]
    per_layer = ("norm1_g", "w_in", "attn_sinks", "rwkv_mu", "rwkv_w0", "rwkv_w_up", "rwkv_a0", "rwkv_a_up", "rwkv_g_up", "rwkv_k_k",
                 "rwkv_k_a", "rwkv_r_k", "rwkv_lnx_g", "rwkv_lnx_b", "proj_a", "proj_b", "proj_c", "w_out", "norm2_g", "ffn_up",
                 "ffn_conv", "ffn_down")
    for li in range(nl):
        final = (li == nl - 1)
        key = (Sq, Lp, d_ff, final)
        if key not in _NC_CACHE:
            _NC_CACHE[key] = build_full(Sq, Lp, d_ff, heads_a, heads_c, heads_b, final=final)
        nc = _NC_CACHE[key]
        sub = dict(inp)
        for k in per_layer:
            sub[k] = np.asarray(inp[k])[li * Lp:(li + 1) * Lp]
        shared = host_inputs(sub, Lp, d_ff, heads_b)
        in_maps = []
        for b in range(Bn):
            m = dict(shared)
            m["xT"] = xTs[b]
            in_maps.append(m)
        res = run_bass_kernel_spmd(nc, in_maps, core_ids=list(range(Bn)))
        xTs = [np.ascontiguousarray(r["outT"]) for r in res.results]
    out = np.stack([np.ascontiguousarray(t.T) for t in xTs], axis=0)
    return out.astype(np.float32)
```

```python
import concourse.bass as bass
import concourse.mybir as mybir

ENGS = ("pe", "dve", "act", "pool", "sp")


_PH = [0]


def uname(n):
    return "%s_%d" % (n, _PH[0])


class Sched:
    _uid = 0

    def __init__(self, nc, n_dma_sems=24):
        self.nc = nc
        self.streams = {e: [] for e in ENGS}
        self.seq = {e: 0 for e in ENGS}
        self.waited = {}
        self.lastw = {}
        self.readers = {}
        self.n_dma = n_dma_sems
        self.dma_cnt = [0] * n_dma_sems
        self.dma_rr = 0
        self.sems = {}
        self.n_wait = 0
        self.alias = {}
        self.hist = {e: [] for e in ENGS}
        self.dma_snap = {}

    def _clock(self, e):
        return {p: v for (c, p), v in self.waited.items() if c == e}

    def _merge(self, cons, snap):
        for p, v in snap.items():
            if p == ("eng", cons):
                continue
            if self.waited.get((cons, p), 0) < v:
                self.waited[(cons, p)] = v

    def _absorb(self, cons, waits):
        import bisect
        for p, v in waits:
            if p[0] == "eng":
                h = self.hist[p[1]]
                if h:
                    i = bisect.bisect_right(h, v, key=lambda t: t[0]) - 1
                    if i >= 0:
                        self._merge(cons, h[i][1])
            else:
                snap = self.dma_snap.get((p[1], v))
                if snap:
                    self._merge(cons, snap)

    def canon(self, keys):
        return [self.alias.get(k, k) for k in keys]

    def eng(self, e):
        nc = self.nc
        return {"pe": nc.tensor, "dve": nc.vector, "act": nc.scalar, "pool": nc.gpsimd, "sp": nc.sync}[e]

    def _need(self, cons, deps):
        best = {}
        for p, v in deps:
            if p is None:
                continue
            if v > best.get(p, 0):
                best[p] = v
        out = []
        for p, v in sorted(best.items(), key=lambda kv: (kv[0][0] != "eng", -kv[1])):
            if self.waited.get((cons, p), 0) >= v:
                continue
            self.waited[(cons, p)] = v
            out.append((p, v))
            self._absorb(cons, [(p, v)])
        return out

    def _deps(self, e, reads, writes, same_engine_war=False):
        deps = []
        me = ("eng", e)
        for k in reads:
            w = self.lastw.get(k)
            if w is not None:
                deps.append(w)
        for k in writes:
            w = self.lastw.get(k)
            if w is not None:
                deps.append(w)
            for p, v in self.readers.get(k, {}).items():
                deps.append((p, v))
        return deps

    def _record(self, prod, val, reads, writes):
        for k in reads:
            d = self.readers.setdefault(k, {})
            if d.get(prod, 0) < val:
                d[prod] = val
        for k in writes:
            self.lastw[k] = (prod, val)
            self.readers[k] = {}

    def op(self, e, fn, reads=(), writes=()):
        reads = self.canon(reads); writes = self.canon(writes)
        deps = self._deps(e, reads, writes)
        if e == "pe":
            deps = [d for d in deps if d[0] != ("eng", "pe")]
        waits = self._need(e, deps)
        self.seq[e] += 1
        val = self.seq[e]
        if waits or not self.hist[e]:
            self.hist[e].append((val, self._clock(e)))
        self.streams[e].append(("op", fn, waits, None))
        self._record(("eng", e), val, reads, writes)
        return val

    def dma(self, q, fn, reads=(), writes=()):
        reads = self.canon(reads); writes = self.canon(writes)
        deps = self._deps(q, reads, writes, same_engine_war=True)
        i = self.dma_rr
        self.dma_rr = (self.dma_rr + 1) % self.n_dma
        prod = ("dma", i)
        if self.dma_cnt[i] > 0:
            deps.append((prod, self.dma_cnt[i]))
        waits = self._need(q, deps)
        self.dma_cnt[i] += 16
        val = self.dma_cnt[i]
        snap = self._clock(q)
        if q != "sp" and self.seq[q] > 0:
            snap[("eng", q)] = max(snap.get(("eng", q), 0), 0)
        self.dma_snap[(i, val)] = snap
        self.streams[q].append(("dma", fn, waits, (i, 16)))
        self._record(prod, val, reads, writes)
        return prod, val

    def finish_waits(self, e="sp"):
        deps = [(("dma", i), c) for i, c in enumerate(self.dma_cnt) if c > 0]
        deps += [(("eng", x), self.seq[x]) for x in ENGS if self.seq[x] > 0 and x != e]
        waits = self._need(e, deps)
        self.streams[e].append(("wait", None, waits, None))

    def barrier_all(self):
        for e in ENGS:
            deps = [(("dma", i), c) for i, c in enumerate(self.dma_cnt) if c > 0]
            deps += [(("eng", x), self.seq[x]) for x in ENGS if self.seq[x] > 0 and x != e]
            waits = self._need(e, deps)
            self.streams[e].append(("wait", None, waits, None))

    def emit(self):
        nc = self.nc
        Sched._uid += 1
        u = Sched._uid
        esem = {e: nc.alloc_semaphore("s%d_%s" % (u, e)) for e in ENGS}
        dsem = [nc.alloc_semaphore("d%d_%d" % (u, i)) for i in range(self.n_dma)]

        def semof(p):
            return esem[p[1]] if p[0] == "eng" else dsem[p[1]]

        def run(e):
            def body(engine):
                for kind, fn, waits, dinfo in self.streams[e]:
                    for p, v in waits:
                        engine.wait_ge(semof(p), v)
                        self.n_wait += 1
                    if kind == "op":
                        fn(engine).then_inc(esem[e], 1)
                    elif kind == "dma":
                        fn(engine).then_inc(dsem[dinfo[0]], dinfo[1])
            return body

        with nc.Block() as block:
            block.tensor(run("pe"))
            block.vector(run("dve"))
            block.scalar(run("act"))
            block.gpsimd(run("pool"))
            block.sync(run("sp"))
        nc.all_engine_barrier()
        nc.clear_and_free_semaphores(list(esem.values()) + dsem)
        nc.all_engine_barrier()


import numpy as np
from concourse.bass_utils import run_bass_kernel_spmd

F32 = mybir.dt.float32
BF16 = mybir.dt.bfloat16
ALU = mybir.AluOpType
AF = mybir.ActivationFunctionType
AX = mybir.AxisListType

D_MODEL = 2048
NORM_EPS = 1e-5
KC = D_MODEL // 128


def emit_rmsnorm_T(S, nc, xT, hT, g_sb, ones_bf, sq, ps, rstd, ntok, keys):
    kx, kh, ksq, kps, krs = keys["x"], keys["h"], keys["sq"], keys["ps"], keys["rstd"]
    for c in range(KC):
        S.op("act", lambda e, c=c: e.activation(out=sq[:, c, :], in_=xT[:, c, :], func=AF.Square),
             reads=[kx], writes=[(ksq, c)])
    for c in range(KC):
        S.op("pe", lambda e, c=c: e.matmul(ps, ones_bf, sq[:, c, :], start=(c == 0), stop=(c == KC - 1)),
             reads=[(ksq, c)], writes=[kps])
    S.op("dve", lambda e: e.tensor_scalar(out=rstd, in0=ps, scalar1=1.0 / D_MODEL, scalar2=NORM_EPS,
                                          op0=ALU.mult, op1=ALU.add), reads=[kps], writes=[krs])
    S.op("act", lambda e: e.activation(out=rstd, in_=rstd, func=AF.Sqrt), reads=[krs], writes=[krs])
    S.op("dve", lambda e: e.reciprocal(out=rstd, in_=rstd), reads=[krs], writes=[krs])
    for c in range(KC):
        S.op("dve", lambda e, c=c: e.scalar_tensor_tensor(out=hT[:, c, :], in0=xT[:, c, :], scalar=g_sb[:, c:c + 1],
                                                         in1=rstd, op0=ALU.mult, op1=ALU.mult),
             reads=[kx, krs], writes=[kh])


def build_proj(T, ncols, TT=512):
    nc = bass.Bass("TRN2", target_bir_lowering=False)
    xT = nc.dram_tensor("xT", [D_MODEL, T], F32, kind="ExternalInput").ap()
    g = nc.dram_tensor("g", [128, KC], F32, kind="ExternalInput").ap()
    W = nc.dram_tensor("W", [D_MODEL, ncols], F32, kind="ExternalInput").ap()
    pT = nc.dram_tensor("pT", [ncols, T], F32, kind="ExternalOutput").ap()
    ntt = T // TT
    CB = 512
    ncb = (ncols + CB - 1) // CB
    import contextlib
    with contextlib.ExitStack() as st:
        sb = lambda name, shape, dt: st.enter_context(nc.sbuf_tensor(uname(name), shape, dt))
        ones_bf = sb("ones", [128, 128], BF16)
        g_sb = sb("g_sb", [128, KC], F32)
        hT = sb("hT", [128, KC, T], BF16)
        xin = [sb("xin%d" % i, [128, KC, TT], F32) for i in range(2)]
        sq = sb("sq", [128, KC, TT], BF16)
        rstd = sb("rstd", [128, TT], F32)
        wt = [sb("wt%d" % i, [128, KC, CB], BF16) for i in range(2)]
        ot = [sb("ot%d" % i, [128, TT], F32) for i in range(4)]
        pss = [st.enter_context(nc.psum_tensor(uname("ps%d" % i), [128, 512], F32)) for i in range(8)]
        _PH[0] += 1
        S = Sched(nc)
        S.op("pool", lambda e: e.memset(ones_bf[:], 1.0), writes=["ones"])
        S.dma("sp", lambda e: e.dma_start(out=g_sb[:], in_=g), writes=["g"])
        xTv = xT.rearrange("(c p) t -> p c t", p=128)
        Wv = W.rearrange("(c p) n -> p c n", p=128)
        for tt in range(ntt):
            xb = xin[tt % 2]
            S.dma("sp", lambda e, xb=xb, tt=tt: e.dma_start(out=xb[:], in_=xTv[:, :, tt * TT:(tt + 1) * TT]),
                  writes=[("xin", tt % 2)])
            keys = dict(x=("xin", tt % 2), h=("h", tt), sq="sq", ps=("ps", 0), rstd="rstd")
            emit_rmsnorm_T(S, nc, xb[:], hT[:, :, tt * TT:(tt + 1) * TT], g_sb[:], ones_bf[:], sq[:], pss[0][:, :TT],
                           rstd[:], TT, keys)
        n_o = 0
        n_ps = 0
        for cb in range(ncb):
            c0 = cb * CB
            cw = min(CB, ncols - c0)
            wb = wt[cb % 2]
            S.dma("pool", lambda e, wb=wb, c0=c0, cw=cw: e.dma_start(out=wb[:, :, :cw], in_=Wv[:, :, c0:c0 + cw]),
                  writes=[("wt", cb % 2)])
            for m0 in range(0, cw, 128):
                mw = min(128, cw - m0)
                for tt in range(ntt):
                    pi = 1 + (n_ps % 7); n_ps += 1
                    ps = pss[pi]
                    for c in range(KC):
                        S.op("pe", lambda e, ps=ps, wb=wb, c=c, m0=m0, mw=mw, tt=tt:
                             e.matmul(ps[:mw, :TT], wb[:, c, m0:m0 + mw], hT[:, c, tt * TT:(tt + 1) * TT],
                                      start=(c == 0), stop=(c == KC - 1)),
                             reads=[("wt", cb % 2), ("h", tt), "ones", "g"], writes=[("ps", pi)])
                    oi = n_o % 4; n_o += 1
                    ob = ot[oi]
                    eng = "act" if (n_o % 2) else "dve"
                    if eng == "act":
                        S.op("act", lambda e, ob=ob, ps=ps, mw=mw: e.copy(out=ob[:mw, :], in_=ps[:mw, :TT]),
                             reads=[("ps", pi)], writes=[("ot", oi)])
                    else:
                        S.op("dve", lambda e, ob=ob, ps=ps, mw=mw: e.tensor_copy(out=ob[:mw, :], in_=ps[:mw, :TT]),
                             reads=[("ps", pi)], writes=[("ot", oi)])
                    S.dma("sp", lambda e, ob=ob, mw=mw, r0=c0 + m0, tt=tt:
                          e.dma_start(out=pT[r0:r0 + mw, tt * TT:(tt + 1) * TT], in_=ob[:mw, :]),
                          reads=[("ot", oi)])
        S.finish_waits("sp")
        S.emit()
    return nc


import math

ROW_AQ, ROW_AK, ROW_AV = 0, 512, 640
ROW_BR, ROW_BK, ROW_BV, ROW_WD, ROW_AD, ROW_GD = 768, 1536, 2304, 3072, 3168, 3264
ROW_CQ, ROW_CK, ROW_CV = 3520, 4288, 5056
NP_ROWS = 5824
YROW_A, YROW_B, YROW_C = 0, 512, 1280
C_DILS = (1, 4, 16)


def ssl(c0, n, d):
    return slice(c0, c0 + d * (n - 1) + 1, d)


def t5_bucket_np(dist):
    dist = np.asarray(dist, np.int64)
    nf = np.maximum(dist, 1).astype(np.float32)
    large = 16 + (np.log(nf / np.float32(16)) / np.float32(math.log(2048 / 16)) * np.float32(16)).astype(np.int32)
    return np.where(dist < 16, dist, np.minimum(large, 31))


def attn_tables(rel_bias):
    i = np.arange(128)[None, :]
    j = np.arange(128)[:, None]
    dists = (i - j, i + 128 - j)
    specs = [(h, 1, 127) for h in range(8)] + [(8 + g * 4 + hh, dil, 128) for g, dil in enumerate(C_DILS)
                                              for hh in range(4)]
    gathered = np.zeros((20, 128, 256), np.float32)
    mul = np.zeros((20, 128, 256), np.float32)
    add = np.zeros((20, 128, 256), np.float32)
    for n, (col, dil, ms) in enumerate(specs):
        for half, d in enumerate(dists):
            valid = (d >= 0) & (d <= ms)
            idx = t5_bucket_np(np.maximum(d, 0) * dil)
            gathered[n, :, half * 128:(half + 1) * 128] = rel_bias[idx, col]
            mul[n, :, half * 128:(half + 1) * 128] = np.where(valid, 8.0, 0.0)
            add[n, :, half * 128:(half + 1) * 128] = np.where(valid, 0.0, -240000.0)
    return gathered, mul, add


def emit_attention(S, nc, st, T, pT, yT, tabs_g, tabs_m, tabs_a, sinks_rep, ident_d, pss, heads_a, heads_c, between=None):
    sb = lambda name, shape, dt: st.enter_context(nc.sbuf_tensor(uname(name), shape, dt))
    NB = T // 128
    q_bf = sb("at_q", [64, T], BF16)
    k_bf = sb("at_k", [64, T], BF16)
    v_f = sb("at_v", [64, T], F32)
    stg = sb("at_stg", [64, T], F32)
    vaug = sb("at_vaug", [128, NB, 65], BF16)
    acc = sb("at_acc", [65, T], F32)
    tb_g = sb("at_tbg", [128, 256], F32)
    tb_m = sb("at_tbm", [128, 256], F32)
    tb_a = sb("at_tba", [128, 256], F32)
    tb = sb("at_tb", [128, 256], BF16)
    pt_sb = [sb("at_pt%d" % i, [128, 512], BF16) for i in range(2)]
    ident_f = sb("at_identf", [128, 128], F32)
    ident_b = sb("at_identb", [128, 128], BF16)
    sel = sb("at_sel", [65, 64], F32)
    esink = sb("at_esink", [64, 8], F32)
    den = sb("at_den", [64, 512], F32)
    yo = [sb("at_yo%d" % i, [64, 512], F32) for i in range(2)]

    S.dma("sp", lambda e: e.dma_start(out=ident_f[:], in_=ident_d), writes=["at_identf"])
    S.op("dve", lambda e: e.tensor_copy(out=ident_b[:], in_=ident_f[:]), reads=["at_identf"], writes=["at_identb"])
    S.op("pool", lambda e: e.memset(sel[:], 0.0), writes=["at_sel"])
    S.op("pool", lambda e: e.memset(sel[64:65, :], 1.0), writes=["at_sel"])
    S.op("pool", lambda e: e.memset(vaug[:, :, 64:65], 1.0), writes=["at_vaug1"])
    S.dma("sp", lambda e: e.dma_start(out=esink[:], in_=sinks_rep), writes=["at_esink"])
    S.op("act", lambda e: e.activation(out=esink[:], in_=esink[:], func=AF.Exp), reads=["at_esink"],
         writes=["at_esink"])

    ps_s = [pss[0], pss[1]]
    ps_o = [pss[2], pss[3]]
    ps_t = pss[4]
    ps_d = pss[5]
    cnt = dict(s=0, o=0, y=0)

    def load_table(n):
        S.dma("sp", lambda e: e.dma_start(out=tb_g[:], in_=tabs_g[n]), writes=["at_tbg"])
        S.dma("sp", lambda e: e.dma_start(out=tb_m[:], in_=tabs_m[n]), writes=["at_tbm"])
        S.dma("sp", lambda e: e.dma_start(out=tb_a[:], in_=tabs_a[n]), writes=["at_tba"])
        S.op("pool", lambda e: e.tensor_tensor(out=tb_g[:], in0=tb_g[:], in1=tb_m[:], op=ALU.mult),
             reads=["at_tbg", "at_tbm"], writes=["at_tbg"])
        S.op("pool", lambda e: e.tensor_tensor(out=tb[:], in0=tb_g[:], in1=tb_a[:], op=ALU.add),
             reads=["at_tbg", "at_tba"], writes=["at_tb"])

    def load_kv(krow, vrow, dil):
        S.dma("act", lambda e: e.dma_start(out=stg[:], in_=pT[krow:krow + 64, :]), reads=["pT"], writes=["at_stg"])
        S.op("pool", lambda e: e.tensor_copy(out=k_bf[:], in_=stg[:]), reads=["at_stg"], writes=["at_k"])
        S.dma("sp", lambda e: e.dma_start(out=v_f[:], in_=pT[vrow:vrow + 64, :]), reads=["pT"], writes=["at_v"])
        Lf = T // dil
        bps = Lf // 128
        for vb0 in range(0, NB, 8):
            for u in range(8):
                vb = vb0 + u
                s_, jb = vb // bps, vb % bps
                c0 = s_ + dil * 128 * jb
                src = v_f[0:64, ssl(c0, 128, dil)]
                S.op("pe", lambda e, u=u, src=src: e.transpose(ps_t[:, u * 64:(u + 1) * 64], src, ident_f[0:64, 0:64]),
                     reads=["at_v", "at_identf"], writes=["ps_t"])
            S.op("dve", lambda e, vb0=vb0: e.tensor_copy(out=vaug[:, vb0:vb0 + 8, 0:64],
                                                        in_=ps_t[:, :].rearrange("p (u d) -> p u d", d=64)),
                 reads=["ps_t"], writes=["at_vaug"])

    def run_seq(dil, first_group):
        Lf = T // dil
        bps = Lf // 128
        nbt = min(4, bps)
        for s_ in range(dil):
            for jb0 in range(0, bps, nbt):
                oi = cnt["o"] % 2; cnt["o"] += 1
                po = ps_o[oi]
                for half in range(0, nbt, 2):
                    si = cnt["s"] % 2; cnt["s"] += 1
                    pst = ps_s[si]
                    ptb = pt_sb[si]
                    nb2 = min(2, nbt - half)
                    lo = 512
                    for r in range(nb2):
                        jb = jb0 + half + r
                        qs = s_ + dil * 128 * jb
                        qv = q_bf[:, ssl(qs, 128, dil)]
                        kv = k_bf[:, ssl(qs, 128, dil)]
                        sl_prev = slice((2 * r) * 128, (2 * r + 1) * 128)
                        sl_cur = slice((2 * r + 1) * 128, (2 * r + 2) * 128)
                        if jb > 0:
                            ks = s_ + dil * 128 * (jb - 1)
                            kpv = k_bf[:, ssl(ks, 128, dil)]
                            S.op("pe", lambda e, pst=pst, sl=sl_prev, kpv=kpv, qv=qv:
                                 e.matmul(pst[:, sl], kpv, qv, start=True, stop=False),
                                 reads=["at_k", "at_q"], writes=[("ps_s", si)])
                            S.op("pe", lambda e, pst=pst, sl=sl_prev:
                                 e.matmul(pst[:, sl], ident_b[:], tb[:, 128:256], start=False, stop=True),
                                 reads=["at_tb", "at_identb"], writes=[("ps_s", si)])
                            lo = min(lo, sl_prev.start)
                        S.op("pe", lambda e, pst=pst, sl=sl_cur, kv=kv, qv=qv:
                             e.matmul(pst[:, sl], kv, qv, start=True, stop=False),
                             reads=["at_k", "at_q"], writes=[("ps_s", si)])
                        S.op("pe", lambda e, pst=pst, sl=sl_cur:
                             e.matmul(pst[:, sl], ident_b[:], tb[:, 0:128], start=False, stop=True),
                             reads=["at_tb", "at_identb"], writes=[("ps_s", si)])
                        lo = min(lo, sl_cur.start)
                    hi = nb2 * 256
                    S.op("act", lambda e, ptb=ptb, pst=pst, lo=lo, hi=hi:
                         e.activation(out=ptb[:, lo:hi], in_=pst[:, lo:hi], func=AF.Exp, scale=0.125),
                         reads=[("ps_s", si)], writes=[("at_pt", si)])
                    for r in range(nb2):
                        jb = jb0 + half + r
                        vb = s_ * bps + jb
                        osl = slice((half + r) * 128, (half + r + 1) * 128)
                        S.op("pe", lambda e, po=po, osl=osl, vb=vb, ptb=ptb, r=r, last=(jb == 0):
                             e.matmul(po[0:65, osl], vaug[:, vb, :], ptb[:, (2 * r + 1) * 128:(2 * r + 2) * 128],
                                      start=True, stop=last),
                             reads=["at_vaug", "at_vaug1", ("at_pt", si)], writes=[("ps_o", oi)])
                        if jb > 0:
                            S.op("pe", lambda e, po=po, osl=osl, vb=vb, ptb=ptb, r=r:
                                 e.matmul(po[0:65, osl], vaug[:, vb - 1, :], ptb[:, (2 * r) * 128:(2 * r + 1) * 128],
                                          start=False, stop=True),
                                 reads=["at_vaug", "at_vaug1", ("at_pt", si)], writes=[("ps_o", oi)])
                t0 = s_ + dil * 128 * jb0
                n_el = nbt * 128
                av = acc[:, ssl(t0, n_el, dil)]
                if first_group:
                    S.op("dve", lambda e, av=av, po=po, n_el=n_el: e.tensor_copy(out=av, in_=po[0:65, 0:n_el]),
                         reads=[("ps_o", oi)], writes=["at_acc"])
                else:
                    S.op("dve", lambda e, av=av, po=po, n_el=n_el:
                         e.tensor_tensor(out=av, in0=po[0:65, 0:n_el], in1=av, op=ALU.add),
                         reads=[("ps_o", oi), "at_acc"], writes=["at_acc"])

    def normalize(yrow, sink_col):
        for t0 in range(0, T, 512):
            S.op("pe", lambda e, t0=t0: e.matmul(ps_d[0:64, :], sel[:], acc[:, t0:t0 + 512], start=True, stop=True),
                 reads=["at_acc", "at_sel"], writes=["ps_d"])
            if sink_col is not None:
                S.op("dve", lambda e: e.tensor_scalar(out=den[:], in0=ps_d[0:64, :],
                                                      scalar1=esink[:, sink_col:sink_col + 1], scalar2=None,
                                                      op0=ALU.add),
                     reads=["ps_d", "at_esink"], writes=["at_den"])
                S.op("dve", lambda e: e.reciprocal(out=den[:], in_=den[:]), reads=["at_den"], writes=["at_den"])
            else:
                S.op("dve", lambda e: e.reciprocal(out=den[:], in_=ps_d[0:64, :]), reads=["ps_d"], writes=["at_den"])
            yi = cnt["y"] % 2; cnt["y"] += 1
            yb = yo[yi]
            S.op("dve", lambda e, yb=yb, t0=t0: e.tensor_tensor(out=yb[:], in0=acc[0:64, t0:t0 + 512], in1=den[:],
                                                                op=ALU.mult),
                 reads=["at_acc", "at_den"], writes=[("at_yo", yi)])
            S.dma("sp", lambda e, yb=yb, t0=t0: e.dma_start(out=yT[yrow:yrow + 64, t0:t0 + 512], in_=yb[:]),
                  reads=[("at_yo", yi)], writes=["yT"])

    last_kv = None
    for h in heads_a:
        kvh = h // 4
        if last_kv != kvh:
            load_kv(ROW_AK + 64 * kvh, ROW_AV + 64 * kvh, 1)
            last_kv = kvh
        S.dma("act", lambda e, h=h: e.dma_start(out=stg[:], in_=pT[ROW_AQ + 64 * h:ROW_AQ + 64 * h + 64, :]),
              reads=["pT"], writes=["at_stg"])
        S.op("pool", lambda e: e.tensor_copy(out=q_bf[:], in_=stg[:]), reads=["at_stg"], writes=["at_q"])
        load_table(h)
        run_seq(1, True)
        normalize(YROW_A + 64 * h, h)
        if between is not None:
            between()
    for hh in heads_c:
        for g, dil in enumerate(C_DILS):
            off = g * 256 + hh * 64
            load_kv(ROW_CK + off, ROW_CV + off, dil)
            S.dma("act", lambda e, off=off: e.dma_start(out=stg[:], in_=pT[ROW_CQ + off:ROW_CQ + off + 64, :]),
                  reads=["pT"], writes=["at_stg"])
            S.op("pool", lambda e: e.tensor_copy(out=q_bf[:], in_=stg[:]), reads=["at_stg"], writes=["at_q"])
            load_table(8 + g * 4 + hh)
            run_seq(dil, g == 0)
            if between is not None:
                between()
        normalize(YROW_C + 64 * hh, None)


def build_attn_test(T, heads_a, heads_c):
    nc = bass.Bass("TRN2", target_bir_lowering=False)
    pT = nc.dram_tensor("pT", [NP_ROWS, T], F32, kind="ExternalInput").ap()
    tg = nc.dram_tensor("tabs_g", [20, 128, 256], F32, kind="ExternalInput").ap()
    tm = nc.dram_tensor("tabs_m", [20, 128, 256], F32, kind="ExternalInput").ap()
    ta = nc.dram_tensor("tabs_a", [20, 128, 256], F32, kind="ExternalInput").ap()
    sk = nc.dram_tensor("sinks", [64, 8], F32, kind="ExternalInput").ap()
    idd = nc.dram_tensor("ident", [128, 128], F32, kind="ExternalInput").ap()
    yT = nc.dram_tensor("yT", [1536, T], F32, kind="ExternalOutput").ap()
    import contextlib
    with contextlib.ExitStack() as st:
        pss = [st.enter_context(nc.psum_tensor(uname("ps%d" % i), [128, 512], F32)) for i in range(8)]
        _PH[0] += 1
        S = Sched(nc)
        emit_attention(S, nc, st, T, pT, yT, tg, tm, ta, sk, idd, pss, heads_a, heads_c)
        S.finish_waits("sp")
        S.emit()
    return nc


CH = 64
B_GN_EPS = 64e-5


def rwkv_consts():
    s_ = np.arange(64)[:, None]; t_ = np.arange(64)[None, :]
    lt = (s_ < t_).astype(np.float32); le = (s_ <= t_).astype(np.float32); gt = (s_ > t_).astype(np.float32)
    m_lt2 = np.ascontiguousarray(np.broadcast_to(np.concatenate([lt, lt], 1)[:, None, :], (64, 3, 128)))
    m_le2 = np.ascontiguousarray(np.broadcast_to(np.concatenate([le, le], 1)[:, None, :], (64, 3, 128)))
    m_gt = np.ascontiguousarray(np.broadcast_to(gt[:, None, :], (64, 3, 64)))
    rst = np.ones((64, 512), np.float32); rst[:, ::64] = 0.0
    return dict(m_lt2=m_lt2, m_le2=m_le2, m_gt=m_gt, rst=rst)


def phase_rwkv(nc, T, pT, yT, prm_d, lmu_d, wup_d, aup_d, gup_d, ident_d, mlt_d, mle_d, mgt_d, rst_d, heads, dbg=None, stop=None):
    import contextlib
    HG = 3
    TT = 512
    NCK = TT // CH
    NH = len(heads)
    with contextlib.ExitStack() as st:
        sb = lambda name, shape, dt=F32: st.enter_context(nc.sbuf_tensor(uname(name), shape, dt))
        pss = [st.enter_context(nc.psum_tensor(uname("rps%d" % i), [128, 512], F32)) for i in range(8)]
        _PH[0] += 1
        S = Sched(nc)
        ident = sb("rw_ident", [128, 128]); m_lt2 = sb("rw_mlt", [64, 3, 128]); m_le2 = sb("rw_mle", [64, 3, 128])
        m_gt = sb("rw_mgt", [64, 3, 64]); rst = sb("rw_rst", [64, 512])
        prm = sb("rw_prm", [64, NH, 10]); lmu = sb("rw_lmu", [128, 4])
        wup = sb("rw_wup", [96, NH * 64]); aup = sb("rw_aup", [96, NH * 64]); gup = sb("rw_gup", [128, 2, NH * 64])
        ones64 = sb("rw_ones", [64, 64]); avg64 = sb("rw_avg", [64, 64]); rkb = sb("rw_rkb", [64, NH, 64])
        for t_, d_ in ((ident, ident_d), (m_lt2, mlt_d), (m_le2, mle_d), (m_gt, mgt_d), (rst, rst_d), (prm, prm_d),
                       (lmu, lmu_d), (wup, wup_d), (aup, aup_d)):
            S.dma("sp", lambda e, t_=t_, d_=d_: e.dma_start(out=t_[:], in_=d_), writes=["const"])
        S.dma("sp", lambda e: e.dma_start(out=gup[:], in_=gup_d.rearrange("(c p) n -> p c n", p=128)), writes=["const"])
        S.op("pool", lambda e: e.memset(ones64[:], 1.0), writes=["const"])
        S.op("pool", lambda e: e.memset(avg64[:], 1.0 / 64), writes=["const"])
        for hi in range(NH):
            S.op("dve", lambda e, hi=hi: e.tensor_scalar(out=rkb[:, hi, :], in0=ones64[:], scalar1=prm[:, hi, 7:8],
                                                        scalar2=None, op0=ALU.mult), reads=["const"], writes=["const"])
        lin = [sb("rw_lin%d" % i, [128, TT + 1]) for i in range(4)]
        ltmp = sb("rw_ltmp", [128, TT])
        th = sb("rw_th", [96, TT]); adm = sb("rw_adm", [96, TT]); sg = sb("rw_sg", [128, 2, TT])
        xin = [sb("rw_xin%d" % i, [64, HG, TT + 1]) for i in range(3)]
        names = ["rm", "km", "vm", "logw", "iclr", "g", "kkn", "k2", "sbon", "Lc", "G", "t0", "t1", "t2",
                 "at"]
        B = {n: sb("rw_" + n, [64, HG, TT]) for n in names}
        for n in ("rt", "bt", "kt", "atb"):
            B[n] = sb("rw_" + n, [64, HG, TT], BF16)
        B["bh"] = B["iclr"]; B["kh"] = B["kkn"]; B["y"] = B["logw"]
        S.alias.update({"bh": "iclr", "kh": "kkn", "y": "logw", ("yo", 0): "Lc", ("yo", 1): "Lc"})
        RhT = sb("rw_RhT", [64, NCK, HG, 64]); Y0T = sb("rw_Y0T", [64, NCK, HG, 64])
        MTa = sb("rw_MT", [64, NCK, HG, 64]); Na = sb("rw_N", [64, NCK, HG, 64])
        Wp = [sb("rw_W%d" % p, [64, HG, 128], BF16) for p in range(2)]; Vtokp = [sb("rw_Vtok%d" % p, [64, HG, 64], BF16) for p in range(2)]
        BKp = [sb("rw_BK%d" % p, [64, HG, 128], BF16) for p in range(2)]; AQp = [sb("rw_AQ%d" % p, [64, HG, 128], BF16) for p in range(2)]
        QPp = [[sb("rw_QP%d_%d" % (p, i), [64, HG, 128], BF16) for i in range(2)] for p in range(2)]
        ARKp = [sb("rw_ARK%d" % p, [64, HG, 128], BF16) for p in range(2)]
        Hs = [sb("rw_H%d" % i, [64, HG, 64]) for i in range(2)]
        yout = [B["Lc"], B["Lc"]]
        cnt = dict(l=0, m=0, y=0)

        def ps_l():
            i = cnt["l"] % 2; cnt["l"] += 1
            return pss[i], ("ps", i)

        def ps_m():
            i = 4 + cnt["m"] % 2; cnt["m"] += 1
            return pss[i], ("ps", i)

        def dve(fn, r, w): S.op("dve", fn, reads=r, writes=w)
        def act(fn, r, w): S.op("act", fn, reads=r, writes=w)
        def pool(fn, r, w): S.op("pool", fn, reads=r, writes=w)
        def pe(fn, r, w): S.op("pe", fn, reads=r, writes=w)

        for g0 in range(0, NH, HG):
            hs = heads[g0:g0 + HG]
            pool(lambda e: e.memset(Hs[0][:], 0.0), [], ["H0"])
            st_h = dict(hcur=0)

            def do_tile(ti, g0=g0, hs=hs, st_h=st_h):
                t0 = ti * TT
                srcs = [(ROW_WD, 96), (ROW_AD, 96), (ROW_GD, 128), (ROW_GD + 128, 128)]
                for i, (row, n) in enumerate(srcs):
                    if t0 == 0:
                        pool(lambda e, i=i, n=n: e.memset(lin[i][0:n, 0:1], 0.0), [], ["lin%d" % i])
                        S.dma("sp", lambda e, i=i, row=row, n=n: e.dma_start(out=lin[i][0:n, 1:TT + 1],
                                                                          in_=pT[row:row + n, 0:TT]),
                              reads=["pT"], writes=["lin%d" % i])
                    else:
                        S.dma("sp", lambda e, i=i, row=row, n=n: e.dma_start(out=lin[i][0:n, :],
                                                                          in_=pT[row:row + n, t0 - 1:t0 + TT]),
                              reads=["pT"], writes=["lin%d" % i])
                for i, row0 in enumerate((ROW_BR, ROW_BK, ROW_BV)):
                    for j, h in enumerate(hs):
                        row = row0 + 64 * h
                        if t0 == 0:
                            pool(lambda e, i=i, j=j: e.memset(xin[i][:, j, 0:1], 0.0), [], ["xin%d" % i])
                            S.dma("act", lambda e, i=i, j=j, row=row: e.dma_start(out=xin[i][:, j, 1:TT + 1],
                                                                              in_=pT[row:row + 64, 0:TT]),
                                  reads=["pT"], writes=["xin%d" % i])
                        else:
                            S.dma("act", lambda e, i=i, j=j, row=row: e.dma_start(out=xin[i][:, j, :],
                                                                              in_=pT[row:row + 64, t0 - 1:t0 + TT]),
                                  reads=["pT"], writes=["xin%d" % i])
                outs = [th, adm, sg[:, 0, :], sg[:, 1, :]]
                for i, (row, n) in enumerate(srcs):
                    pool(lambda e, i=i, n=n: e.tensor_tensor(out=ltmp[0:n, :], in0=lin[i][0:n, 0:TT], in1=lin[i][0:n, 1:TT + 1],
                                                           op=ALU.subtract), ["lin%d" % i], ["ltmp"])
                    o = outs[i]
                    dve(lambda e, i=i, n=n, o=o: e.scalar_tensor_tensor(out=o[0:n, :] if i < 2 else o, in0=ltmp[0:n, :],
                                                                       scalar=lmu[0:n, i:i + 1], in1=lin[i][0:n, 1:TT + 1],
                                                                       op0=ALU.mult, op1=ALU.add),
                        ["ltmp", "lin%d" % i, "const"], ["lo%d" % i])
                act(lambda e: e.activation(out=th[:], in_=th[:], func=AF.Tanh), ["lo0"], ["lo0"])
                act(lambda e: e.activation(out=sg[:, 0, :], in_=sg[:, 0, :], func=AF.Sigmoid), ["lo2"], ["lo2"])
                act(lambda e: e.activation(out=sg[:, 1, :], in_=sg[:, 1, :], func=AF.Sigmoid), ["lo3"], ["lo3"])
                for i, nm in enumerate(("rm", "km", "vm")):
                    pool(lambda e, i=i: e.tensor_tensor(out=B["t0"][:], in0=xin[i][:, :, 0:TT], in1=xin[i][:, :, 1:TT + 1],
                                                      op=ALU.subtract), ["xin%d" % i], ["t0"])
                    for j in range(HG):
                        dve(lambda e, i=i, j=j, nm=nm: e.scalar_tensor_tensor(
                            out=B[nm][:, j, :], in0=B["t0"][:, j, :], scalar=prm[:, g0 + j, i:i + 1],
                            in1=xin[i][:, j, 1:TT + 1], op0=ALU.mult, op1=ALU.add),
                            ["t0", "xin%d" % i, "const"], [nm])
                for j in range(HG):
                    hj = g0 + j
                    cs = slice(hj * 64, hj * 64 + 64)
                    p1, k1 = ps_l()
                    pe(lambda e, p1=p1, cs=cs: e.matmul(p1[0:64, :], wup[:, cs], th[:], start=True, stop=True),
                       ["lo0", "const"], [k1])
                    act(lambda e, p1=p1, j=j, hj=hj: e.activation(out=B["logw"][:, j, :], in_=p1[0:64, :], func=AF.Sigmoid,
                                                               bias=prm[:, hj, 3:4]), [k1, "const"], ["logw"])
                    p2, k2_ = ps_l()
                    pe(lambda e, p2=p2, cs=cs: e.matmul(p2[0:64, :], aup[:, cs], adm[:], start=True, stop=True),
                       ["lo1", "const"], [k2_])
                    act(lambda e, p2=p2, j=j, hj=hj: e.activation(out=B["iclr"][:, j, :], in_=p2[0:64, :], func=AF.Sigmoid,
                                                               bias=prm[:, hj, 4:5]), [k2_, "const"], ["iclr"])
                    p3, k3 = ps_l()
                    for c in range(2):
                        pe(lambda e, p3=p3, cs=cs, c=c: e.matmul(p3[0:64, :], gup[:, c, cs], sg[:, c, :], start=(c == 0),
                                                               stop=(c == 1)), ["lo2", "lo3", "const"], [k3])
                    act(lambda e, p3=p3, j=j: e.copy(out=B["g"][:, j, :], in_=p3[0:64, :]), [k3], ["g"])
                    dve(lambda e, j=j, hj=hj: e.tensor_scalar(out=B["kkn"][:, j, :], in0=B["km"][:, j, :],
                                                             scalar1=prm[:, hj, 5:6], scalar2=None, op0=ALU.mult),
                        ["km", "const"], ["kkn"])
                    pool(lambda e, j=j: e.tensor_tensor(out=B["t1"][:, j, :], in0=B["kkn"][:, j, :], in1=B["kkn"][:, j, :],
                                                      op=ALU.mult), ["kkn"], ["t1"])
                    p4, k4 = ps_l()
                    pe(lambda e, p4=p4, j=j: e.matmul(p4[0:64, :], ones64[:], B["t1"][:, j, :], start=True, stop=True),
                       ["t1", "const"], [k4])
                    act(lambda e, p4=p4, j=j: e.activation(out=B["t2"][:, j, :], in_=p4[0:64, :], func=AF.Sqrt), [k4], ["t2"])
                    dve(lambda e, j=j: e.tensor_scalar(out=B["t2"][:, j, :], in0=B["t2"][:, j, :], scalar1=1e-12, scalar2=None,
                                                      op0=ALU.max), ["t2"], ["t2"])
                    dve(lambda e, j=j: e.reciprocal(out=B["t2"][:, j, :], in_=B["t2"][:, j, :]), ["t2"], ["t2"])
                    dve(lambda e, j=j, hj=hj: e.tensor_scalar(out=B["k2"][:, j, :], in0=B["iclr"][:, j, :], scalar1=-1.0,
                                                             scalar2=prm[:, hj, 6:7], op0=ALU.add, op1=ALU.mult),
                        ["iclr", "const"], ["k2"])
                dve(lambda e: e.tensor_tensor(out=B["kkn"][:], in0=B["kkn"][:], in1=B["t2"][:], op=ALU.mult),
                    ["kkn", "t2"], ["kkn"])
                dve(lambda e: e.scalar_tensor_tensor(out=B["k2"][:], in0=B["k2"][:], scalar=1.0, in1=B["km"][:],
                                                     op0=ALU.add, op1=ALU.mult), ["k2", "km"], ["k2"])
                pool(lambda e: e.tensor_tensor(out=B["t1"][:], in0=B["rm"][:], in1=B["k2"][:], op=ALU.mult),
                     ["rm", "k2"], ["t1"])
                for j in range(HG):
                    p5, k5 = ps_l()
                    pe(lambda e, p5=p5, j=j: e.matmul(p5[0:64, :], rkb[:, g0 + j, :], B["t1"][:, j, :], start=True, stop=True),
                       ["t1", "const"], [k5])
                    act(lambda e, p5=p5, j=j: e.copy(out=B["sbon"][:, j, :], in_=p5[0:64, :]), [k5], ["sbon"])
                dve(lambda e: e.tensor_scalar(out=B["logw"][:], in0=B["logw"][:], scalar1=-math.exp(-0.5), scalar2=None,
                                              op0=ALU.mult), ["logw"], ["logw"])
                for j in range(HG):
                    dve(lambda e, j=j: e.tensor_tensor_scan(out=B["Lc"][:, j, :], data0=rst[:], data1=B["logw"][:, j, :],
                                                           initial=0.0, op0=ALU.mult, op1=ALU.add),
                        ["logw", "const"], ["Lc"])
                act(lambda e: e.activation(out=B["G"][:], in_=B["Lc"][:], func=AF.Exp), ["Lc"], ["G"])
                pool(lambda e: e.tensor_tensor(out=B["rt"][:], in0=B["rm"][:], in1=B["G"][:], op=ALU.mult), ["rm", "G"], ["rt"])
                dve(lambda e: e.tensor_tensor(out=B["t0"][:], in0=B["Lc"][:], in1=B["logw"][:], op=ALU.subtract),
                    ["Lc", "logw"], ["t0"])
                act(lambda e: e.activation(out=B["t0"][:], in_=B["t0"][:], func=AF.Exp), ["t0"], ["t0"])
                dve(lambda e: e.scalar_tensor_tensor(out=B["at"][:], in0=B["kkn"][:], scalar=-1.0, in1=B["t0"][:],
                                                     op0=ALU.mult, op1=ALU.mult), ["kkn", "t0"], ["at"])
                pool(lambda e: e.tensor_copy(out=B["atb"][:], in_=B["at"][:]), ["at"], ["atb"])
                pool(lambda e: e.tensor_tensor(out=B["t2"][:], in0=B["kkn"][:], in1=B["iclr"][:], op=ALU.mult),
                     ["kkn", "iclr"], ["t2"])
                act(lambda e: e.activation(out=B["t1"][:], in_=B["Lc"][:], func=AF.Exp, scale=-1.0), ["Lc", "t1"], ["t1"])
                dve(lambda e: e.tensor_tensor(out=B["bt"][:], in0=B["t2"][:], in1=B["t1"][:], op=ALU.mult), ["t2", "t1"], ["bt"])
                pool(lambda e: e.tensor_tensor(out=B["kt"][:], in0=B["k2"][:], in1=B["t1"][:], op=ALU.mult), ["k2", "t1"], ["kt"])
                for j in range(HG):
                    lc3 = B["Lc"][:, j, :].rearrange("p (c t) -> p c t", t=CH)
                    o3 = B["t0"][:, j, :].rearrange("p (c t) -> p c t", t=CH)
                    dve(lambda e, lc3=lc3, o3=o3: e.tensor_tensor(out=o3, in0=lc3[:, :, CH - 1:CH].to_broadcast([64, NCK, CH]),
                                                                 in1=lc3, op=ALU.subtract), ["Lc", "at"], ["t0"])
                act(lambda e: e.activation(out=B["t0"][:], in_=B["t0"][:], func=AF.Exp), ["t0"], ["t0"])
                dve(lambda e: e.tensor_tensor(out=B["bh"][:], in0=B["t2"][:], in1=B["t0"][:], op=ALU.mult), ["t2", "t0"], ["bh"])
                pool(lambda e: e.tensor_tensor(out=B["kh"][:], in0=B["k2"][:], in1=B["t0"][:], op=ALU.mult), ["k2", "t0"], ["kh"])

                if dbg is not None and ti == 0 and g0 == 0:
                    for di_, nm_ in enumerate(["rm", "km", "vm", "logw", "iclr", "g", "kkn", "k2", "sbon", "Lc", "G", "rt", "at", "bt", "kt", "bh", "kh"]):
                        S.dma("sp", lambda e, di_=di_, nm_=nm_: e.dma_start(out=dbg[di_], in_=B[nm_][:]), reads=[nm_], writes=["dbg"])
                if stop == "prep":
                    return
                def do_chunk(c, pb):
                    W, Vtok, BK, AQ, QP, ARK = Wp[pb], Vtokp[pb], BKp[pb], AQp[pb], QPp[pb], ARKp[pb]
                    kW, kV, kBK, kAQ, kARK = 'W%d' % pb, 'Vtok%d' % pb, 'BK%d' % pb, 'AQ%d' % pb, 'ARK%d' % pb
                    pw_i, pq_i = (6, 0) if pb == 0 else (7, 1)
                    csl = slice(c * CH, (c + 1) * CH)
                    tp1, ktp1 = pss[2], ("ps", 2)
                    tp2, ktp2 = pss[3], ("ps", 3)
                    for j in range(HG):
                        pe(lambda e, j=j: e.transpose(tp1[0:64, j * 128:j * 128 + 64], B["at"][:, j, csl], ident[0:64, 0:64]),
                           ["at", "const"], [ktp1])
                        pe(lambda e, j=j: e.transpose(tp1[0:64, j * 128 + 64:j * 128 + 128], B["vm"][:, j, csl], ident[0:64, 0:64]),
                           ["vm", "const"], [ktp1])
                        pe(lambda e, j=j: e.transpose(tp2[0:64, j * 128:j * 128 + 64], B["bh"][:, j, csl], ident[0:64, 0:64]),
                           ["bh", "const"], [ktp2])
                        pe(lambda e, j=j: e.transpose(tp2[0:64, j * 128 + 64:j * 128 + 128], B["kh"][:, j, csl], ident[0:64, 0:64]),
                           ["kh", "const"], [ktp2])
                    tp1v = tp1[0:64, 0:HG * 128].rearrange("p (h x) -> p h x", x=128)
                    tp2v = tp2[0:64, 0:HG * 128].rearrange("p (h x) -> p h x", x=128)
                    act(lambda e, tp1v=tp1v: e.copy(out=W[:, :, 0:64], in_=tp1v[:, :, 0:64]), [ktp1], [kW])
                    act(lambda e, tp1v=tp1v: e.copy(out=Vtok[:], in_=tp1v[:, :, 64:128]), [ktp1], [kV])
                    act(lambda e, tp2v=tp2v: e.copy(out=BK[:], in_=tp2v), [ktp2], [kBK])
                    yield
                    if stop == "c1":
                        return
                    m1, km1 = ps_m()
                    for j in range(HG):
                        pe(lambda e, m1=m1, j=j: e.matmul(m1[0:64, j * 128:j * 128 + 64], B["kt"][:, j, csl], B["atb"][:, j, csl],
                                                        start=True, stop=True), ["kt", "atb"], [km1])
                        pe(lambda e, m1=m1, j=j: e.matmul(m1[0:64, j * 128 + 64:j * 128 + 128], B["bt"][:, j, csl], B["atb"][:, j, csl],
                                                        start=True, stop=True), ["bt", "atb"], [km1])
                    m1v = m1[0:64, 0:HG * 128].rearrange("p (h x) -> p h x", x=128)
                    dve(lambda e, m1v=m1v: e.tensor_tensor(out=AQ[:], in0=m1v, in1=m_lt2[:], op=ALU.mult), [km1, "const"], [kAQ])
                    m2, km2 = ps_m()
                    for j in range(HG):
                        pe(lambda e, m2=m2, j=j: e.matmul(m2[0:64, j * 64:j * 64 + 64], B["atb"][:, j, csl], B["bt"][:, j, csl],
                                                        start=True, stop=True), ["bt", "atb"], [km2])
                    m2v = m2[0:64, 0:HG * 64].rearrange("p (h x) -> p h x", x=64)
                    qp = 0
                    dve(lambda e: e.tensor_copy(out=QP[0][:, :, 0:64], in_=AQ[:, :, 64:128]), [kAQ], ["QP%d_0" % pb])
                    dve(lambda e, m2v=m2v: e.tensor_tensor(out=QP[0][:, :, 64:128], in0=m2v, in1=m_gt[:], op=ALU.mult),
                        [km2, "const"], ["QP%d_0" % pb])
                    m3, km3 = ps_m()
                    for j in range(HG):
                        pe(lambda e, m3=m3, j=j: e.matmul(m3[0:64, j * 128:j * 128 + 64], B["bt"][:, j, csl], B["rt"][:, j, csl],
                                                        start=True, stop=True), ["bt", "rt"], [km3])
                        pe(lambda e, m3=m3, j=j: e.matmul(m3[0:64, j * 128 + 64:j * 128 + 128], B["kt"][:, j, csl], B["rt"][:, j, csl],
                                                        start=True, stop=True), ["kt", "rt"], [km3])
                    m3v = m3[0:64, 0:HG * 128].rearrange("p (h x) -> p h x", x=128)
                    dve(lambda e, m3v=m3v: e.tensor_tensor(out=ARK[:], in0=m3v, in1=m_le2[:], op=ALU.mult), [km3, "const"], [kARK])
                    yield
                    if stop == "c2":
                        return
                    m4, km4 = ps_m()
                    for j in range(HG):
                        pe(lambda e, m4=m4, j=j: e.matmul(m4[0:64, j * 64:j * 64 + 64], AQ[:, j, 0:64], Vtok[:, j, :],
                                                        start=True, stop=True), [kAQ, kV], [km4])
                    m4v = m4[0:64, 0:HG * 64].rearrange("p (h x) -> p h x", x=64)
                    act(lambda e, m4v=m4v: e.copy(out=W[:, :, 64:128], in_=m4v), [km4], [kW])
                    yield
                    for it in range(6):
                        Qb = QP[qp]
                        kq = "QP%d_%d" % (pb, qp)
                        pw, kpw = pss[pw_i], ("ps", pw_i)
                        for j in range(HG):
                            pe(lambda e, j=j, Qb=Qb: e.matmul(pw[0:64, j * 128:j * 128 + 128], Qb[:, j, 0:64], W[:, j, :],
                                                             start=True, stop=True), [kq, kW], [kpw])
                        pwv = pw[0:64, 0:HG * 128].rearrange("p (h x) -> p h x", x=128)
                        dve(lambda e, pwv=pwv: e.tensor_tensor(out=W[:], in0=pwv, in1=W[:], op=ALU.add), [kpw, kW], [kW])
                        yield
                        if it < 5:
                            pq, kpq = pss[pq_i], ("ps", pq_i)
                            for j in range(HG):
                                pe(lambda e, j=j, Qb=Qb: e.matmul(pq[0:64, j * 128:j * 128 + 64], Qb[:, j, 64:128], Qb[:, j, 0:64],
                                                                 start=True, stop=True), [kq], [kpq])
                                pe(lambda e, j=j, Qb=Qb: e.matmul(pq[0:64, j * 128 + 64:j * 128 + 128], Qb[:, j, 0:64], Qb[:, j, 64:128],
                                                                 start=True, stop=True), [kq], [kpq])
                            pqv = pq[0:64, 0:HG * 128].rearrange("p (h x) -> p h x", x=128)
                            qn = 1 - qp
                            act(lambda e, pqv=pqv, qn=qn: e.copy(out=QP[qn][:], in_=pqv), [kpq], ["QP%d_%d" % (pb, qn)])
                            qp = qn
                            yield
                    if stop == "c3":
                        return
                    m5, km5 = ps_m()
                    for j in range(HG):
                        pe(lambda e, m5=m5, j=j: e.matmul(m5[0:64, j * 64:j * 64 + 64], W[:, j, 0:64], ARK[:, j, 0:64],
                                                        start=True, stop=True), [kW, kARK], [km5])
                    m5b, km5b = ps_m()
                    for j in range(HG):
                        pe(lambda e, m5b=m5b, j=j: e.matmul(m5b[0:64, j * 64:j * 64 + 64], W[:, j, 64:128], ARK[:, j, 0:64],
                                                          start=True, stop=False), [kW, kARK], [km5b])
                        pe(lambda e, m5b=m5b, j=j: e.matmul(m5b[0:64, j * 64:j * 64 + 64], Vtok[:, j, :], ARK[:, j, 64:128],
                                                          start=False, stop=True), [kV, kARK], [km5b])
                    m5v = m5[0:64, 0:HG * 64].rearrange("p (h x) -> p h x", x=64)
                    m5bv = m5b[0:64, 0:HG * 64].rearrange("p (h x) -> p h x", x=64)
                    dve(lambda e, m5v=m5v, c=c: e.tensor_tensor(out=RhT[:, c, :, :], in0=m5v, in1=B["rt"][:, :, csl],
                                                              op=ALU.add), [km5, "rt"], ["RhT"])
                    act(lambda e, m5bv=m5bv, c=c: e.copy(out=Y0T[:, c, :, :], in_=m5bv), [km5b], ["Y0T"])
                    if stop == "c4":
                        return
                    m6, km6 = ps_m()
                    for j in range(HG):
                        pe(lambda e, m6=m6, j=j: e.matmul(m6[0:64, j * 64:j * 64 + 64], W[:, j, 0:64], BK[:, j, 0:64],
                                                        start=True, stop=True), [kW, kBK], [km6])
                    m6b, km6b = ps_m()
                    for j in range(HG):
                        pe(lambda e, m6b=m6b, j=j: e.matmul(m6b[0:64, j * 64:j * 64 + 64], BK[:, j, 0:64], W[:, j, 64:128],
                                                          start=True, stop=False), [kW, kBK], [km6b])
                        pe(lambda e, m6b=m6b, j=j: e.matmul(m6b[0:64, j * 64:j * 64 + 64], BK[:, j, 64:128], Vtok[:, j, :],
                                                          start=False, stop=True), [kV, kBK], [km6b])
                    m6v = m6[0:64, 0:HG * 64].rearrange("p (h x) -> p h x", x=64)
                    m6bv = m6b[0:64, 0:HG * 64].rearrange("p (h x) -> p h x", x=64)
                    for j in range(HG):
                        gc = B["G"][:, j, c * CH + CH - 1:c * CH + CH]
                        dve(lambda e, m6v=m6v, j=j, c=c, gc=gc: e.scalar_tensor_tensor(
                            out=MTa[:, c, j, :], in0=ident[0:64, 0:64], scalar=gc, in1=m6v[:, j, :],
                            op0=ALU.mult, op1=ALU.add), [km6, "G", "const"], ["MT"])
                    act(lambda e, m6bv=m6bv, c=c: e.copy(out=Na[:, c, :, :], in_=m6bv), [km6b], ["N"])

                for c in range(0, NCK, 2):
                    alive = [do_chunk(c, 0), do_chunk(c + 1, 1)]
                    while alive:
                        for g_ in list(alive):
                            try:
                                next(g_)
                            except StopIteration:
                                alive.remove(g_)
                if stop in ("chunk", "c1", "c2", "c3", "c4"):
                    return

                def do_seq(c):
                    hcur = st_h["hcur"]
                    Hc = Hs[hcur]; Hn = Hs[1 - hcur]
                    kh_, kn_ = "H%d" % hcur, "H%d" % (1 - hcur)
                    py, kpy = ps_m()
                    for j in range(HG):
                        pe(lambda e, py=py, j=j, Hc=Hc, c=c: e.matmul(py[0:64, j * 64:j * 64 + 64], Hc[:, j, :], RhT[:, c, j, :],
                                                                    start=True, stop=True), [kh_, "RhT"], [kpy])
                    pyv = py[0:64, 0:HG * 64].rearrange("p (h x) -> p h x", x=64)
                    dve(lambda e, pyv=pyv, c=c: e.tensor_tensor(out=B["y"][:, :, c * CH:(c + 1) * CH], in0=pyv, in1=Y0T[:, c, :, :],
                                                              op=ALU.add), [kpy, "Y0T"], ["y"])
                    ph, kph = ps_m()
                    for j in range(HG):
                        pe(lambda e, ph=ph, j=j, Hc=Hc, c=c: e.matmul(ph[0:64, j * 64:j * 64 + 64], MTa[:, c, j, :], Hc[:, j, :],
                                                                    start=True, stop=True), [kh_, "MT"], [kph])
                    phv = ph[0:64, 0:HG * 64].rearrange("p (h x) -> p h x", x=64)
                    dve(lambda e, phv=phv, c=c, Hn=Hn: e.tensor_tensor(out=Hn[:], in0=phv, in1=Na[:, c, :, :], op=ALU.add),
                        [kph, "N"], [kn_])
                    st_h["hcur"] = 1 - hcur

                for c in range(NCK):
                    do_seq(c)
                if dbg is not None and ti == 0 and g0 == 0:
                    S.dma("sp", lambda e: e.dma_start(out=dbg[17], in_=B["y"][:]), reads=["y"], writes=["dbg"])
                    for di_, nm_ in enumerate([RhT, Y0T, MTa, Na]):
                        S.dma("sp", lambda e, di_=di_, nm_=nm_: e.dma_start(out=dbg[18 + di_].rearrange("p h x -> p (h x)"), in_=nm_[:].rearrange("p c h x -> p (c h x)")),
                              reads=["RhT", "Y0T", "MT", "N"], writes=["dbg"])
                yi = cnt["y"] % 2; cnt["y"] += 1
                yb = yout[yi]
                for j in range(HG):
                    hj = g0 + j
                    p6, k6 = ps_l()
                    pe(lambda e, p6=p6, j=j: e.matmul(p6[0:64, :], avg64[:], B["y"][:, j, :], start=True, stop=True),
                       ["y", "const"], [k6])
                    dve(lambda e, p6=p6, j=j: e.tensor_tensor(out=B["t0"][:, j, :], in0=B["y"][:, j, :], in1=p6[0:64, :],
                                                            op=ALU.subtract), [k6, "y"], ["t0"])
                    pool(lambda e, j=j: e.tensor_tensor(out=B["t1"][:, j, :], in0=B["t0"][:, j, :], in1=B["t0"][:, j, :],
                                                      op=ALU.mult), ["t0"], ["t1"])
                    p7, k7 = ps_l()
                    pe(lambda e, p7=p7, j=j: e.matmul(p7[0:64, :], avg64[:], B["t1"][:, j, :], start=True, stop=True),
                       ["t1", "const"], [k7])
                    dve(lambda e, p7=p7, j=j: e.tensor_scalar(out=B["t2"][:, j, :], in0=p7[0:64, :], scalar1=B_GN_EPS, scalar2=None,
                                                            op0=ALU.add), [k7], ["t2"])
                    act(lambda e, j=j: e.activation(out=B["t2"][:, j, :], in_=B["t2"][:, j, :], func=AF.Sqrt), ["t2"], ["t2"])
                    dve(lambda e, j=j: e.reciprocal(out=B["t2"][:, j, :], in_=B["t2"][:, j, :]), ["t2"], ["t2"])
                    dve(lambda e, j=j: e.tensor_tensor(out=B["t0"][:, j, :], in0=B["t0"][:, j, :], in1=B["t2"][:, j, :],
                                                      op=ALU.mult), ["t0", "t2"], ["t0"])
                    dve(lambda e, j=j, hj=hj: e.tensor_scalar(out=B["t0"][:, j, :], in0=B["t0"][:, j, :], scalar1=prm[:, hj, 8:9],
                                                             scalar2=prm[:, hj, 9:10], op0=ALU.mult, op1=ALU.add),
                        ["t0", "const"], ["t0"])
                pool(lambda e: e.tensor_tensor(out=B["t1"][:], in0=B["sbon"][:], in1=B["vm"][:], op=ALU.mult),
                     ["sbon", "vm", "t1"], ["t1"])
                dve(lambda e: e.tensor_tensor(out=B["t0"][:], in0=B["t0"][:], in1=B["t1"][:], op=ALU.add), ["t0", "t1"], ["t0"])
                dve(lambda e, yb=yb: e.tensor_tensor(out=yb[:], in0=B["t0"][:], in1=B["g"][:], op=ALU.mult),
                    ["t0", "g"], [("yo", yi)])
                for j, h in enumerate(hs):
                    S.dma("sp", lambda e, yb=yb, j=j, h=h: e.dma_start(out=yT[YROW_B + 64 * h:YROW_B + 64 * h + 64, t0:t0 + TT],
                                                                     in_=yb[:, j, :]), reads=[("yo", yi)], writes=["yT"])

            for ti in range(T // TT):
                do_tile(ti)
        S.barrier_all()
        S.emit()


def build_rwkv_test(T, heads, debug=False, stop=None):
    nc = bass.Bass("TRN2", target_bir_lowering=False)
    NH = len(heads)
    di = lambda n, s: nc.dram_tensor(n, s, F32, kind="ExternalInput").ap()
    pT = di("pT", [NP_ROWS, T]); prm = di("prm", [64, NH, 10]); lmu = di("lmu", [128, 4])
    wup = di("wup", [96, NH * 64]); aup = di("aup", [96, NH * 64]); gup = di("gup", [256, NH * 64])
    ident = di("ident", [128, 128]); mlt = di("m_lt2", [64, 3, 128]); mle = di("m_le2", [64, 3, 128])
    mgt = di("m_gt", [64, 3, 64]); rst = di("rst", [64, 512])
    yT = nc.dram_tensor("yT", [1536, T], F32, kind="ExternalOutput").ap()
    dbg = nc.dram_tensor("dbg", [22, 64, 3, 512], F32, kind="ExternalOutput").ap() if debug else None
    phase_rwkv(nc, T, pT, yT, prm, lmu, wup, aup, gup, ident, mlt, mle, mgt, rst, heads, dbg=dbg, stop=stop)
    return nc


def rwkv_host_params(heads, mu, w0, w_up, a0, a_up, g_up, k_k, k_a, r_k, lnx_g, lnx_b):
    NH = len(heads)
    prm = np.zeros((64, NH, 10), np.float32)
    cols = np.concatenate([np.arange(64 * h, 64 * h + 64) for h in heads])
    for i, h in enumerate(heads):
        sl = slice(64 * h, 64 * h + 64)
        prm[:, i, 0] = mu[0:768][sl]; prm[:, i, 1] = mu[768:1536][sl]; prm[:, i, 2] = mu[1536:2304][sl]
        prm[:, i, 3] = w0[sl]; prm[:, i, 4] = a0[sl]; prm[:, i, 5] = k_k[sl]; prm[:, i, 6] = k_a[sl]
        prm[:, i, 7] = r_k.reshape(-1)[sl]; prm[:, i, 8] = lnx_g[sl]; prm[:, i, 9] = lnx_b[sl]
    lmu = np.zeros((128, 4), np.float32)
    lmu[0:96, 0] = mu[2304:2400]; lmu[0:96, 1] = mu[2400:2496]; lmu[:, 2] = mu[2496:2624]; lmu[:, 3] = mu[2624:2752]
    return dict(prm=prm, lmu=lmu, wup=np.ascontiguousarray(w_up[:, cols]), aup=np.ascontiguousarray(a_up[:, cols]),
                gup=np.ascontiguousarray(g_up[:, cols]))


class WeightStream:
    def __init__(self, S, nc, st, name, max_elems, n_stage=2, n_bf=2, direct=False):
        self.S, self.nc, self.name = S, nc, name
        if direct:
            n_stage = 0
        self.stage = [st.enter_context(nc.sbuf_tensor(uname("%s_st%d" % (name, i)), [128, max_elems], F32)) for i in range(n_stage)]
        self.bf = [st.enter_context(nc.sbuf_tensor(uname("%s_bf%d" % (name, i)), [128, max_elems], BF16)) for i in range(n_bf)]
        self.i = 0
        self.q = 0

    def load_bf(self, scr, shapes, dram_key):
        S = self.S
        bi = self.i % len(self.bf); self.i += 1
        bfb = self.bf[bi]
        n_tot = sum(int(np.prod(shp[1:])) for shp in shapes)
        q = ("sp", "act")[self.q % 2]; self.q += 1
        S.dma(q, lambda e, bfb=bfb, n_tot=n_tot: e.dma_start(out=bfb[:, 0:n_tot], in_=scr[:, 0:n_tot]), reads=[dram_key],
              writes=[(self.name, "bf", bi)])
        off = 0
        outs = []
        for shp in shapes:
            n = int(np.prod(shp[1:]))
            pat = "p (a b) -> p a b" if len(shp) == 3 else "p (a b c) -> p a b c"
            kw = dict(a=shp[1], b=shp[2]) if len(shp) == 3 else dict(a=shp[1], b=shp[2], c=shp[3])
            outs.append(bfb[:, off:off + n].rearrange(pat, **kw))
            off += n
        return outs, (self.name, "bf", bi)

    def load(self, src_views, shapes, dram_key):
        S = self.S
        si = self.i % len(self.stage); bi = self.i % len(self.bf); self.i += 1
        stg, bfb = self.stage[si], self.bf[bi]
        off = 0
        outs = []
        for v, shp in zip(src_views, shapes):
            n = int(np.prod(shp[1:]))
            pat = "p (a b) -> p a b" if len(shp) == 3 else "p (a b c) -> p a b c"
            kw = dict(a=shp[1], b=shp[2]) if len(shp) == 3 else dict(a=shp[1], b=shp[2], c=shp[3])
            dst = stg[:, off:off + n].rearrange(pat, **kw)
            q = ("sp", "act")[self.q % 2]; self.q += 1
            S.dma(q, lambda e, dst=dst, v=v: e.dma_start(out=dst, in_=v), reads=[dram_key],
                  writes=[(self.name, "st", si)])
            outs.append(bfb[:, off:off + n].rearrange(pat, **kw))
            off += n
        S.op("pool", lambda e, stg=stg, bfb=bfb, off=off: e.tensor_copy(out=bfb[:, 0:off], in_=stg[:, 0:off]),
             reads=[(self.name, "st", si)], writes=[(self.name, "bf", bi)])
        return outs, (self.name, "bf", bi)


def phase_precast(nc, panels, scr, max_elems):
    import contextlib
    with contextlib.ExitStack() as st:
        _PH[0] += 1
        S = Sched(nc)
        ws = WeightStream(S, nc, st, "pc_w", max_elems)
        for i, (views, shapes) in enumerate(panels):
            outs, kw = ws.load(views, shapes, "Wsrc")
            n_tot = sum(int(np.prod(shp[1:])) for shp in shapes)
            bfb = ws.bf[(ws.i - 1) % len(ws.bf)]
            S.dma("sp", lambda e, i=i, bfb=bfb, n_tot=n_tot: e.dma_start(out=scr[i][:, 0:n_tot], in_=bfb[:, 0:n_tot]),
                  reads=[kw], writes=["scr"])
        S.barrier_all()
        S.emit()


def precast_gen(S, ws, panels, scr, tag):
    for i, (views, shapes) in enumerate(panels):
        outs, kw = ws.load(views, shapes, "Wsrc")
        n_tot = sum(int(np.prod(shp[1:])) for shp in shapes)
        bfb = ws.bf[(ws.i - 1) % len(ws.bf)]
        S.dma("sp", lambda e, i=i, bfb=bfb, n_tot=n_tot: e.dma_start(out=scr[i][:, 0:n_tot], in_=bfb[:, 0:n_tot]),
              reads=[kw], writes=[("scr", tag)])
        yield


def emit_norm_T(S, nc, x_sb, kx, h_out, kh, g_col, ones_bf, sq, rstd, ps, kps, n):
    for c in range(KC):
        S.op("act", lambda e, c=c: e.activation(out=sq[:, c, :n], in_=x_sb[:, c, :], func=AF.Square), reads=[kx], writes=["nrm_sq"])
    for c in range(KC):
        S.op("pe", lambda e, c=c: e.matmul(ps[:, :n], ones_bf[:], sq[:, c, :n], start=(c == 0), stop=(c == KC - 1)),
             reads=["nrm_sq", "ones"], writes=[kps])
    S.op("dve", lambda e: e.tensor_scalar(out=rstd[:, :n], in0=ps[:, :n], scalar1=1.0 / D_MODEL, scalar2=NORM_EPS,
                                          op0=ALU.mult, op1=ALU.add), reads=[kps], writes=["nrm_rstd"])
    S.op("act", lambda e: e.activation(out=rstd[:, :n], in_=rstd[:, :n], func=AF.Sqrt), reads=["nrm_rstd"], writes=["nrm_rstd"])
    S.op("dve", lambda e: e.reciprocal(out=rstd[:, :n], in_=rstd[:, :n]), reads=["nrm_rstd"], writes=["nrm_rstd"])
    for c in range(KC):
        S.op("dve", lambda e, c=c: e.scalar_tensor_tensor(out=h_out[:, c, :], in0=x_sb[:, c, :], scalar=g_col[:, c:c + 1],
                                                         in1=rstd[:, :n], op0=ALU.mult, op1=ALU.mult),
             reads=[kx, "nrm_rstd", "gcol"], writes=[kh])


def phase_proj(nc, TL, xT, g_d, W, pT, ncols, scr=None):
    import contextlib
    TS = min(2048, TL); TT = 512; CB = 256
    Wv0 = W.rearrange("(c p) n -> p c n", p=128)
    if scr is not None:
        panels = []
        for cb0 in range(0, ncols, CB):
            cw = min(CB, ncols - cb0)
            panels.append(([Wv0[:, :, cb0:cb0 + cw]], [(128, KC, cw)]))
        phase_precast(nc, panels, scr, KC * CB)
    with contextlib.ExitStack() as st:
        sb = lambda name, shape, dt=F32: st.enter_context(nc.sbuf_tensor(uname(name), shape, dt))
        pss = [st.enter_context(nc.psum_tensor(uname("pps%d" % i), [128, 512], F32)) for i in range(8)]
        _PH[0] += 1
        S = Sched(nc)
        ones_bf = sb("pj_ones", [128, 128], BF16); g_sb = sb("pj_g", [128, KC])
        hT = sb("pj_h", [128, KC, TS], BF16)
        xin = [sb("pj_x%d" % i, [128, KC, TT]) for i in range(2)]
        sq = sb("pj_sq", [128, KC, TT], BF16); rstd = sb("pj_rstd", [128, TT])
        ot = [sb("pj_o%d" % i, [128, TT]) for i in range(4)]
        ws = WeightStream(S, nc, st, "pj_w", KC * CB, direct=scr is not None)
        S.op("pool", lambda e: e.memset(ones_bf[:], 1.0), writes=["ones"])
        S.dma("sp", lambda e: e.dma_start(out=g_sb[:], in_=g_d), writes=["gcol"])
        xTv = xT.rearrange("(c p) t -> p c t", p=128)
        Wv = W.rearrange("(c p) n -> p c n", p=128)
        cnt = dict(o=0, ps=0)
        for s0 in range(0, TL, TS):
            for tt in range(TS // TT):
                c0 = s0 + tt * TT
                xb_ = xin[tt % 2]
                S.dma("act", lambda e, c0=c0, xb_=xb_: e.dma_start(out=xb_[:], in_=xTv[:, :, c0:c0 + TT]), reads=["xT"], writes=[("pj_x", tt % 2)])
                emit_norm_T(S, nc, xb_, ("pj_x", tt % 2), hT[:, :, tt * TT:(tt + 1) * TT], ("pj_h", tt), g_sb, ones_bf, sq, rstd,
                            pss[0], ("ps", 0), TT)
            for cb0 in range(0, ncols, CB):
                cw = min(CB, ncols - cb0)
                if scr is not None:
                    (wv,), kw = ws.load_bf(scr[cb0 // CB], [(128, KC, cw)], "Wscr")
                else:
                    (wv,), kw = ws.load([Wv[:, :, cb0:cb0 + cw]], [(128, KC, cw)], "W")
                for m0 in range(0, cw, 128):
                    mw = min(128, cw - m0)
                    for tt in range(TS // TT):
                        pi = 1 + cnt["ps"] % 7; cnt["ps"] += 1
                        ps = pss[pi]
                        for c in range(KC):
                            S.op("pe", lambda e, ps=ps, wv=wv, c=c, m0=m0, mw=mw, tt=tt:
                                 e.matmul(ps[:mw, :TT], wv[:, c, m0:m0 + mw], hT[:, c, tt * TT:(tt + 1) * TT],
                                          start=(c == 0), stop=(c == KC - 1)),
                                 reads=[kw, ("pj_h", tt)], writes=[("ps", pi)])
                        oi = cnt["o"] % 4; cnt["o"] += 1
                        ob = ot[oi]
                        if oi % 2:
                            S.op("act", lambda e, ob=ob, ps=ps, mw=mw: e.copy(out=ob[:mw, :], in_=ps[:mw, :TT]),
                                 reads=[("ps", pi)], writes=[("pj_o", oi)])
                        else:
                            S.op("dve", lambda e, ob=ob, ps=ps, mw=mw: e.tensor_copy(out=ob[:mw, :], in_=ps[:mw, :TT]),
                                 reads=[("ps", pi)], writes=[("pj_o", oi)])
                        r0 = cb0 + m0; t0 = s0 + tt * TT
                        S.dma("sp", lambda e, ob=ob, mw=mw, r0=r0, t0=t0: e.dma_start(out=pT[r0:r0 + mw, t0:t0 + TT], in_=ob[:mw, :]),
                              reads=[("pj_o", oi)], writes=["pT"])
        S.barrier_all()
        S.emit()


def merge_panels(Wg, gate_col0, projw, wout):
    Wgv0 = Wg.rearrange("(c p) n -> p c n", p=128)
    Pv0 = projw.rearrange("(c p) n -> p c n", p=128)
    Wov0 = wout.rearrange("(c p) n -> p c n", p=128)
    pa = []
    for m in range(KC):
        views = [Wgv0[:, :, gate_col0 + i * D_MODEL + m * 128:gate_col0 + i * D_MODEL + m * 128 + 128] for i in range(3)]
        views.append(Pv0[:, :, m * 128:(m + 1) * 128])
        pa.append((views, [(128, KC, 128)] * 3 + [(128, 12, 128)]))
    pb = [([Wov0[:, :, m * 128:(m + 1) * 128]], [(128, KC, 128)]) for m in range(KC)]
    return pa, pb


def ffn_panels(wup, wdown, d_ff):
    NF = d_ff // 128
    Wuv0 = wup.rearrange("(c p) n -> p c n", p=128)
    Wdv0 = wdown.rearrange("(f p) n -> p f n", p=128)
    pu = [([Wuv0[:, :, f * 128:(f + 1) * 128], Wuv0[:, :, d_ff + f * 128:d_ff + (f + 1) * 128]], [(128, KC, 128)] * 2) for f in range(NF)]
    pd = [([Wdv0[:, :, m * 128:(m + 1) * 128]], [(128, NF, 128)]) for m in range(KC)]
    return pu, pd


def phase_merge(nc, TL, xT, g_d, Wg, gate_col0, yT, projw, wout, x1T, scr=None, scr_o=None, precast_done=False):
    import contextlib
    TT = 512
    YK = (4, 6, 2)
    YO = (0, 4, 10)
    NHALF = 2 if (scr is not None and TL % (2 * TT) == 0) else 1
    TB = TT * NHALF
    Wgv0 = Wg.rearrange("(c p) n -> p c n", p=128)
    Pv0 = projw.rearrange("(c p) n -> p c n", p=128)
    Wov0 = wout.rearrange("(c p) n -> p c n", p=128)

    def mg_views(m):
        views = [Wgv0[:, :, gate_col0 + i * D_MODEL + m * 128:gate_col0 + i * D_MODEL + m * 128 + 128] for i in range(3)]
        views.append(Pv0[:, :, m * 128:(m + 1) * 128])
        return views, [(128, KC, 128)] * 3 + [(128, 12, 128)]

    if scr is not None and not precast_done:
        phase_precast(nc, [mg_views(m) for m in range(KC)], scr, KC * 3 * 128 + 12 * 128)
        phase_precast(nc, [([Wov0[:, :, m * 128:(m + 1) * 128]], [(128, KC, 128)]) for m in range(KC)], scr_o, KC * 128)
    with contextlib.ExitStack() as st:
        sb = lambda name, shape, dt=F32: st.enter_context(nc.sbuf_tensor(uname(name), shape, dt))
        pss = [st.enter_context(nc.psum_tensor(uname("mps%d" % i), [128, 512], F32)) for i in range(8)]
        _PH[0] += 1
        S = Sched(nc)
        ones_bf = sb("mg_ones", [128, 128], BF16); g_sb = sb("mg_g", [128, KC])
        xin = sb("mg_x", [128, KC, TT]); hT = sb("mg_h", [128, KC, TB], BF16)
        xr = [sb("mg_xr%d" % i, [128, TT]) for i in range(2)]
        rstd = sb("mg_rstd", [128, TT])
        yst = sb("mg_yst", [128, 12, TT]); ybf = sb("mg_ybf", [128, 12, TB], BF16)
        mrgb = sb("mg_mrgb", [128, KC, TB], BF16)
        sq = mrgb[:, :, 0:TT]
        S.alias["nrm_sq"] = "mg_mrgb"
        sig = [sb("mg_sig%d" % i, [128, TT]) for i in range(2)]
        tmp = sb("mg_tmp", [128, TT]); mrg = sb("mg_mrg", [128, TT])
        xo = [sb("mg_xo%d" % i, [128, TT]) for i in range(2)]
        ws = WeightStream(S, nc, st, "mg_w", KC * 3 * 128 + 12 * 128, n_stage=1, n_bf=3 if scr is not None else 2, direct=scr is not None)
        S.op("pool", lambda e: e.memset(ones_bf[:], 1.0), writes=["ones"])
        S.dma("sp", lambda e: e.dma_start(out=g_sb[:], in_=g_d), writes=["gcol"])
        xTv = xT.rearrange("(c p) t -> p c t", p=128)
        yTv = yT.rearrange("(c p) t -> p c t", p=128)
        Wgv = Wg.rearrange("(c p) n -> p c n", p=128)
        Pv = projw.rearrange("(c p) n -> p c n", p=128)
        Wov = wout.rearrange("(c p) n -> p c n", p=128)
        cnt = dict(ps=0, s=0, o=0)

        def nps():
            i = 1 + cnt["ps"] % 7; cnt["ps"] += 1
            return pss[i], ("ps", i)

        for t0 in range(0, TL, TB):
            for h in range(NHALF):
                hs = slice(h * TT, (h + 1) * TT)
                tk = t0 + h * TT
                S.dma("sp", lambda e, tk=tk: e.dma_start(out=xin[:], in_=xTv[:, :, tk:tk + TT]), reads=["xT"], writes=["mg_x"])
                S.dma("act", lambda e, tk=tk: e.dma_start(out=yst[:], in_=yTv[:, :, tk:tk + TT]), reads=["yT"], writes=["mg_yst"])
                S.op("pool", lambda e, hs=hs: e.tensor_copy(out=ybf[:, :, hs], in_=yst[:]), reads=["mg_yst"], writes=[("mg_ybf", h)])
                emit_norm_T(S, nc, xin, "mg_x", hT[:, :, hs], ("mg_h", h), g_sb, ones_bf, sq, rstd, pss[0], ("ps", 0), TT)
            for m in range(KC):
                views = [Wgv[:, :, gate_col0 + i * D_MODEL + m * 128:gate_col0 + i * D_MODEL + m * 128 + 128] for i in range(3)]
                views.append(Pv[:, :, m * 128:(m + 1) * 128])
                shapes = [(128, KC, 128)] * 3 + [(128, 12, 128)]
                if scr is not None:
                    (w0v, w1v, w2v, pv), kw = ws.load_bf(scr[m], shapes, "Wmgs")
                else:
                    (w0v, w1v, w2v, pv), kw = ws.load(views, shapes, "Wmg")
                wgs = (w0v, w1v, w2v)
                for h in range(NHALF):
                    hs = slice(h * TT, (h + 1) * TT)
                    for i in range(3):
                        pg, kpg = nps()
                        for c in range(KC):
                            S.op("pe", lambda e, pg=pg, i=i, c=c, wgs=wgs, hs=hs: e.matmul(pg[:, :TT], wgs[i][:, c, :], hT[:, c, hs],
                                                                                       start=(c == 0), stop=(c == KC - 1)),
                                 reads=[kw, ("mg_h", h)], writes=[kpg])
                        pz, kpz = nps()
                        for u in range(YK[i]):
                            S.op("pe", lambda e, pz=pz, i=i, u=u, pv=pv, hs=hs: e.matmul(pz[:, :TT], pv[:, YO[i] + u, :], ybf[:, YO[i] + u, hs],
                                                                                      start=(u == 0), stop=(u == YK[i] - 1)),
                                 reads=[kw, ("mg_ybf", h)], writes=[kpz])
                        si = cnt["s"] % 2; cnt["s"] += 1
                        sg = sig[si]
                        S.op("act", lambda e, sg=sg, pg=pg: e.activation(out=sg[:], in_=pg[:, :TT], func=AF.Sigmoid), reads=[kpg],
                             writes=[("mg_sig", si)])
                        if i == 0:
                            S.op("dve", lambda e, sg=sg, pz=pz: e.tensor_tensor(out=mrg[:], in0=pz[:, :TT], in1=sg[:], op=ALU.mult),
                                 reads=[kpz, ("mg_sig", si)], writes=["mg_mrg"])
                        else:
                            S.op("dve", lambda e, sg=sg, pz=pz: e.tensor_tensor(out=tmp[:], in0=pz[:, :TT], in1=sg[:], op=ALU.mult),
                                 reads=[kpz, ("mg_sig", si)], writes=["mg_tmp"])
                            if i == 1:
                                S.op("pool", lambda e: e.tensor_tensor(out=mrg[:], in0=mrg[:], in1=tmp[:], op=ALU.add),
                                     reads=["mg_mrg", "mg_tmp"], writes=["mg_mrg"])
                            else:
                                S.op("pool", lambda e, m=m, hs=hs: e.tensor_tensor(out=mrgb[:, m, hs], in0=mrg[:], in1=tmp[:], op=ALU.add),
                                     reads=["mg_mrg", "mg_tmp"], writes=["mg_mrgb"])
            for m in range(KC):
                if scr is not None:
                    (wov,), kw = ws.load_bf(scr_o[m], [(128, KC, 128)], "Wouts")
                else:
                    (wov,), kw = ws.load([Wov[:, :, m * 128:(m + 1) * 128]], [(128, KC, 128)], "Wout")
                for h in range(NHALF):
                    hs = slice(h * TT, (h + 1) * TT)
                    tk = t0 + h * TT
                    po, kpo = nps()
                    for c in range(KC):
                        S.op("pe", lambda e, po=po, c=c, wov=wov, hs=hs: e.matmul(po[:, :TT], wov[:, c, :], mrgb[:, c, hs], start=(c == 0),
                                                                               stop=(c == KC - 1)), reads=[kw, "mg_mrgb"], writes=[kpo])
                    oi = cnt["o"] % 2; cnt["o"] += 1
                    ob = xo[oi]; xrb = xr[oi]
                    S.dma("act", lambda e, xrb=xrb, m=m, tk=tk: e.dma_start(out=xrb[:], in_=xT[m * 128:(m + 1) * 128, tk:tk + TT]),
                          reads=["xT"], writes=[("mg_xr", oi)])
                    S.op("dve", lambda e, ob=ob, po=po, xrb=xrb: e.tensor_tensor(out=ob[:], in0=po[:, :TT], in1=xrb[:], op=ALU.add),
                         reads=[kpo, ("mg_xr", oi)], writes=[("mg_xo", oi)])
                    S.dma("sp", lambda e, ob=ob, m=m, tk=tk: e.dma_start(out=x1T[m * 128:(m + 1) * 128, tk:tk + TT], in_=ob[:]),
                          reads=[("mg_xo", oi)], writes=["x1T"])
        S.barrier_all()
        S.emit()


def phase_ffn(nc, TL, x1T, g_d, wup, conv_d, wdown, xoT, d_ff, halo_d=None, scr_u=None, scr_d=None, precast_done=False):
    import contextlib
    TT = 512
    NF = d_ff // 128
    NHALF = 2 if (scr_u is not None and TL % (2 * TT) == 0) else 1
    TB = TT * NHALF
    Wuv0 = wup.rearrange("(c p) n -> p c n", p=128)
    Wdv0 = wdown.rearrange("(f p) n -> p f n", p=128)
    if scr_u is not None and not precast_done:
        phase_precast(nc, [([Wuv0[:, :, f * 128:(f + 1) * 128], Wuv0[:, :, d_ff + f * 128:d_ff + (f + 1) * 128]], [(128, KC, 128)] * 2)
                           for f in range(NF)], scr_u, KC * 256)
        phase_precast(nc, [([Wdv0[:, :, m * 128:(m + 1) * 128]], [(128, NF, 128)]) for m in range(KC)], scr_d, NF * 128)
    with contextlib.ExitStack() as st:
        sb = lambda name, shape, dt=F32: st.enter_context(nc.sbuf_tensor(uname(name), shape, dt))
        pss = [st.enter_context(nc.psum_tensor(uname("fps%d" % i), [128, 512], F32)) for i in range(8)]
        _PH[0] += 1
        S = Sched(nc)
        ones_bf = sb("ff_ones", [128, 128], BF16); g_sb = sb("ff_g", [128, KC]); cw = sb("ff_cw", [128, NF, 3])
        xin = sb("ff_x", [128, KC, TT]); hT = sb("ff_h", [128, KC, TB], BF16)
        rstd = sb("ff_rstd", [128, TT])
        actb = sb("ff_act", [128, max(NF, KC), TB], BF16)
        sq = actb[:, 0:KC, 0:TT]
        S.alias["nrm_sq"] = "ff_act"
        xr = [sb("ff_xr%d" % i, [128, TT]) for i in range(2)]
        carry = sb("ff_carry", [128, NF, 2])
        gbuf = [sb("ff_gb%d" % i, [128, TT + 2]) for i in range(2)]
        cv = [sb("ff_cv%d" % i, [128, TT]) for i in range(2)]
        xo = [sb("ff_xo%d" % i, [128, TT]) for i in range(2)]
        ws = WeightStream(S, nc, st, "ff_w", max(KC * 256, NF * 128), n_stage=2, n_bf=3 if scr_u is not None else 2,
                          direct=scr_u is not None)
        S.op("pool", lambda e: e.memset(ones_bf[:], 1.0), writes=["ones"])
        S.dma("sp", lambda e: e.dma_start(out=g_sb[:], in_=g_d), writes=["gcol"])
        S.dma("sp", lambda e: e.dma_start(out=cw[:], in_=conv_d), writes=["ff_cw"])
        if halo_d is None:
            S.op("pool", lambda e: e.memset(carry[:], 0.0), writes=["ff_carry"])
        else:
            S.dma("sp", lambda e: e.dma_start(out=carry[:], in_=halo_d), writes=["ff_carry"])
        xv = x1T.rearrange("(c p) t -> p c t", p=128)
        Wuv = wup.rearrange("(c p) n -> p c n", p=128)
        Wdv = wdown.rearrange("(f p) n -> p f n", p=128)
        cnt = dict(ps=0, g=0, o=0)

        def nps():
            i = 1 + cnt["ps"] % 7; cnt["ps"] += 1
            return pss[i], ("ps", i)

        for t0 in range(0, TL, TB):
            for h in range(NHALF):
                S.dma("sp", lambda e, t0=t0, h=h: e.dma_start(out=xin[:], in_=xv[:, :, t0 + h * TT:t0 + (h + 1) * TT]),
                      reads=["x1T"], writes=["ff_x"])
                emit_norm_T(S, nc, xin, "ff_x", hT[:, :, h * TT:(h + 1) * TT], ("ff_h", h), g_sb, ones_bf, sq, rstd, pss[0],
                            ("ps", 0), TT)
            for f in range(NF):
                views = [Wuv[:, :, f * 128:(f + 1) * 128], Wuv[:, :, d_ff + f * 128:d_ff + (f + 1) * 128]]
                if scr_u is not None:
                    (wg, wv), kw = ws.load_bf(scr_u[f], [(128, KC, 128)] * 2, "Wups")
                else:
                    (wg, wv), kw = ws.load(views, [(128, KC, 128)] * 2, "Wup")
                for h in range(NHALF):
                    hs = slice(h * TT, (h + 1) * TT)
                    pg, kpg = nps()
                    for c in range(KC):
                        S.op("pe", lambda e, pg=pg, c=c, wg=wg, hs=hs: e.matmul(pg[:, :TT], wg[:, c, :], hT[:, c, hs], start=(c == 0),
                                                                             stop=(c == KC - 1)), reads=[kw, ("ff_h", h)], writes=[kpg])
                    pv, kpv = nps()
                    for c in range(KC):
                        S.op("pe", lambda e, pv=pv, c=c, wv=wv, hs=hs: e.matmul(pv[:, :TT], wv[:, c, :], hT[:, c, hs], start=(c == 0),
                                                                             stop=(c == KC - 1)), reads=[kw, ("ff_h", h)], writes=[kpv])
                    gi = cnt["g"] % 2; cnt["g"] += 1
                    gb = gbuf[gi]; cb = cv[gi]
                    kgb, kcb = ("ff_gb", gi), ("ff_cv", gi)
                    S.op("pool", lambda e, gb=gb, f=f: e.tensor_copy(out=gb[:, 0:2], in_=carry[:, f, :]), reads=["ff_carry"], writes=[kgb])
                    S.op("act", lambda e, gb=gb, pg=pg: e.copy(out=gb[:, 2:TT + 2], in_=pg[:, :TT]), reads=[kpg], writes=[kgb])
                    S.op("pool", lambda e, gb=gb, f=f: e.tensor_copy(out=carry[:, f, :], in_=gb[:, TT:TT + 2]), reads=[kgb],
                         writes=["ff_carry"])
                    S.op("dve", lambda e, gb=gb, cb=cb, f=f: e.tensor_scalar(out=cb[:], in0=gb[:, 0:TT], scalar1=cw[:, f, 0:1], scalar2=None,
                                                                           op0=ALU.mult), reads=[kgb, "ff_cw"], writes=[kcb])
                    S.op("dve", lambda e, gb=gb, cb=cb, f=f: e.scalar_tensor_tensor(out=cb[:], in0=gb[:, 1:TT + 1], scalar=cw[:, f, 1:2],
                                                                                  in1=cb[:], op0=ALU.mult, op1=ALU.add),
                         reads=[kgb, "ff_cw", kcb], writes=[kcb])
                    S.op("dve", lambda e, gb=gb, cb=cb, f=f: e.scalar_tensor_tensor(out=cb[:], in0=gb[:, 2:TT + 2], scalar=cw[:, f, 2:3],
                                                                                  in1=cb[:], op0=ALU.mult, op1=ALU.add),
                         reads=[kgb, "ff_cw", kcb], writes=[kcb])
                    S.op("act", lambda e, cb=cb: e.activation(out=cb[:], in_=cb[:], func=AF.Silu), reads=[kcb], writes=[kcb])
                    S.op("dve", lambda e, cb=cb, pv=pv, f=f, hs=hs: e.tensor_tensor(out=actb[:, f, hs], in0=pv[:, :TT], in1=cb[:], op=ALU.mult),
                         reads=[kpv, kcb], writes=["ff_act"])
            for m in range(KC):
                if scr_d is not None:
                    (wd,), kw = ws.load_bf(scr_d[m], [(128, NF, 128)], "Wdowns")
                else:
                    (wd,), kw = ws.load([Wdv[:, :, m * 128:(m + 1) * 128]], [(128, NF, 128)], "Wdown")
                for h in range(NHALF):
                    hs = slice(h * TT, (h + 1) * TT)
                    tk = t0 + h * TT
                    po, kpo = nps()
                    for f in range(NF):
                        S.op("pe", lambda e, po=po, f=f, wd=wd, hs=hs: e.matmul(po[:, :TT], wd[:, f, :], actb[:, f, hs], start=(f == 0),
                                                                             stop=(f == NF - 1)), reads=[kw, "ff_act"], writes=[kpo])
                    oi = cnt["o"] % 2; cnt["o"] += 1
                    ob = xo[oi]; xrb = xr[oi]
                    S.dma("act", lambda e, xrb=xrb, m=m, tk=tk: e.dma_start(out=xrb[:], in_=x1T[m * 128:(m + 1) * 128, tk:tk + TT]),
                          reads=["x1T"], writes=[("ff_xr", oi)])
                    S.op("dve", lambda e, ob=ob, po=po, xrb=xrb: e.tensor_tensor(out=ob[:], in0=po[:, :TT], in1=xrb[:], op=ALU.add),
                         reads=[kpo, ("ff_xr", oi)], writes=[("ff_xo", oi)])
                    S.dma("sp", lambda e, ob=ob, m=m, tk=tk: e.dma_start(out=xoT[m * 128:(m + 1) * 128, tk:tk + TT], in_=ob[:]),
                          reads=[("ff_xo", oi)], writes=["xoT"])
        S.barrier_all()
        S.emit()


def phase_final_norm(nc, TL, xT, g_d, outT):
    import contextlib
    TT = 512
    with contextlib.ExitStack() as st:
        sb = lambda name, shape, dt=F32: st.enter_context(nc.sbuf_tensor(uname(name), shape, dt))
        pss = [st.enter_context(nc.psum_tensor(uname("nps%d" % i), [128, 512], F32)) for i in range(2)]
        _PH[0] += 1
        S = Sched(nc)
        ones_bf = sb("fn_ones", [128, 128], BF16); g_sb = sb("fn_g", [128, KC])
        xin = [sb("fn_x%d" % i, [128, KC, TT]) for i in range(2)]
        ho = [sb("fn_h%d" % i, [128, KC, TT]) for i in range(2)]
        sq = sb("fn_sq", [128, KC, TT], BF16); rstd = sb("fn_rstd", [128, TT])
        S.op("pool", lambda e: e.memset(ones_bf[:], 1.0), writes=["ones"])
        S.dma("sp", lambda e: e.dma_start(out=g_sb[:], in_=g_d), writes=["gcol"])
        xv = xT.rearrange("(c p) t -> p c t", p=128)
        ov = outT.rearrange("(c p) t -> p c t", p=128)
        for i, t0 in enumerate(range(0, TL, TT)):
            b = i % 2
            S.dma("sp", lambda e, t0=t0, b=b: e.dma_start(out=xin[b][:], in_=xv[:, :, t0:t0 + TT]), reads=["xT"], writes=[("fn_x", b)])
            emit_norm_T(S, nc, xin[b], ("fn_x", b), ho[b], ("fn_h", b), g_sb, ones_bf, sq, rstd, pss[0], ("ps", 0), TT)
            S.dma("act", lambda e, t0=t0, b=b: e.dma_start(out=ov[:, :, t0:t0 + TT], in_=ho[b][:]), reads=[("fn_h", b)], writes=["outT"])
        S.barrier_all()
        S.emit()


def build_dense_test(TL, d_ff, ncols_p):
    nc = bass.Bass("TRN2", target_bir_lowering=False)
    di = lambda n, s: nc.dram_tensor(n, s, F32, kind="ExternalInput").ap()
    xT = di("xT", [D_MODEL, TL]); g1 = di("g1", [128, KC]); g2 = di("g2", [128, KC]); gf = di("gf", [128, KC])
    w_in = di("w_in", [D_MODEL, ncols_p + 6144]); yT = di("yT", [1536, TL]); projw = di("projw", [1536, D_MODEL])
    wout = di("wout", [D_MODEL, D_MODEL]); wup = di("wup", [D_MODEL, 2 * d_ff]); conv = di("conv", [128, d_ff // 128, 3])
    wdown = di("wdown", [d_ff, D_MODEL])
    pT = nc.dram_tensor("pT", [ncols_p, TL], F32, kind="ExternalOutput").ap()
    x1T = nc.dram_tensor("x1T", [D_MODEL, TL], F32, kind="ExternalOutput").ap()
    x2T = nc.dram_tensor("x2T", [D_MODEL, TL], F32, kind="ExternalOutput").ap()
    outT = nc.dram_tensor("outT", [D_MODEL, TL], F32, kind="ExternalOutput").ap()
    phase_proj(nc, TL, xT, g1, w_in, pT, ncols_p)
    phase_merge(nc, TL, xT, g1, w_in, ncols_p, yT, projw, wout, x1T)
    phase_ffn(nc, TL, x1T, g2, wup, conv, wdown, x2T, d_ff)
    phase_final_norm(nc, TL, x2T, gf, outT)
    return nc


def build_full(TL, n_layers, d_ff, heads_a, heads_c, heads_b, final=True):
    nc = bass.Bass("TRN2", target_bir_lowering=False)
    di = lambda n, s: nc.dram_tensor(n, list(s), F32, kind="ExternalInput").ap()
    L = n_layers
    NHB = len(heads_b)
    xT = di("xT", [D_MODEL, TL])
    g1 = di("g1", [L, 128, KC]); g2 = di("g2", [L, 128, KC]); gf = di("gf", [128, KC])
    w_in = di("w_in", [L, D_MODEL, NP_ROWS + 3 * D_MODEL])
    projw = di("projw", [L, 1536, D_MODEL]); wout = di("w_out", [L, D_MODEL, D_MODEL])
    wup = di("ffn_up", [L, D_MODEL, 2 * d_ff]); conv = di("conv", [L, 128, d_ff // 128, 3]); wdown = di("ffn_down", [L, d_ff, D_MODEL])
    tabs_g = di("tabs_g", [20, 128, 256]); tabs_m = di("tabs_m", [20, 128, 256]); tabs_a = di("tabs_a", [20, 128, 256])
    sinks = di("sinks", [L, 64, 8]); ident = di("ident", [128, 128])
    prm = di("prm", [L, 64, NHB, 10]); lmu = di("lmu", [L, 128, 4])
    rwup = di("rw_up", [L, 96, NHB * 64]); raup = di("ra_up", [L, 96, NHB * 64]); rgup = di("rg_up", [L, 256, NHB * 64])
    mlt = di("m_lt2", [64, 3, 128]); mle = di("m_le2", [64, 3, 128]); mgt = di("m_gt", [64, 3, 64]); rst = di("rst", [64, 512])
    outT = nc.dram_tensor("outT", [D_MODEL, TL], F32, kind="ExternalOutput").ap()
    pT = nc.dram_tensor("pT_s", [NP_ROWS, TL], F32).ap()
    yT = nc.dram_tensor("yT_s", [1536, TL], F32).ap()
    x1T = nc.dram_tensor("x1T_s", [D_MODEL, TL], F32).ap()
    xs = [nc.dram_tensor("xs%d" % i, [D_MODEL, TL], F32).ap() for i in range(2)]
    NF = d_ff // 128
    sc_pj = nc.dram_tensor("sc_pj", [(NP_ROWS + 255) // 256, 128, KC * 256], BF16).ap()
    sc_mg = nc.dram_tensor("sc_mg", [KC, 128, KC * 3 * 128 + 12 * 128], BF16).ap()
    sc_wo = nc.dram_tensor("sc_wo", [KC, 128, KC * 128], BF16).ap()
    sc_up = nc.dram_tensor("sc_up", [NF, 128, KC * 256], BF16).ap()
    sc_dn = nc.dram_tensor("sc_dn", [KC, 128, NF * 128], BF16).ap()
    cur = xT
    for l in range(L):
        phase_proj(nc, TL, cur, g1[l], w_in[l], pT, NP_ROWS, scr=sc_pj)
        pa, pb = merge_panels(w_in[l], NP_ROWS, projw[l], wout[l])
        pu, pd = ffn_panels(wup[l], wdown[l], d_ff)
        phase_attn(nc, TL, pT, yT, tabs_g, tabs_m, tabs_a, sinks[l], ident, heads_a, heads_c,
                   precast_jobs=[(pa, sc_mg), (pb, sc_wo), (pu, sc_up), (pd, sc_dn)])
        phase_rwkv(nc, TL, pT, yT, prm[l], lmu[l], rwup[l], raup[l], rgup[l], ident, mlt, mle, mgt, rst, heads_b)
        phase_merge(nc, TL, cur, g1[l], w_in[l], NP_ROWS, yT, projw[l], wout[l], x1T, scr=sc_mg, scr_o=sc_wo, precast_done=True)
        nxt = outT if (l == L - 1 and not final) else xs[l % 2]
        phase_ffn(nc, TL, x1T, g2[l], wup[l], conv[l], wdown[l], nxt, d_ff, scr_u=sc_up, scr_d=sc_dn, precast_done=True)
        cur = nxt
    if final:
        phase_final_norm(nc, TL, cur, gf, outT)
    return nc


def phase_attn(nc, T, pT, yT, tg, tm, ta, sk, idd, heads_a, heads_c, precast_jobs=None):
    import contextlib
    with contextlib.ExitStack() as st:
        pss = [st.enter_context(nc.psum_tensor(uname("aps%d" % i), [128, 512], F32)) for i in range(8)]
        _PH[0] += 1
        S = Sched(nc)
        between = None
        gen = None
        if precast_jobs:
            mx = max(sum(int(np.prod(shp[1:])) for shp in shapes) for panels, _ in precast_jobs for _, shapes in panels)
            ws = WeightStream(S, nc, st, "apc_w", mx, n_stage=1, n_bf=1)
            n_pan = sum(len(p) for p, _ in precast_jobs)
            n_units = len(heads_a) + 3 * len(heads_c)
            per = (n_pan + n_units - 1) // n_units

            def chain():
                for ji, (panels, scr) in enumerate(precast_jobs):
                    yield from precast_gen(S, ws, panels, scr, ji)

            gen = chain()

            def between():
                for _ in range(per):
                    try:
                        next(gen)
                    except StopIteration:
                        return
        emit_attention(S, nc, st, T, pT, yT, tg, tm, ta, sk, idd, pss, heads_a, heads_c, between=between)
        if gen is not None:
            for _ in gen:
                pass
        S.barrier_all()
        S.emit()


def host_inputs(inp, n_layers, d_ff, heads_b):
    L = n_layers
    f32 = lambda a: np.ascontiguousarray(np.asarray(a, dtype=np.float32))
    gl = lambda g: np.ascontiguousarray(np.asarray(g, np.float32).reshape(-1, KC, 128).transpose(0, 2, 1))
    tg, tm, ta = attn_tables(np.asarray(inp["rel_bias"], np.float32))
    d = dict(g1=gl(inp["norm1_g"][:L]), g2=gl(inp["norm2_g"][:L]), gf=gl(inp["final_g"])[0],
             w_in=f32(inp["w_in"][:L]),
             projw=f32(np.concatenate([np.asarray(inp["proj_a"][:L]), np.asarray(inp["proj_b"][:L]), np.asarray(inp["proj_c"][:L])], axis=1)),
             w_out=f32(inp["w_out"][:L]), ffn_up=f32(inp["ffn_up"][:L]), ffn_down=f32(inp["ffn_down"][:L]),
             conv=f32(np.asarray(inp["ffn_conv"][:L]).transpose(0, 2, 1).reshape(L, d_ff // 128, 128, 3).transpose(0, 2, 1, 3)),
             tabs_g=tg, tabs_m=tm, tabs_a=ta,
             sinks=f32(np.broadcast_to(np.asarray(inp["attn_sinks"][:L])[:, None, :], (L, 64, 8))),
             ident=np.eye(128, dtype=np.float32))
    prm, lmu, wu, au, gu = [], [], [], [], []
    for l in range(L):
        hp = rwkv_host_params(heads_b, *[np.asarray(inp[k][l], np.float32) for k in
                                         ("rwkv_mu", "rwkv_w0", "rwkv_w_up", "rwkv_a0", "rwkv_a_up", "rwkv_g_up", "rwkv_k_k",
                                          "rwkv_k_a", "rwkv_r_k", "rwkv_lnx_g", "rwkv_lnx_b")])
        prm.append(hp["prm"]); lmu.append(hp["lmu"]); wu.append(hp["wup"]); au.append(hp["aup"]); gu.append(hp["gup"])
    d.update(prm=np.stack(prm), lmu=np.stack(lmu), rw_up=np.stack(wu), ra_up=np.stack(au), rg_up=np.stack(gu))
    d.update(rwkv_consts())
    return d


_NC_CACHE = {}
N_LAUNCH = 1


def kernel(**inp):
    x = np.asarray(inp["x"], np.float32)
    Bn, Sq, Dm = x.shape
    L = np.asarray(inp["w_in"]).shape[0]
    d_ff = np.asarray(inp["ffn_down"]).shape[1]
    heads_a, heads_c, heads_b = list(range(8)), list(range(4)), list(range(12))
    nl = N_LAUNCH if L % N_LAUNCH == 0 else 1
    Lp = L // nl
    xTs = [np.ascontiguousarray(x[b].T) for b in range(Bn)]
    per_layer = ("norm1_g", "w_in", "attn_sinks", "rwkv_mu", "rwkv_w0", "rwkv_w_up", "rwkv_a0", "rwkv_a_up", "rwkv_g_up", "rwkv_k_k",
                 "rwkv_k_a", "rwkv_r_k", "rwkv_lnx_g", "rwkv_lnx_b", "proj_a", "proj_b", "proj_c", "w_out", "norm2_g", "ffn_up",
                 "ffn_conv", "ffn_down")
    for li in range(nl):
        final = (li == nl - 1)
        key = (Sq, Lp, d_ff, final)
        if key not in _NC_CACHE:
            _NC_CACHE[key] = build_full(Sq, Lp, d_ff, heads_a, heads_c, heads_b, final=final)
        nc = _NC_CACHE[key]
        sub = dict(inp)
        for k in per_layer:
            sub[k] = np.asarray(inp[k])[li * Lp:(li + 1) * Lp]
        shared = host_inputs(sub, Lp, d_ff, heads_b)
        in_maps = []
        for b in range(Bn):
            m = dict(shared)
            m["xT"] = xTs[b]
            in_maps.append(m)
        res = run_bass_kernel_spmd(nc, in_maps, core_ids=list(range(Bn)))
        xTs = [np.ascontiguousarray(r["outT"]) for r in res.results]
    out = np.stack([np.ascontiguousarray(t.T) for t in xTs], axis=0)
    return out.astype(np.float32)
```
